# Optimizing a Trainium2 kernel written in Bass

```python
import math
import jax, jax.numpy as jnp
from jax import lax
import numpy as np

D_MODEL = 1024
BATCH = 8
SEQ = 4096
DEPTH = 2

CHUNK = 64
POOL_WIDTH = D_MODEL // 2
POOL_WINDOWS = (2, 4, 8, 16)
N_POOL_GROUPS = len(POOL_WINDOWS)
POOL_GROUP = POOL_WIDTH // N_POOL_GROUPS
SB_WIDTH = D_MODEL - POOL_WIDTH
SB_HEAD_DIM = 64
SB_HEADS = SB_WIDTH // SB_HEAD_DIM
Q_BLOCK = 128
IN_WIDTH = POOL_WIDTH + 3 * SB_WIDTH
D_FF = 2816
N_EXPERTS = 8
TOP_K = 2
EXPERT_FF = 2816
MOE_BLOCK = 256
N_DENSE = (DEPTH + 1) // 2
N_MOE = DEPTH // 2
DN_ALPHA = (2 * DEPTH) ** 0.25
DN_BETA = (8 * DEPTH) ** -0.25
LN_EPS = 1e-5

kernel_name = "hymba_pool_stickbreak_deepnorm_moe"


def layer_norm(x, g, b):
    xf = x.astype(jnp.float32)
    mu = jnp.mean(xf, axis=-1, keepdims=True)
    xc = xf - mu
    var = jnp.mean(xc * xc, axis=-1, keepdims=True)
    y = xc * lax.rsqrt(var + LN_EPS) * g.astype(jnp.float32) + b.astype(jnp.float32)
    return y.astype(x.dtype)


def pool_mixer(u, pool_w, pool_scale):
    B, S, _ = u.shape
    ug = u.astype(jnp.float32).reshape(B, S, N_POOL_GROUPS, POOL_GROUP)
    cs = jnp.cumsum(ug, axis=1)
    pos = jnp.arange(S)
    outs = []
    for gi, w in enumerate(POOL_WINDOWS):
        c = cs[:, :, gi]
        lagged = jnp.pad(c, ((0, 0), (w, 0), (0, 0)))[:, :S]
        cnt = jnp.minimum(pos + 1, w).astype(jnp.float32)[None, :, None]
        outs.append((c - lagged) / cnt - ug[:, :, gi])
    d = jnp.stack(outs, axis=2)
    y = jnp.einsum('bsgc,gcd->bsgd', d, pool_w.astype(jnp.float32))
    y = y.reshape(B, S, POOL_WIDTH) * pool_scale.astype(jnp.float32)
    return y.astype(u.dtype)


def stick_breaking_attention(q, k, v):
    S = q.shape[2]
    scale = 1.0 / math.sqrt(SB_HEAD_DIM)
    outs = []
    for blk in range(S // Q_BLOCK):
        start = blk * Q_BLOCK
        end = start + Q_BLOCK
        qb = q[:, :, start:end].astype(jnp.float32)
        kb = k[:, :, :end].astype(jnp.float32)
        vb = v[:, :, :end].astype(jnp.float32)
        z = jnp.einsum('bhtd,bhsd->bhts', qb, kb) * scale
        t_idx = start + jnp.arange(Q_BLOCK)[:, None]
        s_idx = jnp.arange(end)[None, :]
        mask = s_idx < t_idx
        log_1m_beta = jnp.where(mask, jax.nn.log_sigmoid(-z), 0.0)
        between = lax.cumsum(log_1m_beta, axis=3, reverse=True) - log_1m_beta
        att = jnp.where(mask, jnp.exp(jax.nn.log_sigmoid(z) + between), 0.0)
        outs.append(jnp.einsum('bhts,bhsd->bhtd', att, vb))
    return jnp.concatenate(outs, axis=2).astype(q.dtype)


def swiglu(x, wg, wu, wd):
    return (jax.nn.silu(x @ wg) * (x @ wu)) @ wd


def moe_ffn(x, router, wg, wu, wd):
    B, S, D = x.shape
    T = B * S
    A = T * TOP_K
    xf = x.reshape(T, D)
    logits = xf.astype(jnp.float32) @ router.astype(jnp.float32)
    top_logits, top_idx = lax.top_k(logits, TOP_K)
    gates = jax.nn.softmax(top_logits, axis=-1)
    flat_e = top_idx.reshape(-1)
    flat_tok = jnp.repeat(jnp.arange(T, dtype=jnp.int32), TOP_K)
    flat_g = gates.reshape(-1)
    order = jnp.argsort(flat_e)
    sorted_e = flat_e[order]
    counts = jnp.zeros((N_EXPERTS,), jnp.int32).at[flat_e].add(1)
    padded = ((counts + MOE_BLOCK - 1) // MOE_BLOCK) * MOE_BLOCK
    start = jnp.cumsum(counts) - counts
    pend = jnp.cumsum(padded)
    pstart = pend - padded
    rank = jnp.arange(A, dtype=jnp.int32) - start[sorted_e]
    dest = pstart[sorted_e] + rank
    n_slots = ((A + MOE_BLOCK - 1) // MOE_BLOCK) * MOE_BLOCK + N_EXPERTS * MOE_BLOCK
    n_blocks = n_slots // MOE_BLOCK
    slot_tok = jnp.zeros((n_slots,), jnp.int32).at[dest].set(flat_tok[order])
    slot_g = jnp.zeros((n_slots,), jnp.float32).at[dest].set(flat_g[order])
    block_e = jnp.minimum(
        jnp.searchsorted(pend, jnp.arange(n_blocks, dtype=jnp.int32) * MOE_BLOCK, side='right'),
        N_EXPERTS - 1)
    xs = xf[slot_tok].reshape(n_blocks, MOE_BLOCK, D)

    def expert_block(args):
        xb, e = args
        return swiglu(xb, wg[e], wu[e], wd[e])

    ys = lax.map(expert_block, (xs, block_e)).reshape(n_slots, D)
    out = jnp.zeros((T, D), jnp.float32).at[slot_tok].add(ys.astype(jnp.float32) * slot_g[:, None])
    return out.astype(x.dtype).reshape(B, S, D)


def setup_inputs(seed: int = 0) -> dict:
    key = jax.random.key(seed)
    ks = jax.random.split(key, 24)
    f32 = jnp.float32
    nrm = lambda k, shape, s: jax.random.normal(k, shape, f32) * s
    x = jax.random.normal(ks[0], (BATCH, SEQ, D_MODEL), f32)
    ln_in_g = 1.0 + nrm(ks[1], (D_MODEL,), 0.02)
    ln_in_b = nrm(ks[2], (D_MODEL,), 0.02)
    w_in = nrm(ks[3], (DEPTH, D_MODEL, IN_WIDTH), D_MODEL ** -0.5)
    w_in = w_in.at[:, :, POOL_WIDTH + 2 * SB_WIDTH:].multiply(DN_BETA)
    pool_w = nrm(ks[4], (DEPTH, N_POOL_GROUPS, POOL_GROUP, POOL_GROUP), POOL_GROUP ** -0.5)
    pool_scale = 1.0 + nrm(ks[5], (DEPTH, POOL_WIDTH), 0.1)
    w_out = nrm(ks[6], (DEPTH, D_MODEL, D_MODEL), D_MODEL ** -0.5) * DN_BETA
    ln1_g = 1.0 + nrm(ks[7], (DEPTH, D_MODEL), 0.02)
    ln1_b = nrm(ks[8], (DEPTH, D_MODEL), 0.02)
    ffn_wg = nrm(ks[9], (N_DENSE, D_MODEL, D_FF), D_MODEL ** -0.5)
    ffn_wu = nrm(ks[10], (N_DENSE, D_MODEL, D_FF), D_MODEL ** -0.5)
    ffn_wd = nrm(ks[11], (N_DENSE, D_FF, D_MODEL), D_FF ** -0.5) * DN_BETA
    moe_router = nrm(ks[12], (N_MOE, D_MODEL, N_EXPERTS), D_MODEL ** -0.5)
    moe_wg = nrm(ks[13], (N_MOE, N_EXPERTS, D_MODEL, EXPERT_FF), D_MODEL ** -0.5)
    moe_wu = nrm(ks[14], (N_MOE, N_EXPERTS, D_MODEL, EXPERT_FF), D_MODEL ** -0.5)
    moe_wd = nrm(ks[15], (N_MOE, N_EXPERTS, EXPERT_FF, D_MODEL), EXPERT_FF ** -0.5) * DN_BETA
    ln2_g = 1.0 + nrm(ks[16], (DEPTH, D_MODEL), 0.02)
    ln2_b = nrm(ks[17], (DEPTH, D_MODEL), 0.02)
    return {"x": x, "ln_in_g": ln_in_g, "ln_in_b": ln_in_b, "w_in": w_in, "pool_w": pool_w,
            "pool_scale": pool_scale, "w_out": w_out, "ln1_g": ln1_g, "ln1_b": ln1_b,
            "ffn_wg": ffn_wg, "ffn_wu": ffn_wu, "ffn_wd": ffn_wd, "moe_router": moe_router,
            "moe_wg": moe_wg, "moe_wu": moe_wu, "moe_wd": moe_wd, "ln2_g": ln2_g, "ln2_b": ln2_b}


def reference(x, ln_in_g, ln_in_b, w_in, pool_w, pool_scale, w_out, ln1_g, ln1_b,
              ffn_wg, ffn_wu, ffn_wd, moe_router, moe_wg, moe_wu, moe_wd, ln2_g, ln2_b):
    B, S, _ = x.shape
    h = layer_norm(x, ln_in_g, ln_in_b)
    for layer in range(DEPTH):
        proj = h @ w_in[layer]
        u_pool = proj[..., :POOL_WIDTH]
        qkv = proj[..., POOL_WIDTH:].reshape(B, S, 3, SB_HEADS, SB_HEAD_DIM)
        qkv = jnp.transpose(qkv, (2, 0, 3, 1, 4))
        y_pool = pool_mixer(u_pool, pool_w[layer], pool_scale[layer])
        y_sb = stick_breaking_attention(qkv[0], qkv[1], qkv[2])
        y_sb = jnp.transpose(y_sb, (0, 2, 1, 3)).reshape(B, S, SB_WIDTH)
        mix = jnp.concatenate([y_pool, y_sb], axis=-1) @ w_out[layer]
        h = layer_norm(DN_ALPHA * h + mix, ln1_g[layer], ln1_b[layer])
        if layer % 2 == 0:
            i = layer // 2
            f = swiglu(h, ffn_wg[i], ffn_wu[i], ffn_wd[i])
        else:
            i = layer // 2
            f = moe_ffn(h, moe_router[i], moe_wg[i], moe_wu[i], moe_wd[i])
        h = layer_norm(DN_ALPHA * h + f, ln2_g[layer], ln2_b[layer])
    return h
```

```python
import numpy as np
from contextlib import ExitStack
import concourse.bass as bass
import concourse.mybir as mybir
from concourse.bass_utils import run_bass_kernel_spmd

F32 = mybir.dt.float32
BF16 = mybir.dt.bfloat16
I32 = mybir.dt.int32
U32 = mybir.dt.uint32
AF = mybir.ActivationFunctionType
ALU = mybir.AluOpType
AX = mybir.AxisListType

NCORES = 8
T = 4096
D = 1024
NT = T // 128
DEPTH = 2
DFF = 2816
NFC = DFF // 128
NE = 8
CAP = 1280
NST = CAP // 128
ALPHA = float((2 * DEPTH) ** 0.25)
EPS = 1e-5
ENG = ("sync", "act", "pe", "dve", "pool")


_UNIQ = [0]


def uniq(n):
    _UNIQ[0] += 1
    return f"{n}_{_UNIQ[0]}"


def I(meth, *a, **k):
    return lambda e: getattr(e, meth)(*a, **k)


class TT:
    __slots__ = ("t", "w", "r", "name", "excl")

    def __init__(self, t, name="", excl=False):
        self.t = t
        self.w = None
        self.r = {}
        self.name = name
        self.excl = excl


class Prog:
    def __init__(self, nc, es):
        self.nc = nc
        self.es = es
        self.q = {e: [] for e in ENG}
        self.waited = {e: {} for e in ENG}
        self.psem = {e: self._mksem("p_" + e) for e in ENG}
        self.dsems = []
        self.nops = 0

    def _mksem(self, name):
        h = self.es.enter_context(self.nc.semaphore(name))
        return {"h": h, "n": 0, "name": name}

    def dma_sem(self, name):
        if getattr(self, "dpool", None):
            s = self.dpool.pop()
        else:
            s = self._mksem(uniq(name))
        self.dsems.append(s)
        return s

    def _wait(self, eng, dep):
        s, v = dep
        if eng == "pe" and s is self.psem["pe"]:
            return
        if self.waited[eng].get(s["name"], 0) >= v:
            return
        self.waited[eng][s["name"]] = v
        self.q[eng].append(("wait", s["h"], v))

    def _deps(self, R, W):
        deps = []
        for b in R:
            if b.w is not None:
                deps.append(b.w)
        for b in W:
            if b.w is not None:
                deps.append(b.w)
            deps.extend(b.r.values())
        return deps

    def _mark(self, dep, R, W):
        for b in R:
            b.r[dep[0]["name"]] = dep
        for b in W:
            b.w = dep
            b.r = {}

    def op(self, eng, fn, R=(), W=()):
        if any(b.excl for b in R):
            W = list(W) + [b for b in R if b.excl and b not in W]
            R = [b for b in R if not b.excl]
        for d in self._deps(R, W):
            self._wait(eng, d)
        s = self.psem[eng]
        s["n"] += 1
        dep = (s, s["n"])
        self.q[eng].append(("op", fn, s["h"], 1))
        self._mark(dep, R, W)
        self.nops += 1
        return dep

    def dma(self, eng, out, in_, sem, R=(), W=()):
        for d in self._deps(R, W):
            if d[0] is sem:
                continue
            self._wait(eng, d)
        sem["n"] += 16
        dep = (sem, sem["n"])
        self.q[eng].append(("op", lambda e: e.dma_start(out=out, in_=in_), sem["h"], 16))
        self._mark(dep, R, W)
        self.nops += 1
        return dep

    def batch(self, sem, tts):
        for b in tts:
            b.w = (sem, sem["n"])

    def gather(self, out, in_, idx_ap, sem, R=(), W=()):
        eng = "pool"
        for d in self._deps(R, W):
            self._wait(eng, d)
        sem["n"] += 16
        dep = (sem, sem["n"])
        self.q[eng].append(("op", lambda e: e.indirect_dma_start(
            out=out, out_offset=None, in_=in_,
            in_offset=bass.IndirectOffsetOnAxis(ap=idx_ap, axis=0)), sem["h"], 16))
        self._mark(dep, R, W)
        self.nops += 1
        return dep

    def barrier(self):
        sems = list(self.psem.values()) + self.dsems
        for e in ENG:
            for s in sems:
                if s["n"] > 0:
                    self._wait(e, (s, s["n"]))
        if not hasattr(self, "dpool"):
            self.dpool = []
        self.dpool.extend(self.dsems)
        self.dsems = []
        for e in ENG:
            if self.psem[e]["n"] > 8000:
                self.psem[e] = self._mksem(uniq("p_" + e))

    def flush(self):
        nc = self.nc
        q = self.q

        def run(e, lst):
            for it in lst:
                if it[0] == "wait":
                    e.wait_ge(it[1], it[2])
                else:
                    ins = it[1](e)
                    ins.then_inc(it[2], it[3])

        with nc.Block() as block:
            @block.sync
            def _(e):
                run(e, q["sync"])

            @block.scalar
            def _(e):
                run(e, q["act"])

            @block.tensor
            def _(e):
                run(e, q["pe"])

            @block.vector
            def _(e):
                run(e, q["dve"])

            @block.gpsimd
            def _(e):
                run(e, q["pool"])


class Ctx:
    pass


def bcast_row(ap1d, n):
    return bass.AP(ap1d.tensor, ap1d.offset, [[0, 128], [1, n]])


def alloc_ln(C, es, want_T=True):
    nc, P = C.nc, C.P
    L = Ctx()
    sb = lambda n, s, d: es.enter_context(nc.sbuf_tensor(uniq(n), s, d))
    L.stats = [TT(sb(f"ln_stats{i}", [128, 12], F32)) for i in range(2)]
    L.mv = [TT(sb(f"ln_mv{i}", [128, 8], F32)) for i in range(2)]
    L.xn = [TT(sb(f"ln_xn{i}", [128, D], F32)) for i in range(2)]
    L.ho = [TT(sb(f"ln_ho{i}", [128, D], F32)) for i in range(2)]
    L.want_T = want_T
    if want_T:
        L.hTs = [TT(sb(f"ln_hTs{i}", [128, 8, 512], BF16)) for i in range(2)]
    L.sem_st = [P.dma_sem("ln_st0"), P.dma_sem("ln_st1")]
    L.sem_sT = [P.dma_sem("ln_sT0"), P.dma_sem("ln_sT1")]
    L.cnt = 0
    L.pend = []
    return L


def ln_tile(C, L, tt, pre, gam, bet, h_dst, hT_dst, tr_banks=None, router=None):
    i = L.cnt
    L.cnt += 1
    L.pend.append(dict(i=i, tt=tt, pre=pre, gam=gam, bet=bet, h_dst=h_dst, hT_dst=hT_dst, tr=tr_banks, router=router))
    _ln_step(C, L)


def ln_flush(C, L):
    for _ in range(2):
        L.pend.append(None)
        _ln_step(C, L)
    L.pend = []


def _ln_step(C, L):
    P = C.P
    n = len(L.pend)
    t0 = L.pend[n - 1]
    t1 = L.pend[n - 2] if n >= 2 else None
    t2 = L.pend[n - 3] if n >= 3 else None
    if t0 is not None:
        b = t0["i"] % 2
        st, mv, pre = L.stats[b], L.mv[b], t0["pre"]
        P.op("dve", I("bn_stats", st.t[:, 0:6], pre.t[:, 0:512]), R=[pre], W=[st])
        P.op("dve", I("bn_stats", st.t[:, 6:12], pre.t[:, 512:1024]), R=[pre], W=[st])
        P.op("dve", I("bn_aggr", mv.t[:, 0:2], st.t[:, :]), R=[st], W=[mv])
        P.op("dve", I("tensor_scalar_add", mv.t[:, 2:3], mv.t[:, 1:2], EPS), R=[mv], W=[mv])
    if t0 is not None:
        b = t0["i"] % 2
        mv = L.mv[b]
        P.op("act", I("activation", out=mv.t[:, 3:4], in_=mv.t[:, 2:3], func=AF.Ln), R=[mv], W=[mv])
        P.op("act", I("activation", out=mv.t[:, 4:5], in_=mv.t[:, 3:4], func=AF.Exp, scale=-0.5), R=[mv], W=[mv])
    if t1 is not None:
        b = t1["i"] % 2
        mv, xn, ho, pre = L.mv[b], L.xn[b], L.ho[b], t1["pre"]
        P.op("dve", I("scalar_tensor_tensor", xn.t[:, :], pre.t[:, :], mv.t[:, 0:1], t1["gam"].t[:, :], op0=ALU.subtract, op1=ALU.mult),
             R=[pre, mv, t1["gam"]], W=[xn])
        P.op("dve", I("scalar_tensor_tensor", ho.t[:, :], xn.t[:, :], mv.t[:, 4:5], t1["bet"].t[:, :], op0=ALU.mult, op1=ALU.add),
             R=[xn, mv, t1["bet"]], W=[ho])
        P.dma("sync", t1["h_dst"][1], ho.t[:, :], L.sem_st[b], R=[ho], W=[t1["h_dst"][0]])
        if t1["router"] is not None:
            t1["router"](t1["tt"], ho)
    if t2 is not None and t2["hT_dst"] is not None:
        b = t2["i"] % 2
        ho = L.ho[b]
        tt = t2["tt"]
        slot = tt % 4
        hTs = L.hTs[(tt // 4) % 2]
        bk = t2["tr"]
        flat = C.ps2(bk)
        for k in range(8):
            P.op("pe", I("transpose", flat[:, k * 128:(k + 1) * 128], ho.t[:, k * 128:(k + 1) * 128], C.ident.t[:, :]),
                 R=[ho, C.ident], W=[C.PS[bk], C.PS[bk + 1]])
        P.op("act", I("activation", out=hTs.t[:, :, slot * 128:(slot + 1) * 128],
                      in_=C.ps2v(bk, 8, 128), func=AF.Copy),
             R=[C.PS[bk], C.PS[bk + 1]], W=[hTs])
        if slot == 3:
            c0 = (tt - 3) * 128
            dst = t2["hT_dst"][1].rearrange("(c p) t -> p c t", p=128)[:, :, c0:c0 + 512]
            P.dma("sync", dst, hTs.t[:, :, :], L.sem_sT[(tt // 4) % 2], R=[hTs], W=[t2["hT_dst"][0]])


def phase_ln_in(C):
    nc, P = C.nc, C.P
    with ExitStack() as es:
        sb = lambda n, s, d: es.enter_context(nc.sbuf_tensor(uniq(n), s, d))
        gam = TT(sb("a_gam", [128, D], F32))
        bet = TT(sb("a_bet", [128, D], F32))
        xin = [TT(sb(f"a_xin{i}", [128, D], F32)) for i in range(2)]
        sem_c = P.dma_sem("a_c")
        sem_x = [P.dma_sem("a_x0"), P.dma_sem("a_x1")]
        P.dma("sync", gam.t[:, :], bcast_row(C.d["ln_in_g"], D), sem_c, W=[gam])
        P.dma("sync", bet.t[:, :], bcast_row(C.d["ln_in_b"], D), sem_c, W=[bet])
        P.batch(sem_c, [gam, bet])
        L = alloc_ln(C, es)
        for tt in range(NT):
            b = tt % 2
            P.dma("sync", xin[b].t[:, :], C.d["x"][tt * 128:(tt + 1) * 128, :], sem_x[b], W=[xin[b]])
            ln_tile(C, L, tt, xin[b], gam, bet, (C.Dha, C.ha[tt * 128:(tt + 1) * 128, :]), (C.DhTa, C.hTa), tr_banks=0 + 2 * (tt % 2))
        ln_flush(C, L)
        P.barrier()


def phase_proj(C, l):
    nc, P = C.nc, C.P
    with ExitStack() as es:
        sb = lambda n, s, d: es.enter_context(nc.sbuf_tensor(uniq(n), s, d))
        wi = TT(sb("b_wi", [128, 8, 2048], BF16))
        hT = [TT(sb(f"b_hT{k}", [128, T], BF16)) for k in range(8)]
        pw = TT(sb("b_pw", [128, 4, 128], BF16))
        psc = TT(sb("b_psc", [128, 4], F32))
        invc = TT(sb("b_invc", [128, 4, 16], F32))
        U = TT(sb("b_U", [128, 16 + T], F32))
        SA = TT(sb("b_SA", [128, 16 + T], F32))
        SB = TT(sb("b_SB", [128, 16 + T], F32))
        dd = TT(sb("b_d", [128, T], BF16))
        stg = [TT(sb(f"b_stg{i}", [128, T], BF16)) for i in range(2)]
        vst = [TT(sb(f"b_vst{i}", [128, 4, 512], BF16)) for i in range(2)]
        sem_w = P.dma_sem("b_w")
        sem_h = P.dma_sem("b_h")
        sem_c = P.dma_sem("b_c")
        sem_s = [P.dma_sem("b_s0"), P.dma_sem("b_s1")]
        sem_v = [P.dma_sem("b_v0"), P.dma_sem("b_v1")]
        for q4 in range(4):
            P.dma("pool", wi.t[:, :, q4 * 512:(q4 + 1) * 512], C.d["w_in"][l][:, :, q4 * 512:(q4 + 1) * 512], sem_w, W=[wi])
        for k in range(8):
            P.dma("sync", hT[k].t[:, :], C.hTa[k * 128:(k + 1) * 128, :], sem_h, R=[C.DhTa], W=[hT[k]])
        P.dma("pool", pw.t[:, :, :], C.d["pool_w"][l], sem_c, W=[pw])
        P.dma("sync", psc.t[:, :], C.d["pool_scale"][l], sem_c, W=[psc])
        P.dma("sync", invc.t[:, :, :], C.d["c_invc"], sem_c, W=[invc])
        P.batch(sem_c, [pw, psc, invc])
        P.batch(sem_h, hT)
        for buf in (U, SA, SB):
            P.op("dve", I("memset", buf.t[:, 0:16], 0.0), W=[buf])

        nbank = [0]

        def next_bank():
            b = nbank[0] % 4
            nbank[0] += 1
            return b

        evac_eng = [0]

        def evac(out_ap, in_ap, R, W, scale=None):
            e = evac_eng[0] % 2
            evac_eng[0] += 1
            if e == 0:
                if scale is None:
                    P.op("act", I("activation", out=out_ap, in_=in_ap, func=AF.Copy), R=R, W=W)
                else:
                    P.op("act", I("activation", out=out_ap, in_=in_ap, func=AF.Identity, scale=scale), R=R, W=W)
            else:
                if scale is None:
                    P.op("dve", I("tensor_copy", out_ap, in_ap), R=R, W=W)
                else:
                    P.op("dve", I("tensor_scalar_mul", out_ap, in_ap, scale), R=R, W=W)

        def fm_chunk(c, out_fn):
            for tg in range(8):
                bk = next_bank()
                for k in range(8):
                    P.op("pe", I("matmul", C.bank(bk), wi.t[:, k, c * 128:(c + 1) * 128], hT[k].t[:, tg * 512:(tg + 1) * 512],
                                 start=(k == 0), stop=(k == 7)), R=[wi, hT[k]], W=[C.PS[bk]])
                out_fn(tg, bk)

        sidx = [0]
        for g in range(4):
            w = 2 << g
            fm_chunk(g, lambda tg, bk: evac(U.t[:, 16 + tg * 512:16 + (tg + 1) * 512], C.bank(bk), [C.PS[bk]], [U]))
            src = U
            bufs = [SA, SB]
            nlev = g + 1
            for lev in range(nlev):
                dst = bufs[lev % 2]
                sh = 1 << lev
                eng = "dve"
                P.op(eng, I("tensor_tensor", dst.t[:, 16:16 + T], src.t[:, 16:16 + T], src.t[:, 16 - sh:16 + T - sh], op=ALU.add),
                     R=[src], W=[dst])
                src = dst
            P.op("dve", I("scalar_tensor_tensor", dd.t[:, :], src.t[:, 16:16 + T], 1.0 / w, U.t[:, 16:16 + T],
                          op0=ALU.mult, op1=ALU.subtract), R=[src, U], W=[dd])
            P.op("dve", I("tensor_tensor", SA.t[:, 0:16] if src is SB else SB.t[:, 0:16], src.t[:, 16:32], invc.t[:, g, :], op=ALU.mult),
                 R=[src, invc], W=[SA if src is SB else SB])
            tmp = SA if src is SB else SB
            P.op("dve", I("tensor_tensor", dd.t[:, 0:16], tmp.t[:, 0:16], U.t[:, 16:32], op=ALU.subtract),
                 R=[tmp, U], W=[dd])
            P.op("dve", I("memset", tmp.t[:, 0:16], 0.0), W=[tmp])
            so = stg[sidx[0] % 2]
            ss = sem_s[sidx[0] % 2]
            sidx[0] += 1
            for tg in range(8):
                bk = next_bank()
                P.op("pe", I("matmul", C.bank(bk), pw.t[:, g, :], dd.t[:, tg * 512:(tg + 1) * 512], start=True, stop=True),
                     R=[pw, dd], W=[C.PS[bk]])
                evac(so.t[:, tg * 512:(tg + 1) * 512], C.bank(bk), [C.PS[bk], psc], [so], scale=psc.t[:, g:g + 1])
            P.dma("sync", C.ycT[g * 128:(g + 1) * 128, :], so.t[:, :], ss, R=[so], W=[C.DycT[g]])
        for c in range(4, 12):
            so = stg[sidx[0] % 2]
            ss = sem_s[sidx[0] % 2]
            sidx[0] += 1
            sc = 0.125 if c < 8 else None
            fm_chunk(c, lambda tg, bk, so=so, sc=sc: evac(so.t[:, tg * 512:(tg + 1) * 512], C.bank(bk), [C.PS[bk]], [so], scale=sc))
            if c < 8:
                P.dma("sync", C.qT[(c - 4) * 128:(c - 3) * 128, :], so.t[:, :], ss, R=[so], W=[C.DqT[c - 4]])
            else:
                P.dma("sync", C.kT[(c - 8) * 128:(c - 7) * 128, :], so.t[:, :], ss, R=[so], W=[C.DkT[c - 8]])
        vv = C.v.rearrange("(n p) c -> p n c", p=128)
        for tt in range(NT):
            bk = next_bank()
            for k in range(8):
                P.op("pe", I("matmul", C.bank(bk), hT[k].t[:, tt * 128:(tt + 1) * 128], wi.t[:, k, 1536:2048],
                             start=(k == 0), stop=(k == 7)), R=[wi, hT[k]], W=[C.PS[bk]])
            vs = vst[(tt // 4) % 2]
            evac(vs.t[:, tt % 4, :], C.bank(bk), [C.PS[bk]], [vs])
            if tt % 4 == 3:
                P.dma("sync", vv[:, tt - 3:tt + 1, :], vs.t[:, :, :], sem_v[(tt // 4) % 2], R=[vs], W=[C.Dv])
        P.barrier()


def phase_attn(C, l):
    nc, P = C.nc, C.P
    with ExitStack() as es:
        sb = lambda n, s, d: es.enter_context(nc.sbuf_tensor(uniq(n), s, d))
        qT = [TT(sb(f"c_qT{i}", [128, T], BF16)) for i in range(4)]
        kT = [TT(sb(f"c_kT{i}", [128, T], BF16)) for i in range(4)]
        V = TT(sb("c_V", [128, NT, 512], BF16))
        E = [TT(sb(f"c_E{i}", [128, 2048], F32)) for i in range(2)]
        SP = [TT(sb(f"c_SP{i}", [128, 2048], BF16)) for i in range(4)]
        CA = [TT(sb(f"c_CA{i}", [128, 2048], BF16)) for i in range(4)]
        AT = [TT(sb(f"c_AT{i}", [128, 2048], BF16)) for i in range(3)]
        ys = [TT(sb(f"c_ys{i}", [128, T], BF16)) for i in range(2)]
        sem_l = P.dma_sem("c_l")
        sem_s = [P.dma_sem("c_s0"), P.dma_sem("c_s1")]
        for i in range(4):
            P.dma("sync", qT[i].t[:, :], C.qT[i * 128:(i + 1) * 128, :], sem_l, R=[C.DqT[i]], W=[qT[i]])
            P.dma("sync", kT[i].t[:, :], C.kT[i * 128:(i + 1) * 128, :], sem_l, R=[C.DkT[i]], W=[kT[i]])
        vv = C.v.rearrange("(n p) c -> p n c", p=128)
        for i in range(4):
            P.dma("sync", V.t[:, i * 8:(i + 1) * 8, :], vv[:, i * 8:(i + 1) * 8, :], sem_l, R=[C.Dv], W=[V])
        P.batch(sem_l, qT + kT + [V])

        ZP = [0, 2]
        BP = 4
        OB = [6, 7]
        quads = []
        for hp in range(4):
            for e in range(2):
                for g in range(8):
                    nq = g + 1
                    for qi in range(nq):
                        js = [4 * g + 3 - 4 * qi - k for k in range(4)]
                        quads.append(dict(hp=hp, e=e, g=g, js=js, first=(qi == 0), last=(qi == nq - 1)))
        n = len(quads)
        masks2 = C.masks.t[:, :, :].rearrange("p a b -> p (a b)")

        def zq(s):
            q = quads[s]
            hp, e, g = q["hp"], q["e"], q["g"]
            pr = slice(64 * e, 64 * e + 64)
            for k in range(4):
                bk = ZP[k // 2] + k % 2
                j = q["js"][k]
                P.op("pe", I("matmul", C.bank(bk), kT[hp].t[pr, j * 128:(j + 1) * 128], qT[hp].t[pr, g * 512:(g + 1) * 512],
                             start=True, stop=True), R=[kT[hp], qT[hp]], W=[C.PS[bk]])

        def act1(s):
            Eb = E[s % 2]
            for p in range(2):
                P.op("act", I("activation", out=Eb.t[:, p * 1024:(p + 1) * 1024], in_=C.ps2(ZP[p]), func=AF.Exp),
                     R=[C.PS[ZP[p]], C.PS[ZP[p] + 1]], W=[Eb])

        def act2(s):
            P.op("act", I("activation", out=SP[s % 4].t[:, :], in_=E[s % 2].t[:, :], func=AF.Ln, bias=1.0), R=[E[s % 2]], W=[SP[s % 4]])

        def dve3(s):
            q = quads[s]
            S, Cq, Cn = SP[s % 4], CA[s % 4], CA[(s + 1) % 4]
            sl = lambda k: slice(k * 512, (k + 1) * 512)
            if q["first"]:
                P.op("dve", I("tensor_tensor", S.t[:, :], S.t[:, :], masks2, op=ALU.mult), R=[S, C.masks], W=[S])
                P.op("dve", I("tensor_tensor", Cq.t[:, sl(2)], S.t[:, sl(0)], S.t[:, sl(1)], op=ALU.add), R=[S], W=[Cq])
            else:
                P.op("dve", I("tensor_tensor", Cq.t[:, sl(1)], Cq.t[:, sl(0)], S.t[:, sl(0)], op=ALU.add), R=[S, Cq], W=[Cq])
                P.op("dve", I("tensor_tensor", Cq.t[:, sl(2)], Cq.t[:, sl(1)], S.t[:, sl(1)], op=ALU.add), R=[S, Cq], W=[Cq])
            P.op("dve", I("tensor_tensor", Cq.t[:, sl(3)], Cq.t[:, sl(2)], S.t[:, sl(2)], op=ALU.add), R=[S, Cq], W=[Cq])
            if not q["last"]:
                P.op("dve", I("tensor_tensor", Cn.t[:, sl(0)], Cq.t[:, sl(3)], S.t[:, sl(3)], op=ALU.add), R=[S, Cq], W=[Cn])

        def bq(s, p):
            q = quads[s]
            hp, e, g = q["hp"], q["e"], q["g"]
            pr = slice(64 * e, 64 * e + 64)
            S, Cq = SP[s % 4], CA[s % 4]
            for k in (2 * p, 2 * p + 1):
                bk = BP + k % 2
                j = q["js"][k]
                if q["first"] and k == 0:
                    car = None
                elif q["first"] and k == 1:
                    car = (S, S.t[:, 0:512])
                else:
                    car = (Cq, Cq.t[:, k * 512:(k + 1) * 512])
                P.op("pe", I("matmul", C.bank(bk), kT[hp].t[pr, j * 128:(j + 1) * 128], qT[hp].t[pr, g * 512:(g + 1) * 512],
                             start=True, stop=False), R=[kT[hp], qT[hp]], W=[C.PS[bk]])
                P.op("pe", I("matmul", C.bank(bk), C.negU.t[:, :], S.t[:, k * 512:(k + 1) * 512], start=False, stop=(car is None)),
                     R=[C.negU, S], W=[C.PS[bk]])
                if car is not None:
                    P.op("pe", I("matmul", C.bank(bk), C.negO.t[:, :], car[1], start=False, stop=True),
                         R=[C.negO, car[0]], W=[C.PS[bk]])

        def act3(s, p):
            A = AT[s % 3]
            P.op("act", I("activation", out=A.t[:, p * 1024:(p + 1) * 1024], in_=C.ps2(BP), func=AF.Exp),
                 R=[C.PS[BP], C.PS[BP + 1]], W=[A])

        def mask_att(s):
            q = quads[s]
            if q["first"]:
                A = AT[s % 3]
                P.op("dve", I("tensor_tensor", A.t[:, :], A.t[:, :], masks2, op=ALU.mult), R=[A, C.masks], W=[A])

        def av(s):
            q = quads[s]
            hp, e, g = q["hp"], q["e"], q["g"]
            gi = (hp * 2 + e) * 8 + g
            ob = OB[gi % 2]
            A = AT[s % 3]
            pr = slice(64 * e, 64 * e + 64)
            for k in range(4):
                j = q["js"][k]
                P.op("pe", I("matmul", C.bank(ob)[pr, :], V.t[:, j, hp * 128 + 64 * e: hp * 128 + 64 * e + 64], A.t[:, k * 512:(k + 1) * 512],
                             start=(q["first"] and k == 0), stop=(q["last"] and k == 3)), R=[V, A], W=[C.PS[ob]])
            if q["last"]:
                yb = ys[hp % 2]
                P.op("dve", I("tensor_copy", yb.t[pr, g * 512:(g + 1) * 512], C.bank(ob)[pr, :]), R=[C.PS[ob]], W=[yb])
                if e == 1 and g == 7:
                    P.dma("sync", C.ycT[512 + hp * 128:512 + (hp + 1) * 128, :], yb.t[:, :], sem_s[hp % 2], R=[yb], W=[C.DycT[4 + hp]])

        for s in range(n + 3):
            if s < n:
                zq(s)
            if 0 <= s - 2 < n:
                bq(s - 2, 0)
                act3(s - 2, 0)
            if s < n:
                act1(s)
            if 0 <= s - 3 < n:
                av(s - 3)
            if 0 <= s - 2 < n:
                bq(s - 2, 1)
                act3(s - 2, 1)
            if s < n:
                act2(s)
                dve3(s)
            if 0 <= s - 2 < n:
                mask_att(s - 2)
        P.barrier()


def phase_mix_ln1(C, l):
    nc, P = C.nc, C.P
    moe = (l % 2 == 1)
    with ExitStack() as es:
        sb = lambda n, s, d: es.enter_context(nc.sbuf_tensor(uniq(n), s, d))
        yc = [TT(sb(f"d_yc{k}", [128, T], BF16)) for k in range(8)]
        wo = TT(sb("d_wo", [128, 8, D], BF16))
        gam = TT(sb("d_gam", [128, D], F32))
        bet = TT(sb("d_bet", [128, D], F32))
        hin = [TT(sb(f"d_hin{i}", [128, D], F32)) for i in range(2)]
        pre = [TT(sb(f"d_pre{i}", [128, D], F32)) for i in range(2)]
        sem_c = P.dma_sem("d_c")
        sem_y = P.dma_sem("d_y")
        sem_h = [P.dma_sem("d_h0"), P.dma_sem("d_h1")]
        P.dma("pool", wo.t[:, :, :], C.d["w_out"][l], sem_c, W=[wo])
        P.dma("sync", gam.t[:, :], bcast_row(C.d["ln1_g"][l], D), sem_c, W=[gam])
        P.dma("sync", bet.t[:, :], bcast_row(C.d["ln1_b"][l], D), sem_c, W=[bet])
        for k in range(8):
            P.dma("sync", yc[k].t[:, :], C.ycT[k * 128:(k + 1) * 128, :], sem_y, R=[C.DycT[k]], W=[yc[k]])
        P.batch(sem_y, yc)
        L = alloc_ln(C, es)
        router = None
        P.batch(sem_c, [wo, gam, bet])
        if moe:
            rt = TT(sb("d_rt", [128, 8, NE], F32))
            P.dma("sync", rt.t[:, :, :], C.d["moe_router"][l // 2], sem_c, W=[rt])
            P.batch(sem_c, [wo, gam, bet, rt])
            hT32 = [TT(sb(f"d_hT32{i}", [128, 8, 128], F32)) for i in range(2)]
            lg = [TT(sb(f"d_lg{i}", [128, 64], F32)) for i in range(2)]

            def router(tt, ho):
                b = tt % 2
                bk = 4 + 2 * b
                flat = C.ps2(bk)
                for k in range(8):
                    P.op("pe", I("transpose", flat[:, k * 128:(k + 1) * 128], ho.t[:, k * 128:(k + 1) * 128], C.ident.t[:, :]),
                         R=[ho, C.ident], W=[C.PS[bk], C.PS[bk + 1]])
                P.op("dve", I("tensor_copy", hT32[b].t[:, :, :], C.ps2v(bk, 8, 128)), R=[C.PS[bk], C.PS[bk + 1]], W=[hT32[b]])
                lb = C.bank(bk)[:, 0:NE]
                for k in range(8):
                    P.op("pe", I("matmul", lb, hT32[b].t[:, k, :], rt.t[:, k, :], start=(k == 0), stop=(k == 7)),
                         R=[hT32[b], rt], W=[C.PS[bk], C.PS[bk + 1]])
                g_ = lg[b]
                G = g_.t
                P.op("dve", I("tensor_copy", G[:, 0:8], lb), R=[C.PS[bk], C.PS[bk + 1]], W=[g_])
                P.op("dve", I("tensor_reduce", G[:, 8:9], G[:, 0:8], AX.X, ALU.max), R=[g_], W=[g_])
                P.op("dve", I("tensor_scalar", G[:, 9:10], G[:, 8:9], -1.0, None, op0=ALU.mult), R=[g_], W=[g_])
                P.op("dve", I("tensor_scalar", G[:, 10:18], G[:, 0:8], G[:, 8:9], -1e30, op0=ALU.is_equal, op1=ALU.mult), R=[g_], W=[g_])
                P.op("dve", I("tensor_tensor", G[:, 10:18], G[:, 10:18], G[:, 0:8], op=ALU.add), R=[g_], W=[g_])
                P.op("dve", I("tensor_reduce", G[:, 18:19], G[:, 10:18], AX.X, ALU.max), R=[g_], W=[g_])
                P.op("act", I("activation", out=G[:, 19:27], in_=G[:, 0:8], func=AF.Exp, bias=G[:, 9:10]), R=[g_], W=[g_])
                P.op("dve", I("tensor_scalar", G[:, 27:35], G[:, 0:8], G[:, 18:19], None, op0=ALU.is_ge), R=[g_], W=[g_])
                P.op("dve", I("tensor_tensor", G[:, 19:27], G[:, 19:27], G[:, 27:35], op=ALU.mult), R=[g_], W=[g_])
                P.op("dve", I("tensor_reduce", G[:, 35:36], G[:, 19:27], AX.X, ALU.add), R=[g_], W=[g_])
                P.op("dve", I("reciprocal", G[:, 36:37], G[:, 35:36]), R=[g_], W=[g_])
                P.op("dve", I("tensor_scalar", C.gate.t[:, tt, :], G[:, 19:27], G[:, 36:37], None, op0=ALU.mult), R=[g_], W=[C.gate])
                P.op("dve", I("tensor_copy", C.sel.t[:, tt, :], G[:, 27:35]), R=[g_], W=[C.sel])

        for tt in range(NT):
            b = tt % 2
            P.dma("sync", hin[b].t[:, :], C.ha[tt * 128:(tt + 1) * 128, :], sem_h[b], R=[C.Dha], W=[hin[b]])
            bk = 0 + 2 * b
            for nh in range(2):
                for k in range(8):
                    P.op("pe", I("matmul", C.bank(bk + nh), yc[k].t[:, tt * 128:(tt + 1) * 128], wo.t[:, k, nh * 512:(nh + 1) * 512],
                                 start=(k == 0), stop=(k == 7)), R=[yc[k], wo], W=[C.PS[bk], C.PS[bk + 1]])
            P.op("dve", I("scalar_tensor_tensor", pre[b].t[:, :], hin[b].t[:, :], ALPHA, C.ps2(bk), op0=ALU.mult, op1=ALU.add),
                 R=[hin[b], C.PS[bk], C.PS[bk + 1]], W=[pre[b]])
            ln_tile(C, L, tt, pre[b], gam, bet, (C.Dhb, C.hb[tt * 128:(tt + 1) * 128, :]),
                    None if moe else (C.DhTb, C.hTb), tr_banks=4 + 2 * b, router=router)
        ln_flush(C, L)
        P.barrier()


def alloc_ffn(C, es, ntok):
    nc, P = C.nc, C.P
    F = Ctx()
    sb = lambda n, s, d: es.enter_context(nc.sbuf_tensor(uniq(n), s, d))
    F.ntok = ntok
    F.wd = TT(sb("f_wd", [128, NFC, D], BF16))
    F.wg = [TT(sb(f"f_wg{i}", [128, 2, 8, 128], BF16)) for i in range(3)]
    F.wu = [TT(sb(f"f_wu{i}", [128, 2, 8, 128], BF16)) for i in range(3)]
    F.act = TT(sb("f_act", [128, NFC, ntok], BF16))
    F.sg = [TT(sb(f"f_sg{i}", [128, 512], BF16)) for i in range(2)]
    F.sem_wd = P.dma_sem("f_wd")
    F.sem_wg = [P.dma_sem(f"f_wg{i}") for i in range(3)]
    F.sem_wu = [P.dma_sem(f"f_wu{i}") for i in range(3)]
    F.cnt = 0
    F.blk = 0
    return F


def ffn_pass(C, F, xT, wg_d, wu_d, wd_d, consume, GB=(0, 1), UB=(2, 3), DB=4, blk_hook=None):
    P = C.P
    ntok = F.ntok
    subs = []
    t0 = 0
    while t0 < ntok:
        w = min(512, ntok - t0)
        subs.append((t0, w))
        t0 += w
    wdv = wd_d.rearrange("(c p) n -> p c n", p=128)
    for fb in range(NFC // 2):
        if fb == 2:
            P.dma("pool", F.wd.t[:, 0:11, :], wdv[:, 0:11, :], F.sem_wd, W=[F.wd])
            P.dma("pool", F.wd.t[:, 11:22, :], wdv[:, 11:22, :], F.sem_wd, W=[F.wd])
        bi = F.blk % 3
        F.blk += 1
        wg, wu = F.wg[bi], F.wu[bi]
        P.dma("pool", wg.t[:, :, :, :], wg_d[2 * fb:2 * fb + 2].rearrange("c p k f -> p c k f"), F.sem_wg[bi], W=[wg])
        P.dma("pool", wu.t[:, :, :, :], wu_d[2 * fb:2 * fb + 2].rearrange("c p k f -> p c k f"), F.sem_wu[bi], W=[wu])
        for ci in range(2):
            fc = 2 * fb + ci
            for (t0, w) in subs:
                i = F.cnt
                F.cnt += 1
                gb, ub = GB[i % 2], UB[i % 2]
                for k in range(8):
                    P.op("pe", I("matmul", C.bank(gb)[:, 0:w], wg.t[:, ci, k, :], xT[k].t[:, t0:t0 + w], start=(k == 0), stop=(k == 7)),
                         R=[wg, xT[k]], W=[C.PS[gb]])
                for k in range(8):
                    P.op("pe", I("matmul", C.bank(ub)[:, 0:w], wu.t[:, ci, k, :], xT[k].t[:, t0:t0 + w], start=(k == 0), stop=(k == 7)),
                         R=[wu, xT[k]], W=[C.PS[ub]])
                sg = F.sg[i % 2]
                P.op("act", I("activation", out=sg.t[:, 0:w], in_=C.bank(gb)[:, 0:w], func=AF.Silu), R=[C.PS[gb]], W=[sg])
                P.op("dve", I("tensor_tensor", F.act.t[:, fc, t0:t0 + w], sg.t[:, 0:w], C.bank(ub)[:, 0:w], op=ALU.mult),
                     R=[sg, C.PS[ub]], W=[F.act])
        if blk_hook is not None:
            blk_hook(fb)
    for tt in range(ntok // 128):
        for nh in range(2):
            for fc in range(NFC):
                P.op("pe", I("matmul", C.bank(DB + nh), F.act.t[:, fc, tt * 128:(tt + 1) * 128], F.wd.t[:, fc, nh * 512:(nh + 1) * 512],
                             start=(fc == 0), stop=(fc == NFC - 1)), R=[F.act, F.wd], W=[C.PS[DB], C.PS[DB + 1]])
        consume(tt, DB)


def phase_ffn_dense(C, l, final):
    nc, P = C.nc, C.P
    TG = 1024
    with ExitStack() as es:
        sb = lambda n, s, d: es.enter_context(nc.sbuf_tensor(uniq(n), s, d))
        gam = TT(sb("e_gam", [128, D], F32))
        bet = TT(sb("e_bet", [128, D], F32))
        hin = [TT(sb(f"e_hin{i}", [128, D], F32)) for i in range(2)]
        pre = [TT(sb(f"e_pre{i}", [128, D], F32)) for i in range(2)]
        xT = [[TT(sb(f"e_xT{j}_{k}", [128, TG], BF16)) for k in range(8)] for j in range(1)]
        sem_c = P.dma_sem("e_c")
        sem_x = [P.dma_sem("e_x0"), P.dma_sem("e_x1")]
        sem_h = [P.dma_sem("e_h0"), P.dma_sem("e_h1")]
        P.dma("sync", gam.t[:, :], bcast_row(C.d["ln2_g"][l], D), sem_c, W=[gam])
        P.dma("sync", bet.t[:, :], bcast_row(C.d["ln2_b"][l], D), sem_c, W=[bet])
        P.batch(sem_c, [gam, bet])
        L = alloc_ln(C, es, want_T=not final)
        F = alloc_ffn(C, es, TG)
        i = l // 2
        cnt = [0]
        for grp in range(T // TG):
            xb = xT[0]
            for k in range(8):
                P.dma("sync", xb[k].t[:, :], C.hTb[k * 128:(k + 1) * 128, grp * TG:(grp + 1) * TG], sem_x[grp % 2], R=[C.DhTb], W=[xb[k]])
            P.batch(sem_x[grp % 2], xb)

            def consume(tt, db, grp=grp):
                gt = grp * (TG // 128) + tt
                b = cnt[0] % 2
                cnt[0] += 1
                P.dma("sync", hin[b].t[:, :], C.hb[gt * 128:(gt + 1) * 128, :], sem_h[b], R=[C.Dhb], W=[hin[b]])
                P.op("dve", I("scalar_tensor_tensor", pre[b].t[:, :], hin[b].t[:, :], ALPHA, C.ps2(db), op0=ALU.mult, op1=ALU.add),
                     R=[hin[b], C.PS[db], C.PS[db + 1]], W=[pre[b]])
                if final:
                    ln_tile(C, L, gt, pre[b], gam, bet, (C.Dout, C.out[gt * 128:(gt + 1) * 128, :]), None)
                else:
                    ln_tile(C, L, gt, pre[b], gam, bet, (C.Dha, C.ha[gt * 128:(gt + 1) * 128, :]), (C.DhTa, C.hTa), tr_banks=6)

            ffn_pass(C, F, xb, C.d["ffn_wg"][i], C.d["ffn_wu"][i], C.d["ffn_wd"][i], consume)
        ln_flush(C, L)
        P.barrier()


def phase_moe(C, l, final, stop=3):
    nc, P = C.nc, C.P
    mi = l // 2
    with ExitStack() as es:
        sb = lambda n, s, d: es.enter_context(nc.sbuf_tensor(uniq(n), s, d))
        selb = TT(sb("m_selb", [128, NT, NE], BF16))
        csel = TT(sb("m_csel", [128, NT, NE], BF16))
        cum = TT(sb("m_cum", [128, NT, NE], F32))
        pos = TT(sb("m_pos", [128, NT, NE], F32))
        pre_ = TT(sb("m_pre", [128, NT, NE], F32))
        tmp = TT(sb("m_tmp", [128, NT, NE], F32))
        tmp2 = TT(sb("m_tmp2", [128, NT, NE], F32))
        iot = TT(sb("m_iot", [128, CAP], F32))
        ecap = TT(sb("m_ecap", [128, NT, NE], F32))
        A = [TT(sb(f"m_A{i}", [128, NT, CAP], BF16)) for i in range(2)]
        tokf = TT(sb("m_tokf", [128, NE, NST], F32))
        sem_c = P.dma_sem("m_c")
        P.dma("sync", iot.t[:, :], C.d["c_iota"], sem_c, W=[iot])
        P.dma("sync", ecap.t[:, :, :], C.d["c_ecap"], sem_c, W=[ecap])
        P.batch(sem_c, [iot, ecap])
        P.op("dve", I("tensor_copy", selb.t[:, :, :], C.sel.t[:, :, :]), R=[C.sel], W=[selb])
        P.op("dve", I("memset", csel.t[:, 0, :], 0.0), W=[csel])
        for tt in range(1, NT):
            P.op("dve", I("tensor_tensor", csel.t[:, tt, :], csel.t[:, tt - 1, :], selb.t[:, tt - 1, :], op=ALU.add), R=[csel, selb], W=[csel])
        bk = 0
        for tt in range(NT):
            o = C.bank(bk)[:, tt * NE:(tt + 1) * NE]
            P.op("pe", I("matmul", o, C.lowI.t[:, :], selb.t[:, tt, :], start=True, stop=False), R=[C.lowI, selb], W=[C.PS[bk]])
            P.op("pe", I("matmul", o, C.ones.t[:, :], csel.t[:, tt, :], start=False, stop=True), R=[C.ones, csel], W=[C.PS[bk]])
        P.op("dve", I("tensor_copy", cum.t[:, :, :], C.bank(bk)[:, 0:NT * NE].rearrange("p (a b) -> p a b", b=NE)), R=[C.PS[bk]], W=[cum])
        P.op("dve", I("tensor_tensor", pos.t[:, :, :], cum.t[:, :, :], C.sel.t[:, :, :], op=ALU.subtract), R=[cum, C.sel], W=[pos])
        P.op("dve", I("memset", pre_.t[:, :, :], 0.0), W=[pre_])
        P.op("dve", I("tensor_copy", pre_.t[:, :, 1:NE], C.sel.t[:, :, 0:NE - 1]), R=[C.sel], W=[pre_])
        for sh in (1, 2, 4):
            P.op("dve", I("tensor_copy", tmp.t[:, :, :], pre_.t[:, :, :]), R=[pre_], W=[tmp])
            P.op("dve", I("tensor_tensor", pre_.t[:, :, sh:NE], tmp.t[:, :, sh:NE], tmp.t[:, :, 0:NE - sh], op=ALU.add), R=[tmp], W=[pre_])
        P.op("dve", I("tensor_tensor", pos.t[:, :, :], pos.t[:, :, :], ecap.t[:, :, :], op=ALU.add), R=[pos, ecap], W=[pos])
        for kk in range(2):
            P.op("dve", I("tensor_scalar", tmp.t[:, :, :], pre_.t[:, :, :], float(kk), None, op0=ALU.is_equal), R=[pre_], W=[tmp])
            P.op("dve", I("tensor_tensor", tmp.t[:, :, :], tmp.t[:, :, :], C.sel.t[:, :, :], op=ALU.mult), R=[tmp, C.sel], W=[tmp])
            P.op("dve", I("tensor_tensor", tmp2.t[:, :, :], tmp.t[:, :, :], pos.t[:, :, :], op=ALU.mult), R=[tmp, pos], W=[tmp2])
            P.op("dve", I("tensor_reduce", C.slotf.t[:, kk, :], tmp2.t[:, :, :], AX.X, ALU.add), R=[tmp2], W=[C.slotf])
            P.op("dve", I("tensor_tensor", tmp2.t[:, :, :], tmp.t[:, :, :], C.gate.t[:, :, :], op=ALU.mult), R=[tmp, C.gate], W=[tmp2])
            P.op("dve", I("tensor_reduce", C.gsel.t[:, kk, :], tmp2.t[:, :, :], AX.X, ALU.add), R=[tmp2], W=[C.gsel])
        P.op("dve", I("tensor_copy", C.sloti.t[:, :, :], C.slotf.t[:, :, :]), R=[C.slotf], W=[C.sloti])
        ai = 0
        for e in range(NE):
            bk = 1 + (e % 2)
            Ab = A[e % 2]
            for tt in range(NT):
                eng = "dve"
                ai += 1
                P.op(eng, I("tensor_scalar", Ab.t[:, tt, :], iot.t[:, :], cum.t[:, tt, e:e + 1], None, op0=ALU.is_ge), R=[iot, cum], W=[Ab])
            for st in range(NST):
                for tt in range(NT):
                    P.op("pe", I("matmul", C.bank(bk)[:, st:st + 1], Ab.t[:, tt, st * 128:(st + 1) * 128], C.ones.t[:, 0:1],
                                 start=(tt == 0), stop=(tt == NT - 1)), R=[Ab, C.ones], W=[C.PS[bk]])
            P.op("dve", I("tensor_scalar", tokf.t[:, e, :], C.bank(bk)[:, 0:NST], float(T - 1), None, op0=ALU.min), R=[C.PS[bk]], W=[tokf])
        P.op("dve", I("tensor_copy", C.toki.t[:, :, :], tokf.t[:, :, :]), R=[tokf], W=[C.toki])
        P.barrier()
    if stop < 2:
        return
    with ExitStack() as es:
        sb = lambda n, s, d: es.enter_context(nc.sbuf_tensor(uniq(n), s, d))
        xg = [TT(sb(f"m_xg{i}", [128, D], F32)) for i in range(2)]
        xT = [[TT(sb(f"m_xT{j}_{k}", [128, CAP], BF16)) for k in range(8)] for j in range(2)]
        yst = [TT(sb(f"m_yst{i}", [128, D], F32)) for i in range(2)]
        sem_g = [P.dma_sem("m_g0"), P.dma_sem("m_g1")]
        sem_y = [P.dma_sem("m_y0"), P.dma_sem("m_y1")]
        F = alloc_ffn(C, es, CAP)
        yi = [0]

        def emit_gather(e, st):
            b = st % 2
            P.gather(xg[b].t[:, :], C.hb, C.toki.t[:, e, st:st + 1], sem_g[b], R=[C.Dhb, C.toki], W=[xg[b]])

        def emit_tr(e, st):
            b = st % 2
            xb = xT[e % 2]
            bk = 6
            flat = C.ps2(bk)
            for k in range(8):
                P.op("pe", I("transpose", flat[:, k * 128:(k + 1) * 128], xg[b].t[:, k * 128:(k + 1) * 128], C.ident.t[:, :]),
                     R=[xg[b], C.ident], W=[C.PS[bk], C.PS[bk + 1]])
            for k in range(8):
                if k % 2 == 0:
                    P.op("act", I("activation", out=xb[k].t[:, st * 128:(st + 1) * 128], in_=flat[:, k * 128:(k + 1) * 128], func=AF.Copy),
                         R=[C.PS[bk], C.PS[bk + 1]], W=[xb[k]])
                else:
                    P.op("dve", I("tensor_copy", xb[k].t[:, st * 128:(st + 1) * 128], flat[:, k * 128:(k + 1) * 128]),
                         R=[C.PS[bk], C.PS[bk + 1]], W=[xb[k]])

        for st in range(NST):
            emit_gather(0, st)
            emit_tr(0, st)
        for e in range(NE):
            xb = xT[e % 2]

            def consume(tt, db, e=e):
                b = yi[0] % 2
                yi[0] += 1
                if b == 0:
                    P.op("act", I("activation", out=yst[b].t[:, :], in_=C.ps2(db), func=AF.Copy), R=[C.PS[db], C.PS[db + 1]], W=[yst[b]])
                else:
                    P.op("dve", I("tensor_copy", yst[b].t[:, :], C.ps2(db)), R=[C.PS[db], C.PS[db + 1]], W=[yst[b]])
                r0 = e * CAP + tt * 128
                P.dma("sync", C.ybuf[r0:r0 + 128, :], yst[b].t[:, :], sem_y[b], R=[yst[b]], W=[C.Dyb])

            def hook(fb, e=e):
                if e + 1 < NE:
                    if fb < NST:
                        emit_gather(e + 1, fb)
                    if 1 <= fb <= NST:
                        emit_tr(e + 1, fb - 1)

            ffn_pass(C, F, xb, C.d["moe_wg"][mi][e], C.d["moe_wu"][mi][e], C.d["moe_wd"][mi][e], consume, blk_hook=hook)
        P.barrier()
    if stop < 3:
        return
    with ExitStack() as es:
        sb = lambda n, s, d: es.enter_context(nc.sbuf_tensor(uniq(n), s, d))
        gam = TT(sb("n_gam", [128, D], F32))
        bet = TT(sb("n_bet", [128, D], F32))
        hin = [TT(sb(f"n_hin{i}", [128, D], F32)) for i in range(2)]
        y0 = [TT(sb(f"n_y0{i}", [128, D], F32)) for i in range(2)]
        y1 = [TT(sb(f"n_y1{i}", [128, D], F32)) for i in range(2)]
        pre = [TT(sb(f"n_pre{i}", [128, D], F32)) for i in range(2)]
        sem_c = P.dma_sem("n_c")
        sem_h = [P.dma_sem("n_h0"), P.dma_sem("n_h1")]
        sem_a = [P.dma_sem("n_a0"), P.dma_sem("n_a1")]
        sem_b = [P.dma_sem("n_b0"), P.dma_sem("n_b1")]
        P.dma("sync", gam.t[:, :], bcast_row(C.d["ln2_g"][l], D), sem_c, W=[gam])
        P.dma("sync", bet.t[:, :], bcast_row(C.d["ln2_b"][l], D), sem_c, W=[bet])
        P.batch(sem_c, [gam, bet])
        L = alloc_ln(C, es, want_T=not final)
        for tt in range(NT):
            b = tt % 2
            P.dma("sync", hin[b].t[:, :], C.hb[tt * 128:(tt + 1) * 128, :], sem_h[b], R=[C.Dhb], W=[hin[b]])
            P.gather(y0[b].t[:, :], C.ybuf, C.sloti.t[:, 0, tt:tt + 1], sem_a[b], R=[C.Dyb, C.sloti], W=[y0[b]])
            P.gather(y1[b].t[:, :], C.ybuf, C.sloti.t[:, 1, tt:tt + 1], sem_b[b], R=[C.Dyb, C.sloti], W=[y1[b]])
            P.op("act", I("activation", out=y0[b].t[:, :], in_=y0[b].t[:, :], func=AF.Identity, scale=C.gsel.t[:, 0, tt:tt + 1]),
                 R=[y0[b], C.gsel], W=[y0[b]])
            P.op("dve", I("scalar_tensor_tensor", pre[b].t[:, :], hin[b].t[:, :], ALPHA, y0[b].t[:, :], op0=ALU.mult, op1=ALU.add),
                 R=[y0[b], hin[b]], W=[pre[b]])
            P.op("dve", I("scalar_tensor_tensor", pre[b].t[:, :], y1[b].t[:, :], C.gsel.t[:, 1, tt:tt + 1], pre[b].t[:, :], op0=ALU.mult, op1=ALU.add),
                 R=[y1[b], C.gsel, pre[b]], W=[pre[b]])
            if final:
                ln_tile(C, L, tt, pre[b], gam, bet, (C.Dout, C.out[tt * 128:(tt + 1) * 128, :]), None)
            else:
                ln_tile(C, L, tt, pre[b], gam, bet, (C.Dha, C.ha[tt * 128:(tt + 1) * 128, :]), (C.DhTa, C.hTa), tr_banks=0 + 2 * b)
        ln_flush(C, L)
        P.barrier()


IN_SPECS = None


def host_constants():
    c = {}
    c["c_ident"] = np.eye(128, dtype=np.float32)
    j = np.arange(128)[:, None]
    s = np.arange(128)[None, :]
    c["c_negU"] = -(j >= s).astype(np.float32)
    c["c_negO"] = -np.ones((128, 128), np.float32)
    c["c_lowI"] = (j <= s).astype(np.float32)
    c["c_ones"] = np.ones((128, 128), np.float32)
    p = np.arange(128)[:, None, None]
    dj = np.arange(4)[None, :, None]
    col = np.arange(512)[None, None, :]
    c["c_masks"] = ((128 * (3 - dj) + p) < col).astype(np.float32)
    invc = np.zeros((128, 4, 16), np.float32)
    for g in range(4):
        w = 2 << g
        invc[:, g, :] = 1.0 / np.minimum(np.arange(16) + 1, w)
    c["c_invc"] = invc
    c["c_iota"] = np.broadcast_to(np.arange(CAP, dtype=np.float32)[None, :], (128, CAP)).copy()
    c["c_ecap"] = np.broadcast_to((np.arange(NE, dtype=np.float32) * CAP)[None, None, :], (128, NT, NE)).copy()
    return c


def host_layout(inp):
    o = {}
    f = lambda a: np.ascontiguousarray(a, dtype=np.float32)
    o["ln_in_g"] = f(inp["ln_in_g"])
    o["ln_in_b"] = f(inp["ln_in_b"])
    o["w_in"] = f(inp["w_in"].reshape(DEPTH, 8, 128, 2048).transpose(0, 2, 1, 3))
    o["pool_w"] = f(inp["pool_w"].transpose(0, 2, 1, 3))
    o["pool_scale"] = f(inp["pool_scale"].reshape(DEPTH, 4, 128).transpose(0, 2, 1))
    o["w_out"] = f(inp["w_out"].reshape(DEPTH, 8, 128, D).transpose(0, 2, 1, 3))
    for nm in ("ln1_g", "ln1_b", "ln2_g", "ln2_b"):
        o[nm] = f(inp[nm])
    o["ffn_wg"] = f(inp["ffn_wg"].reshape(-1, 8, 128, NFC, 128).transpose(0, 3, 2, 1, 4))
    o["ffn_wu"] = f(inp["ffn_wu"].reshape(-1, 8, 128, NFC, 128).transpose(0, 3, 2, 1, 4))
    o["ffn_wd"] = f(inp["ffn_wd"])
    o["moe_router"] = f(inp["moe_router"].reshape(-1, 8, 128, NE).transpose(0, 2, 1, 3))
    o["moe_wg"] = f(inp["moe_wg"].reshape(-1, NE, 8, 128, NFC, 128).transpose(0, 1, 4, 3, 2, 5))
    o["moe_wu"] = f(inp["moe_wu"].reshape(-1, NE, 8, 128, NFC, 128).transpose(0, 1, 4, 3, 2, 5))
    o["moe_wd"] = f(inp["moe_wd"])
    o.update(host_constants())
    return o


def build(shapes, phases="all", debug_out=()):
    nc = bass.Bass("TRN2", target_bir_lowering=False)
    C = Ctx()
    C.nc = nc
    C.d = {}
    for name, shp in shapes.items():
        C.d[name] = nc.dram_tensor(name, list(shp), F32, kind="ExternalInput").ap()
    kind = lambda n: "ExternalOutput" if n in debug_out else "Internal"
    C.out = nc.dram_tensor("out", [T, D], F32, kind="ExternalOutput").ap()
    C.ha = nc.dram_tensor("s_ha", [T, D], F32, kind=kind("s_ha")).ap()
    C.hb = nc.dram_tensor("s_hb", [T, D], F32, kind=kind("s_hb")).ap()
    C.hTa = nc.dram_tensor("s_hTa", [D, T], BF16, kind=kind("s_hTa")).ap()
    C.hTb = nc.dram_tensor("s_hTb", [D, T], BF16, kind=kind("s_hTb")).ap()
    C.qT = nc.dram_tensor("s_qT", [512, T], BF16, kind=kind("s_qT")).ap()
    C.kT = nc.dram_tensor("s_kT", [512, T], BF16, kind=kind("s_kT")).ap()
    C.v = nc.dram_tensor("s_v", [T, 512], BF16, kind=kind("s_v")).ap()
    C.ycT = nc.dram_tensor("s_ycT", [D, T], BF16, kind=kind("s_ycT")).ap()
    C.ybuf = nc.dram_tensor("s_ybuf", [NE * CAP, D], F32, kind=kind("s_ybuf")).ap()
    C.Dout, C.Dha, C.Dhb, C.DhTa, C.DhTb, C.Dv, C.Dyb = TT(None), TT(None), TT(None), TT(None), TT(None), TT(None), TT(None)
    C.DqT = [TT(None) for _ in range(4)]
    C.DkT = [TT(None) for _ in range(4)]
    C.DycT = [TT(None) for _ in range(8)]
    with ExitStack() as es:
        P = Prog(nc, es)
        C.P = P
        sb = lambda n, s, d: es.enter_context(nc.sbuf_tensor(uniq(n), s, d))
        ps = es.enter_context(nc.psum_tensor("ps", [128, 8, 512], F32))
        C.ps = ps
        C.PS = [TT(None, f"bank{i}", excl=True) for i in range(8)]
        C.bank = lambda b: ps[:, b, :]
        C.ps2 = lambda b: ps[:, b:b + 2, :].rearrange("p a b -> p (a b)")
        C.ps2v = lambda b, a, w: ps[:, b:b + 2, :].rearrange("p a b -> p (a b)").rearrange("p (a w) -> p a w", w=w)
        C.ident = TT(sb("k_ident", [128, 128], F32))
        C.negU = TT(sb("k_negU", [128, 128], BF16))
        C.negO = TT(sb("k_negO", [128, 128], BF16))
        C.lowI = TT(sb("k_lowI", [128, 128], BF16))
        C.ones = TT(sb("k_ones", [128, 128], BF16))
        C.masks = TT(sb("k_masks", [128, 4, 512], BF16))
        C.gate = TT(sb("k_gate", [128, NT, NE], F32))
        C.sel = TT(sb("k_sel", [128, NT, NE], F32))
        C.slotf = TT(sb("k_slotf", [128, 2, NT], F32))
        C.sloti = TT(sb("k_sloti", [128, 2, NT], U32))
        C.gsel = TT(sb("k_gsel", [128, 2, NT], F32))
        C.toki = TT(sb("k_toki", [128, NE, NST], U32))
        sem_k = P.dma_sem("k_c")
        P.dma("sync", C.ident.t[:, :], C.d["c_ident"], sem_k, W=[C.ident])
        P.dma("pool", C.negU.t[:, :], C.d["c_negU"], sem_k, W=[C.negU])
        P.dma("pool", C.negO.t[:, :], C.d["c_negO"], sem_k, W=[C.negO])
        P.dma("pool", C.lowI.t[:, :], C.d["c_lowI"], sem_k, W=[C.lowI])
        P.dma("pool", C.ones.t[:, :], C.d["c_ones"], sem_k, W=[C.ones])
        P.dma("pool", C.masks.t[:, :, :], C.d["c_masks"], sem_k, W=[C.masks])
        P.batch(sem_k, [C.ident, C.negU, C.negO, C.lowI, C.ones, C.masks])

        def want(p):
            if phases == "all":
                return not (len(p) == 3 and p[2] in "ab")
            return p in phases

        if want("A"):
            phase_ln_in(C)
        for l in range(DEPTH):
            final = (l == DEPTH - 1)
            if want(f"B{l}"):
                phase_proj(C, l)
            if want(f"C{l}"):
                phase_attn(C, l)
            if want(f"D{l}"):
                phase_mix_ln1(C, l)
            if want(f"E{l}") or want(f"E{l}a") or want(f"E{l}b"):
                if l % 2 == 0:
                    phase_ffn_dense(C, l, final)
                else:
                    phase_moe(C, l, final, stop=(1 if want(f"E{l}a") else 2 if want(f"E{l}b") else 3))
        if "k_dbg" in debug_out:
            sem_d = P.dma_sem("dbg")
            for nm, tt_, shp, dt in (("k_gate", C.gate, [128, NT, NE], F32), ("k_sel", C.sel, [128, NT, NE], F32),
                                     ("k_slotf", C.slotf, [128, 2, NT], F32), ("k_sloti", C.sloti, [128, 2, NT], U32),
                                     ("k_gsel", C.gsel, [128, 2, NT], F32), ("k_toki", C.toki, [128, NE, NST], U32)):
                dd_ = nc.dram_tensor("dbg_" + nm, shp, dt, kind="ExternalOutput").ap()
                P.dma("sync", dd_, tt_.t[:, :, :], sem_d, R=[tt_])
        P.barrier()
        print("bass ops:", P.nops, {e: len(P.q[e]) for e in ENG})
        P.flush()
    return nc


_CACHE = {}


def kernel(**inputs):
    lay = host_layout(inputs)
    x = np.ascontiguousarray(inputs["x"], dtype=np.float32)
    shapes = {"x": (T, D)}
    for k, v in lay.items():
        shapes[k] = v.shape
    if "nc" not in _CACHE:
        import os
        dbg = ("s_ha", "s_hb", "s_hTa", "s_hTb", "s_qT", "s_kT", "s_v", "s_ycT", "s_ybuf") if os.environ.get("SCRATCH_OUT") == "1" else ()
        _CACHE["nc"] = build(shapes, debug_out=dbg)
    nc = _CACHE["nc"]
    in_maps = []
    for b in range(NCORES):
        m = {"x": x[b]}
        m.update(lay)
        in_maps.append(m)
    res = run_bass_kernel_spmd(nc, in_maps, core_ids=list(range(NCORES)))
    out = np.stack([np.asarray(res.results[b]["out"], dtype=np.float32) for b in range(NCORES)], axis=0)
    return out
```

```python
import numpy as np
from contextlib import ExitStack
import concourse.bass as bass
import concourse.mybir as mybir
from concourse.bass_utils import run_bass_kernel_spmd

F32 = mybir.dt.float32
BF16 = mybir.dt.bfloat16
I32 = mybir.dt.int32
U32 = mybir.dt.uint32
AF = mybir.ActivationFunctionType
ALU = mybir.AluOpType
AX = mybir.AxisListType

NCORES = 8
T = 4096
D = 1024
NT = T // 128
DEPTH = 2
DFF = 2816
NFC = DFF // 128
NE = 8
CAP = 1280
NST = CAP // 128
ALPHA = float((2 * DEPTH) ** 0.25)
EPS = 1e-5
ENG = ("sync", "act", "pe", "dve", "pool")


_UNIQ = [0]


def uniq(n):
    _UNIQ[0] += 1
    return f"{n}_{_UNIQ[0]}"


def I(meth, *a, **k):
    return lambda e: getattr(e, meth)(*a, **k)


class TT:
    __slots__ = ("t", "w", "r", "name", "excl")

    def __init__(self, t, name="", excl=False):
        self.t = t
        self.w = None
        self.r = {}
        self.name = name
        self.excl = excl


class Prog:
    def __init__(self, nc, es):
        self.nc = nc
        self.es = es
        self.q = {e: [] for e in ENG}
        self.waited = {e: {} for e in ENG}
        self.psem = {e: self._mksem("p_" + e) for e in ENG}
        self.dsems = []
        self.dpool_sw = []
        self.dpool_hw = []
        self.nops = 0

    def _mksem(self, name):
        h = self.es.enter_context(self.nc.semaphore(name))
        return {"h": h, "n": 0, "name": name}

    def dma_sem(self, name, sw=False):
        pool = self.dpool_sw if sw else self.dpool_hw
        if pool:
            s = pool.pop()
        else:
            s = self._mksem(uniq(name))
            s["sw"] = sw
        self.dsems.append(s)
        return s

    def _wait(self, eng, dep):
        s, v = dep
        if eng == "pe" and s is self.psem["pe"]:
            return
        if self.waited[eng].get(s["name"], 0) >= v:
            return
        self.waited[eng][s["name"]] = v
        self.q[eng].append(("wait", s["h"], v))

    def _deps(self, R, W):
        deps = []
        for b in R:
            if b.w is not None:
                deps.append(b.w)
        for b in W:
            if b.w is not None:
                deps.append(b.w)
            deps.extend(b.r.values())
        return deps

    def _mark(self, dep, R, W):
        for b in R:
            b.r[dep[0]["name"]] = dep
        for b in W:
            b.w = dep
            b.r = {}

    def op(self, eng, fn, R=(), W=()):
        if any(b.excl for b in R):
            W = list(W) + [b for b in R if b.excl and b not in W]
            R = [b for b in R if not b.excl]
        for d in self._deps(R, W):
            self._wait(eng, d)
        s = self.psem[eng]
        s["n"] += 1
        dep = (s, s["n"])
        self.q[eng].append(("op", fn, s["h"], 1))
        self._mark(dep, R, W)
        self.nops += 1
        return dep

    def dma(self, eng, out, in_, sem, R=(), W=()):
        assert bool(sem.get("sw")) == (eng == "pool"), (eng, sem["name"])
        for d in self._deps(R, W):
            if d[0] is sem:
                continue
            self._wait(eng, d)
        sem["n"] += 16
        dep = (sem, sem["n"])
        self.q[eng].append(("op", lambda e: e.dma_start(out=out, in_=in_), sem["h"], 16))
        self._mark(dep, R, W)
        self.nops += 1
        return dep

    def batch(self, sem, tts):
        for b in tts:
            b.w = (sem, sem["n"])

    def gather(self, out, in_, idx_ap, sem, R=(), W=()):
        eng = "pool"
        for d in self._deps(R, W):
            self._wait(eng, d)
        sem["n"] += 16
        dep = (sem, sem["n"])
        self.q[eng].append(("op", lambda e: e.indirect_dma_start(
            out=out, out_offset=None, in_=in_,
            in_offset=bass.IndirectOffsetOnAxis(ap=idx_ap, axis=0)), sem["h"], 16))
        self._mark(dep, R, W)
        self.nops += 1
        return dep

    def barrier(self):
        sems = list(self.psem.values()) + self.dsems
        for e in ENG:
            for s in sems:
                if s["n"] > 0:
                    self._wait(e, (s, s["n"]))
        for s_ in self.dsems:
            (self.dpool_sw if s_.get("sw") else self.dpool_hw).append(s_)
        self.dsems = []
        for e in ENG:
            if self.psem[e]["n"] > 8000:
                self.psem[e] = self._mksem(uniq("p_" + e))

    def flush(self):
        nc = self.nc
        q = self.q

        def run(e, lst):
            for it in lst:
                if it[0] == "wait":
                    e.wait_ge(it[1], it[2])
                else:
                    ins = it[1](e)
                    ins.then_inc(it[2], it[3])

        with nc.Block() as block:
            @block.sync
            def _(e):
                run(e, q["sync"])

            @block.scalar
            def _(e):
                run(e, q["act"])

            @block.tensor
            def _(e):
                run(e, q["pe"])

            @block.vector
            def _(e):
                run(e, q["dve"])

            @block.gpsimd
            def _(e):
                run(e, q["pool"])


class Ctx:
    pass


def bcast_row(ap1d, n):
    return bass.AP(ap1d.tensor, ap1d.offset, [[0, 128], [1, n]])


def alloc_ln(C, es, want_T=True):
    nc, P = C.nc, C.P
    L = Ctx()
    sb = lambda n, s, d: es.enter_context(nc.sbuf_tensor(uniq(n), s, d))
    L.stats = [TT(sb(f"ln_stats{i}", [128, 12], F32)) for i in range(2)]
    L.mv = [TT(sb(f"ln_mv{i}", [128, 8], F32)) for i in range(2)]
    L.xn = [TT(sb(f"ln_xn{i}", [128, D], F32)) for i in range(2)]
    L.ho = [TT(sb(f"ln_ho{i}", [128, D], F32)) for i in range(2)]
    L.want_T = want_T
    if want_T:
        L.hTs = [TT(sb(f"ln_hTs{i}", [128, 8, 512], BF16)) for i in range(2)]
    L.sem_st = [P.dma_sem("ln_st0"), P.dma_sem("ln_st1")]
    L.sem_sT = [P.dma_sem("ln_sT0"), P.dma_sem("ln_sT1")]
    L.cnt = 0
    L.pend = []
    return L


def ln_tile(C, L, tt, pre, gam, bet, h_dst, hT_dst, tr_banks=None, router=None):
    i = L.cnt
    L.cnt += 1
    L.pend.append(dict(i=i, tt=tt, pre=pre, gam=gam, bet=bet, h_dst=h_dst, hT_dst=hT_dst, tr=tr_banks, router=router))
    _ln_step(C, L)


def ln_flush(C, L):
    for _ in range(2):
        L.pend.append(None)
        _ln_step(C, L)
    L.pend = []


def _ln_step(C, L):
    P = C.P
    n = len(L.pend)
    t0 = L.pend[n - 1]
    t1 = L.pend[n - 2] if n >= 2 else None
    t2 = L.pend[n - 3] if n >= 3 else None
    if t0 is not None:
        b = t0["i"] % 2
        st, mv, pre = L.stats[b], L.mv[b], t0["pre"]
        P.op("dve", I("bn_stats", st.t[:, 0:6], pre.t[:, 0:512]), R=[pre], W=[st])
        P.op("dve", I("bn_stats", st.t[:, 6:12], pre.t[:, 512:1024]), R=[pre], W=[st])
        P.op("dve", I("bn_aggr", mv.t[:, 0:2], st.t[:, :]), R=[st], W=[mv])
        P.op("dve", I("tensor_scalar_add", mv.t[:, 2:3], mv.t[:, 1:2], EPS), R=[mv], W=[mv])
    if t0 is not None:
        b = t0["i"] % 2
        mv = L.mv[b]
        P.op("act", I("activation", out=mv.t[:, 3:4], in_=mv.t[:, 2:3], func=AF.Ln), R=[mv], W=[mv])
        P.op("act", I("activation", out=mv.t[:, 4:5], in_=mv.t[:, 3:4], func=AF.Exp, scale=-0.5), R=[mv], W=[mv])
    if t1 is not None:
        b = t1["i"] % 2
        mv, xn, ho, pre = L.mv[b], L.xn[b], L.ho[b], t1["pre"]
        P.op("dve", I("scalar_tensor_tensor", xn.t[:, :], pre.t[:, :], mv.t[:, 0:1], t1["gam"].t[:, :], op0=ALU.subtract, op1=ALU.mult),
             R=[pre, mv, t1["gam"]], W=[xn])
        P.op("dve", I("scalar_tensor_tensor", ho.t[:, :], xn.t[:, :], mv.t[:, 4:5], t1["bet"].t[:, :], op0=ALU.mult, op1=ALU.add),
             R=[xn, mv, t1["bet"]], W=[ho])
        P.dma("sync", t1["h_dst"][1], ho.t[:, :], L.sem_st[b], R=[ho], W=[t1["h_dst"][0]])
        if t1["router"] is not None:
            t1["router"](t1["tt"], ho)
    if t2 is not None and t2["hT_dst"] is not None:
        b = t2["i"] % 2
        ho = L.ho[b]
        tt = t2["tt"]
        slot = tt % 4
        hTs = L.hTs[(tt // 4) % 2]
        bk = t2["tr"]
        flat = C.ps2(bk)
        for k in range(8):
            P.op("pe", I("transpose", flat[:, k * 128:(k + 1) * 128], ho.t[:, k * 128:(k + 1) * 128], C.ident.t[:, :]),
                 R=[ho, C.ident], W=[C.PS[bk], C.PS[bk + 1]])
        P.op("act", I("activation", out=hTs.t[:, :, slot * 128:(slot + 1) * 128],
                      in_=C.ps2v(bk, 8, 128), func=AF.Copy),
             R=[C.PS[bk], C.PS[bk + 1]], W=[hTs])
        if slot == 3:
            c0 = (tt - 3) * 128
            dst = t2["hT_dst"][1].rearrange("(c p) t -> p c t", p=128)[:, :, c0:c0 + 512]
            P.dma("sync", dst, hTs.t[:, :, :], L.sem_sT[(tt // 4) % 2], R=[hTs], W=[t2["hT_dst"][0]])


def phase_ln_in(C):
    nc, P = C.nc, C.P
    with ExitStack() as es:
        sb = lambda n, s, d: es.enter_context(nc.sbuf_tensor(uniq(n), s, d))
        gam = TT(sb("a_gam", [128, D], F32))
        bet = TT(sb("a_bet", [128, D], F32))
        xin = [TT(sb(f"a_xin{i}", [128, D], F32)) for i in range(2)]
        sem_c = P.dma_sem("a_c")
        sem_x = [P.dma_sem("a_x0"), P.dma_sem("a_x1")]
        P.dma("sync", gam.t[:, :], bcast_row(C.d["ln_in_g"], D), sem_c, W=[gam])
        P.dma("sync", bet.t[:, :], bcast_row(C.d["ln_in_b"], D), sem_c, W=[bet])
        P.batch(sem_c, [gam, bet])
        L = alloc_ln(C, es)
        for tt in range(NT):
            b = tt % 2
            P.dma("sync", xin[b].t[:, :], C.d["x"][tt * 128:(tt + 1) * 128, :], sem_x[b], W=[xin[b]])
            ln_tile(C, L, tt, xin[b], gam, bet, (C.Dha, C.ha[tt * 128:(tt + 1) * 128, :]), (C.DhTa, C.hTa), tr_banks=0 + 2 * (tt % 2))
        ln_flush(C, L)
        P.barrier()


def phase_proj(C, l):
    nc, P = C.nc, C.P
    with ExitStack() as es:
        sb = lambda n, s, d: es.enter_context(nc.sbuf_tensor(uniq(n), s, d))
        wi = TT(sb("b_wi", [128, 8, 2048], BF16))
        hT = [TT(sb(f"b_hT{k}", [128, T], BF16)) for k in range(8)]
        pw = TT(sb("b_pw", [128, 4, 128], BF16))
        psc = TT(sb("b_psc", [128, 4], F32))
        invc = TT(sb("b_invc", [128, 4, 16], F32))
        U = TT(sb("b_U", [128, 16 + T], F32))
        SA = TT(sb("b_SA", [128, 16 + T], F32))
        SB = TT(sb("b_SB", [128, 16 + T], F32))
        dd = TT(sb("b_d", [128, T], BF16))
        stg = [TT(sb(f"b_stg{i}", [128, T], BF16)) for i in range(2)]
        vst = [TT(sb(f"b_vst{i}", [128, 4, 512], BF16)) for i in range(2)]
        sem_w = P.dma_sem("b_w", sw=True)
        sem_h = P.dma_sem("b_h")
        sem_c = P.dma_sem("b_c")
        sem_cw = P.dma_sem("b_cw", sw=True)
        sem_s = [P.dma_sem("b_s0"), P.dma_sem("b_s1")]
        sem_v = [P.dma_sem("b_v0"), P.dma_sem("b_v1")]
        for q4 in range(4):
            P.dma("pool", wi.t[:, :, q4 * 512:(q4 + 1) * 512], C.d["w_in"][l][:, :, q4 * 512:(q4 + 1) * 512], sem_w, W=[wi])
        for k in range(8):
            P.dma("sync", hT[k].t[:, :], C.hTa[k * 128:(k + 1) * 128, :], sem_h, R=[C.DhTa], W=[hT[k]])
        P.dma("pool", pw.t[:, :, :], C.d["pool_w"][l], sem_cw, W=[pw])
        P.dma("sync", psc.t[:, :], C.d["pool_scale"][l], sem_c, W=[psc])
        P.dma("sync", invc.t[:, :, :], C.d["c_invc"], sem_c, W=[invc])
        P.batch(sem_c, [psc, invc])
        P.batch(sem_h, hT)
        for buf in (U, SA, SB):
            P.op("dve", I("memset", buf.t[:, 0:16], 0.0), W=[buf])

        nbank = [0]

        def next_bank():
            b = nbank[0] % 4
            nbank[0] += 1
            return b

        evac_eng = [0]

        def evac(out_ap, in_ap, R, W, scale=None):
            e = evac_eng[0] % 2
            evac_eng[0] += 1
            if e == 0:
                if scale is None:
                    P.op("act", I("activation", out=out_ap, in_=in_ap, func=AF.Copy), R=R, W=W)
                else:
                    P.op("act", I("activation", out=out_ap, in_=in_ap, func=AF.Identity, scale=scale), R=R, W=W)
            else:
                if scale is None:
                    P.op("dve", I("tensor_copy", out_ap, in_ap), R=R, W=W)
                else:
                    P.op("dve", I("tensor_scalar_mul", out_ap, in_ap, scale), R=R, W=W)

        def fm_chunk(c, out_fn):
            for tg in range(8):
                bk = next_bank()
                for k in range(8):
                    P.op("pe", I("matmul", C.bank(bk), wi.t[:, k, c * 128:(c + 1) * 128], hT[k].t[:, tg * 512:(tg + 1) * 512],
                                 start=(k == 0), stop=(k == 7)), R=[wi, hT[k]], W=[C.PS[bk]])
                out_fn(tg, bk)

        sidx = [0]
        for g in range(4):
            w = 2 << g
            fm_chunk(g, lambda tg, bk: evac(U.t[:, 16 + tg * 512:16 + (tg + 1) * 512], C.bank(bk), [C.PS[bk]], [U]))
            src = U
            bufs = [SA, SB]
            nlev = g + 1
            for lev in range(nlev):
                dst = bufs[lev % 2]
                sh = 1 << lev
                eng = "dve"
                P.op(eng, I("tensor_tensor", dst.t[:, 16:16 + T], src.t[:, 16:16 + T], src.t[:, 16 - sh:16 + T - sh], op=ALU.add),
                     R=[src], W=[dst])
                src = dst
            P.op("dve", I("scalar_tensor_tensor", dd.t[:, :], src.t[:, 16:16 + T], 1.0 / w, U.t[:, 16:16 + T],
                          op0=ALU.mult, op1=ALU.subtract), R=[src, U], W=[dd])
            P.op("dve", I("tensor_tensor", SA.t[:, 0:16] if src is SB else SB.t[:, 0:16], src.t[:, 16:32], invc.t[:, g, :], op=ALU.mult),
                 R=[src, invc], W=[SA if src is SB else SB])
            tmp = SA if src is SB else SB
            P.op("dve", I("tensor_tensor", dd.t[:, 0:16], tmp.t[:, 0:16], U.t[:, 16:32], op=ALU.subtract),
                 R=[tmp, U], W=[dd])
            P.op("dve", I("memset", tmp.t[:, 0:16], 0.0), W=[tmp])
            so = stg[sidx[0] % 2]
            ss = sem_s[sidx[0] % 2]
            sidx[0] += 1
            for tg in range(8):
                bk = next_bank()
                P.op("pe", I("matmul", C.bank(bk), pw.t[:, g, :], dd.t[:, tg * 512:(tg + 1) * 512], start=True, stop=True),
                     R=[pw, dd], W=[C.PS[bk]])
                evac(so.t[:, tg * 512:(tg + 1) * 512], C.bank(bk), [C.PS[bk], psc], [so], scale=psc.t[:, g:g + 1])
            P.dma("sync", C.ycT[g * 128:(g + 1) * 128, :], so.t[:, :], ss, R=[so], W=[C.DycT[g]])
        for c in range(4, 12):
            so = stg[sidx[0] % 2]
            ss = sem_s[sidx[0] % 2]
            sidx[0] += 1
            sc = 0.125 if c < 8 else None
            fm_chunk(c, lambda tg, bk, so=so, sc=sc: evac(so.t[:, tg * 512:(tg + 1) * 512], C.bank(bk), [C.PS[bk]], [so], scale=sc))
            if c < 8:
                P.dma("sync", C.qT[(c - 4) * 128:(c - 3) * 128, :], so.t[:, :], ss, R=[so], W=[C.DqT[c - 4]])
            else:
                P.dma("sync", C.kT[(c - 8) * 128:(c - 7) * 128, :], so.t[:, :], ss, R=[so], W=[C.DkT[c - 8]])
        vv = C.v.rearrange("(n p) c -> p n c", p=128)
        for tt in range(NT):
            bk = next_bank()
            for k in range(8):
                P.op("pe", I("matmul", C.bank(bk), hT[k].t[:, tt * 128:(tt + 1) * 128], wi.t[:, k, 1536:2048],
                             start=(k == 0), stop=(k == 7)), R=[wi, hT[k]], W=[C.PS[bk]])
            vs = vst[(tt // 4) % 2]
            evac(vs.t[:, tt % 4, :], C.bank(bk), [C.PS[bk]], [vs])
            if tt % 4 == 3:
                P.dma("sync", vv[:, tt - 3:tt + 1, :], vs.t[:, :, :], sem_v[(tt // 4) % 2], R=[vs], W=[C.Dv])
        P.barrier()


def phase_attn(C, l):
    nc, P = C.nc, C.P
    with ExitStack() as es:
        sb = lambda n, s, d: es.enter_context(nc.sbuf_tensor(uniq(n), s, d))
        qT = [TT(sb(f"c_qT{i}", [128, T], BF16)) for i in range(4)]
        kT = [TT(sb(f"c_kT{i}", [128, T], BF16)) for i in range(4)]
        V = TT(sb("c_V", [128, NT, 512], BF16))
        E = [TT(sb(f"c_E{i}", [128, 2048], F32)) for i in range(2)]
        SP = [TT(sb(f"c_SP{i}", [128, 2048], BF16)) for i in range(4)]
        CA = [TT(sb(f"c_CA{i}", [128, 2048], BF16)) for i in range(4)]
        AT = [TT(sb(f"c_AT{i}", [128, 2048], BF16)) for i in range(3)]
        ys = [TT(sb(f"c_ys{i}", [128, T], BF16)) for i in range(2)]
        sem_l = P.dma_sem("c_l")
        sem_s = [P.dma_sem("c_s0"), P.dma_sem("c_s1")]
        for i in range(4):
            P.dma("sync", qT[i].t[:, :], C.qT[i * 128:(i + 1) * 128, :], sem_l, R=[C.DqT[i]], W=[qT[i]])
            P.dma("sync", kT[i].t[:, :], C.kT[i * 128:(i + 1) * 128, :], sem_l, R=[C.DkT[i]], W=[kT[i]])
        vv = C.v.rearrange("(n p) c -> p n c", p=128)
        for i in range(4):
            P.dma("sync", V.t[:, i * 8:(i + 1) * 8, :], vv[:, i * 8:(i + 1) * 8, :], sem_l, R=[C.Dv], W=[V])
        P.batch(sem_l, qT + kT + [V])

        ZP = [0, 2]
        BP = 4
        OB = [6, 7]
        quads = []
        for hp in range(4):
            for e in range(2):
                for g in range(8):
                    nq = g + 1
                    for qi in range(nq):
                        js = [4 * g + 3 - 4 * qi - k for k in range(4)]
                        quads.append(dict(hp=hp, e=e, g=g, js=js, first=(qi == 0), last=(qi == nq - 1)))
        n = len(quads)
        masks2 = C.masks.t[:, :, :].rearrange("p a b -> p (a b)")

        def zq(s):
            q = quads[s]
            hp, e, g = q["hp"], q["e"], q["g"]
            pr = slice(64 * e, 64 * e + 64)
            for k in range(4):
                bk = ZP[k // 2] + k % 2
                j = q["js"][k]
                P.op("pe", I("matmul", C.bank(bk), kT[hp].t[pr, j * 128:(j + 1) * 128], qT[hp].t[pr, g * 512:(g + 1) * 512],
                             start=True, stop=True), R=[kT[hp], qT[hp]], W=[C.PS[bk]])

        def act1(s):
            Eb = E[s % 2]
            z4 = C.ps[:, 0:4, :].rearrange("p a b -> p (a b)")
            P.op("act", I("activation", out=Eb.t[:, :], in_=z4, func=AF.Exp),
                 R=[C.PS[0], C.PS[1], C.PS[2], C.PS[3]], W=[Eb])

        def act2(s):
            P.op("act", I("activation", out=SP[s % 4].t[:, :], in_=E[s % 2].t[:, :], func=AF.Ln, bias=1.0), R=[E[s % 2]], W=[SP[s % 4]])

        def dve3(s):
            q = quads[s]
            S, Cq, Cn = SP[s % 4], CA[s % 4], CA[(s + 1) % 4]
            sl = lambda k: slice(k * 512, (k + 1) * 512)
            if q["first"]:
                P.op("dve", I("tensor_tensor", S.t[:, :], S.t[:, :], masks2, op=ALU.mult), R=[S, C.masks], W=[S])
                P.op("dve", I("tensor_tensor", Cq.t[:, sl(2)], S.t[:, sl(0)], S.t[:, sl(1)], op=ALU.add), R=[S], W=[Cq])
            else:
                P.op("dve", I("tensor_tensor", Cq.t[:, sl(1)], Cq.t[:, sl(0)], S.t[:, sl(0)], op=ALU.add), R=[S, Cq], W=[Cq])
                P.op("dve", I("tensor_tensor", Cq.t[:, sl(2)], Cq.t[:, sl(1)], S.t[:, sl(1)], op=ALU.add), R=[S, Cq], W=[Cq])
            P.op("dve", I("tensor_tensor", Cq.t[:, sl(3)], Cq.t[:, sl(2)], S.t[:, sl(2)], op=ALU.add), R=[S, Cq], W=[Cq])
            if not q["last"]:
                P.op("dve", I("tensor_tensor", Cn.t[:, sl(0)], Cq.t[:, sl(3)], S.t[:, sl(3)], op=ALU.add), R=[S, Cq], W=[Cn])

        def bq(s, p):
            q = quads[s]
            hp, e, g = q["hp"], q["e"], q["g"]
            pr = slice(64 * e, 64 * e + 64)
            S, Cq = SP[s % 4], CA[s % 4]
            for k in (2 * p, 2 * p + 1):
                bk = BP + k % 2
                j = q["js"][k]
                if q["first"] and k == 0:
                    car = None
                elif q["first"] and k == 1:
                    car = (S, S.t[:, 0:512])
                else:
                    car = (Cq, Cq.t[:, k * 512:(k + 1) * 512])
                P.op("pe", I("matmul", C.bank(bk), kT[hp].t[pr, j * 128:(j + 1) * 128], qT[hp].t[pr, g * 512:(g + 1) * 512],
                             start=True, stop=False), R=[kT[hp], qT[hp]], W=[C.PS[bk]])
                P.op("pe", I("matmul", C.bank(bk), C.negU.t[:, :], S.t[:, k * 512:(k + 1) * 512], start=False, stop=(car is None)),
                     R=[C.negU, S], W=[C.PS[bk]])
                if car is not None:
                    P.op("pe", I("matmul", C.bank(bk), C.negO.t[:, :], car[1], start=False, stop=True),
                         R=[C.negO, car[0]], W=[C.PS[bk]])

        def act3(s, p):
            A = AT[s % 3]
            P.op("act", I("activation", out=A.t[:, p * 1024:(p + 1) * 1024], in_=C.ps2(BP), func=AF.Exp),
                 R=[C.PS[BP], C.PS[BP + 1]], W=[A])

        def mask_att(s):
            q = quads[s]
            if q["first"]:
                A = AT[s % 3]
                P.op("dve", I("tensor_tensor", A.t[:, :], A.t[:, :], masks2, op=ALU.mult), R=[A, C.masks], W=[A])

        def av(s):
            q = quads[s]
            hp, e, g = q["hp"], q["e"], q["g"]
            gi = (hp * 2 + e) * 8 + g
            ob = OB[gi % 2]
            A = AT[s % 3]
            pr = slice(64 * e, 64 * e + 64)
            for k in range(4):
                j = q["js"][k]
                P.op("pe", I("matmul", C.bank(ob)[pr, :], V.t[:, j, hp * 128 + 64 * e: hp * 128 + 64 * e + 64], A.t[:, k * 512:(k + 1) * 512],
                             start=(q["first"] and k == 0), stop=(q["last"] and k == 3)), R=[V, A], W=[C.PS[ob]])
            if q["last"]:
                yb = ys[hp % 2]
                P.op("dve", I("tensor_copy", yb.t[pr, g * 512:(g + 1) * 512], C.bank(ob)[pr, :]), R=[C.PS[ob]], W=[yb])
                if e == 1 and g == 7:
                    P.dma("sync", C.ycT[512 + hp * 128:512 + (hp + 1) * 128, :], yb.t[:, :], sem_s[hp % 2], R=[yb], W=[C.DycT[4 + hp]])

        for s in range(n + 3):
            if s < n:
                zq(s)
            if 0 <= s - 2 < n:
                bq(s - 2, 0)
                act3(s - 2, 0)
            if s < n:
                act1(s)
            if 0 <= s - 3 < n:
                av(s - 3)
            if 0 <= s - 2 < n:
                bq(s - 2, 1)
                act3(s - 2, 1)
            if s < n:
                act2(s)
                dve3(s)
            if 0 <= s - 2 < n:
                mask_att(s - 2)
        P.barrier()


def phase_mix_ln1(C, l):
    nc, P = C.nc, C.P
    moe = (l % 2 == 1)
    with ExitStack() as es:
        sb = lambda n, s, d: es.enter_context(nc.sbuf_tensor(uniq(n), s, d))
        yc = [TT(sb(f"d_yc{k}", [128, T], BF16)) for k in range(8)]
        wo = TT(sb("d_wo", [128, 8, D], BF16))
        gam = TT(sb("d_gam", [128, D], F32))
        bet = TT(sb("d_bet", [128, D], F32))
        hin = [TT(sb(f"d_hin{i}", [128, D], F32)) for i in range(2)]
        pre = [TT(sb(f"d_pre{i}", [128, D], F32)) for i in range(2)]
        sem_c = P.dma_sem("d_c")
        sem_cw = P.dma_sem("d_cw", sw=True)
        sem_y = P.dma_sem("d_y")
        sem_h = [P.dma_sem("d_h0"), P.dma_sem("d_h1")]
        P.dma("pool", wo.t[:, :, :], C.d["w_out"][l], sem_cw, W=[wo])
        P.dma("sync", gam.t[:, :], bcast_row(C.d["ln1_g"][l], D), sem_c, W=[gam])
        P.dma("sync", bet.t[:, :], bcast_row(C.d["ln1_b"][l], D), sem_c, W=[bet])
        for k in range(8):
            P.dma("sync", yc[k].t[:, :], C.ycT[k * 128:(k + 1) * 128, :], sem_y, R=[C.DycT[k]], W=[yc[k]])
        P.batch(sem_y, yc)
        L = alloc_ln(C, es)
        router = None
        P.batch(sem_c, [gam, bet])
        if moe:
            rt = TT(sb("d_rt", [128, 8, NE], F32))
            P.dma("sync", rt.t[:, :, :], C.d["moe_router"][l // 2], sem_c, W=[rt])
            P.batch(sem_c, [gam, bet, rt])
            hT32 = [TT(sb(f"d_hT32{i}", [128, 8, 128], F32)) for i in range(2)]
            lg = [TT(sb(f"d_lg{i}", [128, 64], F32)) for i in range(2)]

            def router(tt, ho):
                b = tt % 2
                bk = 4 + 2 * b
                flat = C.ps2(bk)
                for k in range(8):
                    P.op("pe", I("transpose", flat[:, k * 128:(k + 1) * 128], ho.t[:, k * 128:(k + 1) * 128], C.ident.t[:, :]),
                         R=[ho, C.ident], W=[C.PS[bk], C.PS[bk + 1]])
                P.op("dve", I("tensor_copy", hT32[b].t[:, :, :], C.ps2v(bk, 8, 128)), R=[C.PS[bk], C.PS[bk + 1]], W=[hT32[b]])
                lb = C.bank(bk)[:, 0:NE]
                for k in range(8):
                    P.op("pe", I("matmul", lb, hT32[b].t[:, k, :], rt.t[:, k, :], start=(k == 0), stop=(k == 7)),
                         R=[hT32[b], rt], W=[C.PS[bk], C.PS[bk + 1]])
                g_ = lg[b]
                G = g_.t
                P.op("dve", I("tensor_copy", G[:, 0:8], lb), R=[C.PS[bk], C.PS[bk + 1]], W=[g_])
                P.op("dve", I("tensor_reduce", G[:, 8:9], G[:, 0:8], AX.X, ALU.max), R=[g_], W=[g_])
                P.op("dve", I("tensor_scalar", G[:, 9:10], G[:, 8:9], -1.0, None, op0=ALU.mult), R=[g_], W=[g_])
                P.op("dve", I("tensor_scalar", G[:, 10:18], G[:, 0:8], G[:, 8:9], -1e30, op0=ALU.is_equal, op1=ALU.mult), R=[g_], W=[g_])
                P.op("dve", I("tensor_tensor", G[:, 10:18], G[:, 10:18], G[:, 0:8], op=ALU.add), R=[g_], W=[g_])
                P.op("dve", I("tensor_reduce", G[:, 18:19], G[:, 10:18], AX.X, ALU.max), R=[g_], W=[g_])
                P.op("act", I("activation", out=G[:, 19:27], in_=G[:, 0:8], func=AF.Exp, bias=G[:, 9:10]), R=[g_], W=[g_])
                P.op("dve", I("tensor_scalar", G[:, 27:35], G[:, 0:8], G[:, 18:19], None, op0=ALU.is_ge), R=[g_], W=[g_])
                P.op("dve", I("tensor_tensor", G[:, 19:27], G[:, 19:27], G[:, 27:35], op=ALU.mult), R=[g_], W=[g_])
                P.op("dve", I("tensor_reduce", G[:, 35:36], G[:, 19:27], AX.X, ALU.add), R=[g_], W=[g_])
                P.op("dve", I("reciprocal", G[:, 36:37], G[:, 35:36]), R=[g_], W=[g_])
                P.op("dve", I("tensor_scalar", C.gate.t[:, tt, :], G[:, 19:27], G[:, 36:37], None, op0=ALU.mult), R=[g_], W=[C.gate])
                P.op("dve", I("tensor_copy", C.sel.t[:, tt, :], G[:, 27:35]), R=[g_], W=[C.sel])

        for tt in range(NT):
            b = tt % 2
            P.dma("sync", hin[b].t[:, :], C.ha[tt * 128:(tt + 1) * 128, :], sem_h[b], R=[C.Dha], W=[hin[b]])
            bk = 0 + 2 * b
            for nh in range(2):
                for k in range(8):
                    P.op("pe", I("matmul", C.bank(bk + nh), yc[k].t[:, tt * 128:(tt + 1) * 128], wo.t[:, k, nh * 512:(nh + 1) * 512],
                                 start=(k == 0), stop=(k == 7)), R=[yc[k], wo], W=[C.PS[bk], C.PS[bk + 1]])
            P.op("dve", I("scalar_tensor_tensor", pre[b].t[:, :], hin[b].t[:, :], ALPHA, C.ps2(bk), op0=ALU.mult, op1=ALU.add),
                 R=[hin[b], C.PS[bk], C.PS[bk + 1]], W=[pre[b]])
            ln_tile(C, L, tt, pre[b], gam, bet, (C.Dhb, C.hb[tt * 128:(tt + 1) * 128, :]),
                    None if moe else (C.DhTb, C.hTb), tr_banks=4 + 2 * b, router=router)
        ln_flush(C, L)
        P.barrier()


def alloc_ffn(C, es, ntok):
    nc, P = C.nc, C.P
    F = Ctx()
    sb = lambda n, s, d: es.enter_context(nc.sbuf_tensor(uniq(n), s, d))
    F.ntok = ntok
    F.wd = TT(sb("f_wd", [128, NFC, D], BF16))
    F.wg = [TT(sb(f"f_wg{i}", [128, 2, 8, 128], BF16)) for i in range(3)]
    F.wu = [TT(sb(f"f_wu{i}", [128, 2, 8, 128], BF16)) for i in range(3)]
    F.act = TT(sb("f_act", [128, NFC, ntok], BF16))
    F.sg = [TT(sb(f"f_sg{i}", [128, 512], BF16)) for i in range(2)]
    F.sem_wd = P.dma_sem("f_wd", sw=True)
    F.sem_wg = [P.dma_sem(f"f_wg{i}", sw=True) for i in range(3)]
    F.sem_wu = [P.dma_sem(f"f_wu{i}", sw=True) for i in range(3)]
    F.cnt = 0
    F.blk = 0
    return F


def ffn_pass(C, F, xT, wg_d, wu_d, wd_d, consume, GB=(0, 1), UB=(2, 3), DB=4, blk_hook=None):
    P = C.P
    ntok = F.ntok
    subs = []
    t0 = 0
    while t0 < ntok:
        w = min(512, ntok - t0)
        subs.append((t0, w))
        t0 += w
    wdv = wd_d.rearrange("(c p) n -> p c n", p=128)
    for fb in range(NFC // 2):
        if fb == 2:
            P.dma("pool", F.wd.t[:, 0:11, :], wdv[:, 0:11, :], F.sem_wd, W=[F.wd])
            P.dma("pool", F.wd.t[:, 11:22, :], wdv[:, 11:22, :], F.sem_wd, W=[F.wd])
        bi = F.blk % 3
        F.blk += 1
        wg, wu = F.wg[bi], F.wu[bi]
        P.dma("pool", wg.t[:, :, :, :], wg_d[2 * fb:2 * fb + 2].rearrange("c p k f -> p c k f"), F.sem_wg[bi], W=[wg])
        P.dma("pool", wu.t[:, :, :, :], wu_d[2 * fb:2 * fb + 2].rearrange("c p k f -> p c k f"), F.sem_wu[bi], W=[wu])
        for ci in range(2):
            fc = 2 * fb + ci
            for (t0, w) in subs:
                i = F.cnt
                F.cnt += 1
                gb, ub = GB[i % 2], UB[i % 2]
                for k in range(8):
                    P.op("pe", I("matmul", C.bank(gb)[:, 0:w], wg.t[:, ci, k, :], xT[k].t[:, t0:t0 + w], start=(k == 0), stop=(k == 7)),
                         R=[wg, xT[k]], W=[C.PS[gb]])
                for k in range(8):
                    P.op("pe", I("matmul", C.bank(ub)[:, 0:w], wu.t[:, ci, k, :], xT[k].t[:, t0:t0 + w], start=(k == 0), stop=(k == 7)),
                         R=[wu, xT[k]], W=[C.PS[ub]])
                sg = F.sg[i % 2]
                P.op("act", I("activation", out=sg.t[:, 0:w], in_=C.bank(gb)[:, 0:w], func=AF.Silu), R=[C.PS[gb]], W=[sg])
                P.op("dve", I("tensor_tensor", F.act.t[:, fc, t0:t0 + w], sg.t[:, 0:w], C.bank(ub)[:, 0:w], op=ALU.mult),
                     R=[sg, C.PS[ub]], W=[F.act])
        if blk_hook is not None:
            blk_hook(fb)
    for tt in range(ntok // 128):
        for nh in range(2):
            for fc in range(NFC):
                P.op("pe", I("matmul", C.bank(DB + nh), F.act.t[:, fc, tt * 128:(tt + 1) * 128], F.wd.t[:, fc, nh * 512:(nh + 1) * 512],
                             start=(fc == 0), stop=(fc == NFC - 1)), R=[F.act, F.wd], W=[C.PS[DB], C.PS[DB + 1]])
        consume(tt, DB)


def phase_ffn_dense(C, l, final):
    nc, P = C.nc, C.P
    TG = 1024
    with ExitStack() as es:
        sb = lambda n, s, d: es.enter_context(nc.sbuf_tensor(uniq(n), s, d))
        gam = TT(sb("e_gam", [128, D], F32))
        bet = TT(sb("e_bet", [128, D], F32))
        hin = [TT(sb(f"e_hin{i}", [128, D], F32)) for i in range(2)]
        pre = [TT(sb(f"e_pre{i}", [128, D], F32)) for i in range(2)]
        xT = [[TT(sb(f"e_xT{j}_{k}", [128, TG], BF16)) for k in range(8)] for j in range(1)]
        sem_c = P.dma_sem("e_c")
        sem_x = [P.dma_sem("e_x0"), P.dma_sem("e_x1")]
        sem_h = [P.dma_sem("e_h0"), P.dma_sem("e_h1")]
        P.dma("sync", gam.t[:, :], bcast_row(C.d["ln2_g"][l], D), sem_c, W=[gam])
        P.dma("sync", bet.t[:, :], bcast_row(C.d["ln2_b"][l], D), sem_c, W=[bet])
        P.batch(sem_c, [gam, bet])
        L = alloc_ln(C, es, want_T=not final)
        F = alloc_ffn(C, es, TG)
        i = l // 2
        cnt = [0]
        for grp in range(T // TG):
            xb = xT[0]
            for k in range(8):
                P.dma("sync", xb[k].t[:, :], C.hTb[k * 128:(k + 1) * 128, grp * TG:(grp + 1) * TG], sem_x[grp % 2], R=[C.DhTb], W=[xb[k]])
            P.batch(sem_x[grp % 2], xb)

            def consume(tt, db, grp=grp):
                gt = grp * (TG // 128) + tt
                b = cnt[0] % 2
                cnt[0] += 1
                P.dma("sync", hin[b].t[:, :], C.hb[gt * 128:(gt + 1) * 128, :], sem_h[b], R=[C.Dhb], W=[hin[b]])
                P.op("dve", I("scalar_tensor_tensor", pre[b].t[:, :], hin[b].t[:, :], ALPHA, C.ps2(db), op0=ALU.mult, op1=ALU.add),
                     R=[hin[b], C.PS[db], C.PS[db + 1]], W=[pre[b]])
                if final:
                    ln_tile(C, L, gt, pre[b], gam, bet, (C.Dout, C.out[gt * 128:(gt + 1) * 128, :]), None)
                else:
                    ln_tile(C, L, gt, pre[b], gam, bet, (C.Dha, C.ha[gt * 128:(gt + 1) * 128, :]), (C.DhTa, C.hTa), tr_banks=6)

            ffn_pass(C, F, xb, C.d["ffn_wg"][i], C.d["ffn_wu"][i], C.d["ffn_wd"][i], consume)
        ln_flush(C, L)
        P.barrier()


def phase_moe(C, l, final, stop=3):
    nc, P = C.nc, C.P
    mi = l // 2
    with ExitStack() as es:
        sb = lambda n, s, d: es.enter_context(nc.sbuf_tensor(uniq(n), s, d))
        selb = TT(sb("m_selb", [128, NT, NE], BF16))
        csel = TT(sb("m_csel", [128, NT, NE], BF16))
        cum = TT(sb("m_cum", [128, NT, NE], F32))
        pos = TT(sb("m_pos", [128, NT, NE], F32))
        pre_ = TT(sb("m_pre", [128, NT, NE], F32))
        tmp = TT(sb("m_tmp", [128, NT, NE], F32))
        tmp2 = TT(sb("m_tmp2", [128, NT, NE], F32))
        iot = TT(sb("m_iot", [128, CAP], F32))
        ecap = TT(sb("m_ecap", [128, NT, NE], F32))
        A = [TT(sb(f"m_A{i}", [128, NT, CAP], BF16)) for i in range(2)]
        tokf = TT(sb("m_tokf", [128, NE, NST], F32))
        sem_c = P.dma_sem("m_c")
        P.dma("sync", iot.t[:, :], C.d["c_iota"], sem_c, W=[iot])
        P.dma("sync", ecap.t[:, :, :], C.d["c_ecap"], sem_c, W=[ecap])
        P.batch(sem_c, [iot, ecap])
        P.op("dve", I("tensor_copy", selb.t[:, :, :], C.sel.t[:, :, :]), R=[C.sel], W=[selb])
        P.op("dve", I("memset", csel.t[:, 0, :], 0.0), W=[csel])
        for tt in range(1, NT):
            P.op("dve", I("tensor_tensor", csel.t[:, tt, :], csel.t[:, tt - 1, :], selb.t[:, tt - 1, :], op=ALU.add), R=[csel, selb], W=[csel])
        bk = 0
        for tt in range(NT):
            o = C.bank(bk)[:, tt * NE:(tt + 1) * NE]
            P.op("pe", I("matmul", o, C.lowI.t[:, :], selb.t[:, tt, :], start=True, stop=False), R=[C.lowI, selb], W=[C.PS[bk]])
            P.op("pe", I("matmul", o, C.ones.t[:, :], csel.t[:, tt, :], start=False, stop=True), R=[C.ones, csel], W=[C.PS[bk]])
        P.op("dve", I("tensor_copy", cum.t[:, :, :], C.bank(bk)[:, 0:NT * NE].rearrange("p (a b) -> p a b", b=NE)), R=[C.PS[bk]], W=[cum])
        P.op("dve", I("tensor_tensor", pos.t[:, :, :], cum.t[:, :, :], C.sel.t[:, :, :], op=ALU.subtract), R=[cum, C.sel], W=[pos])
        P.op("dve", I("memset", pre_.t[:, :, :], 0.0), W=[pre_])
        P.op("dve", I("tensor_copy", pre_.t[:, :, 1:NE], C.sel.t[:, :, 0:NE - 1]), R=[C.sel], W=[pre_])
        for sh in (1, 2, 4):
            P.op("dve", I("tensor_copy", tmp.t[:, :, :], pre_.t[:, :, :]), R=[pre_], W=[tmp])
            P.op("dve", I("tensor_tensor", pre_.t[:, :, sh:NE], tmp.t[:, :, sh:NE], tmp.t[:, :, 0:NE - sh], op=ALU.add), R=[tmp], W=[pre_])
        P.op("dve", I("tensor_tensor", pos.t[:, :, :], pos.t[:, :, :], ecap.t[:, :, :], op=ALU.add), R=[pos, ecap], W=[pos])
        for kk in range(2):
            P.op("dve", I("tensor_scalar", tmp.t[:, :, :], pre_.t[:, :, :], float(kk), None, op0=ALU.is_equal), R=[pre_], W=[tmp])
            P.op("dve", I("tensor_tensor", tmp.t[:, :, :], tmp.t[:, :, :], C.sel.t[:, :, :], op=ALU.mult), R=[tmp, C.sel], W=[tmp])
            P.op("dve", I("tensor_tensor", tmp2.t[:, :, :], tmp.t[:, :, :], pos.t[:, :, :], op=ALU.mult), R=[tmp, pos], W=[tmp2])
            P.op("dve", I("tensor_reduce", C.slotf.t[:, kk, :], tmp2.t[:, :, :], AX.X, ALU.add), R=[tmp2], W=[C.slotf])
            P.op("dve", I("tensor_tensor", tmp2.t[:, :, :], tmp.t[:, :, :], C.gate.t[:, :, :], op=ALU.mult), R=[tmp, C.gate], W=[tmp2])
            P.op("dve", I("tensor_reduce", C.gsel.t[:, kk, :], tmp2.t[:, :, :], AX.X, ALU.add), R=[tmp2], W=[C.gsel])
        P.op("dve", I("tensor_copy", C.sloti.t[:, :, :], C.slotf.t[:, :, :]), R=[C.slotf], W=[C.sloti])
        ai = 0
        for e in range(NE):
            bk = 1 + (e % 2)
            Ab = A[e % 2]
            for tt in range(NT):
                eng = "dve"
                ai += 1
                P.op(eng, I("tensor_scalar", Ab.t[:, tt, :], iot.t[:, :], cum.t[:, tt, e:e + 1], None, op0=ALU.is_ge), R=[iot, cum], W=[Ab])
            for st in range(NST):
                for tt in range(NT):
                    P.op("pe", I("matmul", C.bank(bk)[:, st:st + 1], Ab.t[:, tt, st * 128:(st + 1) * 128], C.ones.t[:, 0:1],
                                 start=(tt == 0), stop=(tt == NT - 1)), R=[Ab, C.ones], W=[C.PS[bk]])
            P.op("dve", I("tensor_scalar", tokf.t[:, e, :], C.bank(bk)[:, 0:NST], float(T - 1), None, op0=ALU.min), R=[C.PS[bk]], W=[tokf])
        P.op("dve", I("tensor_copy", C.toki.t[:, :, :], tokf.t[:, :, :]), R=[tokf], W=[C.toki])
        P.barrier()
    if stop < 2:
        return
    with ExitStack() as es:
        sb = lambda n, s, d: es.enter_context(nc.sbuf_tensor(uniq(n), s, d))
        xg = [TT(sb(f"m_xg{i}", [128, D], F32)) for i in range(2)]
        xT = [[TT(sb(f"m_xT{j}_{k}", [128, CAP], BF16)) for k in range(8)] for j in range(2)]
        yst = [TT(sb(f"m_yst{i}", [128, D], F32)) for i in range(2)]
        sem_g = [P.dma_sem("m_g0", sw=True), P.dma_sem("m_g1", sw=True)]
        sem_y = [P.dma_sem("m_y0"), P.dma_sem("m_y1")]
        F = alloc_ffn(C, es, CAP)
        yi = [0]

        def emit_gather(e, st):
            b = st % 2
            P.gather(xg[b].t[:, :], C.hb, C.toki.t[:, e, st:st + 1], sem_g[b], R=[C.Dhb, C.toki], W=[xg[b]])

        def emit_tr(e, st):
            b = st % 2
            xb = xT[e % 2]
            bk = 6
            flat = C.ps2(bk)
            for k in range(8):
                P.op("pe", I("transpose", flat[:, k * 128:(k + 1) * 128], xg[b].t[:, k * 128:(k + 1) * 128], C.ident.t[:, :]),
                     R=[xg[b], C.ident], W=[C.PS[bk], C.PS[bk + 1]])
            for k in range(8):
                if k % 2 == 0:
                    P.op("act", I("activation", out=xb[k].t[:, st * 128:(st + 1) * 128], in_=flat[:, k * 128:(k + 1) * 128], func=AF.Copy),
                         R=[C.PS[bk], C.PS[bk + 1]], W=[xb[k]])
                else:
                    P.op("dve", I("tensor_copy", xb[k].t[:, st * 128:(st + 1) * 128], flat[:, k * 128:(k + 1) * 128]),
                         R=[C.PS[bk], C.PS[bk + 1]], W=[xb[k]])

        for st in range(NST):
            emit_gather(0, st)
            emit_tr(0, st)
        for e in range(NE):
            xb = xT[e % 2]

            def consume(tt, db, e=e):
                b = yi[0] % 2
                yi[0] += 1
                if b == 0:
                    P.op("act", I("activation", out=yst[b].t[:, :], in_=C.ps2(db), func=AF.Copy), R=[C.PS[db], C.PS[db + 1]], W=[yst[b]])
                else:
                    P.op("dve", I("tensor_copy", yst[b].t[:, :], C.ps2(db)), R=[C.PS[db], C.PS[db + 1]], W=[yst[b]])
                r0 = e * CAP + tt * 128
                P.dma("sync", C.ybuf[r0:r0 + 128, :], yst[b].t[:, :], sem_y[b], R=[yst[b]], W=[C.Dyb])

            def hook(fb, e=e):
                if e + 1 < NE:
                    if fb < NST:
                        emit_gather(e + 1, fb)
                    if 1 <= fb <= NST:
                        emit_tr(e + 1, fb - 1)

            ffn_pass(C, F, xb, C.d["moe_wg"][mi][e], C.d["moe_wu"][mi][e], C.d["moe_wd"][mi][e], consume, blk_hook=hook)
        P.barrier()
    if stop < 3:
        return
    with ExitStack() as es:
        sb = lambda n, s, d: es.enter_context(nc.sbuf_tensor(uniq(n), s, d))
        gam = TT(sb("n_gam", [128, D], F32))
        bet = TT(sb("n_bet", [128, D], F32))
        hin = [TT(sb(f"n_hin{i}", [128, D], F32)) for i in range(2)]
        y0 = [TT(sb(f"n_y0{i}", [128, D], F32)) for i in range(2)]
        y1 = [TT(sb(f"n_y1{i}", [128, D], F32)) for i in range(2)]
        pre = [TT(sb(f"n_pre{i}", [128, D], F32)) for i in range(2)]
        sem_c = P.dma_sem("n_c")
        sem_h = [P.dma_sem("n_h0"), P.dma_sem("n_h1")]
        sem_a = [P.dma_sem("n_a0", sw=True), P.dma_sem("n_a1", sw=True)]
        sem_b = [P.dma_sem("n_b0", sw=True), P.dma_sem("n_b1", sw=True)]
        P.dma("sync", gam.t[:, :], bcast_row(C.d["ln2_g"][l], D), sem_c, W=[gam])
        P.dma("sync", bet.t[:, :], bcast_row(C.d["ln2_b"][l], D), sem_c, W=[bet])
        P.batch(sem_c, [gam, bet])
        L = alloc_ln(C, es, want_T=not final)
        for tt in range(NT):
            b = tt % 2
            P.dma("sync", hin[b].t[:, :], C.hb[tt * 128:(tt + 1) * 128, :], sem_h[b], R=[C.Dhb], W=[hin[b]])
            P.gather(y0[b].t[:, :], C.ybuf, C.sloti.t[:, 0, tt:tt + 1], sem_a[b], R=[C.Dyb, C.sloti], W=[y0[b]])
            P.gather(y1[b].t[:, :], C.ybuf, C.sloti.t[:, 1, tt:tt + 1], sem_b[b], R=[C.Dyb, C.sloti], W=[y1[b]])
            P.op("act", I("activation", out=y0[b].t[:, :], in_=y0[b].t[:, :], func=AF.Identity, scale=C.gsel.t[:, 0, tt:tt + 1]),
                 R=[y0[b], C.gsel], W=[y0[b]])
            P.op("dve", I("scalar_tensor_tensor", pre[b].t[:, :], hin[b].t[:, :], ALPHA, y0[b].t[:, :], op0=ALU.mult, op1=ALU.add),
                 R=[y0[b], hin[b]], W=[pre[b]])
            P.op("dve", I("scalar_tensor_tensor", pre[b].t[:, :], y1[b].t[:, :], C.gsel.t[:, 1, tt:tt + 1], pre[b].t[:, :], op0=ALU.mult, op1=ALU.add),
                 R=[y1[b], C.gsel, pre[b]], W=[pre[b]])
            if final:
                ln_tile(C, L, tt, pre[b], gam, bet, (C.Dout, C.out[tt * 128:(tt + 1) * 128, :]), None)
            else:
                ln_tile(C, L, tt, pre[b], gam, bet, (C.Dha, C.ha[tt * 128:(tt + 1) * 128, :]), (C.DhTa, C.hTa), tr_banks=0 + 2 * b)
        ln_flush(C, L)
        P.barrier()


IN_SPECS = None


def host_constants():
    c = {}
    c["c_ident"] = np.eye(128, dtype=np.float32)
    j = np.arange(128)[:, None]
    s = np.arange(128)[None, :]
    c["c_negU"] = -(j >= s).astype(np.float32)
    c["c_negO"] = -np.ones((128, 128), np.float32)
    c["c_lowI"] = (j <= s).astype(np.float32)
    c["c_ones"] = np.ones((128, 128), np.float32)
    p = np.arange(128)[:, None, None]
    dj = np.arange(4)[None, :, None]
    col = np.arange(512)[None, None, :]
    c["c_masks"] = ((128 * (3 - dj) + p) < col).astype(np.float32)
    invc = np.zeros((128, 4, 16), np.float32)
    for g in range(4):
        w = 2 << g
        invc[:, g, :] = 1.0 / np.minimum(np.arange(16) + 1, w)
    c["c_invc"] = invc
    c["c_iota"] = np.broadcast_to(np.arange(CAP, dtype=np.float32)[None, :], (128, CAP)).copy()
    c["c_ecap"] = np.broadcast_to((np.arange(NE, dtype=np.float32) * CAP)[None, None, :], (128, NT, NE)).copy()
    return c


def host_layout(inp):
    o = {}
    f = lambda a: np.ascontiguousarray(a, dtype=np.float32)
    o["ln_in_g"] = f(inp["ln_in_g"])
    o["ln_in_b"] = f(inp["ln_in_b"])
    o["w_in"] = f(inp["w_in"].reshape(DEPTH, 8, 128, 2048).transpose(0, 2, 1, 3))
    o["pool_w"] = f(inp["pool_w"].transpose(0, 2, 1, 3))
    o["pool_scale"] = f(inp["pool_scale"].reshape(DEPTH, 4, 128).transpose(0, 2, 1))
    o["w_out"] = f(inp["w_out"].reshape(DEPTH, 8, 128, D).transpose(0, 2, 1, 3))
    for nm in ("ln1_g", "ln1_b", "ln2_g", "ln2_b"):
        o[nm] = f(inp[nm])
    o["ffn_wg"] = f(inp["ffn_wg"].reshape(-1, 8, 128, NFC, 128).transpose(0, 3, 2, 1, 4))
    o["ffn_wu"] = f(inp["ffn_wu"].reshape(-1, 8, 128, NFC, 128).transpose(0, 3, 2, 1, 4))
    o["ffn_wd"] = f(inp["ffn_wd"])
    o["moe_router"] = f(inp["moe_router"].reshape(-1, 8, 128, NE).transpose(0, 2, 1, 3))
    o["moe_wg"] = f(inp["moe_wg"].reshape(-1, NE, 8, 128, NFC, 128).transpose(0, 1, 4, 3, 2, 5))
    o["moe_wu"] = f(inp["moe_wu"].reshape(-1, NE, 8, 128, NFC, 128).transpose(0, 1, 4, 3, 2, 5))
    o["moe_wd"] = f(inp["moe_wd"])
    o.update(host_constants())
    return o


def build(shapes, phases="all", debug_out=()):
    nc = bass.Bass("TRN2", target_bir_lowering=False)
    C = Ctx()
    C.nc = nc
    C.d = {}
    for name, shp in shapes.items():
        C.d[name] = nc.dram_tensor(name, list(shp), F32, kind="ExternalInput").ap()
    kind = lambda n: "ExternalOutput" if n in debug_out else "Internal"
    C.out = nc.dram_tensor("out", [T, D], F32, kind="ExternalOutput").ap()
    C.ha = nc.dram_tensor("s_ha", [T, D], F32, kind=kind("s_ha")).ap()
    C.hb = nc.dram_tensor("s_hb", [T, D], F32, kind=kind("s_hb")).ap()
    C.hTa = nc.dram_tensor("s_hTa", [D, T], BF16, kind=kind("s_hTa")).ap()
    C.hTb = nc.dram_tensor("s_hTb", [D, T], BF16, kind=kind("s_hTb")).ap()
    C.qT = nc.dram_tensor("s_qT", [512, T], BF16, kind=kind("s_qT")).ap()
    C.kT = nc.dram_tensor("s_kT", [512, T], BF16, kind=kind("s_kT")).ap()
    C.v = nc.dram_tensor("s_v", [T, 512], BF16, kind=kind("s_v")).ap()
    C.ycT = nc.dram_tensor("s_ycT", [D, T], BF16, kind=kind("s_ycT")).ap()
    C.ybuf = nc.dram_tensor("s_ybuf", [NE * CAP, D], F32, kind=kind("s_ybuf")).ap()
    C.Dout, C.Dha, C.Dhb, C.DhTa, C.DhTb, C.Dv, C.Dyb = TT(None), TT(None), TT(None), TT(None), TT(None), TT(None), TT(None)
    C.DqT = [TT(None) for _ in range(4)]
    C.DkT = [TT(None) for _ in range(4)]
    C.DycT = [TT(None) for _ in range(8)]
    with ExitStack() as es:
        P = Prog(nc, es)
        C.P = P
        sb = lambda n, s, d: es.enter_context(nc.sbuf_tensor(uniq(n), s, d))
        ps = es.enter_context(nc.psum_tensor("ps", [128, 8, 512], F32))
        C.ps = ps
        C.PS = [TT(None, f"bank{i}", excl=True) for i in range(8)]
        C.bank = lambda b: ps[:, b, :]
        C.ps2 = lambda b: ps[:, b:b + 2, :].rearrange("p a b -> p (a b)")
        C.ps2v = lambda b, a, w: ps[:, b:b + 2, :].rearrange("p a b -> p (a b)").rearrange("p (a w) -> p a w", w=w)
        C.ident = TT(sb("k_ident", [128, 128], F32))
        C.negU = TT(sb("k_negU", [128, 128], BF16))
        C.negO = TT(sb("k_negO", [128, 128], BF16))
        C.lowI = TT(sb("k_lowI", [128, 128], BF16))
        C.ones = TT(sb("k_ones", [128, 128], BF16))
        C.masks = TT(sb("k_masks", [128, 4, 512], BF16))
        C.gate = TT(sb("k_gate", [128, NT, NE], F32))
        C.sel = TT(sb("k_sel", [128, NT, NE], F32))
        C.slotf = TT(sb("k_slotf", [128, 2, NT], F32))
        C.sloti = TT(sb("k_sloti", [128, 2, NT], U32))
        C.gsel = TT(sb("k_gsel", [128, 2, NT], F32))
        C.toki = TT(sb("k_toki", [128, NE, NST], U32))
        sem_k = P.dma_sem("k_c", sw=True)
        sem_k2 = P.dma_sem("k_c2")
        P.dma("sync", C.ident.t[:, :], C.d["c_ident"], sem_k2, W=[C.ident])
        P.dma("pool", C.negU.t[:, :], C.d["c_negU"], sem_k, W=[C.negU])
        P.dma("pool", C.negO.t[:, :], C.d["c_negO"], sem_k, W=[C.negO])
        P.dma("pool", C.lowI.t[:, :], C.d["c_lowI"], sem_k, W=[C.lowI])
        P.dma("pool", C.ones.t[:, :], C.d["c_ones"], sem_k, W=[C.ones])
        P.dma("pool", C.masks.t[:, :, :], C.d["c_masks"], sem_k, W=[C.masks])
        P.batch(sem_k, [C.negU, C.negO, C.lowI, C.ones, C.masks])

        def want(p):
            if phases == "all":
                return not (len(p) == 3 and p[2] in "ab")
            return p in phases

        if want("A"):
            phase_ln_in(C)
        for l in range(DEPTH):
            final = (l == DEPTH - 1)
            if want(f"B{l}"):
                phase_proj(C, l)
            if want(f"C{l}"):
                phase_attn(C, l)
            if want(f"D{l}"):
                phase_mix_ln1(C, l)
            if want(f"E{l}") or want(f"E{l}a") or want(f"E{l}b"):
                if l % 2 == 0:
                    phase_ffn_dense(C, l, final)
                else:
                    phase_moe(C, l, final, stop=(1 if want(f"E{l}a") else 2 if want(f"E{l}b") else 3))
        if "k_dbg" in debug_out:
            sem_d = P.dma_sem("dbg")
            for nm, tt_, shp, dt in (("k_gate", C.gate, [128, NT, NE], F32), ("k_sel", C.sel, [128, NT, NE], F32),
                                     ("k_slotf", C.slotf, [128, 2, NT], F32), ("k_sloti", C.sloti, [128, 2, NT], U32),
                                     ("k_gsel", C.gsel, [128, 2, NT], F32), ("k_toki", C.toki, [128, NE, NST], U32)):
                dd_ = nc.dram_tensor("dbg_" + nm, shp, dt, kind="ExternalOutput").ap()
                P.dma("sync", dd_, tt_.t[:, :, :], sem_d, R=[tt_])
        P.barrier()
        print("bass ops:", P.nops, {e: len(P.q[e]) for e in ENG})
        P.flush()
    return nc


_CACHE = {}


def kernel(**inputs):
    lay = host_layout(inputs)
    x = np.ascontiguousarray(inputs["x"], dtype=np.float32)
    shapes = {"x": (T, D)}
    for k, v in lay.items():
        shapes[k] = v.shape
    if "nc" not in _CACHE:
        import os
        dbg = ("s_ha", "s_hb", "s_hTa", "s_hTb", "s_qT", "s_kT", "s_v", "s_ycT", "s_ybuf") if os.environ.get("SCRATCH_OUT") == "1" else ()
        _CACHE["nc"] = build(shapes, debug_out=dbg)
    nc = _CACHE["nc"]
    in_maps = []
    for b in range(NCORES):
        m = {"x": x[b]}
        m.update(lay)
        in_maps.append(m)
    res = run_bass_kernel_spmd(nc, in_maps, core_ids=list(range(NCORES)))
    out = np.stack([np.asarray(res.results[b]["out"], dtype=np.float32) for b in range(NCORES)], axis=0)
    return out
```

```python
import numpy as np
from contextlib import ExitStack
import concourse.bass as bass
import concourse.mybir as mybir
from concourse.bass_utils import run_bass_kernel_spmd

F32 = mybir.dt.float32
BF16 = mybir.dt.bfloat16
I32 = mybir.dt.int32
U32 = mybir.dt.uint32
AF = mybir.ActivationFunctionType
ALU = mybir.AluOpType
AX = mybir.AxisListType

NCORES = 8
T = 4096
D = 1024
NT = T // 128
DEPTH = 2
DFF = 2816
NFC = DFF // 128
NE = 8
CAP = 1280
NST = CAP // 128
ALPHA = float((2 * DEPTH) ** 0.25)
EPS = 1e-5
ENG = ("sync", "act", "pe", "dve", "pool")


_UNIQ = [0]


def uniq(n):
    _UNIQ[0] += 1
    return f"{n}_{_UNIQ[0]}"


def I(meth, *a, **k):
    return lambda e: getattr(e, meth)(*a, **k)


class TT:
    __slots__ = ("t", "w", "r", "name", "excl")

    def __init__(self, t, name="", excl=False):
        self.t = t
        self.w = None
        self.r = {}
        self.name = name
        self.excl = excl


class Prog:
    def __init__(self, nc, es):
        self.nc = nc
        self.es = es
        self.q = {e: [] for e in ENG}
        self.waited = {e: {} for e in ENG}
        self.psem = {e: self._mksem("p_" + e) for e in ENG}
        self.dsems = []
        self.dpool_sw = []
        self.dpool_hw = []
        self.nops = 0

    def _mksem(self, name):
        h = self.es.enter_context(self.nc.semaphore(name))
        return {"h": h, "n": 0, "name": name}

    def dma_sem(self, name, sw=False):
        pool = self.dpool_sw if sw else self.dpool_hw
        if pool:
            s = pool.pop()
        else:
            s = self._mksem(uniq(name))
            s["sw"] = sw
        self.dsems.append(s)
        return s

    def _wait(self, eng, dep):
        s, v = dep
        if eng == "pe" and s is self.psem["pe"]:
            return
        if self.waited[eng].get(s["name"], 0) >= v:
            return
        self.waited[eng][s["name"]] = v
        self.q[eng].append(("wait", s["h"], v))

    def _deps(self, R, W):
        deps = []
        for b in R:
            if b.w is not None:
                deps.append(b.w)
        for b in W:
            if b.w is not None:
                deps.append(b.w)
            deps.extend(b.r.values())
        return deps

    def _mark(self, dep, R, W):
        for b in R:
            b.r[dep[0]["name"]] = dep
        for b in W:
            b.w = dep
            b.r = {}

    def op(self, eng, fn, R=(), W=()):
        if any(b.excl for b in R):
            W = list(W) + [b for b in R if b.excl and b not in W]
            R = [b for b in R if not b.excl]
        for d in self._deps(R, W):
            self._wait(eng, d)
        s = self.psem[eng]
        s["n"] += 1
        dep = (s, s["n"])
        self.q[eng].append(("op", fn, s["h"], 1))
        self._mark(dep, R, W)
        self.nops += 1
        return dep

    def dma(self, eng, out, in_, sem, R=(), W=()):
        assert bool(sem.get("sw")) == (eng == "pool"), (eng, sem["name"])
        for d in self._deps(R, W):
            if d[0] is sem:
                continue
            self._wait(eng, d)
        sem["n"] += 16
        dep = (sem, sem["n"])
        self.q[eng].append(("op", lambda e: e.dma_start(out=out, in_=in_), sem["h"], 16))
        self._mark(dep, R, W)
        self.nops += 1
        return dep

    def batch(self, sem, tts):
        for b in tts:
            b.w = (sem, sem["n"])

    def gather(self, out, in_, idx_ap, sem, R=(), W=()):
        eng = "pool"
        for d in self._deps(R, W):
            self._wait(eng, d)
        sem["n"] += 16
        dep = (sem, sem["n"])
        self.q[eng].append(("op", lambda e: e.indirect_dma_start(
            out=out, out_offset=None, in_=in_,
            in_offset=bass.IndirectOffsetOnAxis(ap=idx_ap, axis=0)), sem["h"], 16))
        self._mark(dep, R, W)
        self.nops += 1
        return dep

    def barrier(self):
        sems = list(self.psem.values()) + self.dsems
        for e in ENG:
            for s in sems:
                if s["n"] > 0:
                    self._wait(e, (s, s["n"]))
        for s_ in self.dsems:
            (self.dpool_sw if s_.get("sw") else self.dpool_hw).append(s_)
        self.dsems = []
        for e in ENG:
            if self.psem[e]["n"] > 8000:
                self.psem[e] = self._mksem(uniq("p_" + e))

    def flush(self):
        nc = self.nc
        q = self.q

        def run(e, lst):
            for it in lst:
                if it[0] == "wait":
                    e.wait_ge(it[1], it[2])
                else:
                    ins = it[1](e)
                    ins.then_inc(it[2], it[3])

        with nc.Block() as block:
            @block.sync
            def _(e):
                run(e, q["sync"])

            @block.scalar
            def _(e):
                run(e, q["act"])

            @block.tensor
            def _(e):
                run(e, q["pe"])

            @block.vector
            def _(e):
                run(e, q["dve"])

            @block.gpsimd
            def _(e):
                run(e, q["pool"])


class Ctx:
    pass


def bcast_row(ap1d, n):
    return bass.AP(ap1d.tensor, ap1d.offset, [[0, 128], [1, n]])


def alloc_ln(C, es, want_T=True):
    nc, P = C.nc, C.P
    L = Ctx()
    sb = lambda n, s, d: es.enter_context(nc.sbuf_tensor(uniq(n), s, d))
    L.stats = [TT(sb(f"ln_stats{i}", [128, 12], F32)) for i in range(2)]
    L.mv = [TT(sb(f"ln_mv{i}", [128, 8], F32)) for i in range(2)]
    L.xn = [TT(sb(f"ln_xn{i}", [128, D], F32)) for i in range(2)]
    L.ho = [TT(sb(f"ln_ho{i}", [128, D], F32)) for i in range(2)]
    L.want_T = want_T
    if want_T:
        L.hTs = [TT(sb(f"ln_hTs{i}", [128, 8, 512], BF16)) for i in range(2)]
    L.sem_st = [P.dma_sem("ln_st0"), P.dma_sem("ln_st1")]
    L.sem_sT = [P.dma_sem("ln_sT0"), P.dma_sem("ln_sT1")]
    L.cnt = 0
    L.pend = []
    return L


def ln_tile(C, L, tt, pre, gam, bet, h_dst, hT_dst, tr_banks=None, router=None):
    i = L.cnt
    L.cnt += 1
    L.pend.append(dict(i=i, tt=tt, pre=pre, gam=gam, bet=bet, h_dst=h_dst, hT_dst=hT_dst, tr=tr_banks, router=router))
    _ln_step(C, L)


def ln_flush(C, L):
    for _ in range(2):
        L.pend.append(None)
        _ln_step(C, L)
    L.pend = []


def _ln_step(C, L):
    P = C.P
    n = len(L.pend)
    t0 = L.pend[n - 1]
    t1 = L.pend[n - 2] if n >= 2 else None
    t2 = L.pend[n - 3] if n >= 3 else None
    if t0 is not None:
        b = t0["i"] % 2
        st, mv, pre = L.stats[b], L.mv[b], t0["pre"]
        P.op("dve", I("bn_stats", st.t[:, 0:6], pre.t[:, 0:512]), R=[pre], W=[st])
        P.op("dve", I("bn_stats", st.t[:, 6:12], pre.t[:, 512:1024]), R=[pre], W=[st])
        P.op("dve", I("bn_aggr", mv.t[:, 0:2], st.t[:, :]), R=[st], W=[mv])
        P.op("dve", I("tensor_scalar_add", mv.t[:, 2:3], mv.t[:, 1:2], EPS), R=[mv], W=[mv])
    if t0 is not None:
        b = t0["i"] % 2
        mv = L.mv[b]
        P.op("act", I("activation", out=mv.t[:, 3:4], in_=mv.t[:, 2:3], func=AF.Ln), R=[mv], W=[mv])
        P.op("act", I("activation", out=mv.t[:, 4:5], in_=mv.t[:, 3:4], func=AF.Exp, scale=-0.5), R=[mv], W=[mv])
    if t1 is not None:
        b = t1["i"] % 2
        mv, xn, ho, pre = L.mv[b], L.xn[b], L.ho[b], t1["pre"]
        P.op("dve", I("scalar_tensor_tensor", xn.t[:, :], pre.t[:, :], mv.t[:, 0:1], t1["gam"].t[:, :], op0=ALU.subtract, op1=ALU.mult),
             R=[pre, mv, t1["gam"]], W=[xn])
        P.op("dve", I("scalar_tensor_tensor", ho.t[:, :], xn.t[:, :], mv.t[:, 4:5], t1["bet"].t[:, :], op0=ALU.mult, op1=ALU.add),
             R=[xn, mv, t1["bet"]], W=[ho])
        P.dma("sync", t1["h_dst"][1], ho.t[:, :], L.sem_st[b], R=[ho], W=[t1["h_dst"][0]])
        if t1["router"] is not None:
            t1["router"](t1["tt"], ho)
    if t2 is not None and t2["hT_dst"] is not None:
        b = t2["i"] % 2
        ho = L.ho[b]
        tt = t2["tt"]
        slot = tt % 4
        hTs = L.hTs[(tt // 4) % 2]
        bk = t2["tr"]
        flat = C.ps2(bk)
        for k in range(8):
            P.op("pe", I("transpose", flat[:, k * 128:(k + 1) * 128], ho.t[:, k * 128:(k + 1) * 128], C.ident.t[:, :]),
                 R=[ho, C.ident], W=[C.PS[bk], C.PS[bk + 1]])
        P.op("act", I("activation", out=hTs.t[:, :, slot * 128:(slot + 1) * 128],
                      in_=C.ps2v(bk, 8, 128), func=AF.Copy),
             R=[C.PS[bk], C.PS[bk + 1]], W=[hTs])
        if slot == 3:
            c0 = (tt - 3) * 128
            dst = t2["hT_dst"][1].rearrange("(c p) t -> p c t", p=128)[:, :, c0:c0 + 512]
            P.dma("sync", dst, hTs.t[:, :, :], L.sem_sT[(tt // 4) % 2], R=[hTs], W=[t2["hT_dst"][0]])


def phase_ln_in(C):
    nc, P = C.nc, C.P
    with ExitStack() as es:
        sb = lambda n, s, d: es.enter_context(nc.sbuf_tensor(uniq(n), s, d))
        gam = TT(sb("a_gam", [128, D], F32))
        bet = TT(sb("a_bet", [128, D], F32))
        xin = [TT(sb(f"a_xin{i}", [128, D], F32)) for i in range(2)]
        sem_c = P.dma_sem("a_c")
        sem_x = [P.dma_sem("a_x0", sw=True), P.dma_sem("a_x1", sw=True)]
        P.dma("sync", gam.t[:, :], bcast_row(C.d["ln_in_g"], D), sem_c, W=[gam])
        P.dma("sync", bet.t[:, :], bcast_row(C.d["ln_in_b"], D), sem_c, W=[bet])
        P.batch(sem_c, [gam, bet])
        L = alloc_ln(C, es)
        for tt in range(NT):
            b = tt % 2
            P.dma("pool", xin[b].t[:, :], C.d["x"][tt * 128:(tt + 1) * 128, :], sem_x[b], W=[xin[b]])
            ln_tile(C, L, tt, xin[b], gam, bet, (C.Dha, C.ha[tt * 128:(tt + 1) * 128, :]), (C.DhTa, C.hTa), tr_banks=0 + 2 * (tt % 2))
        ln_flush(C, L)
        P.barrier()


def phase_proj(C, l):
    nc, P = C.nc, C.P
    with ExitStack() as es:
        sb = lambda n, s, d: es.enter_context(nc.sbuf_tensor(uniq(n), s, d))
        wi = TT(sb("b_wi", [128, 8, 2048], BF16))
        hT = [TT(sb(f"b_hT{k}", [128, T], BF16)) for k in range(8)]
        pw = TT(sb("b_pw", [128, 4, 128], BF16))
        psc = TT(sb("b_psc", [128, 4], F32))
        invc = TT(sb("b_invc", [128, 4, 16], F32))
        U = TT(sb("b_U", [128, 16 + T], F32))
        SA = TT(sb("b_SA", [128, 16 + T], F32))
        SB = TT(sb("b_SB", [128, 16 + T], F32))
        dd = TT(sb("b_d", [128, T], BF16))
        stg = [TT(sb(f"b_stg{i}", [128, T], BF16)) for i in range(2)]
        vst = [TT(sb(f"b_vst{i}", [128, 4, 512], BF16)) for i in range(2)]
        sem_w = P.dma_sem("b_w", sw=True)
        sem_h = P.dma_sem("b_h")
        sem_c = P.dma_sem("b_c")
        sem_cw = P.dma_sem("b_cw", sw=True)
        sem_s = [P.dma_sem("b_s0"), P.dma_sem("b_s1")]
        sem_v = [P.dma_sem("b_v0"), P.dma_sem("b_v1")]
        for q4 in range(4):
            P.dma("pool", wi.t[:, :, q4 * 512:(q4 + 1) * 512], C.d["w_in"][l][:, :, q4 * 512:(q4 + 1) * 512], sem_w, W=[wi])
        for k in range(8):
            P.dma("sync", hT[k].t[:, :], C.hTa[k * 128:(k + 1) * 128, :], sem_h, R=[C.DhTa], W=[hT[k]])
        P.dma("pool", pw.t[:, :, :], C.d["pool_w"][l], sem_cw, W=[pw])
        P.dma("sync", psc.t[:, :], C.d["pool_scale"][l], sem_c, W=[psc])
        P.dma("sync", invc.t[:, :, :], C.d["c_invc"], sem_c, W=[invc])
        P.batch(sem_c, [psc, invc])
        P.batch(sem_h, hT)
        for buf in (U, SA, SB):
            P.op("dve", I("memset", buf.t[:, 0:16], 0.0), W=[buf])

        nbank = [0]

        def next_bank():
            b = nbank[0] % 4
            nbank[0] += 1
            return b

        evac_eng = [0]

        def evac(out_ap, in_ap, R, W, scale=None):
            e = evac_eng[0] % 2
            evac_eng[0] += 1
            if e == 0:
                if scale is None:
                    P.op("act", I("activation", out=out_ap, in_=in_ap, func=AF.Copy), R=R, W=W)
                else:
                    P.op("act", I("activation", out=out_ap, in_=in_ap, func=AF.Identity, scale=scale), R=R, W=W)
            else:
                if scale is None:
                    P.op("dve", I("tensor_copy", out_ap, in_ap), R=R, W=W)
                else:
                    P.op("dve", I("tensor_scalar_mul", out_ap, in_ap, scale), R=R, W=W)

        def fm_chunk(c, out_fn):
            for tg in range(8):
                bk = next_bank()
                for k in range(8):
                    P.op("pe", I("matmul", C.bank(bk), wi.t[:, k, c * 128:(c + 1) * 128], hT[k].t[:, tg * 512:(tg + 1) * 512],
                                 start=(k == 0), stop=(k == 7)), R=[wi, hT[k]], W=[C.PS[bk]])
                out_fn(tg, bk)

        sidx = [0]
        for g in range(4):
            w = 2 << g
            fm_chunk(g, lambda tg, bk: evac(U.t[:, 16 + tg * 512:16 + (tg + 1) * 512], C.bank(bk), [C.PS[bk]], [U]))
            src = U
            bufs = [SA, SB]
            nlev = g + 1
            for lev in range(nlev):
                dst = bufs[lev % 2]
                sh = 1 << lev
                eng = "dve"
                P.op(eng, I("tensor_tensor", dst.t[:, 16:16 + T], src.t[:, 16:16 + T], src.t[:, 16 - sh:16 + T - sh], op=ALU.add),
                     R=[src], W=[dst])
                src = dst
            P.op("dve", I("scalar_tensor_tensor", dd.t[:, :], src.t[:, 16:16 + T], 1.0 / w, U.t[:, 16:16 + T],
                          op0=ALU.mult, op1=ALU.subtract), R=[src, U], W=[dd])
            P.op("dve", I("tensor_tensor", SA.t[:, 0:16] if src is SB else SB.t[:, 0:16], src.t[:, 16:32], invc.t[:, g, :], op=ALU.mult),
                 R=[src, invc], W=[SA if src is SB else SB])
            tmp = SA if src is SB else SB
            P.op("dve", I("tensor_tensor", dd.t[:, 0:16], tmp.t[:, 0:16], U.t[:, 16:32], op=ALU.subtract),
                 R=[tmp, U], W=[dd])
            P.op("dve", I("memset", tmp.t[:, 0:16], 0.0), W=[tmp])
            so = stg[sidx[0] % 2]
            ss = sem_s[sidx[0] % 2]
            sidx[0] += 1
            for tg in range(8):
                bk = next_bank()
                P.op("pe", I("matmul", C.bank(bk), pw.t[:, g, :], dd.t[:, tg * 512:(tg + 1) * 512], start=True, stop=True),
                     R=[pw, dd], W=[C.PS[bk]])
                evac(so.t[:, tg * 512:(tg + 1) * 512], C.bank(bk), [C.PS[bk], psc], [so], scale=psc.t[:, g:g + 1])
            P.dma("sync", C.ycT[g * 128:(g + 1) * 128, :], so.t[:, :], ss, R=[so], W=[C.DycT[g]])
        for c in range(4, 12):
            so = stg[sidx[0] % 2]
            ss = sem_s[sidx[0] % 2]
            sidx[0] += 1
            sc = 0.125 if c < 8 else None
            fm_chunk(c, lambda tg, bk, so=so, sc=sc: evac(so.t[:, tg * 512:(tg + 1) * 512], C.bank(bk), [C.PS[bk]], [so], scale=sc))
            if c < 8:
                P.dma("sync", C.qT[(c - 4) * 128:(c - 3) * 128, :], so.t[:, :], ss, R=[so], W=[C.DqT[c - 4]])
            else:
                P.dma("sync", C.kT[(c - 8) * 128:(c - 7) * 128, :], so.t[:, :], ss, R=[so], W=[C.DkT[c - 8]])
        vv = C.v.rearrange("(n p) c -> p n c", p=128)
        for tt in range(NT):
            bk = next_bank()
            for k in range(8):
                P.op("pe", I("matmul", C.bank(bk), hT[k].t[:, tt * 128:(tt + 1) * 128], wi.t[:, k, 1536:2048],
                             start=(k == 0), stop=(k == 7)), R=[wi, hT[k]], W=[C.PS[bk]])
            vs = vst[(tt // 4) % 2]
            evac(vs.t[:, tt % 4, :], C.bank(bk), [C.PS[bk]], [vs])
            if tt % 4 == 3:
                P.dma("sync", vv[:, tt - 3:tt + 1, :], vs.t[:, :, :], sem_v[(tt // 4) % 2], R=[vs], W=[C.Dv])
        P.barrier()


def phase_attn(C, l):
    nc, P = C.nc, C.P
    with ExitStack() as es:
        sb = lambda n, s, d: es.enter_context(nc.sbuf_tensor(uniq(n), s, d))
        qT = [TT(sb(f"c_qT{i}", [128, T], BF16)) for i in range(4)]
        kT = [TT(sb(f"c_kT{i}", [128, T], BF16)) for i in range(4)]
        V = TT(sb("c_V", [128, NT, 512], BF16))
        E = [TT(sb(f"c_E{i}", [128, 2048], F32)) for i in range(2)]
        SP = [TT(sb(f"c_SP{i}", [128, 2048], BF16)) for i in range(4)]
        CA = [TT(sb(f"c_CA{i}", [128, 2048], BF16)) for i in range(4)]
        AT = [TT(sb(f"c_AT{i}", [128, 2048], BF16)) for i in range(3)]
        ys = [TT(sb(f"c_ys{i}", [128, T], BF16)) for i in range(2)]
        sem_l = P.dma_sem("c_l")
        sem_s = [P.dma_sem("c_s0"), P.dma_sem("c_s1")]
        for i in range(4):
            P.dma("sync", qT[i].t[:, :], C.qT[i * 128:(i + 1) * 128, :], sem_l, R=[C.DqT[i]], W=[qT[i]])
            P.dma("sync", kT[i].t[:, :], C.kT[i * 128:(i + 1) * 128, :], sem_l, R=[C.DkT[i]], W=[kT[i]])
        vv = C.v.rearrange("(n p) c -> p n c", p=128)
        for i in range(4):
            P.dma("sync", V.t[:, i * 8:(i + 1) * 8, :], vv[:, i * 8:(i + 1) * 8, :], sem_l, R=[C.Dv], W=[V])
        P.batch(sem_l, qT + kT + [V])

        ZP = [0, 2]
        BP = 4
        OB = [6, 7]
        quads = []
        for hp in range(4):
            for e in range(2):
                for g in range(8):
                    nq = g + 1
                    for qi in range(nq):
                        js = [4 * g + 3 - 4 * qi - k for k in range(4)]
                        quads.append(dict(hp=hp, e=e, g=g, js=js, first=(qi == 0), last=(qi == nq - 1)))
        n = len(quads)
        masks2 = C.masks.t[:, :, :].rearrange("p a b -> p (a b)")

        def zq(s):
            q = quads[s]
            hp, e, g = q["hp"], q["e"], q["g"]
            pr = slice(64 * e, 64 * e + 64)
            for k in range(4):
                bk = ZP[k // 2] + k % 2
                j = q["js"][k]
                P.op("pe", I("matmul", C.bank(bk), kT[hp].t[pr, j * 128:(j + 1) * 128], qT[hp].t[pr, g * 512:(g + 1) * 512],
                             start=True, stop=True), R=[kT[hp], qT[hp]], W=[C.PS[bk]])

        def act1(s):
            Eb = E[s % 2]
            z4 = C.ps[:, 0:4, :].rearrange("p a b -> p (a b)")
            P.op("act", I("activation", out=Eb.t[:, :], in_=z4, func=AF.Exp),
                 R=[C.PS[0], C.PS[1], C.PS[2], C.PS[3]], W=[Eb])

        def act2(s):
            P.op("act", I("activation", out=SP[s % 4].t[:, :], in_=E[s % 2].t[:, :], func=AF.Ln, bias=1.0), R=[E[s % 2]], W=[SP[s % 4]])

        def dve3(s):
            q = quads[s]
            S, Cq, Cn = SP[s % 4], CA[s % 4], CA[(s + 1) % 4]
            sl = lambda k: slice(k * 512, (k + 1) * 512)
            if q["first"]:
                P.op("dve", I("tensor_tensor", S.t[:, :], S.t[:, :], masks2, op=ALU.mult), R=[S, C.masks], W=[S])
                P.op("dve", I("tensor_tensor", Cq.t[:, sl(2)], S.t[:, sl(0)], S.t[:, sl(1)], op=ALU.add), R=[S], W=[Cq])
            else:
                P.op("dve", I("tensor_tensor", Cq.t[:, sl(1)], Cq.t[:, sl(0)], S.t[:, sl(0)], op=ALU.add), R=[S, Cq], W=[Cq])
                P.op("dve", I("tensor_tensor", Cq.t[:, sl(2)], Cq.t[:, sl(1)], S.t[:, sl(1)], op=ALU.add), R=[S, Cq], W=[Cq])
            P.op("dve", I("tensor_tensor", Cq.t[:, sl(3)], Cq.t[:, sl(2)], S.t[:, sl(2)], op=ALU.add), R=[S, Cq], W=[Cq])
            if not q["last"]:
                P.op("dve", I("tensor_tensor", Cn.t[:, sl(0)], Cq.t[:, sl(3)], S.t[:, sl(3)], op=ALU.add), R=[S, Cq], W=[Cn])

        def bq(s, p):
            q = quads[s]
            hp, e, g = q["hp"], q["e"], q["g"]
            pr = slice(64 * e, 64 * e + 64)
            S, Cq = SP[s % 4], CA[s % 4]
            for k in (2 * p, 2 * p + 1):
                bk = BP + k % 2
                j = q["js"][k]
                if q["first"] and k == 0:
                    car = None
                elif q["first"] and k == 1:
                    car = (S, S.t[:, 0:512])
                else:
                    car = (Cq, Cq.t[:, k * 512:(k + 1) * 512])
                P.op("pe", I("matmul", C.bank(bk), kT[hp].t[pr, j * 128:(j + 1) * 128], qT[hp].t[pr, g * 512:(g + 1) * 512],
                             start=True, stop=False), R=[kT[hp], qT[hp]], W=[C.PS[bk]])
                P.op("pe", I("matmul", C.bank(bk), C.negU.t[:, :], S.t[:, k * 512:(k + 1) * 512], start=False, stop=(car is None)),
                     R=[C.negU, S], W=[C.PS[bk]])
                if car is not None:
                    P.op("pe", I("matmul", C.bank(bk), C.negO.t[:, :], car[1], start=False, stop=True),
                         R=[C.negO, car[0]], W=[C.PS[bk]])

        def act3(s, p):
            A = AT[s % 3]
            P.op("act", I("activation", out=A.t[:, p * 1024:(p + 1) * 1024], in_=C.ps2(BP), func=AF.Exp),
                 R=[C.PS[BP], C.PS[BP + 1]], W=[A])

        def mask_att(s):
            q = quads[s]
            if q["first"]:
                A = AT[s % 3]
                P.op("dve", I("tensor_tensor", A.t[:, :], A.t[:, :], masks2, op=ALU.mult), R=[A, C.masks], W=[A])

        def av(s):
            q = quads[s]
            hp, e, g = q["hp"], q["e"], q["g"]
            gi = (hp * 2 + e) * 8 + g
            ob = OB[gi % 2]
            A = AT[s % 3]
            pr = slice(64 * e, 64 * e + 64)
            for k in range(4):
                j = q["js"][k]
                P.op("pe", I("matmul", C.bank(ob)[pr, :], V.t[:, j, hp * 128 + 64 * e: hp * 128 + 64 * e + 64], A.t[:, k * 512:(k + 1) * 512],
                             start=(q["first"] and k == 0), stop=(q["last"] and k == 3)), R=[V, A], W=[C.PS[ob]])
            if q["last"]:
                yb = ys[hp % 2]
                P.op("dve", I("tensor_copy", yb.t[pr, g * 512:(g + 1) * 512], C.bank(ob)[pr, :]), R=[C.PS[ob]], W=[yb])
                if e == 1 and g == 7:
                    P.dma("sync", C.ycT[512 + hp * 128:512 + (hp + 1) * 128, :], yb.t[:, :], sem_s[hp % 2], R=[yb], W=[C.DycT[4 + hp]])

        for s in range(n + 3):
            if s < n:
                zq(s)
            if 0 <= s - 2 < n:
                bq(s - 2, 0)
                act3(s - 2, 0)
            if s < n:
                act1(s)
            if 0 <= s - 3 < n:
                av(s - 3)
            if 0 <= s - 2 < n:
                bq(s - 2, 1)
                act3(s - 2, 1)
            if s < n:
                act2(s)
                dve3(s)
            if 0 <= s - 2 < n:
                mask_att(s - 2)
        P.barrier()


def phase_mix_ln1(C, l):
    nc, P = C.nc, C.P
    moe = (l % 2 == 1)
    with ExitStack() as es:
        sb = lambda n, s, d: es.enter_context(nc.sbuf_tensor(uniq(n), s, d))
        yc = [TT(sb(f"d_yc{k}", [128, T], BF16)) for k in range(8)]
        wo = TT(sb("d_wo", [128, 8, D], BF16))
        gam = TT(sb("d_gam", [128, D], F32))
        bet = TT(sb("d_bet", [128, D], F32))
        hin = [TT(sb(f"d_hin{i}", [128, D], F32)) for i in range(2)]
        pre = [TT(sb(f"d_pre{i}", [128, D], F32)) for i in range(2)]
        sem_c = P.dma_sem("d_c")
        sem_cw = P.dma_sem("d_cw", sw=True)
        sem_y = P.dma_sem("d_y")
        sem_h = [P.dma_sem("d_h0", sw=True), P.dma_sem("d_h1", sw=True)]
        P.dma("pool", wo.t[:, :, :], C.d["w_out"][l], sem_cw, W=[wo])
        P.dma("sync", gam.t[:, :], bcast_row(C.d["ln1_g"][l], D), sem_c, W=[gam])
        P.dma("sync", bet.t[:, :], bcast_row(C.d["ln1_b"][l], D), sem_c, W=[bet])
        for k in range(8):
            P.dma("sync", yc[k].t[:, :], C.ycT[k * 128:(k + 1) * 128, :], sem_y, R=[C.DycT[k]], W=[yc[k]])
        P.batch(sem_y, yc)
        L = alloc_ln(C, es)
        router = None
        P.batch(sem_c, [gam, bet])
        if moe:
            rt = TT(sb("d_rt", [128, 8, NE], F32))
            P.dma("sync", rt.t[:, :, :], C.d["moe_router"][l // 2], sem_c, W=[rt])
            P.batch(sem_c, [gam, bet, rt])
            hT32 = [TT(sb(f"d_hT32{i}", [128, 8, 128], F32)) for i in range(2)]
            lgt = TT(sb("d_lgt", [128, NT, NE], F32))
            wk = TT(sb("d_wk", [128, NT, NE], F32))
            m1 = TT(sb("d_m1", [128, NT], F32))
            m2 = TT(sb("d_m2", [128, NT], F32))

            def router(tt, ho):
                b = tt % 2
                bk = 4 + 2 * b
                flat = C.ps2(bk)
                for k in range(8):
                    P.op("pe", I("transpose", flat[:, k * 128:(k + 1) * 128], ho.t[:, k * 128:(k + 1) * 128], C.ident.t[:, :]),
                         R=[ho, C.ident], W=[C.PS[bk], C.PS[bk + 1]])
                P.op("dve", I("tensor_copy", hT32[b].t[:, :, :], C.ps2v(bk, 8, 128)), R=[C.PS[bk], C.PS[bk + 1]], W=[hT32[b]])
                lb = C.bank(bk)[:, 0:NE]
                for k in range(8):
                    P.op("pe", I("matmul", lb, hT32[b].t[:, k, :], rt.t[:, k, :], start=(k == 0), stop=(k == 7)),
                         R=[hT32[b], rt], W=[C.PS[bk], C.PS[bk + 1]])
                P.op("dve", I("tensor_copy", lgt.t[:, tt, :], lb), R=[C.PS[bk], C.PS[bk + 1]], W=[lgt])

            def router_finish():
                fl = lambda t_: t_.t[:, :, :].rearrange("p a b -> p (a b)")
                P.op("dve", I("tensor_reduce", m1.t[:, :], lgt.t[:, :, :], AX.X, ALU.max), R=[lgt], W=[m1])
                for e_ in range(NE):
                    P.op("dve", I("tensor_tensor", wk.t[:, :, e_], lgt.t[:, :, e_], m1.t[:, :], op=ALU.is_equal), R=[lgt, m1], W=[wk])
                P.op("dve", I("scalar_tensor_tensor", fl(wk), fl(wk), -1e30, fl(lgt), op0=ALU.mult, op1=ALU.add), R=[wk, lgt], W=[wk])
                P.op("dve", I("tensor_reduce", m2.t[:, :], wk.t[:, :, :], AX.X, ALU.max), R=[wk], W=[m2])
                for e_ in range(NE):
                    P.op("dve", I("tensor_tensor", wk.t[:, :, e_], lgt.t[:, :, e_], m1.t[:, :], op=ALU.subtract), R=[lgt, m1], W=[wk])
                P.op("act", I("activation", out=fl(wk), in_=fl(wk), func=AF.Exp), R=[wk], W=[wk])
                for e_ in range(NE):
                    P.op("dve", I("tensor_tensor", C.sel.t[:, :, e_], lgt.t[:, :, e_], m2.t[:, :], op=ALU.is_ge), R=[lgt, m2], W=[C.sel])
                P.op("dve", I("tensor_tensor", fl(wk), fl(wk), fl(C.sel), op=ALU.mult), R=[wk, C.sel], W=[wk])
                P.op("dve", I("tensor_reduce", m1.t[:, :], wk.t[:, :, :], AX.X, ALU.add), R=[wk], W=[m1])
                P.op("dve", I("reciprocal", m1.t[:, :], m1.t[:, :]), R=[m1], W=[m1])
                for e_ in range(NE):
                    P.op("dve", I("tensor_tensor", C.gate.t[:, :, e_], wk.t[:, :, e_], m1.t[:, :], op=ALU.mult), R=[wk, m1], W=[C.gate])

        for tt in range(NT):
            b = tt % 2
            P.dma("pool", hin[b].t[:, :], C.ha[tt * 128:(tt + 1) * 128, :], sem_h[b], R=[C.Dha], W=[hin[b]])
            bk = 0 + 2 * b
            for nh in range(2):
                for k in range(8):
                    P.op("pe", I("matmul", C.bank(bk + nh), yc[k].t[:, tt * 128:(tt + 1) * 128], wo.t[:, k, nh * 512:(nh + 1) * 512],
                                 start=(k == 0), stop=(k == 7)), R=[yc[k], wo], W=[C.PS[bk], C.PS[bk + 1]])
            P.op("dve", I("scalar_tensor_tensor", pre[b].t[:, :], hin[b].t[:, :], ALPHA, C.ps2(bk), op0=ALU.mult, op1=ALU.add),
                 R=[hin[b], C.PS[bk], C.PS[bk + 1]], W=[pre[b]])
            ln_tile(C, L, tt, pre[b], gam, bet, (C.Dhb, C.hb[tt * 128:(tt + 1) * 128, :]),
                    None if moe else (C.DhTb, C.hTb), tr_banks=4 + 2 * b, router=router)
        ln_flush(C, L)
        if moe:
            router_finish()
        P.barrier()


def alloc_ffn(C, es, ntok):
    nc, P = C.nc, C.P
    F = Ctx()
    sb = lambda n, s, d: es.enter_context(nc.sbuf_tensor(uniq(n), s, d))
    F.ntok = ntok
    F.wd = TT(sb("f_wd", [128, NFC, D], BF16))
    F.wg = [TT(sb(f"f_wg{i}", [128, 2, 8, 128], BF16)) for i in range(3)]
    F.wu = [TT(sb(f"f_wu{i}", [128, 2, 8, 128], BF16)) for i in range(3)]
    F.act = TT(sb("f_act", [128, NFC, ntok], BF16))
    F.sg = [TT(sb(f"f_sg{i}", [128, 512], BF16)) for i in range(2)]
    F.sem_wd = P.dma_sem("f_wd", sw=True)
    F.sem_wg = [P.dma_sem(f"f_wg{i}", sw=True) for i in range(3)]
    F.sem_wu = [P.dma_sem(f"f_wu{i}", sw=True) for i in range(3)]
    F.cnt = 0
    F.blk = 0
    F.pref = None
    return F


def ffn_pass(C, F, xT, wg_d, wu_d, wd_d, consume, GB=(0, 1), UB=(2, 3), DB=4, blk_hook=None, nxt=None, mid_hook=None):
    P = C.P
    ntok = F.ntok
    subs = []
    t0 = 0
    while t0 < ntok:
        w = min(512, ntok - t0)
        subs.append((t0, w))
        t0 += w
    wdv = wd_d.rearrange("(c p) n -> p c n", p=128)
    for fb in range(NFC // 2):
        if fb == 2:
            P.dma("pool", F.wd.t[:, 0:11, :], wdv[:, 0:11, :], F.sem_wd, W=[F.wd])
            P.dma("pool", F.wd.t[:, 11:22, :], wdv[:, 11:22, :], F.sem_wd, W=[F.wd])
        if fb == 0 and getattr(F, "pref", None) is not None:
            bi = F.pref
            F.pref = None
            wg, wu = F.wg[bi], F.wu[bi]
        else:
            bi = F.blk % 3
            F.blk += 1
            wg, wu = F.wg[bi], F.wu[bi]
            P.dma("pool", wg.t[:, :, :, :], wg_d[2 * fb:2 * fb + 2].rearrange("c p k f -> p c k f"), F.sem_wg[bi], W=[wg])
            P.dma("pool", wu.t[:, :, :, :], wu_d[2 * fb:2 * fb + 2].rearrange("c p k f -> p c k f"), F.sem_wu[bi], W=[wu])
        for ci in range(2):
            fc = 2 * fb + ci
            for (t0, w) in subs:
                i = F.cnt
                F.cnt += 1
                gb, ub = GB[i % 2], UB[i % 2]
                for k in range(8):
                    P.op("pe", I("matmul", C.bank(gb)[:, 0:w], wg.t[:, ci, k, :], xT[k].t[:, t0:t0 + w], start=(k == 0), stop=(k == 7)),
                         R=[wg, xT[k]], W=[C.PS[gb]])
                for k in range(8):
                    P.op("pe", I("matmul", C.bank(ub)[:, 0:w], wu.t[:, ci, k, :], xT[k].t[:, t0:t0 + w], start=(k == 0), stop=(k == 7)),
                         R=[wu, xT[k]], W=[C.PS[ub]])
                sg = F.sg[i % 2]
                P.op("act", I("activation", out=sg.t[:, 0:w], in_=C.bank(gb)[:, 0:w], func=AF.Silu), R=[C.PS[gb]], W=[sg])
                P.op("dve", I("tensor_tensor", F.act.t[:, fc, t0:t0 + w], sg.t[:, 0:w], C.bank(ub)[:, 0:w], op=ALU.mult),
                     R=[sg, C.PS[ub]], W=[F.act])
        if blk_hook is not None:
            blk_hook(fb)
    if mid_hook is not None:
        mid_hook()
    if nxt is not None:
        bi = F.blk % 3
        F.blk += 1
        P.dma("pool", F.wg[bi].t[:, :, :, :], nxt[0][0:2].rearrange("c p k f -> p c k f"), F.sem_wg[bi], W=[F.wg[bi]])
        P.dma("pool", F.wu[bi].t[:, :, :, :], nxt[1][0:2].rearrange("c p k f -> p c k f"), F.sem_wu[bi], W=[F.wu[bi]])
        F.pref = bi
    for tt in range(ntok // 128):
        for nh in range(2):
            for fc in range(NFC):
                P.op("pe", I("matmul", C.bank(DB + nh), F.act.t[:, fc, tt * 128:(tt + 1) * 128], F.wd.t[:, fc, nh * 512:(nh + 1) * 512],
                             start=(fc == 0), stop=(fc == NFC - 1)), R=[F.act, F.wd], W=[C.PS[DB], C.PS[DB + 1]])
        consume(tt, DB)


def phase_ffn_dense(C, l, final):
    nc, P = C.nc, C.P
    TG = 1024
    with ExitStack() as es:
        sb = lambda n, s, d: es.enter_context(nc.sbuf_tensor(uniq(n), s, d))
        gam = TT(sb("e_gam", [128, D], F32))
        bet = TT(sb("e_bet", [128, D], F32))
        hin = [TT(sb(f"e_hin{i}", [128, D], F32)) for i in range(2)]
        pre = [TT(sb(f"e_pre{i}", [128, D], F32)) for i in range(2)]
        xT = [[TT(sb(f"e_xT{j}_{k}", [128, TG], BF16)) for k in range(8)] for j in range(1)]
        sem_c = P.dma_sem("e_c")
        sem_x = [P.dma_sem("e_x0"), P.dma_sem("e_x1")]
        sem_h = [P.dma_sem("e_h0", sw=True), P.dma_sem("e_h1", sw=True)]
        P.dma("sync", gam.t[:, :], bcast_row(C.d["ln2_g"][l], D), sem_c, W=[gam])
        P.dma("sync", bet.t[:, :], bcast_row(C.d["ln2_b"][l], D), sem_c, W=[bet])
        P.batch(sem_c, [gam, bet])
        L = alloc_ln(C, es, want_T=not final)
        F = alloc_ffn(C, es, TG)
        i = l // 2
        cnt = [0]
        NG = T // TG
        xb = xT[0]

        def load_x(grp):
            for k in range(8):
                P.dma("sync", xb[k].t[:, :], C.hTb[k * 128:(k + 1) * 128, grp * TG:(grp + 1) * TG], sem_x[grp % 2], R=[C.DhTb], W=[xb[k]])
            P.batch(sem_x[grp % 2], xb)

        load_x(0)
        for grp in range(NG):
            def consume(tt, db, grp=grp):
                gt = grp * (TG // 128) + tt
                b = cnt[0] % 2
                cnt[0] += 1
                P.dma("pool", hin[b].t[:, :], C.hb[gt * 128:(gt + 1) * 128, :], sem_h[b], R=[C.Dhb], W=[hin[b]])
                P.op("dve", I("scalar_tensor_tensor", pre[b].t[:, :], hin[b].t[:, :], ALPHA, C.ps2(db), op0=ALU.mult, op1=ALU.add),
                     R=[hin[b], C.PS[db], C.PS[db + 1]], W=[pre[b]])
                if final:
                    ln_tile(C, L, gt, pre[b], gam, bet, (C.Dout, C.out[gt * 128:(gt + 1) * 128, :]), None)
                else:
                    ln_tile(C, L, gt, pre[b], gam, bet, (C.Dha, C.ha[gt * 128:(gt + 1) * 128, :]), (C.DhTa, C.hTa), tr_banks=6)

            more = grp + 1 < NG
            ffn_pass(C, F, xb, C.d["ffn_wg"][i], C.d["ffn_wu"][i], C.d["ffn_wd"][i], consume,
                     nxt=(C.d["ffn_wg"][i], C.d["ffn_wu"][i]) if more else None,
                     mid_hook=(lambda g=grp: load_x(g + 1)) if more else None)
        ln_flush(C, L)
        P.barrier()


def phase_moe(C, l, final, stop=3):
    nc, P = C.nc, C.P
    mi = l // 2
    with ExitStack() as es:
        sb = lambda n, s, d: es.enter_context(nc.sbuf_tensor(uniq(n), s, d))
        selb = TT(sb("m_selb", [128, NT, NE], BF16))
        csel = TT(sb("m_csel", [128, NT, NE], BF16))
        cum = TT(sb("m_cum", [128, NT, NE], F32))
        pos = TT(sb("m_pos", [128, NT, NE], F32))
        pre_ = TT(sb("m_pre", [128, NT, NE], F32))
        tmp = TT(sb("m_tmp", [128, NT, NE], F32))
        tmp2 = TT(sb("m_tmp2", [128, NT, NE], F32))
        iot = TT(sb("m_iot", [128, CAP], F32))
        ecap = TT(sb("m_ecap", [128, NT, NE], F32))
        A = [TT(sb(f"m_A{i}", [128, NT, CAP], BF16)) for i in range(2)]
        tokf = TT(sb("m_tokf", [128, NE, NST], F32))
        sem_c = P.dma_sem("m_c")
        P.dma("sync", iot.t[:, :], C.d["c_iota"], sem_c, W=[iot])
        P.dma("sync", ecap.t[:, :, :], C.d["c_ecap"], sem_c, W=[ecap])
        P.batch(sem_c, [iot, ecap])
        P.op("dve", I("tensor_copy", selb.t[:, :, :], C.sel.t[:, :, :]), R=[C.sel], W=[selb])
        P.op("dve", I("memset", csel.t[:, 0, :], 0.0), W=[csel])
        for tt in range(1, NT):
            P.op("dve", I("tensor_tensor", csel.t[:, tt, :], csel.t[:, tt - 1, :], selb.t[:, tt - 1, :], op=ALU.add), R=[csel, selb], W=[csel])
        bk = 0
        for tt in range(NT):
            o = C.bank(bk)[:, tt * NE:(tt + 1) * NE]
            P.op("pe", I("matmul", o, C.lowI.t[:, :], selb.t[:, tt, :], start=True, stop=False), R=[C.lowI, selb], W=[C.PS[bk]])
            P.op("pe", I("matmul", o, C.ones.t[:, :], csel.t[:, tt, :], start=False, stop=True), R=[C.ones, csel], W=[C.PS[bk]])
        P.op("dve", I("tensor_copy", cum.t[:, :, :], C.bank(bk)[:, 0:NT * NE].rearrange("p (a b) -> p a b", b=NE)), R=[C.PS[bk]], W=[cum])
        P.op("dve", I("tensor_tensor", pos.t[:, :, :], cum.t[:, :, :], C.sel.t[:, :, :], op=ALU.subtract), R=[cum, C.sel], W=[pos])
        P.op("dve", I("memset", pre_.t[:, :, :], 0.0), W=[pre_])
        P.op("dve", I("tensor_copy", pre_.t[:, :, 1:NE], C.sel.t[:, :, 0:NE - 1]), R=[C.sel], W=[pre_])
        for sh in (1, 2, 4):
            P.op("dve", I("tensor_copy", tmp.t[:, :, :], pre_.t[:, :, :]), R=[pre_], W=[tmp])
            P.op("dve", I("tensor_tensor", pre_.t[:, :, sh:NE], tmp.t[:, :, sh:NE], tmp.t[:, :, 0:NE - sh], op=ALU.add), R=[tmp], W=[pre_])
        P.op("dve", I("tensor_tensor", pos.t[:, :, :], pos.t[:, :, :], ecap.t[:, :, :], op=ALU.add), R=[pos, ecap], W=[pos])
        for kk in range(2):
            P.op("dve", I("tensor_scalar", tmp.t[:, :, :], pre_.t[:, :, :], float(kk), None, op0=ALU.is_equal), R=[pre_], W=[tmp])
            P.op("dve", I("tensor_tensor", tmp.t[:, :, :], tmp.t[:, :, :], C.sel.t[:, :, :], op=ALU.mult), R=[tmp, C.sel], W=[tmp])
            P.op("dve", I("tensor_tensor", tmp2.t[:, :, :], tmp.t[:, :, :], pos.t[:, :, :], op=ALU.mult), R=[tmp, pos], W=[tmp2])
            P.op("dve", I("tensor_reduce", C.slotf.t[:, kk, :], tmp2.t[:, :, :], AX.X, ALU.add), R=[tmp2], W=[C.slotf])
            P.op("dve", I("tensor_tensor", tmp2.t[:, :, :], tmp.t[:, :, :], C.gate.t[:, :, :], op=ALU.mult), R=[tmp, C.gate], W=[tmp2])
            P.op("dve", I("tensor_reduce", C.gsel.t[:, kk, :], tmp2.t[:, :, :], AX.X, ALU.add), R=[tmp2], W=[C.gsel])
        P.op("dve", I("tensor_copy", C.sloti.t[:, :, :], C.slotf.t[:, :, :]), R=[C.slotf], W=[C.sloti])
        ai = 0
        for e in range(NE):
            bk = 1 + (e % 2)
            Ab = A[e % 2]
            for tt in range(NT):
                eng = "dve"
                ai += 1
                P.op(eng, I("tensor_scalar", Ab.t[:, tt, :], iot.t[:, :], cum.t[:, tt, e:e + 1], None, op0=ALU.is_ge), R=[iot, cum], W=[Ab])
            for st in range(NST):
                for tt in range(NT):
                    P.op("pe", I("matmul", C.bank(bk)[:, st:st + 1], Ab.t[:, tt, st * 128:(st + 1) * 128], C.ones.t[:, 0:1],
                                 start=(tt == 0), stop=(tt == NT - 1)), R=[Ab, C.ones], W=[C.PS[bk]])
            P.op("dve", I("tensor_scalar", tokf.t[:, e, :], C.bank(bk)[:, 0:NST], float(T - 1), None, op0=ALU.min), R=[C.PS[bk]], W=[tokf])
        P.op("dve", I("tensor_copy", C.toki.t[:, :, :], tokf.t[:, :, :]), R=[tokf], W=[C.toki])
        P.barrier()
    if stop < 2:
        return
    with ExitStack() as es:
        sb = lambda n, s, d: es.enter_context(nc.sbuf_tensor(uniq(n), s, d))
        xg = [TT(sb(f"m_xg{i}", [128, D], F32)) for i in range(2)]
        xT = [[TT(sb(f"m_xT{j}_{k}", [128, CAP], BF16)) for k in range(8)] for j in range(2)]
        yst = [TT(sb(f"m_yst{i}", [128, D], F32)) for i in range(2)]
        sem_g = [P.dma_sem("m_g0", sw=True), P.dma_sem("m_g1", sw=True)]
        sem_y = [P.dma_sem("m_y0"), P.dma_sem("m_y1")]
        F = alloc_ffn(C, es, CAP)
        yi = [0]

        def emit_gather(e, st):
            b = st % 2
            P.gather(xg[b].t[:, :], C.hb, C.toki.t[:, e, st:st + 1], sem_g[b], R=[C.Dhb, C.toki], W=[xg[b]])

        def emit_tr(e, st):
            b = st % 2
            xb = xT[e % 2]
            bk = 6
            flat = C.ps2(bk)
            for k in range(8):
                P.op("pe", I("transpose", flat[:, k * 128:(k + 1) * 128], xg[b].t[:, k * 128:(k + 1) * 128], C.ident.t[:, :]),
                     R=[xg[b], C.ident], W=[C.PS[bk], C.PS[bk + 1]])
            for k in range(8):
                if k % 2 == 0:
                    P.op("act", I("activation", out=xb[k].t[:, st * 128:(st + 1) * 128], in_=flat[:, k * 128:(k + 1) * 128], func=AF.Copy),
                         R=[C.PS[bk], C.PS[bk + 1]], W=[xb[k]])
                else:
                    P.op("dve", I("tensor_copy", xb[k].t[:, st * 128:(st + 1) * 128], flat[:, k * 128:(k + 1) * 128]),
                         R=[C.PS[bk], C.PS[bk + 1]], W=[xb[k]])

        for st in range(NST):
            emit_gather(0, st)
            emit_tr(0, st)
        for e in range(NE):
            xb = xT[e % 2]

            def consume(tt, db, e=e):
                b = yi[0] % 2
                yi[0] += 1
                if b == 0:
                    P.op("act", I("activation", out=yst[b].t[:, :], in_=C.ps2(db), func=AF.Copy), R=[C.PS[db], C.PS[db + 1]], W=[yst[b]])
                else:
                    P.op("dve", I("tensor_copy", yst[b].t[:, :], C.ps2(db)), R=[C.PS[db], C.PS[db + 1]], W=[yst[b]])
                r0 = e * CAP + tt * 128
                P.dma("sync", C.ybuf[r0:r0 + 128, :], yst[b].t[:, :], sem_y[b], R=[yst[b]], W=[C.Dyb])

            def hook(fb, e=e):
                if e + 1 < NE:
                    if fb < NST:
                        emit_gather(e + 1, fb)
                    if 1 <= fb <= NST:
                        emit_tr(e + 1, fb - 1)

            ffn_pass(C, F, xb, C.d["moe_wg"][mi][e], C.d["moe_wu"][mi][e], C.d["moe_wd"][mi][e], consume, blk_hook=hook,
                     nxt=(C.d["moe_wg"][mi][e + 1], C.d["moe_wu"][mi][e + 1]) if e + 1 < NE else None)
        P.barrier()
    if stop < 3:
        return
    with ExitStack() as es:
        sb = lambda n, s, d: es.enter_context(nc.sbuf_tensor(uniq(n), s, d))
        gam = TT(sb("n_gam", [128, D], F32))
        bet = TT(sb("n_bet", [128, D], F32))
        hin = [TT(sb(f"n_hin{i}", [128, D], F32)) for i in range(2)]
        y0 = [TT(sb(f"n_y0{i}", [128, D], F32)) for i in range(2)]
        y1 = [TT(sb(f"n_y1{i}", [128, D], F32)) for i in range(2)]
        pre = [TT(sb(f"n_pre{i}", [128, D], F32)) for i in range(2)]
        sem_c = P.dma_sem("n_c")
        sem_h = [P.dma_sem("n_h0", sw=True), P.dma_sem("n_h1", sw=True)]
        sem_a = [P.dma_sem("n_a0", sw=True), P.dma_sem("n_a1", sw=True)]
        sem_b = [P.dma_sem("n_b0", sw=True), P.dma_sem("n_b1", sw=True)]
        P.dma("sync", gam.t[:, :], bcast_row(C.d["ln2_g"][l], D), sem_c, W=[gam])
        P.dma("sync", bet.t[:, :], bcast_row(C.d["ln2_b"][l], D), sem_c, W=[bet])
        P.batch(sem_c, [gam, bet])
        L = alloc_ln(C, es, want_T=not final)
        for tt in range(NT):
            b = tt % 2
            P.dma("pool", hin[b].t[:, :], C.hb[tt * 128:(tt + 1) * 128, :], sem_h[b], R=[C.Dhb], W=[hin[b]])
            P.gather(y0[b].t[:, :], C.ybuf, C.sloti.t[:, 0, tt:tt + 1], sem_a[b], R=[C.Dyb, C.sloti], W=[y0[b]])
            P.gather(y1[b].t[:, :], C.ybuf, C.sloti.t[:, 1, tt:tt + 1], sem_b[b], R=[C.Dyb, C.sloti], W=[y1[b]])
            P.op("act", I("activation", out=y0[b].t[:, :], in_=y0[b].t[:, :], func=AF.Identity, scale=C.gsel.t[:, 0, tt:tt + 1]),
                 R=[y0[b], C.gsel], W=[y0[b]])
            P.op("dve", I("scalar_tensor_tensor", pre[b].t[:, :], hin[b].t[:, :], ALPHA, y0[b].t[:, :], op0=ALU.mult, op1=ALU.add),
                 R=[y0[b], hin[b]], W=[pre[b]])
            P.op("dve", I("scalar_tensor_tensor", pre[b].t[:, :], y1[b].t[:, :], C.gsel.t[:, 1, tt:tt + 1], pre[b].t[:, :], op0=ALU.mult, op1=ALU.add),
                 R=[y1[b], C.gsel, pre[b]], W=[pre[b]])
            if final:
                ln_tile(C, L, tt, pre[b], gam, bet, (C.Dout, C.out[tt * 128:(tt + 1) * 128, :]), None)
            else:
                ln_tile(C, L, tt, pre[b], gam, bet, (C.Dha, C.ha[tt * 128:(tt + 1) * 128, :]), (C.DhTa, C.hTa), tr_banks=0 + 2 * b)
        ln_flush(C, L)
        P.barrier()


IN_SPECS = None


def host_constants():
    c = {}
    c["c_ident"] = np.eye(128, dtype=np.float32)
    j = np.arange(128)[:, None]
    s = np.arange(128)[None, :]
    c["c_negU"] = -(j >= s).astype(np.float32)
    c["c_negO"] = -np.ones((128, 128), np.float32)
    c["c_lowI"] = (j <= s).astype(np.float32)
    c["c_ones"] = np.ones((128, 128), np.float32)
    p = np.arange(128)[:, None, None]
    dj = np.arange(4)[None, :, None]
    col = np.arange(512)[None, None, :]
    c["c_masks"] = ((128 * (3 - dj) + p) < col).astype(np.float32)
    invc = np.zeros((128, 4, 16), np.float32)
    for g in range(4):
        w = 2 << g
        invc[:, g, :] = 1.0 / np.minimum(np.arange(16) + 1, w)
    c["c_invc"] = invc
    c["c_iota"] = np.broadcast_to(np.arange(CAP, dtype=np.float32)[None, :], (128, CAP)).copy()
    c["c_ecap"] = np.broadcast_to((np.arange(NE, dtype=np.float32) * CAP)[None, None, :], (128, NT, NE)).copy()
    return c


def host_layout(inp):
    o = {}
    f = lambda a: np.ascontiguousarray(a, dtype=np.float32)
    o["ln_in_g"] = f(inp["ln_in_g"])
    o["ln_in_b"] = f(inp["ln_in_b"])
    o["w_in"] = f(inp["w_in"].reshape(DEPTH, 8, 128, 2048).transpose(0, 2, 1, 3))
    o["pool_w"] = f(inp["pool_w"].transpose(0, 2, 1, 3))
    o["pool_scale"] = f(inp["pool_scale"].reshape(DEPTH, 4, 128).transpose(0, 2, 1))
    o["w_out"] = f(inp["w_out"].reshape(DEPTH, 8, 128, D).transpose(0, 2, 1, 3))
    for nm in ("ln1_g", "ln1_b", "ln2_g", "ln2_b"):
        o[nm] = f(inp[nm])
    o["ffn_wg"] = f(inp["ffn_wg"].reshape(-1, 8, 128, NFC, 128).transpose(0, 3, 2, 1, 4))
    o["ffn_wu"] = f(inp["ffn_wu"].reshape(-1, 8, 128, NFC, 128).transpose(0, 3, 2, 1, 4))
    o["ffn_wd"] = f(inp["ffn_wd"])
    o["moe_router"] = f(inp["moe_router"].reshape(-1, 8, 128, NE).transpose(0, 2, 1, 3))
    o["moe_wg"] = f(inp["moe_wg"].reshape(-1, NE, 8, 128, NFC, 128).transpose(0, 1, 4, 3, 2, 5))
    o["moe_wu"] = f(inp["moe_wu"].reshape(-1, NE, 8, 128, NFC, 128).transpose(0, 1, 4, 3, 2, 5))
    o["moe_wd"] = f(inp["moe_wd"])
    o.update(host_constants())
    return o


def build(shapes, phases="all", debug_out=()):
    nc = bass.Bass("TRN2", target_bir_lowering=False)
    C = Ctx()
    C.nc = nc
    C.d = {}
    for name, shp in shapes.items():
        C.d[name] = nc.dram_tensor(name, list(shp), F32, kind="ExternalInput").ap()
    kind = lambda n: "ExternalOutput" if n in debug_out else "Internal"
    C.out = nc.dram_tensor("out", [T, D], F32, kind="ExternalOutput").ap()
    C.ha = nc.dram_tensor("s_ha", [T, D], F32, kind=kind("s_ha")).ap()
    C.hb = nc.dram_tensor("s_hb", [T, D], F32, kind=kind("s_hb")).ap()
    C.hTa = nc.dram_tensor("s_hTa", [D, T], BF16, kind=kind("s_hTa")).ap()
    C.hTb = nc.dram_tensor("s_hTb", [D, T], BF16, kind=kind("s_hTb")).ap()
    C.qT = nc.dram_tensor("s_qT", [512, T], BF16, kind=kind("s_qT")).ap()
    C.kT = nc.dram_tensor("s_kT", [512, T], BF16, kind=kind("s_kT")).ap()
    C.v = nc.dram_tensor("s_v", [T, 512], BF16, kind=kind("s_v")).ap()
    C.ycT = nc.dram_tensor("s_ycT", [D, T], BF16, kind=kind("s_ycT")).ap()
    C.ybuf = nc.dram_tensor("s_ybuf", [NE * CAP, D], F32, kind=kind("s_ybuf")).ap()
    C.Dout, C.Dha, C.Dhb, C.DhTa, C.DhTb, C.Dv, C.Dyb = TT(None), TT(None), TT(None), TT(None), TT(None), TT(None), TT(None)
    C.DqT = [TT(None) for _ in range(4)]
    C.DkT = [TT(None) for _ in range(4)]
    C.DycT = [TT(None) for _ in range(8)]
    with ExitStack() as es:
        P = Prog(nc, es)
        C.P = P
        sb = lambda n, s, d: es.enter_context(nc.sbuf_tensor(uniq(n), s, d))
        ps = es.enter_context(nc.psum_tensor("ps", [128, 8, 512], F32))
        C.ps = ps
        C.PS = [TT(None, f"bank{i}", excl=True) for i in range(8)]
        C.bank = lambda b: ps[:, b, :]
        C.ps2 = lambda b: ps[:, b:b + 2, :].rearrange("p a b -> p (a b)")
        C.ps2v = lambda b, a, w: ps[:, b:b + 2, :].rearrange("p a b -> p (a b)").rearrange("p (a w) -> p a w", w=w)
        C.ident = TT(sb("k_ident", [128, 128], F32))
        C.negU = TT(sb("k_negU", [128, 128], BF16))
        C.negO = TT(sb("k_negO", [128, 128], BF16))
        C.lowI = TT(sb("k_lowI", [128, 128], BF16))
        C.ones = TT(sb("k_ones", [128, 128], BF16))
        C.masks = TT(sb("k_masks", [128, 4, 512], BF16))
        C.gate = TT(sb("k_gate", [128, NT, NE], F32))
        C.sel = TT(sb("k_sel", [128, NT, NE], F32))
        C.slotf = TT(sb("k_slotf", [128, 2, NT], F32))
        C.sloti = TT(sb("k_sloti", [128, 2, NT], U32))
        C.gsel = TT(sb("k_gsel", [128, 2, NT], F32))
        C.toki = TT(sb("k_toki", [128, NE, NST], U32))
        sem_k = P.dma_sem("k_c", sw=True)
        sem_k2 = P.dma_sem("k_c2")
        P.dma("sync", C.ident.t[:, :], C.d["c_ident"], sem_k2, W=[C.ident])
        P.dma("pool", C.negU.t[:, :], C.d["c_negU"], sem_k, W=[C.negU])
        P.dma("pool", C.negO.t[:, :], C.d["c_negO"], sem_k, W=[C.negO])
        P.dma("pool", C.lowI.t[:, :], C.d["c_lowI"], sem_k, W=[C.lowI])
        P.dma("pool", C.ones.t[:, :], C.d["c_ones"], sem_k, W=[C.ones])
        P.dma("pool", C.masks.t[:, :, :], C.d["c_masks"], sem_k, W=[C.masks])
        P.batch(sem_k, [C.negU, C.negO, C.lowI, C.ones, C.masks])

        def want(p):
            if phases == "all":
                return not (len(p) == 3 and p[2] in "ab")
            return p in phases

        if want("A"):
            phase_ln_in(C)
        for l in range(DEPTH):
            final = (l == DEPTH - 1)
            if want(f"B{l}"):
                phase_proj(C, l)
            if want(f"C{l}"):
                phase_attn(C, l)
            if want(f"D{l}"):
                phase_mix_ln1(C, l)
            if want(f"E{l}") or want(f"E{l}a") or want(f"E{l}b"):
                if l % 2 == 0:
                    phase_ffn_dense(C, l, final)
                else:
                    phase_moe(C, l, final, stop=(1 if want(f"E{l}a") else 2 if want(f"E{l}b") else 3))
        if "k_dbg" in debug_out:
            sem_d = P.dma_sem("dbg")
            for nm, tt_, shp, dt in (("k_gate", C.gate, [128, NT, NE], F32), ("k_sel", C.sel, [128, NT, NE], F32),
                                     ("k_slotf", C.slotf, [128, 2, NT], F32), ("k_sloti", C.sloti, [128, 2, NT], U32),
                                     ("k_gsel", C.gsel, [128, 2, NT], F32), ("k_toki", C.toki, [128, NE, NST], U32)):
                dd_ = nc.dram_tensor("dbg_" + nm, shp, dt, kind="ExternalOutput").ap()
                P.dma("sync", dd_, tt_.t[:, :, :], sem_d, R=[tt_])
        P.barrier()
        print("bass ops:", P.nops, {e: len(P.q[e]) for e in ENG})
        P.flush()
    return nc


_CACHE = {}


def kernel(**inputs):
    lay = host_layout(inputs)
    x = np.ascontiguousarray(inputs["x"], dtype=np.float32)
    shapes = {"x": (T, D)}
    for k, v in lay.items():
        shapes[k] = v.shape
    if "nc" not in _CACHE:
        import os
        dbg = ("s_ha", "s_hb", "s_hTa", "s_hTb", "s_qT", "s_kT", "s_v", "s_ycT", "s_ybuf") if os.environ.get("SCRATCH_OUT") == "1" else ()
        _CACHE["nc"] = build(shapes, debug_out=dbg)
    nc = _CACHE["nc"]
    in_maps = []
    for b in range(NCORES):
        m = {"x": x[b]}
        m.update(lay)
        in_maps.append(m)
    res = run_bass_kernel_spmd(nc, in_maps, core_ids=list(range(NCORES)))
    out = np.stack([np.asarray(res.results[b]["out"], dtype=np.float32) for b in range(NCORES)], axis=0)
    return out
```

```python
import numpy as np
from contextlib import ExitStack
import concourse.bass as bass
import concourse.mybir as mybir
from concourse.bass_utils import run_bass_kernel_spmd

F32 = mybir.dt.float32
BF16 = mybir.dt.bfloat16
I32 = mybir.dt.int32
U32 = mybir.dt.uint32
AF = mybir.ActivationFunctionType
ALU = mybir.AluOpType
AX = mybir.AxisListType

NCORES = 8
T = 4096
D = 1024
NT = T // 128
DEPTH = 2
DFF = 2816
NFC = DFF // 128
NE = 8
CAP = 1280
NST = CAP // 128
ALPHA = float((2 * DEPTH) ** 0.25)
EPS = 1e-5
ENG = ("sync", "act", "pe", "dve", "pool")


_UNIQ = [0]


def uniq(n):
    _UNIQ[0] += 1
    return f"{n}_{_UNIQ[0]}"


def I(meth, *a, **k):
    return lambda e: getattr(e, meth)(*a, **k)


class TT:
    __slots__ = ("t", "w", "r", "name", "excl")

    def __init__(self, t, name="", excl=False):
        self.t = t
        self.w = None
        self.r = {}
        self.name = name
        self.excl = excl


class Prog:
    def __init__(self, nc, es):
        self.nc = nc
        self.es = es
        self.q = {e: [] for e in ENG}
        self.waited = {e: {} for e in ENG}
        self.psem = {e: self._mksem("p_" + e) for e in ENG}
        self.dsems = []
        self.dpool_sw = []
        self.dpool_hw = []
        self.nops = 0

    def _mksem(self, name):
        h = self.es.enter_context(self.nc.semaphore(name))
        return {"h": h, "n": 0, "name": name}

    def dma_sem(self, name, sw=False):
        pool = self.dpool_sw if sw else self.dpool_hw
        if pool:
            s = pool.pop()
        else:
            s = self._mksem(uniq(name))
            s["sw"] = sw
        self.dsems.append(s)
        return s

    def _wait(self, eng, dep):
        s, v = dep
        if eng == "pe" and s is self.psem["pe"]:
            return
        if self.waited[eng].get(s["name"], 0) >= v:
            return
        self.waited[eng][s["name"]] = v
        self.q[eng].append(("wait", s["h"], v))

    def _deps(self, R, W):
        deps = []
        for b in R:
            if b.w is not None:
                deps.append(b.w)
        for b in W:
            if b.w is not None:
                deps.append(b.w)
            deps.extend(b.r.values())
        return deps

    def _mark(self, dep, R, W):
        for b in R:
            b.r[dep[0]["name"]] = dep
        for b in W:
            b.w = dep
            b.r = {}

    def op(self, eng, fn, R=(), W=()):
        if any(b.excl for b in R):
            W = list(W) + [b for b in R if b.excl and b not in W]
            R = [b for b in R if not b.excl]
        for d in self._deps(R, W):
            self._wait(eng, d)
        s = self.psem[eng]
        s["n"] += 1
        dep = (s, s["n"])
        self.q[eng].append(("op", fn, s["h"], 1))
        self._mark(dep, R, W)
        self.nops += 1
        return dep

    def dma(self, eng, out, in_, sem, R=(), W=()):
        assert bool(sem.get("sw")) == (eng == "pool"), (eng, sem["name"])
        for d in self._deps(R, W):
            if d[0] is sem:
                continue
            self._wait(eng, d)
        sem["n"] += 16
        dep = (sem, sem["n"])
        self.q[eng].append(("op", lambda e: e.dma_start(out=out, in_=in_), sem["h"], 16))
        self._mark(dep, R, W)
        self.nops += 1
        return dep

    def batch(self, sem, tts):
        for b in tts:
            b.w = (sem, sem["n"])

    def gather(self, out, in_, idx_ap, sem, R=(), W=()):
        eng = "pool"
        for d in self._deps(R, W):
            self._wait(eng, d)
        sem["n"] += 16
        dep = (sem, sem["n"])
        self.q[eng].append(("op", lambda e: e.indirect_dma_start(
            out=out, out_offset=None, in_=in_,
            in_offset=bass.IndirectOffsetOnAxis(ap=idx_ap, axis=0)), sem["h"], 16))
        self._mark(dep, R, W)
        self.nops += 1
        return dep

    def barrier(self):
        sems = list(self.psem.values()) + self.dsems
        for e in ENG:
            for s in sems:
                if s["n"] > 0:
                    self._wait(e, (s, s["n"]))
        for s_ in self.dsems:
            (self.dpool_sw if s_.get("sw") else self.dpool_hw).append(s_)
        self.dsems = []
        for e in ENG:
            if self.psem[e]["n"] > 8000:
                self.psem[e] = self._mksem(uniq("p_" + e))

    def flush(self):
        nc = self.nc
        q = self.q

        def run(e, lst):
            for it in lst:
                if it[0] == "wait":
                    e.wait_ge(it[1], it[2])
                else:
                    ins = it[1](e)
                    ins.then_inc(it[2], it[3])

        with nc.Block() as block:
            @block.sync
            def _(e):
                run(e, q["sync"])

            @block.scalar
            def _(e):
                run(e, q["act"])

            @block.tensor
            def _(e):
                run(e, q["pe"])

            @block.vector
            def _(e):
                run(e, q["dve"])

            @block.gpsimd
            def _(e):
                run(e, q["pool"])


class Ctx:
    pass


def bcast_row(ap1d, n):
    return bass.AP(ap1d.tensor, ap1d.offset, [[0, 128], [1, n]])


def alloc_ln(C, es, want_T=True):
    nc, P = C.nc, C.P
    L = Ctx()
    sb = lambda n, s, d: es.enter_context(nc.sbuf_tensor(uniq(n), s, d))
    L.stats = [TT(sb(f"ln_stats{i}", [128, 12], F32)) for i in range(2)]
    L.mv = [TT(sb(f"ln_mv{i}", [128, 8], F32)) for i in range(2)]
    L.xn = [TT(sb(f"ln_xn{i}", [128, D], F32)) for i in range(2)]
    L.ho = [TT(sb(f"ln_ho{i}", [128, D], F32)) for i in range(2)]
    L.want_T = want_T
    if want_T:
        L.hTs = [TT(sb(f"ln_hTs{i}", [128, 8, 512], BF16)) for i in range(2)]
    L.sem_st = [P.dma_sem("ln_st0"), P.dma_sem("ln_st1")]
    L.sem_sT = [P.dma_sem("ln_sT0"), P.dma_sem("ln_sT1")]
    L.cnt = 0
    L.pend = []
    return L


def ln_tile(C, L, tt, pre, gam, bet, h_dst, hT_dst, tr_banks=None, router=None):
    i = L.cnt
    L.cnt += 1
    L.pend.append(dict(i=i, tt=tt, pre=pre, gam=gam, bet=bet, h_dst=h_dst, hT_dst=hT_dst, tr=tr_banks, router=router))
    _ln_step(C, L)


def ln_flush(C, L):
    for _ in range(2):
        L.pend.append(None)
        _ln_step(C, L)
    L.pend = []


def _ln_step(C, L):
    P = C.P
    n = len(L.pend)
    t0 = L.pend[n - 1]
    t1 = L.pend[n - 2] if n >= 2 else None
    t2 = L.pend[n - 3] if n >= 3 else None
    if t0 is not None:
        b = t0["i"] % 2
        st, mv, pre = L.stats[b], L.mv[b], t0["pre"]
        P.op("dve", I("bn_stats", st.t[:, 0:6], pre.t[:, 0:512]), R=[pre], W=[st])
        P.op("dve", I("bn_stats", st.t[:, 6:12], pre.t[:, 512:1024]), R=[pre], W=[st])
        P.op("dve", I("bn_aggr", mv.t[:, 0:2], st.t[:, :]), R=[st], W=[mv])
        P.op("dve", I("tensor_scalar_add", mv.t[:, 2:3], mv.t[:, 1:2], EPS), R=[mv], W=[mv])
    if t0 is not None:
        b = t0["i"] % 2
        mv = L.mv[b]
        P.op("act", I("activation", out=mv.t[:, 3:4], in_=mv.t[:, 2:3], func=AF.Ln), R=[mv], W=[mv])
        P.op("act", I("activation", out=mv.t[:, 4:5], in_=mv.t[:, 3:4], func=AF.Exp, scale=-0.5), R=[mv], W=[mv])
    if t1 is not None:
        b = t1["i"] % 2
        mv, xn, ho, pre = L.mv[b], L.xn[b], L.ho[b], t1["pre"]
        P.op("dve", I("scalar_tensor_tensor", xn.t[:, :], pre.t[:, :], mv.t[:, 0:1], t1["gam"].t[:, :], op0=ALU.subtract, op1=ALU.mult),
             R=[pre, mv, t1["gam"]], W=[xn])
        P.op("dve", I("scalar_tensor_tensor", ho.t[:, :], xn.t[:, :], mv.t[:, 4:5], t1["bet"].t[:, :], op0=ALU.mult, op1=ALU.add),
             R=[xn, mv, t1["bet"]], W=[ho])
        P.dma("sync", t1["h_dst"][1], ho.t[:, :], L.sem_st[b], R=[ho], W=[t1["h_dst"][0]])
        if t1["router"] is not None:
            t1["router"](t1["tt"], ho)
    if t2 is not None and t2["hT_dst"] is not None:
        b = t2["i"] % 2
        ho = L.ho[b]
        tt = t2["tt"]
        slot = tt % 4
        hTs = L.hTs[(tt // 4) % 2]
        bk = t2["tr"]
        flat = C.ps2(bk)
        for k in range(8):
            P.op("pe", I("transpose", flat[:, k * 128:(k + 1) * 128], ho.t[:, k * 128:(k + 1) * 128], C.ident.t[:, :]),
                 R=[ho, C.ident], W=[C.PS[bk], C.PS[bk + 1]])
        P.op("act", I("activation", out=hTs.t[:, :, slot * 128:(slot + 1) * 128],
                      in_=C.ps2v(bk, 8, 128), func=AF.Copy),
             R=[C.PS[bk], C.PS[bk + 1]], W=[hTs])
        if slot == 3:
            c0 = (tt - 3) * 128
            dst = t2["hT_dst"][1].rearrange("(c p) t -> p c t", p=128)[:, :, c0:c0 + 512]
            P.dma("sync", dst, hTs.t[:, :, :], L.sem_sT[(tt // 4) % 2], R=[hTs], W=[t2["hT_dst"][0]])


def phase_ln_in(C):
    nc, P = C.nc, C.P
    with ExitStack() as es:
        sb = lambda n, s, d: es.enter_context(nc.sbuf_tensor(uniq(n), s, d))
        gam = TT(sb("a_gam", [128, D], F32))
        bet = TT(sb("a_bet", [128, D], F32))
        xin = [TT(sb(f"a_xin{i}", [128, D], F32)) for i in range(2)]
        sem_c = P.dma_sem("a_c")
        sem_x = [P.dma_sem("a_x0", sw=True), P.dma_sem("a_x1", sw=True)]
        P.dma("sync", gam.t[:, :], bcast_row(C.d["ln_in_g"], D), sem_c, W=[gam])
        P.dma("sync", bet.t[:, :], bcast_row(C.d["ln_in_b"], D), sem_c, W=[bet])
        P.batch(sem_c, [gam, bet])
        L = alloc_ln(C, es)
        for tt in range(NT):
            b = tt % 2
            P.dma("pool", xin[b].t[:, :], C.d["x"][tt * 128:(tt + 1) * 128, :], sem_x[b], W=[xin[b]])
            ln_tile(C, L, tt, xin[b], gam, bet, (C.Dha, C.ha[tt * 128:(tt + 1) * 128, :]), (C.DhTa, C.hTa), tr_banks=0 + 2 * (tt % 2))
        ln_flush(C, L)
        P.barrier()


def phase_proj(C, l):
    nc, P = C.nc, C.P
    with ExitStack() as es:
        sb = lambda n, s, d: es.enter_context(nc.sbuf_tensor(uniq(n), s, d))
        wi = TT(sb("b_wi", [128, 8, 2048], BF16))
        hT = [TT(sb(f"b_hT{k}", [128, T], BF16)) for k in range(8)]
        pw = TT(sb("b_pw", [128, 4, 128], BF16))
        psc = TT(sb("b_psc", [128, 4], F32))
        invc = TT(sb("b_invc", [128, 4, 16], F32))
        U = TT(sb("b_U", [128, 16 + T], F32))
        SA = TT(sb("b_SA", [128, 16 + T], F32))
        SB = TT(sb("b_SB", [128, 16 + T], F32))
        dd = TT(sb("b_d", [128, T], BF16))
        stg = [TT(sb(f"b_stg{i}", [128, T], BF16)) for i in range(2)]
        vst = [TT(sb(f"b_vst{i}", [128, 4, 512], BF16)) for i in range(2)]
        sem_w = P.dma_sem("b_w", sw=True)
        sem_h = P.dma_sem("b_h")
        sem_c = P.dma_sem("b_c")
        sem_cw = P.dma_sem("b_cw", sw=True)
        sem_s = [P.dma_sem("b_s0"), P.dma_sem("b_s1")]
        sem_v = [P.dma_sem("b_v0"), P.dma_sem("b_v1")]
        for q4 in range(4):
            P.dma("pool", wi.t[:, :, q4 * 512:(q4 + 1) * 512], C.d["w_in"][l][:, :, q4 * 512:(q4 + 1) * 512], sem_w, W=[wi])
        for k in range(8):
            P.dma("sync", hT[k].t[:, :], C.hTa[k * 128:(k + 1) * 128, :], sem_h, R=[C.DhTa], W=[hT[k]])
        P.dma("pool", pw.t[:, :, :], C.d["pool_w"][l], sem_cw, W=[pw])
        P.dma("sync", psc.t[:, :], C.d["pool_scale"][l], sem_c, W=[psc])
        P.dma("sync", invc.t[:, :, :], C.d["c_invc"], sem_c, W=[invc])
        P.batch(sem_c, [psc, invc])
        P.batch(sem_h, hT)
        for buf in (U, SA, SB):
            P.op("dve", I("memset", buf.t[:, 0:16], 0.0), W=[buf])

        nbank = [0]

        def next_bank():
            b = nbank[0] % 4
            nbank[0] += 1
            return b

        evac_eng = [0]

        def evac(out_ap, in_ap, R, W, scale=None):
            e = evac_eng[0] % 2
            evac_eng[0] += 1
            if e == 0:
                if scale is None:
                    P.op("act", I("activation", out=out_ap, in_=in_ap, func=AF.Copy), R=R, W=W)
                else:
                    P.op("act", I("activation", out=out_ap, in_=in_ap, func=AF.Identity, scale=scale), R=R, W=W)
            else:
                if scale is None:
                    P.op("dve", I("tensor_copy", out_ap, in_ap), R=R, W=W)
                else:
                    P.op("dve", I("tensor_scalar_mul", out_ap, in_ap, scale), R=R, W=W)

        def fm_chunk(c, out_fn):
            for tg in range(8):
                bk = next_bank()
                for k in range(8):
                    P.op("pe", I("matmul", C.bank(bk), wi.t[:, k, c * 128:(c + 1) * 128], hT[k].t[:, tg * 512:(tg + 1) * 512],
                                 start=(k == 0), stop=(k == 7)), R=[wi, hT[k]], W=[C.PS[bk]])
                out_fn(tg, bk)

        sidx = [0]
        for g in range(4):
            w = 2 << g
            fm_chunk(g, lambda tg, bk: evac(U.t[:, 16 + tg * 512:16 + (tg + 1) * 512], C.bank(bk), [C.PS[bk]], [U]))
            src = U
            bufs = [SA, SB]
            nlev = g + 1
            for lev in range(nlev):
                dst = bufs[lev % 2]
                sh = 1 << lev
                eng = "dve"
                P.op(eng, I("tensor_tensor", dst.t[:, 16:16 + T], src.t[:, 16:16 + T], src.t[:, 16 - sh:16 + T - sh], op=ALU.add),
                     R=[src], W=[dst])
                src = dst
            P.op("dve", I("scalar_tensor_tensor", dd.t[:, :], src.t[:, 16:16 + T], 1.0 / w, U.t[:, 16:16 + T],
                          op0=ALU.mult, op1=ALU.subtract), R=[src, U], W=[dd])
            P.op("dve", I("tensor_tensor", SA.t[:, 0:16] if src is SB else SB.t[:, 0:16], src.t[:, 16:32], invc.t[:, g, :], op=ALU.mult),
                 R=[src, invc], W=[SA if src is SB else SB])
            tmp = SA if src is SB else SB
            P.op("dve", I("tensor_tensor", dd.t[:, 0:16], tmp.t[:, 0:16], U.t[:, 16:32], op=ALU.subtract),
                 R=[tmp, U], W=[dd])
            P.op("dve", I("memset", tmp.t[:, 0:16], 0.0), W=[tmp])
            so = stg[sidx[0] % 2]
            ss = sem_s[sidx[0] % 2]
            sidx[0] += 1
            for tg in range(8):
                bk = next_bank()
                P.op("pe", I("matmul", C.bank(bk), pw.t[:, g, :], dd.t[:, tg * 512:(tg + 1) * 512], start=True, stop=True),
                     R=[pw, dd], W=[C.PS[bk]])
                evac(so.t[:, tg * 512:(tg + 1) * 512], C.bank(bk), [C.PS[bk], psc], [so], scale=psc.t[:, g:g + 1])
            P.dma("sync", C.ycT[g * 128:(g + 1) * 128, :], so.t[:, :], ss, R=[so], W=[C.DycT[g]])
        for c in range(4, 12):
            so = stg[sidx[0] % 2]
            ss = sem_s[sidx[0] % 2]
            sidx[0] += 1
            sc = 0.125 if c < 8 else None
            fm_chunk(c, lambda tg, bk, so=so, sc=sc: evac(so.t[:, tg * 512:(tg + 1) * 512], C.bank(bk), [C.PS[bk]], [so], scale=sc))
            if c < 8:
                P.dma("sync", C.qT[(c - 4) * 128:(c - 3) * 128, :], so.t[:, :], ss, R=[so], W=[C.DqT[c - 4]])
            else:
                P.dma("sync", C.kT[(c - 8) * 128:(c - 7) * 128, :], so.t[:, :], ss, R=[so], W=[C.DkT[c - 8]])
        vv = C.v.rearrange("(n p) c -> p n c", p=128)
        for tt in range(NT):
            bk = next_bank()
            for k in range(8):
                P.op("pe", I("matmul", C.bank(bk), hT[k].t[:, tt * 128:(tt + 1) * 128], wi.t[:, k, 1536:2048],
                             start=(k == 0), stop=(k == 7)), R=[wi, hT[k]], W=[C.PS[bk]])
            vs = vst[(tt // 4) % 2]
            evac(vs.t[:, tt % 4, :], C.bank(bk), [C.PS[bk]], [vs])
            if tt % 4 == 3:
                P.dma("sync", vv[:, tt - 3:tt + 1, :], vs.t[:, :, :], sem_v[(tt // 4) % 2], R=[vs], W=[C.Dv])
        P.barrier()


def phase_attn(C, l):
    nc, P = C.nc, C.P
    with ExitStack() as es:
        sb = lambda n, s, d: es.enter_context(nc.sbuf_tensor(uniq(n), s, d))
        qT = [TT(sb(f"c_qT{i}", [128, T], BF16)) for i in range(4)]
        kT = [TT(sb(f"c_kT{i}", [128, T], BF16)) for i in range(4)]
        V = TT(sb("c_V", [128, NT, 512], BF16))
        E = [TT(sb(f"c_E{i}", [128, 2048], F32)) for i in range(2)]
        SP = [TT(sb(f"c_SP{i}", [128, 2048], BF16)) for i in range(4)]
        CA = [TT(sb(f"c_CA{i}", [128, 2048], BF16)) for i in range(4)]
        AT = [TT(sb(f"c_AT{i}", [128, 2048], BF16)) for i in range(3)]
        ys = [TT(sb(f"c_ys{i}", [128, T], BF16)) for i in range(2)]
        sem_l = P.dma_sem("c_l")
        sem_s = [P.dma_sem("c_s0"), P.dma_sem("c_s1")]
        for i in range(4):
            P.dma("sync", qT[i].t[:, :], C.qT[i * 128:(i + 1) * 128, :], sem_l, R=[C.DqT[i]], W=[qT[i]])
            P.dma("sync", kT[i].t[:, :], C.kT[i * 128:(i + 1) * 128, :], sem_l, R=[C.DkT[i]], W=[kT[i]])
        vv = C.v.rearrange("(n p) c -> p n c", p=128)
        for i in range(4):
            P.dma("sync", V.t[:, i * 8:(i + 1) * 8, :], vv[:, i * 8:(i + 1) * 8, :], sem_l, R=[C.Dv], W=[V])
        P.batch(sem_l, qT + kT + [V])

        ZP = [0, 2]
        BP = 4
        OB = [6, 7]
        quads = []
        for hp in range(4):
            for e in range(2):
                for g in range(8):
                    nq = g + 1
                    for qi in range(nq):
                        js = [4 * g + 3 - 4 * qi - k for k in range(4)]
                        quads.append(dict(hp=hp, e=e, g=g, js=js, first=(qi == 0), last=(qi == nq - 1)))
        n = len(quads)
        masks2 = C.masks.t[:, :, :].rearrange("p a b -> p (a b)")

        def zq(s):
            q = quads[s]
            hp, e, g = q["hp"], q["e"], q["g"]
            pr = slice(64 * e, 64 * e + 64)
            for k in range(4):
                bk = ZP[k // 2] + k % 2
                j = q["js"][k]
                P.op("pe", I("matmul", C.bank(bk), kT[hp].t[pr, j * 128:(j + 1) * 128], qT[hp].t[pr, g * 512:(g + 1) * 512],
                             start=True, stop=True), R=[kT[hp], qT[hp]], W=[C.PS[bk]])

        def act1(s):
            Eb = E[s % 2]
            z4 = C.ps[:, 0:4, :].rearrange("p a b -> p (a b)")
            P.op("act", I("activation", out=Eb.t[:, :], in_=z4, func=AF.Exp),
                 R=[C.PS[0], C.PS[1], C.PS[2], C.PS[3]], W=[Eb])

        def act2(s):
            P.op("act", I("activation", out=SP[s % 4].t[:, :], in_=E[s % 2].t[:, :], func=AF.Ln, bias=1.0), R=[E[s % 2]], W=[SP[s % 4]])

        def dve3(s):
            q = quads[s]
            S, Cq, Cn = SP[s % 4], CA[s % 4], CA[(s + 1) % 4]
            sl = lambda k: slice(k * 512, (k + 1) * 512)
            if q["first"]:
                P.op("dve", I("tensor_tensor", S.t[:, :], S.t[:, :], masks2, op=ALU.mult), R=[S, C.masks], W=[S])
                P.op("dve", I("tensor_tensor", Cq.t[:, sl(2)], S.t[:, sl(0)], S.t[:, sl(1)], op=ALU.add), R=[S], W=[Cq])
            else:
                P.op("dve", I("tensor_tensor", Cq.t[:, sl(1)], Cq.t[:, sl(0)], S.t[:, sl(0)], op=ALU.add), R=[S, Cq], W=[Cq])
                P.op("dve", I("tensor_tensor", Cq.t[:, sl(2)], Cq.t[:, sl(1)], S.t[:, sl(1)], op=ALU.add), R=[S, Cq], W=[Cq])
            P.op("dve", I("tensor_tensor", Cq.t[:, sl(3)], Cq.t[:, sl(2)], S.t[:, sl(2)], op=ALU.add), R=[S, Cq], W=[Cq])
            if not q["last"]:
                P.op("dve", I("tensor_tensor", Cn.t[:, sl(0)], Cq.t[:, sl(3)], S.t[:, sl(3)], op=ALU.add), R=[S, Cq], W=[Cn])

        def bq(s, p):
            q = quads[s]
            hp, e, g = q["hp"], q["e"], q["g"]
            pr = slice(64 * e, 64 * e + 64)
            S, Cq = SP[s % 4], CA[s % 4]
            for k in (2 * p, 2 * p + 1):
                bk = BP + k % 2
                j = q["js"][k]
                if q["first"] and k == 0:
                    car = None
                elif q["first"] and k == 1:
                    car = (S, S.t[:, 0:512])
                else:
                    car = (Cq, Cq.t[:, k * 512:(k + 1) * 512])
                P.op("pe", I("matmul", C.bank(bk), kT[hp].t[pr, j * 128:(j + 1) * 128], qT[hp].t[pr, g * 512:(g + 1) * 512],
                             start=True, stop=False), R=[kT[hp], qT[hp]], W=[C.PS[bk]])
                P.op("pe", I("matmul", C.bank(bk), C.negU.t[:, :], S.t[:, k * 512:(k + 1) * 512], start=False, stop=(car is None)),
                     R=[C.negU, S], W=[C.PS[bk]])
                if car is not None:
                    P.op("pe", I("matmul", C.bank(bk), C.negO.t[:, :], car[1], start=False, stop=True),
                         R=[C.negO, car[0]], W=[C.PS[bk]])

        def act3(s, p):
            A = AT[s % 3]
            P.op("act", I("activation", out=A.t[:, p * 1024:(p + 1) * 1024], in_=C.ps2(BP), func=AF.Exp),
                 R=[C.PS[BP], C.PS[BP + 1]], W=[A])

        def mask_att(s):
            q = quads[s]
            if q["first"]:
                A = AT[s % 3]
                P.op("dve", I("tensor_tensor", A.t[:, :], A.t[:, :], masks2, op=ALU.mult), R=[A, C.masks], W=[A])

        def av(s):
            q = quads[s]
            hp, e, g = q["hp"], q["e"], q["g"]
            gi = (hp * 2 + e) * 8 + g
            ob = OB[gi % 2]
            A = AT[s % 3]
            pr = slice(64 * e, 64 * e + 64)
            for k in range(4):
                j = q["js"][k]
                P.op("pe", I("matmul", C.bank(ob)[pr, :], V.t[:, j, hp * 128 + 64 * e: hp * 128 + 64 * e + 64], A.t[:, k * 512:(k + 1) * 512],
                             start=(q["first"] and k == 0), stop=(q["last"] and k == 3)), R=[V, A], W=[C.PS[ob]])
            if q["last"]:
                yb = ys[hp % 2]
                P.op("dve", I("tensor_copy", yb.t[pr, g * 512:(g + 1) * 512], C.bank(ob)[pr, :]), R=[C.PS[ob]], W=[yb])
                if e == 1 and g == 7:
                    P.dma("sync", C.ycT[512 + hp * 128:512 + (hp + 1) * 128, :], yb.t[:, :], sem_s[hp % 2], R=[yb], W=[C.DycT[4 + hp]])

        for s in range(n + 3):
            if s < n:
                zq(s)
            if 0 <= s - 2 < n:
                bq(s - 2, 0)
                act3(s - 2, 0)
            if s < n:
                act1(s)
            if 0 <= s - 3 < n:
                av(s - 3)
            if 0 <= s - 2 < n:
                bq(s - 2, 1)
                act3(s - 2, 1)
            if s < n:
                act2(s)
                dve3(s)
            if 0 <= s - 2 < n:
                mask_att(s - 2)
        P.barrier()


def phase_mix_ln1(C, l):
    nc, P = C.nc, C.P
    moe = (l % 2 == 1)
    with ExitStack() as es:
        sb = lambda n, s, d: es.enter_context(nc.sbuf_tensor(uniq(n), s, d))
        yc = [TT(sb(f"d_yc{k}", [128, T], BF16)) for k in range(8)]
        wo = TT(sb("d_wo", [128, 8, D], BF16))
        gam = TT(sb("d_gam", [128, D], F32))
        bet = TT(sb("d_bet", [128, D], F32))
        hin = [TT(sb(f"d_hin{i}", [128, D], F32)) for i in range(2)]
        pre = [TT(sb(f"d_pre{i}", [128, D], F32)) for i in range(2)]
        sem_c = P.dma_sem("d_c")
        sem_cw = P.dma_sem("d_cw", sw=True)
        sem_y = P.dma_sem("d_y")
        sem_h = [P.dma_sem("d_h0", sw=True), P.dma_sem("d_h1", sw=True)]
        P.dma("pool", wo.t[:, :, :], C.d["w_out"][l], sem_cw, W=[wo])
        P.dma("sync", gam.t[:, :], bcast_row(C.d["ln1_g"][l], D), sem_c, W=[gam])
        P.dma("sync", bet.t[:, :], bcast_row(C.d["ln1_b"][l], D), sem_c, W=[bet])
        for k in range(8):
            P.dma("sync", yc[k].t[:, :], C.ycT[k * 128:(k + 1) * 128, :], sem_y, R=[C.DycT[k]], W=[yc[k]])
        P.batch(sem_y, yc)
        L = alloc_ln(C, es)
        router = None
        P.batch(sem_c, [gam, bet])
        if moe:
            rt = TT(sb("d_rt", [128, 8, NE], F32))
            P.dma("sync", rt.t[:, :, :], C.d["moe_router"][l // 2], sem_c, W=[rt])
            P.batch(sem_c, [gam, bet, rt])
            hT32 = [TT(sb(f"d_hT32{i}", [128, 8, 128], F32)) for i in range(2)]
            lgt = TT(sb("d_lgt", [128, NT, NE], F32))
            wk = TT(sb("d_wk", [128, NT, NE], F32))
            m1 = TT(sb("d_m1", [128, NT], F32))
            m2 = TT(sb("d_m2", [128, NT], F32))

            def router(tt, ho):
                b = tt % 2
                bk = 4 + 2 * b
                flat = C.ps2(bk)
                for k in range(8):
                    P.op("pe", I("transpose", flat[:, k * 128:(k + 1) * 128], ho.t[:, k * 128:(k + 1) * 128], C.ident.t[:, :]),
                         R=[ho, C.ident], W=[C.PS[bk], C.PS[bk + 1]])
                P.op("dve", I("tensor_copy", hT32[b].t[:, :, :], C.ps2v(bk, 8, 128)), R=[C.PS[bk], C.PS[bk + 1]], W=[hT32[b]])
                lb = C.bank(bk)[:, 0:NE]
                for k in range(8):
                    P.op("pe", I("matmul", lb, hT32[b].t[:, k, :], rt.t[:, k, :], start=(k == 0), stop=(k == 7)),
                         R=[hT32[b], rt], W=[C.PS[bk], C.PS[bk + 1]])
                P.op("dve", I("tensor_copy", lgt.t[:, tt, :], lb), R=[C.PS[bk], C.PS[bk + 1]], W=[lgt])

            def router_finish():
                fl = lambda t_: t_.t[:, :, :].rearrange("p a b -> p (a b)")
                P.op("dve", I("tensor_reduce", m1.t[:, :], lgt.t[:, :, :], AX.X, ALU.max), R=[lgt], W=[m1])
                for e_ in range(NE):
                    P.op("dve", I("tensor_tensor", wk.t[:, :, e_], lgt.t[:, :, e_], m1.t[:, :], op=ALU.is_equal), R=[lgt, m1], W=[wk])
                P.op("dve", I("scalar_tensor_tensor", fl(wk), fl(wk), -1e30, fl(lgt), op0=ALU.mult, op1=ALU.add), R=[wk, lgt], W=[wk])
                P.op("dve", I("tensor_reduce", m2.t[:, :], wk.t[:, :, :], AX.X, ALU.max), R=[wk], W=[m2])
                for e_ in range(NE):
                    P.op("dve", I("tensor_tensor", wk.t[:, :, e_], lgt.t[:, :, e_], m1.t[:, :], op=ALU.subtract), R=[lgt, m1], W=[wk])
                P.op("act", I("activation", out=fl(wk), in_=fl(wk), func=AF.Exp), R=[wk], W=[wk])
                for e_ in range(NE):
                    P.op("dve", I("tensor_tensor", C.sel.t[:, :, e_], lgt.t[:, :, e_], m2.t[:, :], op=ALU.is_ge), R=[lgt, m2], W=[C.sel])
                P.op("dve", I("tensor_tensor", fl(wk), fl(wk), fl(C.sel), op=ALU.mult), R=[wk, C.sel], W=[wk])
                P.op("dve", I("tensor_reduce", m1.t[:, :], wk.t[:, :, :], AX.X, ALU.add), R=[wk], W=[m1])
                P.op("dve", I("reciprocal", m1.t[:, :], m1.t[:, :]), R=[m1], W=[m1])
                for e_ in range(NE):
                    P.op("dve", I("tensor_tensor", C.gate.t[:, :, e_], wk.t[:, :, e_], m1.t[:, :], op=ALU.mult), R=[wk, m1], W=[C.gate])

        for tt in range(NT):
            b = tt % 2
            P.dma("pool", hin[b].t[:, :], C.ha[tt * 128:(tt + 1) * 128, :], sem_h[b], R=[C.Dha], W=[hin[b]])
            bk = 0 + 2 * b
            for nh in range(2):
                for k in range(8):
                    P.op("pe", I("matmul", C.bank(bk + nh), yc[k].t[:, tt * 128:(tt + 1) * 128], wo.t[:, k, nh * 512:(nh + 1) * 512],
                                 start=(k == 0), stop=(k == 7)), R=[yc[k], wo], W=[C.PS[bk], C.PS[bk + 1]])
            P.op("dve", I("scalar_tensor_tensor", pre[b].t[:, :], hin[b].t[:, :], ALPHA, C.ps2(bk), op0=ALU.mult, op1=ALU.add),
                 R=[hin[b], C.PS[bk], C.PS[bk + 1]], W=[pre[b]])
            ln_tile(C, L, tt, pre[b], gam, bet, (C.Dhb, C.hb[tt * 128:(tt + 1) * 128, :]),
                    None if moe else (C.DhTb, C.hTb), tr_banks=4 + 2 * b, router=router)
        ln_flush(C, L)
        if moe:
            router_finish()
        P.barrier()


def alloc_ffn(C, es, ntok):
    nc, P = C.nc, C.P
    F = Ctx()
    sb = lambda n, s, d: es.enter_context(nc.sbuf_tensor(uniq(n), s, d))
    F.ntok = ntok
    F.wd = TT(sb("f_wd", [128, NFC, D], BF16))
    F.wg = [TT(sb(f"f_wg{i}", [128, 2, 8, 128], BF16)) for i in range(3)]
    F.wu = [TT(sb(f"f_wu{i}", [128, 2, 8, 128], BF16)) for i in range(3)]
    F.act = TT(sb("f_act", [128, NFC, ntok], BF16))
    F.sg = [TT(sb(f"f_sg{i}", [128, 512], BF16)) for i in range(2)]
    F.sem_wd = P.dma_sem("f_wd", sw=True)
    F.sem_wg = [P.dma_sem(f"f_wg{i}", sw=True) for i in range(3)]
    F.sem_wu = [P.dma_sem(f"f_wu{i}", sw=True) for i in range(3)]
    F.cnt = 0
    F.blk = 0
    return F


def ffn_pass(C, F, xT, wg_d, wu_d, wd_d, consume, GB=(0, 1), UB=(2, 3), DB=4, blk_hook=None):
    P = C.P
    ntok = F.ntok
    subs = []
    t0 = 0
    while t0 < ntok:
        w = min(512, ntok - t0)
        subs.append((t0, w))
        t0 += w
    wdv = wd_d.rearrange("(c p) n -> p c n", p=128)
    for fb in range(NFC // 2):
        if fb == 2:
            P.dma("pool", F.wd.t[:, 0:11, :], wdv[:, 0:11, :], F.sem_wd, W=[F.wd])
            P.dma("pool", F.wd.t[:, 11:22, :], wdv[:, 11:22, :], F.sem_wd, W=[F.wd])
        bi = F.blk % 3
        F.blk += 1
        wg, wu = F.wg[bi], F.wu[bi]
        P.dma("pool", wg.t[:, :, :, :], wg_d[2 * fb:2 * fb + 2].rearrange("c p k f -> p c k f"), F.sem_wg[bi], W=[wg])
        P.dma("pool", wu.t[:, :, :, :], wu_d[2 * fb:2 * fb + 2].rearrange("c p k f -> p c k f"), F.sem_wu[bi], W=[wu])
        for ci in range(2):
            fc = 2 * fb + ci
            for (t0, w) in subs:
                i = F.cnt
                F.cnt += 1
                gb, ub = GB[i % 2], UB[i % 2]
                for k in range(8):
                    P.op("pe", I("matmul", C.bank(gb)[:, 0:w], wg.t[:, ci, k, :], xT[k].t[:, t0:t0 + w], start=(k == 0), stop=(k == 7)),
                         R=[wg, xT[k]], W=[C.PS[gb]])
                for k in range(8):
                    P.op("pe", I("matmul", C.bank(ub)[:, 0:w], wu.t[:, ci, k, :], xT[k].t[:, t0:t0 + w], start=(k == 0), stop=(k == 7)),
                         R=[wu, xT[k]], W=[C.PS[ub]])
                sg = F.sg[i % 2]
                P.op("act", I("activation", out=sg.t[:, 0:w], in_=C.bank(gb)[:, 0:w], func=AF.Silu), R=[C.PS[gb]], W=[sg])
                P.op("dve", I("tensor_tensor", F.act.t[:, fc, t0:t0 + w], sg.t[:, 0:w], C.bank(ub)[:, 0:w], op=ALU.mult),
                     R=[sg, C.PS[ub]], W=[F.act])
        if blk_hook is not None:
            blk_hook(fb)
    dpairs = (DB, GB[0], UB[0])
    for tt in range(ntok // 128):
        db = dpairs[tt % 3]
        for nh in range(2):
            for fc in range(NFC):
                P.op("pe", I("matmul", C.bank(db + nh), F.act.t[:, fc, tt * 128:(tt + 1) * 128], F.wd.t[:, fc, nh * 512:(nh + 1) * 512],
                             start=(fc == 0), stop=(fc == NFC - 1)), R=[F.act, F.wd], W=[C.PS[db], C.PS[db + 1]])
        consume(tt, db)


def phase_ffn_dense(C, l, final):
    nc, P = C.nc, C.P
    TG = 1024
    with ExitStack() as es:
        sb = lambda n, s, d: es.enter_context(nc.sbuf_tensor(uniq(n), s, d))
        gam = TT(sb("e_gam", [128, D], F32))
        bet = TT(sb("e_bet", [128, D], F32))
        hin = [TT(sb(f"e_hin{i}", [128, D], F32)) for i in range(2)]
        pre = [TT(sb(f"e_pre{i}", [128, D], F32)) for i in range(2)]
        xT = [[TT(sb(f"e_xT{j}_{k}", [128, TG], BF16)) for k in range(8)] for j in range(1)]
        sem_c = P.dma_sem("e_c")
        sem_x = [P.dma_sem("e_x0"), P.dma_sem("e_x1")]
        sem_h = [P.dma_sem("e_h0", sw=True), P.dma_sem("e_h1", sw=True)]
        P.dma("sync", gam.t[:, :], bcast_row(C.d["ln2_g"][l], D), sem_c, W=[gam])
        P.dma("sync", bet.t[:, :], bcast_row(C.d["ln2_b"][l], D), sem_c, W=[bet])
        P.batch(sem_c, [gam, bet])
        L = alloc_ln(C, es, want_T=not final)
        F = alloc_ffn(C, es, TG)
        i = l // 2
        cnt = [0]
        for grp in range(T // TG):
            xb = xT[0]
            for k in range(8):
                P.dma("sync", xb[k].t[:, :], C.hTb[k * 128:(k + 1) * 128, grp * TG:(grp + 1) * TG], sem_x[grp % 2], R=[C.DhTb], W=[xb[k]])
            P.batch(sem_x[grp % 2], xb)

            def consume(tt, db, grp=grp):
                gt = grp * (TG // 128) + tt
                b = cnt[0] % 2
                cnt[0] += 1
                P.dma("pool", hin[b].t[:, :], C.hb[gt * 128:(gt + 1) * 128, :], sem_h[b], R=[C.Dhb], W=[hin[b]])
                P.op("dve", I("scalar_tensor_tensor", pre[b].t[:, :], hin[b].t[:, :], ALPHA, C.ps2(db), op0=ALU.mult, op1=ALU.add),
                     R=[hin[b], C.PS[db], C.PS[db + 1]], W=[pre[b]])
                if final:
                    ln_tile(C, L, gt, pre[b], gam, bet, (C.Dout, C.out[gt * 128:(gt + 1) * 128, :]), None)
                else:
                    ln_tile(C, L, gt, pre[b], gam, bet, (C.Dha, C.ha[gt * 128:(gt + 1) * 128, :]), (C.DhTa, C.hTa), tr_banks=6)

            ffn_pass(C, F, xb, C.d["ffn_wg"][i], C.d["ffn_wu"][i], C.d["ffn_wd"][i], consume)
        ln_flush(C, L)
        P.barrier()


def phase_moe(C, l, final, stop=3):
    nc, P = C.nc, C.P
    mi = l // 2
    with ExitStack() as es:
        sb = lambda n, s, d: es.enter_context(nc.sbuf_tensor(uniq(n), s, d))
        selb = TT(sb("m_selb", [128, NT, NE], BF16))
        csel = TT(sb("m_csel", [128, NT, NE], BF16))
        cum = TT(sb("m_cum", [128, NT, NE], F32))
        pos = TT(sb("m_pos", [128, NT, NE], F32))
        pre_ = TT(sb("m_pre", [128, NT, NE], F32))
        tmp = TT(sb("m_tmp", [128, NT, NE], F32))
        tmp2 = TT(sb("m_tmp2", [128, NT, NE], F32))
        iot = TT(sb("m_iot", [128, CAP], F32))
        ecap = TT(sb("m_ecap", [128, NT, NE], F32))
        A = [TT(sb(f"m_A{i}", [128, NT, CAP], BF16)) for i in range(2)]
        tokf = TT(sb("m_tokf", [128, NE, NST], F32))
        sem_c = P.dma_sem("m_c")
        P.dma("sync", iot.t[:, :], C.d["c_iota"], sem_c, W=[iot])
        P.dma("sync", ecap.t[:, :, :], C.d["c_ecap"], sem_c, W=[ecap])
        P.batch(sem_c, [iot, ecap])
        P.op("dve", I("tensor_copy", selb.t[:, :, :], C.sel.t[:, :, :]), R=[C.sel], W=[selb])
        P.op("dve", I("memset", csel.t[:, 0, :], 0.0), W=[csel])
        for tt in range(1, NT):
            P.op("dve", I("tensor_tensor", csel.t[:, tt, :], csel.t[:, tt - 1, :], selb.t[:, tt - 1, :], op=ALU.add), R=[csel, selb], W=[csel])
        bk = 0
        for tt in range(NT):
            o = C.bank(bk)[:, tt * NE:(tt + 1) * NE]
            P.op("pe", I("matmul", o, C.lowI.t[:, :], selb.t[:, tt, :], start=True, stop=False), R=[C.lowI, selb], W=[C.PS[bk]])
            P.op("pe", I("matmul", o, C.ones.t[:, :], csel.t[:, tt, :], start=False, stop=True), R=[C.ones, csel], W=[C.PS[bk]])
        P.op("dve", I("tensor_copy", cum.t[:, :, :], C.bank(bk)[:, 0:NT * NE].rearrange("p (a b) -> p a b", b=NE)), R=[C.PS[bk]], W=[cum])
        P.op("dve", I("tensor_tensor", pos.t[:, :, :], cum.t[:, :, :], C.sel.t[:, :, :], op=ALU.subtract), R=[cum, C.sel], W=[pos])
        P.op("dve", I("memset", pre_.t[:, :, :], 0.0), W=[pre_])
        P.op("dve", I("tensor_copy", pre_.t[:, :, 1:NE], C.sel.t[:, :, 0:NE - 1]), R=[C.sel], W=[pre_])
        for sh in (1, 2, 4):
            P.op("dve", I("tensor_copy", tmp.t[:, :, :], pre_.t[:, :, :]), R=[pre_], W=[tmp])
            P.op("dve", I("tensor_tensor", pre_.t[:, :, sh:NE], tmp.t[:, :, sh:NE], tmp.t[:, :, 0:NE - sh], op=ALU.add), R=[tmp], W=[pre_])
        P.op("dve", I("tensor_tensor", pos.t[:, :, :], pos.t[:, :, :], ecap.t[:, :, :], op=ALU.add), R=[pos, ecap], W=[pos])
        for kk in range(2):
            P.op("dve", I("tensor_scalar", tmp.t[:, :, :], pre_.t[:, :, :], float(kk), None, op0=ALU.is_equal), R=[pre_], W=[tmp])
            P.op("dve", I("tensor_tensor", tmp.t[:, :, :], tmp.t[:, :, :], C.sel.t[:, :, :], op=ALU.mult), R=[tmp, C.sel], W=[tmp])
            P.op("dve", I("tensor_tensor", tmp2.t[:, :, :], tmp.t[:, :, :], pos.t[:, :, :], op=ALU.mult), R=[tmp, pos], W=[tmp2])
            P.op("dve", I("tensor_reduce", C.slotf.t[:, kk, :], tmp2.t[:, :, :], AX.X, ALU.add), R=[tmp2], W=[C.slotf])
            P.op("dve", I("tensor_tensor", tmp2.t[:, :, :], tmp.t[:, :, :], C.gate.t[:, :, :], op=ALU.mult), R=[tmp, C.gate], W=[tmp2])
            P.op("dve", I("tensor_reduce", C.gsel.t[:, kk, :], tmp2.t[:, :, :], AX.X, ALU.add), R=[tmp2], W=[C.gsel])
        P.op("dve", I("tensor_copy", C.sloti.t[:, :, :], C.slotf.t[:, :, :]), R=[C.slotf], W=[C.sloti])
        ai = 0
        for e in range(NE):
            bk = 1 + (e % 2)
            Ab = A[e % 2]
            for tt in range(NT):
                eng = "dve"
                ai += 1
                P.op(eng, I("tensor_scalar", Ab.t[:, tt, :], iot.t[:, :], cum.t[:, tt, e:e + 1], None, op0=ALU.is_ge), R=[iot, cum], W=[Ab])
            for st in range(NST):
                for tt in range(NT):
                    P.op("pe", I("matmul", C.bank(bk)[:, st:st + 1], Ab.t[:, tt, st * 128:(st + 1) * 128], C.ones.t[:, 0:1],
                                 start=(tt == 0), stop=(tt == NT - 1)), R=[Ab, C.ones], W=[C.PS[bk]])
            P.op("dve", I("tensor_scalar", tokf.t[:, e, :], C.bank(bk)[:, 0:NST], float(T - 1), None, op0=ALU.min), R=[C.PS[bk]], W=[tokf])
        P.op("dve", I("tensor_copy", C.toki.t[:, :, :], tokf.t[:, :, :]), R=[tokf], W=[C.toki])
        P.barrier()
    if stop < 2:
        return
    with ExitStack() as es:
        sb = lambda n, s, d: es.enter_context(nc.sbuf_tensor(uniq(n), s, d))
        xg = [TT(sb(f"m_xg{i}", [128, D], F32)) for i in range(2)]
        xT = [[TT(sb(f"m_xT{j}_{k}", [128, CAP], BF16)) for k in range(8)] for j in range(2)]
        yst = [TT(sb(f"m_yst{i}", [128, D], F32)) for i in range(2)]
        sem_g = [P.dma_sem("m_g0", sw=True), P.dma_sem("m_g1", sw=True)]
        sem_y = [P.dma_sem("m_y0"), P.dma_sem("m_y1")]
        F = alloc_ffn(C, es, CAP)
        yi = [0]

        def emit_gather(e, st):
            b = st % 2
            P.gather(xg[b].t[:, :], C.hb, C.toki.t[:, e, st:st + 1], sem_g[b], R=[C.Dhb, C.toki], W=[xg[b]])

        def emit_tr(e, st):
            b = st % 2
            xb = xT[e % 2]
            bk = 6
            flat = C.ps2(bk)
            for k in range(8):
                P.op("pe", I("transpose", flat[:, k * 128:(k + 1) * 128], xg[b].t[:, k * 128:(k + 1) * 128], C.ident.t[:, :]),
                     R=[xg[b], C.ident], W=[C.PS[bk], C.PS[bk + 1]])
            for k in range(8):
                if k % 2 == 0:
                    P.op("act", I("activation", out=xb[k].t[:, st * 128:(st + 1) * 128], in_=flat[:, k * 128:(k + 1) * 128], func=AF.Copy),
                         R=[C.PS[bk], C.PS[bk + 1]], W=[xb[k]])
                else:
                    P.op("dve", I("tensor_copy", xb[k].t[:, st * 128:(st + 1) * 128], flat[:, k * 128:(k + 1) * 128]),
                         R=[C.PS[bk], C.PS[bk + 1]], W=[xb[k]])

        for st in range(NST):
            emit_gather(0, st)
            emit_tr(0, st)
        for e in range(NE):
            xb = xT[e % 2]

            def consume(tt, db, e=e):
                b = yi[0] % 2
                yi[0] += 1
                if b == 0:
                    P.op("act", I("activation", out=yst[b].t[:, :], in_=C.ps2(db), func=AF.Copy), R=[C.PS[db], C.PS[db + 1]], W=[yst[b]])
                else:
                    P.op("dve", I("tensor_copy", yst[b].t[:, :], C.ps2(db)), R=[C.PS[db], C.PS[db + 1]], W=[yst[b]])
                r0 = e * CAP + tt * 128
                P.dma("sync", C.ybuf[r0:r0 + 128, :], yst[b].t[:, :], sem_y[b], R=[yst[b]], W=[C.Dyb])

            def hook(fb, e=e):
                if e + 1 < NE:
                    if fb < NST:
                        emit_gather(e + 1, fb)
                    if 1 <= fb <= NST:
                        emit_tr(e + 1, fb - 1)

            ffn_pass(C, F, xb, C.d["moe_wg"][mi][e], C.d["moe_wu"][mi][e], C.d["moe_wd"][mi][e], consume, blk_hook=hook)
        P.barrier()
    if stop < 3:
        return
    with ExitStack() as es:
        sb = lambda n, s, d: es.enter_context(nc.sbuf_tensor(uniq(n), s, d))
        gam = TT(sb("n_gam", [128, D], F32))
        bet = TT(sb("n_bet", [128, D], F32))
        hin = [TT(sb(f"n_hin{i}", [128, D], F32)) for i in range(2)]
        y0 = [TT(sb(f"n_y0{i}", [128, D], F32)) for i in range(2)]
        y1 = [TT(sb(f"n_y1{i}", [128, D], F32)) for i in range(2)]
        pre = [TT(sb(f"n_pre{i}", [128, D], F32)) for i in range(2)]
        sem_c = P.dma_sem("n_c")
        sem_h = [P.dma_sem("n_h0", sw=True), P.dma_sem("n_h1", sw=True)]
        sem_a = [P.dma_sem("n_a0", sw=True), P.dma_sem("n_a1", sw=True)]
        sem_b = [P.dma_sem("n_b0", sw=True), P.dma_sem("n_b1", sw=True)]
        P.dma("sync", gam.t[:, :], bcast_row(C.d["ln2_g"][l], D), sem_c, W=[gam])
        P.dma("sync", bet.t[:, :], bcast_row(C.d["ln2_b"][l], D), sem_c, W=[bet])
        P.batch(sem_c, [gam, bet])
        L = alloc_ln(C, es, want_T=not final)
        for tt in range(NT):
            b = tt % 2
            P.dma("pool", hin[b].t[:, :], C.hb[tt * 128:(tt + 1) * 128, :], sem_h[b], R=[C.Dhb], W=[hin[b]])
            P.gather(y0[b].t[:, :], C.ybuf, C.sloti.t[:, 0, tt:tt + 1], sem_a[b], R=[C.Dyb, C.sloti], W=[y0[b]])
            P.gather(y1[b].t[:, :], C.ybuf, C.sloti.t[:, 1, tt:tt + 1], sem_b[b], R=[C.Dyb, C.sloti], W=[y1[b]])
            P.op("act", I("activation", out=y0[b].t[:, :], in_=y0[b].t[:, :], func=AF.Identity, scale=C.gsel.t[:, 0, tt:tt + 1]),
                 R=[y0[b], C.gsel], W=[y0[b]])
            P.op("dve", I("scalar_tensor_tensor", pre[b].t[:, :], hin[b].t[:, :], ALPHA, y0[b].t[:, :], op0=ALU.mult, op1=ALU.add),
                 R=[y0[b], hin[b]], W=[pre[b]])
            P.op("dve", I("scalar_tensor_tensor", pre[b].t[:, :], y1[b].t[:, :], C.gsel.t[:, 1, tt:tt + 1], pre[b].t[:, :], op0=ALU.mult, op1=ALU.add),
                 R=[y1[b], C.gsel, pre[b]], W=[pre[b]])
            if final:
                ln_tile(C, L, tt, pre[b], gam, bet, (C.Dout, C.out[tt * 128:(tt + 1) * 128, :]), None)
            else:
                ln_tile(C, L, tt, pre[b], gam, bet, (C.Dha, C.ha[tt * 128:(tt + 1) * 128, :]), (C.DhTa, C.hTa), tr_banks=0 + 2 * b)
        ln_flush(C, L)
        P.barrier()


IN_SPECS = None


def host_constants():
    c = {}
    c["c_ident"] = np.eye(128, dtype=np.float32)
    j = np.arange(128)[:, None]
    s = np.arange(128)[None, :]
    c["c_negU"] = -(j >= s).astype(np.float32)
    c["c_negO"] = -np.ones((128, 128), np.float32)
    c["c_lowI"] = (j <= s).astype(np.float32)
    c["c_ones"] = np.ones((128, 128), np.float32)
    p = np.arange(128)[:, None, None]
    dj = np.arange(4)[None, :, None]
    col = np.arange(512)[None, None, :]
    c["c_masks"] = ((128 * (3 - dj) + p) < col).astype(np.float32)
    invc = np.zeros((128, 4, 16), np.float32)
    for g in range(4):
        w = 2 << g
        invc[:, g, :] = 1.0 / np.minimum(np.arange(16) + 1, w)
    c["c_invc"] = invc
    c["c_iota"] = np.broadcast_to(np.arange(CAP, dtype=np.float32)[None, :], (128, CAP)).copy()
    c["c_ecap"] = np.broadcast_to((np.arange(NE, dtype=np.float32) * CAP)[None, None, :], (128, NT, NE)).copy()
    return c


def host_layout(inp):
    o = {}
    f = lambda a: np.ascontiguousarray(a, dtype=np.float32)
    o["ln_in_g"] = f(inp["ln_in_g"])
    o["ln_in_b"] = f(inp["ln_in_b"])
    o["w_in"] = f(inp["w_in"].reshape(DEPTH, 8, 128, 2048).transpose(0, 2, 1, 3))
    o["pool_w"] = f(inp["pool_w"].transpose(0, 2, 1, 3))
    o["pool_scale"] = f(inp["pool_scale"].reshape(DEPTH, 4, 128).transpose(0, 2, 1))
    o["w_out"] = f(inp["w_out"].reshape(DEPTH, 8, 128, D).transpose(0, 2, 1, 3))
    for nm in ("ln1_g", "ln1_b", "ln2_g", "ln2_b"):
        o[nm] = f(inp[nm])
    o["ffn_wg"] = f(inp["ffn_wg"].reshape(-1, 8, 128, NFC, 128).transpose(0, 3, 2, 1, 4))
    o["ffn_wu"] = f(inp["ffn_wu"].reshape(-1, 8, 128, NFC, 128).transpose(0, 3, 2, 1, 4))
    o["ffn_wd"] = f(inp["ffn_wd"])
    o["moe_router"] = f(inp["moe_router"].reshape(-1, 8, 128, NE).transpose(0, 2, 1, 3))
    o["moe_wg"] = f(inp["moe_wg"].reshape(-1, NE, 8, 128, NFC, 128).transpose(0, 1, 4, 3, 2, 5))
    o["moe_wu"] = f(inp["moe_wu"].reshape(-1, NE, 8, 128, NFC, 128).transpose(0, 1, 4, 3, 2, 5))
    o["moe_wd"] = f(inp["moe_wd"])
    o.update(host_constants())
    return o


def build(shapes, phases="all", debug_out=()):
    nc = bass.Bass("TRN2", target_bir_lowering=False)
    C = Ctx()
    C.nc = nc
    C.d = {}
    for name, shp in shapes.items():
        C.d[name] = nc.dram_tensor(name, list(shp), F32, kind="ExternalInput").ap()
    kind = lambda n: "ExternalOutput" if n in debug_out else "Internal"
    C.out = nc.dram_tensor("out", [T, D], F32, kind="ExternalOutput").ap()
    C.ha = nc.dram_tensor("s_ha", [T, D], F32, kind=kind("s_ha")).ap()
    C.hb = nc.dram_tensor("s_hb", [T, D], F32, kind=kind("s_hb")).ap()
    C.hTa = nc.dram_tensor("s_hTa", [D, T], BF16, kind=kind("s_hTa")).ap()
    C.hTb = nc.dram_tensor("s_hTb", [D, T], BF16, kind=kind("s_hTb")).ap()
    C.qT = nc.dram_tensor("s_qT", [512, T], BF16, kind=kind("s_qT")).ap()
    C.kT = nc.dram_tensor("s_kT", [512, T], BF16, kind=kind("s_kT")).ap()
    C.v = nc.dram_tensor("s_v", [T, 512], BF16, kind=kind("s_v")).ap()
    C.ycT = nc.dram_tensor("s_ycT", [D, T], BF16, kind=kind("s_ycT")).ap()
    C.ybuf = nc.dram_tensor("s_ybuf", [NE * CAP, D], F32, kind=kind("s_ybuf")).ap()
    C.Dout, C.Dha, C.Dhb, C.DhTa, C.DhTb, C.Dv, C.Dyb = TT(None), TT(None), TT(None), TT(None), TT(None), TT(None), TT(None)
    C.DqT = [TT(None) for _ in range(4)]
    C.DkT = [TT(None) for _ in range(4)]
    C.DycT = [TT(None) for _ in range(8)]
    with ExitStack() as es:
        P = Prog(nc, es)
        C.P = P
        sb = lambda n, s, d: es.enter_context(nc.sbuf_tensor(uniq(n), s, d))
        ps = es.enter_context(nc.psum_tensor("ps", [128, 8, 512], F32))
        C.ps = ps
        C.PS = [TT(None, f"bank{i}", excl=True) for i in range(8)]
        C.bank = lambda b: ps[:, b, :]
        C.ps2 = lambda b: ps[:, b:b + 2, :].rearrange("p a b -> p (a b)")
        C.ps2v = lambda b, a, w: ps[:, b:b + 2, :].rearrange("p a b -> p (a b)").rearrange("p (a w) -> p a w", w=w)
        C.ident = TT(sb("k_ident", [128, 128], F32))
        C.negU = TT(sb("k_negU", [128, 128], BF16))
        C.negO = TT(sb("k_negO", [128, 128], BF16))
        C.lowI = TT(sb("k_lowI", [128, 128], BF16))
        C.ones = TT(sb("k_ones", [128, 128], BF16))
        C.masks = TT(sb("k_masks", [128, 4, 512], BF16))
        C.gate = TT(sb("k_gate", [128, NT, NE], F32))
        C.sel = TT(sb("k_sel", [128, NT, NE], F32))
        C.slotf = TT(sb("k_slotf", [128, 2, NT], F32))
        C.sloti = TT(sb("k_sloti", [128, 2, NT], U32))
        C.gsel = TT(sb("k_gsel", [128, 2, NT], F32))
        C.toki = TT(sb("k_toki", [128, NE, NST], U32))
        sem_k = P.dma_sem("k_c", sw=True)
        sem_k2 = P.dma_sem("k_c2")
        P.dma("sync", C.ident.t[:, :], C.d["c_ident"], sem_k2, W=[C.ident])
        P.dma("pool", C.negU.t[:, :], C.d["c_negU"], sem_k, W=[C.negU])
        P.dma("pool", C.negO.t[:, :], C.d["c_negO"], sem_k, W=[C.negO])
        P.dma("pool", C.lowI.t[:, :], C.d["c_lowI"], sem_k, W=[C.lowI])
        P.dma("pool", C.ones.t[:, :], C.d["c_ones"], sem_k, W=[C.ones])
        P.dma("pool", C.masks.t[:, :, :], C.d["c_masks"], sem_k, W=[C.masks])
        P.batch(sem_k, [C.negU, C.negO, C.lowI, C.ones, C.masks])

        def want(p):
            if phases == "all":
                return not (len(p) == 3 and p[2] in "ab")
            return p in phases

        if want("A"):
            phase_ln_in(C)
        for l in range(DEPTH):
            final = (l == DEPTH - 1)
            if want(f"B{l}"):
                phase_proj(C, l)
            if want(f"C{l}"):
                phase_attn(C, l)
            if want(f"D{l}"):
                phase_mix_ln1(C, l)
            if want(f"E{l}") or want(f"E{l}a") or want(f"E{l}b"):
                if l % 2 == 0:
                    phase_ffn_dense(C, l, final)
                else:
                    phase_moe(C, l, final, stop=(1 if want(f"E{l}a") else 2 if want(f"E{l}b") else 3))
        if "k_dbg" in debug_out:
            sem_d = P.dma_sem("dbg")
            for nm, tt_, shp, dt in (("k_gate", C.gate, [128, NT, NE], F32), ("k_sel", C.sel, [128, NT, NE], F32),
                                     ("k_slotf", C.slotf, [128, 2, NT], F32), ("k_sloti", C.sloti, [128, 2, NT], U32),
                                     ("k_gsel", C.gsel, [128, 2, NT], F32), ("k_toki", C.toki, [128, NE, NST], U32)):
                dd_ = nc.dram_tensor("dbg_" + nm, shp, dt, kind="ExternalOutput").ap()
                P.dma("sync", dd_, tt_.t[:, :, :], sem_d, R=[tt_])
        P.barrier()
        print("bass ops:", P.nops, {e: len(P.q[e]) for e in ENG})
        P.flush()
    return nc


_CACHE = {}


def kernel(**inputs):
    lay = host_layout(inputs)
    x = np.ascontiguousarray(inputs["x"], dtype=np.float32)
    shapes = {"x": (T, D)}
    for k, v in lay.items():
        shapes[k] = v.shape
    if "nc" not in _CACHE:
        import os
        dbg = ("s_ha", "s_hb", "s_hTa", "s_hTb", "s_qT", "s_kT", "s_v", "s_ycT", "s_ybuf") if os.environ.get("SCRATCH_OUT") == "1" else ()
        _CACHE["nc"] = build(shapes, debug_out=dbg)
    nc = _CACHE["nc"]
    in_maps = []
    for b in range(NCORES):
        m = {"x": x[b]}
        m.update(lay)
        in_maps.append(m)
    res = run_bass_kernel_spmd(nc, in_maps, core_ids=list(range(NCORES)))
    out = np.stack([np.asarray(res.results[b]["out"], dtype=np.float32) for b in range(NCORES)], axis=0)
    return out
```

```python
import numpy as np
from contextlib import ExitStack
import concourse.bass as bass
import concourse.mybir as mybir
from concourse.bass_utils import run_bass_kernel_spmd

F32 = mybir.dt.float32
BF16 = mybir.dt.bfloat16
I32 = mybir.dt.int32
U32 = mybir.dt.uint32
AF = mybir.ActivationFunctionType
ALU = mybir.AluOpType
AX = mybir.AxisListType

NCORES = 8
T = 4096
D = 1024
NT = T // 128
DEPTH = 2
DFF = 2816
NFC = DFF // 128
NE = 8
CAP = 1280
NST = CAP // 128
ALPHA = float((2 * DEPTH) ** 0.25)
EPS = 1e-5
ENG = ("sync", "act", "pe", "dve", "pool")


_UNIQ = [0]


def uniq(n):
    _UNIQ[0] += 1
    return f"{n}_{_UNIQ[0]}"


def I(meth, *a, **k):
    return lambda e: getattr(e, meth)(*a, **k)


class TT:
    __slots__ = ("t", "w", "r", "name", "excl")

    def __init__(self, t, name="", excl=False):
        self.t = t
        self.w = None
        self.r = {}
        self.name = name
        self.excl = excl


class Prog:
    def __init__(self, nc, es):
        self.nc = nc
        self.es = es
        self.q = {e: [] for e in ENG}
        self.waited = {e: {} for e in ENG}
        self.psem = {e: self._mksem("p_" + e) for e in ENG}
        self.dsems = []
        self.dpool_sw = []
        self.dpool_hw = []
        self.nops = 0

    def _mksem(self, name):
        h = self.es.enter_context(self.nc.semaphore(name))
        return {"h": h, "n": 0, "name": name}

    def dma_sem(self, name, sw=False):
        pool = self.dpool_sw if sw else self.dpool_hw
        if pool:
            s = pool.pop()
        else:
            s = self._mksem(uniq(name))
            s["sw"] = sw
        self.dsems.append(s)
        return s

    def _wait(self, eng, dep):
        s, v = dep
        if eng == "pe" and s is self.psem["pe"]:
            return
        if self.waited[eng].get(s["name"], 0) >= v:
            return
        self.waited[eng][s["name"]] = v
        self.q[eng].append(("wait", s["h"], v))

    def _deps(self, R, W):
        deps = []
        for b in R:
            if b.w is not None:
                deps.append(b.w)
        for b in W:
            if b.w is not None:
                deps.append(b.w)
            deps.extend(b.r.values())
        return deps

    def _mark(self, dep, R, W):
        for b in R:
            b.r[dep[0]["name"]] = dep
        for b in W:
            b.w = dep
            b.r = {}

    def op(self, eng, fn, R=(), W=()):
        if any(b.excl for b in R):
            W = list(W) + [b for b in R if b.excl and b not in W]
            R = [b for b in R if not b.excl]
        for d in self._deps(R, W):
            self._wait(eng, d)
        s = self.psem[eng]
        s["n"] += 1
        dep = (s, s["n"])
        self.q[eng].append(("op", fn, s["h"], 1))
        self._mark(dep, R, W)
        self.nops += 1
        return dep

    def dma(self, eng, out, in_, sem, R=(), W=()):
        assert bool(sem.get("sw")) == (eng == "pool"), (eng, sem["name"])
        for d in self._deps(R, W):
            if d[0] is sem:
                continue
            self._wait(eng, d)
        sem["n"] += 16
        dep = (sem, sem["n"])
        self.q[eng].append(("op", lambda e: e.dma_start(out=out, in_=in_), sem["h"], 16))
        self._mark(dep, R, W)
        self.nops += 1
        return dep

    def batch(self, sem, tts):
        for b in tts:
            b.w = (sem, sem["n"])

    def gather(self, out, in_, idx_ap, sem, R=(), W=()):
        eng = "pool"
        for d in self._deps(R, W):
            self._wait(eng, d)
        sem["n"] += 16
        dep = (sem, sem["n"])
        self.q[eng].append(("op", lambda e: e.indirect_dma_start(
            out=out, out_offset=None, in_=in_,
            in_offset=bass.IndirectOffsetOnAxis(ap=idx_ap, axis=0)), sem["h"], 16))
        self._mark(dep, R, W)
        self.nops += 1
        return dep

    def barrier(self):
        sems = list(self.psem.values()) + self.dsems
        for e in ENG:
            for s in sems:
                if s["n"] > 0:
                    self._wait(e, (s, s["n"]))
        for s_ in self.dsems:
            (self.dpool_sw if s_.get("sw") else self.dpool_hw).append(s_)
        self.dsems = []
        for e in ENG:
            if self.psem[e]["n"] > 8000:
                self.psem[e] = self._mksem(uniq("p_" + e))

    def flush(self):
        nc = self.nc
        q = self.q

        def run(e, lst):
            for it in lst:
                if it[0] == "wait":
                    e.wait_ge(it[1], it[2])
                else:
                    ins = it[1](e)
                    ins.then_inc(it[2], it[3])

        with nc.Block() as block:
            @block.sync
            def _(e):
                run(e, q["sync"])

            @block.scalar
            def _(e):
                run(e, q["act"])

            @block.tensor
            def _(e):
                run(e, q["pe"])

            @block.vector
            def _(e):
                run(e, q["dve"])

            @block.gpsimd
            def _(e):
                run(e, q["pool"])


class Ctx:
    pass


def bcast_row(ap1d, n):
    return bass.AP(ap1d.tensor, ap1d.offset, [[0, 128], [1, n]])


def alloc_ln(C, es, want_T=True):
    nc, P = C.nc, C.P
    L = Ctx()
    sb = lambda n, s, d: es.enter_context(nc.sbuf_tensor(uniq(n), s, d))
    L.stats = [TT(sb(f"ln_stats{i}", [128, 12], F32)) for i in range(2)]
    L.mv = [TT(sb(f"ln_mv{i}", [128, 8], F32)) for i in range(2)]
    L.xn = [TT(sb(f"ln_xn{i}", [128, D], F32)) for i in range(2)]
    L.ho = [TT(sb(f"ln_ho{i}", [128, D], F32)) for i in range(2)]
    L.want_T = want_T
    if want_T:
        L.hTs = [TT(sb(f"ln_hTs{i}", [128, 8, 512], BF16)) for i in range(2)]
    L.sem_st = [P.dma_sem("ln_st0"), P.dma_sem("ln_st1")]
    L.sem_sT = [P.dma_sem("ln_sT0"), P.dma_sem("ln_sT1")]
    L.cnt = 0
    L.pend = []
    return L


def ln_tile(C, L, tt, pre, gam, bet, h_dst, hT_dst, tr_banks=None, router=None):
    i = L.cnt
    L.cnt += 1
    L.pend.append(dict(i=i, tt=tt, pre=pre, gam=gam, bet=bet, h_dst=h_dst, hT_dst=hT_dst, tr=tr_banks, router=router))
    _ln_step(C, L)


def ln_flush(C, L):
    for _ in range(2):
        L.pend.append(None)
        _ln_step(C, L)
    L.pend = []


def _ln_step(C, L):
    P = C.P
    n = len(L.pend)
    t0 = L.pend[n - 1]
    t1 = L.pend[n - 2] if n >= 2 else None
    t2 = L.pend[n - 3] if n >= 3 else None
    if t0 is not None:
        b = t0["i"] % 2
        st, mv, pre = L.stats[b], L.mv[b], t0["pre"]
        P.op("dve", I("bn_stats", st.t[:, 0:6], pre.t[:, 0:512]), R=[pre], W=[st])
        P.op("dve", I("bn_stats", st.t[:, 6:12], pre.t[:, 512:1024]), R=[pre], W=[st])
        P.op("dve", I("bn_aggr", mv.t[:, 0:2], st.t[:, :]), R=[st], W=[mv])
        P.op("dve", I("tensor_scalar_add", mv.t[:, 2:3], mv.t[:, 1:2], EPS), R=[mv], W=[mv])
    if t0 is not None:
        b = t0["i"] % 2
        mv = L.mv[b]
        P.op("act", I("activation", out=mv.t[:, 3:4], in_=mv.t[:, 2:3], func=AF.Ln), R=[mv], W=[mv])
        P.op("act", I("activation", out=mv.t[:, 4:5], in_=mv.t[:, 3:4], func=AF.Exp, scale=-0.5), R=[mv], W=[mv])
    if t1 is not None:
        b = t1["i"] % 2
        mv, xn, ho, pre = L.mv[b], L.xn[b], L.ho[b], t1["pre"]
        P.op("dve", I("scalar_tensor_tensor", xn.t[:, :], pre.t[:, :], mv.t[:, 0:1], t1["gam"].t[:, :], op0=ALU.subtract, op1=ALU.mult),
             R=[pre, mv, t1["gam"]], W=[xn])
        P.op("dve", I("scalar_tensor_tensor", ho.t[:, :], xn.t[:, :], mv.t[:, 4:5], t1["bet"].t[:, :], op0=ALU.mult, op1=ALU.add),
             R=[xn, mv, t1["bet"]], W=[ho])
        P.dma("sync", t1["h_dst"][1], ho.t[:, :], L.sem_st[b], R=[ho], W=[t1["h_dst"][0]])
        if t1["router"] is not None:
            t1["router"](t1["tt"], ho)
    if t2 is not None and t2["hT_dst"] is not None:
        b = t2["i"] % 2
        ho = L.ho[b]
        tt = t2["tt"]
        slot = tt % 4
        hTs = L.hTs[(tt // 4) % 2]
        bk = t2["tr"]
        flat = C.ps2(bk)
        for k in range(8):
            P.op("pe", I("transpose", flat[:, k * 128:(k + 1) * 128], ho.t[:, k * 128:(k + 1) * 128], C.ident.t[:, :]),
                 R=[ho, C.ident], W=[C.PS[bk], C.PS[bk + 1]])
        P.op("act", I("activation", out=hTs.t[:, :, slot * 128:(slot + 1) * 128],
                      in_=C.ps2v(bk, 8, 128), func=AF.Copy),
             R=[C.PS[bk], C.PS[bk + 1]], W=[hTs])
        if slot == 3:
            c0 = (tt - 3) * 128
            dst = t2["hT_dst"][1].rearrange("(c p) t -> p c t", p=128)[:, :, c0:c0 + 512]
            P.dma("sync", dst, hTs.t[:, :, :], L.sem_sT[(tt // 4) % 2], R=[hTs], W=[t2["hT_dst"][0]])


def phase_ln_in(C):
    nc, P = C.nc, C.P
    with ExitStack() as es:
        sb = lambda n, s, d: es.enter_context(nc.sbuf_tensor(uniq(n), s, d))
        gam = TT(sb("a_gam", [128, D], F32))
        bet = TT(sb("a_bet", [128, D], F32))
        xin = [TT(sb(f"a_xin{i}", [128, D], F32)) for i in range(2)]
        sem_c = P.dma_sem("a_c")
        sem_x = [P.dma_sem("a_x0", sw=True), P.dma_sem("a_x1", sw=True)]
        P.dma("sync", gam.t[:, :], bcast_row(C.d["ln_in_g"], D), sem_c, W=[gam])
        P.dma("sync", bet.t[:, :], bcast_row(C.d["ln_in_b"], D), sem_c, W=[bet])
        P.batch(sem_c, [gam, bet])
        L = alloc_ln(C, es)
        for tt in range(NT):
            b = tt % 2
            P.dma("pool", xin[b].t[:, :], C.d["x"][tt * 128:(tt + 1) * 128, :], sem_x[b], W=[xin[b]])
            ln_tile(C, L, tt, xin[b], gam, bet, (C.Dha, C.ha[tt * 128:(tt + 1) * 128, :]), (C.DhTa, C.hTa), tr_banks=0 + 2 * (tt % 2))
        ln_flush(C, L)
        P.barrier()


def phase_proj(C, l):
    nc, P = C.nc, C.P
    with ExitStack() as es:
        sb = lambda n, s, d: es.enter_context(nc.sbuf_tensor(uniq(n), s, d))
        wi = TT(sb("b_wi", [128, 8, 2048], BF16))
        hT = [TT(sb(f"b_hT{k}", [128, T], BF16)) for k in range(8)]
        pw = TT(sb("b_pw", [128, 4, 128], BF16))
        psc = TT(sb("b_psc", [128, 4], F32))
        invc = TT(sb("b_invc", [128, 4, 16], F32))
        U = TT(sb("b_U", [128, 16 + T], F32))
        SA = TT(sb("b_SA", [128, 16 + T], F32))
        SB = TT(sb("b_SB", [128, 16 + T], F32))
        dd = TT(sb("b_d", [128, T], BF16))
        stg = [TT(sb(f"b_stg{i}", [128, T], BF16)) for i in range(2)]
        vst = [TT(sb(f"b_vst{i}", [128, 4, 512], BF16)) for i in range(2)]
        sem_w = P.dma_sem("b_w", sw=True)
        sem_h = P.dma_sem("b_h")
        sem_c = P.dma_sem("b_c")
        sem_cw = P.dma_sem("b_cw", sw=True)
        sem_s = [P.dma_sem("b_s0"), P.dma_sem("b_s1")]
        sem_v = [P.dma_sem("b_v0"), P.dma_sem("b_v1")]
        for q4 in range(4):
            P.dma("pool", wi.t[:, :, q4 * 512:(q4 + 1) * 512], C.d["w_in"][l][:, :, q4 * 512:(q4 + 1) * 512], sem_w, W=[wi])
        for k in range(8):
            P.dma("sync", hT[k].t[:, :], C.hTa[k * 128:(k + 1) * 128, :], sem_h, R=[C.DhTa], W=[hT[k]])
        P.dma("pool", pw.t[:, :, :], C.d["pool_w"][l], sem_cw, W=[pw])
        P.dma("sync", psc.t[:, :], C.d["pool_scale"][l], sem_c, W=[psc])
        P.dma("sync", invc.t[:, :, :], C.d["c_invc"], sem_c, W=[invc])
        P.batch(sem_c, [psc, invc])
        P.batch(sem_h, hT)
        for buf in (U, SA, SB):
            P.op("dve", I("memset", buf.t[:, 0:16], 0.0), W=[buf])

        nbank = [0]

        def next_bank():
            b = nbank[0] % 4
            nbank[0] += 1
            return b

        evac_eng = [0]

        def evac(out_ap, in_ap, R, W, scale=None, only_act=False):
            e = 0 if only_act else evac_eng[0] % 2
            evac_eng[0] += 1
            if e == 0:
                if scale is None:
                    P.op("act", I("activation", out=out_ap, in_=in_ap, func=AF.Copy), R=R, W=W)
                else:
                    P.op("act", I("activation", out=out_ap, in_=in_ap, func=AF.Identity, scale=scale), R=R, W=W)
            else:
                if scale is None:
                    P.op("dve", I("tensor_copy", out_ap, in_ap), R=R, W=W)
                else:
                    P.op("dve", I("tensor_scalar_mul", out_ap, in_ap, scale), R=R, W=W)

        def fm_chunk(c, out_fn):
            for tg in range(8):
                bk = next_bank()
                for k in range(8):
                    P.op("pe", I("matmul", C.bank(bk), wi.t[:, k, c * 128:(c + 1) * 128], hT[k].t[:, tg * 512:(tg + 1) * 512],
                                 start=(k == 0), stop=(k == 7)), R=[wi, hT[k]], W=[C.PS[bk]])
                out_fn(tg, bk)

        def qk_chunk(c, only_act=False):
            so = stg[sidx[0] % 2]
            ss = sem_s[sidx[0] % 2]
            sidx[0] += 1
            sc = 0.125 if c < 8 else None
            fm_chunk(c, lambda tg, bk, so=so, sc=sc: evac(so.t[:, tg * 512:(tg + 1) * 512], C.bank(bk), [C.PS[bk]], [so], scale=sc, only_act=only_act))
            if c < 8:
                P.dma("sync", C.qT[(c - 4) * 128:(c - 3) * 128, :], so.t[:, :], ss, R=[so], W=[C.DqT[c - 4]])
            else:
                P.dma("sync", C.kT[(c - 8) * 128:(c - 7) * 128, :], so.t[:, :], ss, R=[so], W=[C.DkT[c - 8]])

        sidx = [0]
        for g in range(4):
            w = 2 << g
            fm_chunk(g, lambda tg, bk: evac(U.t[:, 16 + tg * 512:16 + (tg + 1) * 512], C.bank(bk), [C.PS[bk]], [U]))
            src = U
            bufs = [SA, SB]
            nlev = g + 1
            for lev in range(nlev):
                dst = bufs[lev % 2]
                sh = 1 << lev
                eng = "dve"
                P.op(eng, I("tensor_tensor", dst.t[:, 16:16 + T], src.t[:, 16:16 + T], src.t[:, 16 - sh:16 + T - sh], op=ALU.add),
                     R=[src], W=[dst])
                src = dst
            P.op("dve", I("scalar_tensor_tensor", dd.t[:, :], src.t[:, 16:16 + T], 1.0 / w, U.t[:, 16:16 + T],
                          op0=ALU.mult, op1=ALU.subtract), R=[src, U], W=[dd])
            P.op("dve", I("tensor_tensor", SA.t[:, 0:16] if src is SB else SB.t[:, 0:16], src.t[:, 16:32], invc.t[:, g, :], op=ALU.mult),
                 R=[src, invc], W=[SA if src is SB else SB])
            tmp = SA if src is SB else SB
            P.op("dve", I("tensor_tensor", dd.t[:, 0:16], tmp.t[:, 0:16], U.t[:, 16:32], op=ALU.subtract),
                 R=[tmp, U], W=[dd])
            P.op("dve", I("memset", tmp.t[:, 0:16], 0.0), W=[tmp])
            qk_chunk(4 + 2 * g, only_act=True)
            qk_chunk(5 + 2 * g, only_act=True)
            so = stg[sidx[0] % 2]
            ss = sem_s[sidx[0] % 2]
            sidx[0] += 1
            for tg in range(8):
                bk = next_bank()
                P.op("pe", I("matmul", C.bank(bk), pw.t[:, g, :], dd.t[:, tg * 512:(tg + 1) * 512], start=True, stop=True),
                     R=[pw, dd], W=[C.PS[bk]])
                evac(so.t[:, tg * 512:(tg + 1) * 512], C.bank(bk), [C.PS[bk], psc], [so], scale=psc.t[:, g:g + 1])
            P.dma("sync", C.ycT[g * 128:(g + 1) * 128, :], so.t[:, :], ss, R=[so], W=[C.DycT[g]])
        vv = C.v.rearrange("(n p) c -> p n c", p=128)
        for tt in range(NT):
            bk = next_bank()
            for k in range(8):
                P.op("pe", I("matmul", C.bank(bk), hT[k].t[:, tt * 128:(tt + 1) * 128], wi.t[:, k, 1536:2048],
                             start=(k == 0), stop=(k == 7)), R=[wi, hT[k]], W=[C.PS[bk]])
            vs = vst[(tt // 4) % 2]
            evac(vs.t[:, tt % 4, :], C.bank(bk), [C.PS[bk]], [vs])
            if tt % 4 == 3:
                P.dma("sync", vv[:, tt - 3:tt + 1, :], vs.t[:, :, :], sem_v[(tt // 4) % 2], R=[vs], W=[C.Dv])
        P.barrier()


def phase_attn(C, l):
    nc, P = C.nc, C.P
    with ExitStack() as es:
        sb = lambda n, s, d: es.enter_context(nc.sbuf_tensor(uniq(n), s, d))
        qT = [TT(sb(f"c_qT{i}", [128, T], BF16)) for i in range(4)]
        kT = [TT(sb(f"c_kT{i}", [128, T], BF16)) for i in range(4)]
        V = TT(sb("c_V", [128, NT, 512], BF16))
        E = [TT(sb(f"c_E{i}", [128, 2048], F32)) for i in range(2)]
        SP = [TT(sb(f"c_SP{i}", [128, 2048], BF16)) for i in range(4)]
        CA = [TT(sb(f"c_CA{i}", [128, 2048], BF16)) for i in range(4)]
        AT = [TT(sb(f"c_AT{i}", [128, 2048], BF16)) for i in range(3)]
        ys = [TT(sb(f"c_ys{i}", [128, T], BF16)) for i in range(2)]
        sem_l = P.dma_sem("c_l")
        sem_s = [P.dma_sem("c_s0"), P.dma_sem("c_s1")]
        sem_l0 = P.dma_sem("c_l0")
        sem_lv = P.dma_sem("c_lv")
        vv = C.v.rearrange("(n p) c -> p n c", p=128)
        P.dma("sync", qT[0].t[:, :], C.qT[0:128, :], sem_l0, R=[C.DqT[0]], W=[qT[0]])
        P.dma("sync", kT[0].t[:, :], C.kT[0:128, :], sem_l0, R=[C.DkT[0]], W=[kT[0]])
        P.batch(sem_l0, [qT[0], kT[0]])
        for i in range(4):
            P.dma("sync", V.t[:, i * 8:(i + 1) * 8, :], vv[:, i * 8:(i + 1) * 8, :], sem_lv, R=[C.Dv], W=[V])
        for i in range(1, 4):
            P.dma("sync", qT[i].t[:, :], C.qT[i * 128:(i + 1) * 128, :], sem_l, R=[C.DqT[i]], W=[qT[i]])
            P.dma("sync", kT[i].t[:, :], C.kT[i * 128:(i + 1) * 128, :], sem_l, R=[C.DkT[i]], W=[kT[i]])
        P.batch(sem_l, qT[1:] + kT[1:])

        ZP = [0, 2]
        BP = 4
        OB = [6, 7]
        quads = []
        for hp in range(4):
            for e in range(2):
                for g in range(8):
                    nq = g + 1
                    for qi in range(nq):
                        js = [4 * g + 3 - 4 * qi - k for k in range(4)]
                        quads.append(dict(hp=hp, e=e, g=g, js=js, first=(qi == 0), last=(qi == nq - 1)))
        n = len(quads)
        masks2 = C.masks.t[:, :, :].rearrange("p a b -> p (a b)")

        def zq(s):
            q = quads[s]
            hp, e, g = q["hp"], q["e"], q["g"]
            pr = slice(64 * e, 64 * e + 64)
            for k in range(4):
                bk = ZP[k // 2] + k % 2
                j = q["js"][k]
                P.op("pe", I("matmul", C.bank(bk), kT[hp].t[pr, j * 128:(j + 1) * 128], qT[hp].t[pr, g * 512:(g + 1) * 512],
                             start=True, stop=True), R=[kT[hp], qT[hp]], W=[C.PS[bk]])

        def act1(s):
            Eb = E[s % 2]
            z4 = C.ps[:, 0:4, :].rearrange("p a b -> p (a b)")
            P.op("act", I("activation", out=Eb.t[:, :], in_=z4, func=AF.Exp),
                 R=[C.PS[0], C.PS[1], C.PS[2], C.PS[3]], W=[Eb])

        def act2(s):
            P.op("act", I("activation", out=SP[s % 4].t[:, :], in_=E[s % 2].t[:, :], func=AF.Ln, bias=1.0), R=[E[s % 2]], W=[SP[s % 4]])

        def dve3(s):
            q = quads[s]
            S, Cq, Cn = SP[s % 4], CA[s % 4], CA[(s + 1) % 4]
            sl = lambda k: slice(k * 512, (k + 1) * 512)
            if q["first"]:
                P.op("dve", I("tensor_tensor", S.t[:, :], S.t[:, :], masks2, op=ALU.mult), R=[S, C.masks], W=[S])
                P.op("dve", I("tensor_tensor", Cq.t[:, sl(2)], S.t[:, sl(0)], S.t[:, sl(1)], op=ALU.add), R=[S], W=[Cq])
            else:
                P.op("dve", I("tensor_tensor", Cq.t[:, sl(1)], Cq.t[:, sl(0)], S.t[:, sl(0)], op=ALU.add), R=[S, Cq], W=[Cq])
                P.op("dve", I("tensor_tensor", Cq.t[:, sl(2)], Cq.t[:, sl(1)], S.t[:, sl(1)], op=ALU.add), R=[S, Cq], W=[Cq])
            P.op("dve", I("tensor_tensor", Cq.t[:, sl(3)], Cq.t[:, sl(2)], S.t[:, sl(2)], op=ALU.add), R=[S, Cq], W=[Cq])
            if not q["last"]:
                P.op("dve", I("tensor_tensor", Cn.t[:, sl(0)], Cq.t[:, sl(3)], S.t[:, sl(3)], op=ALU.add), R=[S, Cq], W=[Cn])

        def bq(s, p):
            q = quads[s]
            hp, e, g = q["hp"], q["e"], q["g"]
            pr = slice(64 * e, 64 * e + 64)
            S, Cq = SP[s % 4], CA[s % 4]
            for k in (2 * p, 2 * p + 1):
                bk = BP + k % 2
                j = q["js"][k]
                if q["first"] and k == 0:
                    car = None
                elif q["first"] and k == 1:
                    car = (S, S.t[:, 0:512])
                else:
                    car = (Cq, Cq.t[:, k * 512:(k + 1) * 512])
                P.op("pe", I("matmul", C.bank(bk), kT[hp].t[pr, j * 128:(j + 1) * 128], qT[hp].t[pr, g * 512:(g + 1) * 512],
                             start=True, stop=False), R=[kT[hp], qT[hp]], W=[C.PS[bk]])
                P.op("pe", I("matmul", C.bank(bk), C.negU.t[:, :], S.t[:, k * 512:(k + 1) * 512], start=False, stop=(car is None)),
                     R=[C.negU, S], W=[C.PS[bk]])
                if car is not None:
                    P.op("pe", I("matmul", C.bank(bk), C.negO.t[:, :], car[1], start=False, stop=True),
                         R=[C.negO, car[0]], W=[C.PS[bk]])

        def act3(s, p):
            A = AT[s % 3]
            P.op("act", I("activation", out=A.t[:, p * 1024:(p + 1) * 1024], in_=C.ps2(BP), func=AF.Exp),
                 R=[C.PS[BP], C.PS[BP + 1]], W=[A])

        def mask_att(s):
            q = quads[s]
            if q["first"]:
                A = AT[s % 3]
                P.op("dve", I("tensor_tensor", A.t[:, :], A.t[:, :], masks2, op=ALU.mult), R=[A, C.masks], W=[A])

        def av(s):
            q = quads[s]
            hp, e, g = q["hp"], q["e"], q["g"]
            gi = (hp * 2 + e) * 8 + g
            ob = OB[gi % 2]
            A = AT[s % 3]
            pr = slice(64 * e, 64 * e + 64)
            for k in range(4):
                j = q["js"][k]
                P.op("pe", I("matmul", C.bank(ob)[pr, :], V.t[:, j, hp * 128 + 64 * e: hp * 128 + 64 * e + 64], A.t[:, k * 512:(k + 1) * 512],
                             start=(q["first"] and k == 0), stop=(q["last"] and k == 3)), R=[V, A], W=[C.PS[ob]])
            if q["last"]:
                yb = ys[hp % 2]
                P.op("dve", I("tensor_copy", yb.t[pr, g * 512:(g + 1) * 512], C.bank(ob)[pr, :]), R=[C.PS[ob]], W=[yb])
                if e == 1 and g == 7:
                    P.dma("sync", C.ycT[512 + hp * 128:512 + (hp + 1) * 128, :], yb.t[:, :], sem_s[hp % 2], R=[yb], W=[C.DycT[4 + hp]])

        for s in range(n + 3):
            if s < n:
                zq(s)
            if 0 <= s - 2 < n:
                bq(s - 2, 0)
                act3(s - 2, 0)
            if s < n:
                act1(s)
            if 0 <= s - 3 < n:
                av(s - 3)
            if 0 <= s - 2 < n:
                bq(s - 2, 1)
                act3(s - 2, 1)
            if s < n:
                act2(s)
                dve3(s)
            if 0 <= s - 2 < n:
                mask_att(s - 2)
        P.barrier()


def phase_mix_ln1(C, l):
    nc, P = C.nc, C.P
    moe = (l % 2 == 1)
    with ExitStack() as es:
        sb = lambda n, s, d: es.enter_context(nc.sbuf_tensor(uniq(n), s, d))
        yc = [TT(sb(f"d_yc{k}", [128, T], BF16)) for k in range(8)]
        wo = TT(sb("d_wo", [128, 8, D], BF16))
        gam = TT(sb("d_gam", [128, D], F32))
        bet = TT(sb("d_bet", [128, D], F32))
        hin = [TT(sb(f"d_hin{i}", [128, D], F32)) for i in range(2)]
        pre = [TT(sb(f"d_pre{i}", [128, D], F32)) for i in range(2)]
        sem_c = P.dma_sem("d_c")
        sem_cw = P.dma_sem("d_cw", sw=True)
        sem_y = P.dma_sem("d_y")
        sem_h = [P.dma_sem("d_h0", sw=True), P.dma_sem("d_h1", sw=True)]
        P.dma("pool", wo.t[:, :, :], C.d["w_out"][l], sem_cw, W=[wo])
        P.dma("sync", gam.t[:, :], bcast_row(C.d["ln1_g"][l], D), sem_c, W=[gam])
        P.dma("sync", bet.t[:, :], bcast_row(C.d["ln1_b"][l], D), sem_c, W=[bet])
        for k in range(8):
            P.dma("sync", yc[k].t[:, :], C.ycT[k * 128:(k + 1) * 128, :], sem_y, R=[C.DycT[k]], W=[yc[k]])
        P.batch(sem_y, yc)
        L = alloc_ln(C, es)
        router = None
        P.batch(sem_c, [gam, bet])
        if moe:
            rt = TT(sb("d_rt", [128, 8, NE], F32))
            P.dma("sync", rt.t[:, :, :], C.d["moe_router"][l // 2], sem_c, W=[rt])
            P.batch(sem_c, [gam, bet, rt])
            hT32 = [TT(sb(f"d_hT32{i}", [128, 8, 128], F32)) for i in range(2)]
            lgt = TT(sb("d_lgt", [128, NT, NE], F32))
            wk = TT(sb("d_wk", [128, NT, NE], F32))
            m1 = TT(sb("d_m1", [128, NT], F32))
            m2 = TT(sb("d_m2", [128, NT], F32))

            def router(tt, ho):
                b = tt % 2
                bk = 4 + 2 * b
                flat = C.ps2(bk)
                for k in range(8):
                    P.op("pe", I("transpose", flat[:, k * 128:(k + 1) * 128], ho.t[:, k * 128:(k + 1) * 128], C.ident.t[:, :]),
                         R=[ho, C.ident], W=[C.PS[bk], C.PS[bk + 1]])
                P.op("dve", I("tensor_copy", hT32[b].t[:, :, :], C.ps2v(bk, 8, 128)), R=[C.PS[bk], C.PS[bk + 1]], W=[hT32[b]])
                lb = C.bank(bk)[:, 0:NE]
                for k in range(8):
                    P.op("pe", I("matmul", lb, hT32[b].t[:, k, :], rt.t[:, k, :], start=(k == 0), stop=(k == 7)),
                         R=[hT32[b], rt], W=[C.PS[bk], C.PS[bk + 1]])
                P.op("dve", I("tensor_copy", lgt.t[:, tt, :], lb), R=[C.PS[bk], C.PS[bk + 1]], W=[lgt])

            def router_finish():
                fl = lambda t_: t_.t[:, :, :].rearrange("p a b -> p (a b)")
                P.op("dve", I("tensor_reduce", m1.t[:, :], lgt.t[:, :, :], AX.X, ALU.max), R=[lgt], W=[m1])
                for e_ in range(NE):
                    P.op("dve", I("tensor_tensor", wk.t[:, :, e_], lgt.t[:, :, e_], m1.t[:, :], op=ALU.is_equal), R=[lgt, m1], W=[wk])
                P.op("dve", I("scalar_tensor_tensor", fl(wk), fl(wk), -1e30, fl(lgt), op0=ALU.mult, op1=ALU.add), R=[wk, lgt], W=[wk])
                P.op("dve", I("tensor_reduce", m2.t[:, :], wk.t[:, :, :], AX.X, ALU.max), R=[wk], W=[m2])
                for e_ in range(NE):
                    P.op("dve", I("tensor_tensor", wk.t[:, :, e_], lgt.t[:, :, e_], m1.t[:, :], op=ALU.subtract), R=[lgt, m1], W=[wk])
                P.op("act", I("activation", out=fl(wk), in_=fl(wk), func=AF.Exp), R=[wk], W=[wk])
                for e_ in range(NE):
                    P.op("dve", I("tensor_tensor", C.sel.t[:, :, e_], lgt.t[:, :, e_], m2.t[:, :], op=ALU.is_ge), R=[lgt, m2], W=[C.sel])
                P.op("dve", I("tensor_tensor", fl(wk), fl(wk), fl(C.sel), op=ALU.mult), R=[wk, C.sel], W=[wk])
                P.op("dve", I("tensor_reduce", m1.t[:, :], wk.t[:, :, :], AX.X, ALU.add), R=[wk], W=[m1])
                P.op("dve", I("reciprocal", m1.t[:, :], m1.t[:, :]), R=[m1], W=[m1])
                for e_ in range(NE):
                    P.op("dve", I("tensor_tensor", C.gate.t[:, :, e_], wk.t[:, :, e_], m1.t[:, :], op=ALU.mult), R=[wk, m1], W=[C.gate])

        for tt in range(NT):
            b = tt % 2
            P.dma("pool", hin[b].t[:, :], C.ha[tt * 128:(tt + 1) * 128, :], sem_h[b], R=[C.Dha], W=[hin[b]])
            bk = 0 + 2 * b
            for nh in range(2):
                for k in range(8):
                    P.op("pe", I("matmul", C.bank(bk + nh), yc[k].t[:, tt * 128:(tt + 1) * 128], wo.t[:, k, nh * 512:(nh + 1) * 512],
                                 start=(k == 0), stop=(k == 7)), R=[yc[k], wo], W=[C.PS[bk], C.PS[bk + 1]])
            P.op("dve", I("scalar_tensor_tensor", pre[b].t[:, :], hin[b].t[:, :], ALPHA, C.ps2(bk), op0=ALU.mult, op1=ALU.add),
                 R=[hin[b], C.PS[bk], C.PS[bk + 1]], W=[pre[b]])
            ln_tile(C, L, tt, pre[b], gam, bet, (C.Dhb, C.hb[tt * 128:(tt + 1) * 128, :]),
                    None if moe else (C.DhTb, C.hTb), tr_banks=4 + 2 * b, router=router)
        ln_flush(C, L)
        if moe:
            router_finish()
        P.barrier()


def alloc_ffn(C, es, ntok):
    nc, P = C.nc, C.P
    F = Ctx()
    sb = lambda n, s, d: es.enter_context(nc.sbuf_tensor(uniq(n), s, d))
    F.ntok = ntok
    F.wd = TT(sb("f_wd", [128, NFC, D], BF16))
    F.wg = [TT(sb(f"f_wg{i}", [128, 2, 8, 128], BF16)) for i in range(3)]
    F.wu = [TT(sb(f"f_wu{i}", [128, 2, 8, 128], BF16)) for i in range(3)]
    F.act = TT(sb("f_act", [128, NFC, ntok], BF16))
    F.sg = [TT(sb(f"f_sg{i}", [128, 512], BF16)) for i in range(2)]
    F.sem_wd = P.dma_sem("f_wd", sw=True)
    F.sem_wg = [P.dma_sem(f"f_wg{i}", sw=True) for i in range(3)]
    F.sem_wu = [P.dma_sem(f"f_wu{i}", sw=True) for i in range(3)]
    F.cnt = 0
    F.blk = 0
    return F


def ffn_pass(C, F, xT, wg_d, wu_d, wd_d, consume, GB=(0, 1), UB=(2, 3), DB=4, blk_hook=None):
    P = C.P
    ntok = F.ntok
    subs = []
    t0 = 0
    while t0 < ntok:
        w = min(512, ntok - t0)
        subs.append((t0, w))
        t0 += w
    wdv = wd_d.rearrange("(c p) n -> p c n", p=128)
    for fb in range(NFC // 2):
        if fb == 2:
            P.dma("pool", F.wd.t[:, 0:11, :], wdv[:, 0:11, :], F.sem_wd, W=[F.wd])
            P.dma("pool", F.wd.t[:, 11:22, :], wdv[:, 11:22, :], F.sem_wd, W=[F.wd])
        bi = F.blk % 3
        F.blk += 1
        wg, wu = F.wg[bi], F.wu[bi]
        P.dma("pool", wg.t[:, :, :, :], wg_d[2 * fb:2 * fb + 2].rearrange("c p k f -> p c k f"), F.sem_wg[bi], W=[wg])
        P.dma("pool", wu.t[:, :, :, :], wu_d[2 * fb:2 * fb + 2].rearrange("c p k f -> p c k f"), F.sem_wu[bi], W=[wu])
        for ci in range(2):
            fc = 2 * fb + ci
            for (t0, w) in subs:
                i = F.cnt
                F.cnt += 1
                gb, ub = GB[i % 2], UB[i % 2]
                for k in range(8):
                    P.op("pe", I("matmul", C.bank(gb)[:, 0:w], wg.t[:, ci, k, :], xT[k].t[:, t0:t0 + w], start=(k == 0), stop=(k == 7)),
                         R=[wg, xT[k]], W=[C.PS[gb]])
                for k in range(8):
                    P.op("pe", I("matmul", C.bank(ub)[:, 0:w], wu.t[:, ci, k, :], xT[k].t[:, t0:t0 + w], start=(k == 0), stop=(k == 7)),
                         R=[wu, xT[k]], W=[C.PS[ub]])
                sg = F.sg[i % 2]
                P.op("act", I("activation", out=sg.t[:, 0:w], in_=C.bank(gb)[:, 0:w], func=AF.Silu), R=[C.PS[gb]], W=[sg])
                P.op("dve", I("tensor_tensor", F.act.t[:, fc, t0:t0 + w], sg.t[:, 0:w], C.bank(ub)[:, 0:w], op=ALU.mult),
                     R=[sg, C.PS[ub]], W=[F.act])
        if blk_hook is not None:
            blk_hook(fb)
    dpairs = (DB, GB[0], UB[0])
    for tt in range(ntok // 128):
        db = dpairs[tt % 3]
        for nh in range(2):
            for fc in range(NFC):
                P.op("pe", I("matmul", C.bank(db + nh), F.act.t[:, fc, tt * 128:(tt + 1) * 128], F.wd.t[:, fc, nh * 512:(nh + 1) * 512],
                             start=(fc == 0), stop=(fc == NFC - 1)), R=[F.act, F.wd], W=[C.PS[db], C.PS[db + 1]])
        consume(tt, db)


def phase_ffn_dense(C, l, final):
    nc, P = C.nc, C.P
    TG = 1024
    with ExitStack() as es:
        sb = lambda n, s, d: es.enter_context(nc.sbuf_tensor(uniq(n), s, d))
        gam = TT(sb("e_gam", [128, D], F32))
        bet = TT(sb("e_bet", [128, D], F32))
        hin = [TT(sb(f"e_hin{i}", [128, D], F32)) for i in range(2)]
        pre = [TT(sb(f"e_pre{i}", [128, D], F32)) for i in range(2)]
        xT = [[TT(sb(f"e_xT{j}_{k}", [128, TG], BF16)) for k in range(8)] for j in range(1)]
        sem_c = P.dma_sem("e_c")
        sem_x = [P.dma_sem("e_x0"), P.dma_sem("e_x1")]
        sem_h = [P.dma_sem("e_h0", sw=True), P.dma_sem("e_h1", sw=True)]
        P.dma("sync", gam.t[:, :], bcast_row(C.d["ln2_g"][l], D), sem_c, W=[gam])
        P.dma("sync", bet.t[:, :], bcast_row(C.d["ln2_b"][l], D), sem_c, W=[bet])
        P.batch(sem_c, [gam, bet])
        L = alloc_ln(C, es, want_T=not final)
        F = alloc_ffn(C, es, TG)
        i = l // 2
        cnt = [0]
        for grp in range(T // TG):
            xb = xT[0]
            for k in range(8):
                P.dma("sync", xb[k].t[:, :], C.hTb[k * 128:(k + 1) * 128, grp * TG:(grp + 1) * TG], sem_x[grp % 2], R=[C.DhTb], W=[xb[k]])
            P.batch(sem_x[grp % 2], xb)

            def consume(tt, db, grp=grp):
                gt = grp * (TG // 128) + tt
                b = cnt[0] % 2
                cnt[0] += 1
                P.dma("pool", hin[b].t[:, :], C.hb[gt * 128:(gt + 1) * 128, :], sem_h[b], R=[C.Dhb], W=[hin[b]])
                P.op("dve", I("scalar_tensor_tensor", pre[b].t[:, :], hin[b].t[:, :], ALPHA, C.ps2(db), op0=ALU.mult, op1=ALU.add),
                     R=[hin[b], C.PS[db], C.PS[db + 1]], W=[pre[b]])
                if final:
                    ln_tile(C, L, gt, pre[b], gam, bet, (C.Dout, C.out[gt * 128:(gt + 1) * 128, :]), None)
                else:
                    ln_tile(C, L, gt, pre[b], gam, bet, (C.Dha, C.ha[gt * 128:(gt + 1) * 128, :]), (C.DhTa, C.hTa), tr_banks=6)

            ffn_pass(C, F, xb, C.d["ffn_wg"][i], C.d["ffn_wu"][i], C.d["ffn_wd"][i], consume)
        ln_flush(C, L)
        P.barrier()


def phase_moe(C, l, final, stop=3):
    nc, P = C.nc, C.P
    mi = l // 2
    with ExitStack() as es:
        sb = lambda n, s, d: es.enter_context(nc.sbuf_tensor(uniq(n), s, d))
        selb = TT(sb("m_selb", [128, NT, NE], BF16))
        csel = TT(sb("m_csel", [128, NT, NE], BF16))
        cum = TT(sb("m_cum", [128, NT, NE], F32))
        pos = TT(sb("m_pos", [128, NT, NE], F32))
        pre_ = TT(sb("m_pre", [128, NT, NE], F32))
        tmp = TT(sb("m_tmp", [128, NT, NE], F32))
        tmp2 = TT(sb("m_tmp2", [128, NT, NE], F32))
        iot = TT(sb("m_iot", [128, CAP], F32))
        ecap = TT(sb("m_ecap", [128, NT, NE], F32))
        A = [TT(sb(f"m_A{i}", [128, NT, CAP], BF16)) for i in range(2)]
        tokf = TT(sb("m_tokf", [128, NE, NST], F32))
        sem_c = P.dma_sem("m_c")
        P.dma("sync", iot.t[:, :], C.d["c_iota"], sem_c, W=[iot])
        P.dma("sync", ecap.t[:, :, :], C.d["c_ecap"], sem_c, W=[ecap])
        P.batch(sem_c, [iot, ecap])
        P.op("dve", I("tensor_copy", selb.t[:, :, :], C.sel.t[:, :, :]), R=[C.sel], W=[selb])
        P.op("dve", I("memset", csel.t[:, 0, :], 0.0), W=[csel])
        for tt in range(1, NT):
            P.op("dve", I("tensor_tensor", csel.t[:, tt, :], csel.t[:, tt - 1, :], selb.t[:, tt - 1, :], op=ALU.add), R=[csel, selb], W=[csel])
        bk = 0
        for tt in range(NT):
            o = C.bank(bk)[:, tt * NE:(tt + 1) * NE]
            P.op("pe", I("matmul", o, C.lowI.t[:, :], selb.t[:, tt, :], start=True, stop=False), R=[C.lowI, selb], W=[C.PS[bk]])
            P.op("pe", I("matmul", o, C.ones.t[:, :], csel.t[:, tt, :], start=False, stop=True), R=[C.ones, csel], W=[C.PS[bk]])
        P.op("dve", I("tensor_copy", cum.t[:, :, :], C.bank(bk)[:, 0:NT * NE].rearrange("p (a b) -> p a b", b=NE)), R=[C.PS[bk]], W=[cum])
        P.op("dve", I("tensor_tensor", pos.t[:, :, :], cum.t[:, :, :], C.sel.t[:, :, :], op=ALU.subtract), R=[cum, C.sel], W=[pos])
        P.op("dve", I("memset", pre_.t[:, :, :], 0.0), W=[pre_])
        P.op("dve", I("tensor_copy", pre_.t[:, :, 1:NE], C.sel.t[:, :, 0:NE - 1]), R=[C.sel], W=[pre_])
        for sh in (1, 2, 4):
            P.op("dve", I("tensor_copy", tmp.t[:, :, :], pre_.t[:, :, :]), R=[pre_], W=[tmp])
            P.op("dve", I("tensor_tensor", pre_.t[:, :, sh:NE], tmp.t[:, :, sh:NE], tmp.t[:, :, 0:NE - sh], op=ALU.add), R=[tmp], W=[pre_])
        P.op("dve", I("tensor_tensor", pos.t[:, :, :], pos.t[:, :, :], ecap.t[:, :, :], op=ALU.add), R=[pos, ecap], W=[pos])
        for kk in range(2):
            P.op("dve", I("tensor_scalar", tmp.t[:, :, :], pre_.t[:, :, :], float(kk), None, op0=ALU.is_equal), R=[pre_], W=[tmp])
            P.op("dve", I("tensor_tensor", tmp.t[:, :, :], tmp.t[:, :, :], C.sel.t[:, :, :], op=ALU.mult), R=[tmp, C.sel], W=[tmp])
            P.op("dve", I("tensor_tensor", tmp2.t[:, :, :], tmp.t[:, :, :], pos.t[:, :, :], op=ALU.mult), R=[tmp, pos], W=[tmp2])
            P.op("dve", I("tensor_reduce", C.slotf.t[:, kk, :], tmp2.t[:, :, :], AX.X, ALU.add), R=[tmp2], W=[C.slotf])
            P.op("dve", I("tensor_tensor", tmp2.t[:, :, :], tmp.t[:, :, :], C.gate.t[:, :, :], op=ALU.mult), R=[tmp, C.gate], W=[tmp2])
            P.op("dve", I("tensor_reduce", C.gsel.t[:, kk, :], tmp2.t[:, :, :], AX.X, ALU.add), R=[tmp2], W=[C.gsel])
        P.op("dve", I("tensor_copy", C.sloti.t[:, :, :], C.slotf.t[:, :, :]), R=[C.slotf], W=[C.sloti])
        ai = 0
        for e in range(NE):
            bk = 1 + (e % 2)
            Ab = A[e % 2]
            for tt in range(NT):
                eng = "dve"
                ai += 1
                P.op(eng, I("tensor_scalar", Ab.t[:, tt, :], iot.t[:, :], cum.t[:, tt, e:e + 1], None, op0=ALU.is_ge), R=[iot, cum], W=[Ab])
            for st in range(NST):
                for tt in range(NT):
                    P.op("pe", I("matmul", C.bank(bk)[:, st:st + 1], Ab.t[:, tt, st * 128:(st + 1) * 128], C.ones.t[:, 0:1],
                                 start=(tt == 0), stop=(tt == NT - 1)), R=[Ab, C.ones], W=[C.PS[bk]])
            P.op("dve", I("tensor_scalar", tokf.t[:, e, :], C.bank(bk)[:, 0:NST], float(T - 1), None, op0=ALU.min), R=[C.PS[bk]], W=[tokf])
        P.op("dve", I("tensor_copy", C.toki.t[:, :, :], tokf.t[:, :, :]), R=[tokf], W=[C.toki])
        P.barrier()
    if stop < 2:
        return
    with ExitStack() as es:
        sb = lambda n, s, d: es.enter_context(nc.sbuf_tensor(uniq(n), s, d))
        xg = [TT(sb(f"m_xg{i}", [128, D], F32)) for i in range(2)]
        xT = [[TT(sb(f"m_xT{j}_{k}", [128, CAP], BF16)) for k in range(8)] for j in range(2)]
        yst = [TT(sb(f"m_yst{i}", [128, D], F32)) for i in range(2)]
        sem_g = [P.dma_sem("m_g0", sw=True), P.dma_sem("m_g1", sw=True)]
        sem_y = [P.dma_sem("m_y0"), P.dma_sem("m_y1")]
        F = alloc_ffn(C, es, CAP)
        yi = [0]

        def emit_gather(e, st):
            b = st % 2
            P.gather(xg[b].t[:, :], C.hb, C.toki.t[:, e, st:st + 1], sem_g[b], R=[C.Dhb, C.toki], W=[xg[b]])

        def emit_tr(e, st):
            b = st % 2
            xb = xT[e % 2]
            bk = 6
            flat = C.ps2(bk)
            for k in range(8):
                P.op("pe", I("transpose", flat[:, k * 128:(k + 1) * 128], xg[b].t[:, k * 128:(k + 1) * 128], C.ident.t[:, :]),
                     R=[xg[b], C.ident], W=[C.PS[bk], C.PS[bk + 1]])
            for k in range(8):
                if k % 2 == 0:
                    P.op("act", I("activation", out=xb[k].t[:, st * 128:(st + 1) * 128], in_=flat[:, k * 128:(k + 1) * 128], func=AF.Copy),
                         R=[C.PS[bk], C.PS[bk + 1]], W=[xb[k]])
                else:
                    P.op("dve", I("tensor_copy", xb[k].t[:, st * 128:(st + 1) * 128], flat[:, k * 128:(k + 1) * 128]),
                         R=[C.PS[bk], C.PS[bk + 1]], W=[xb[k]])

        for st in range(NST):
            emit_gather(0, st)
            emit_tr(0, st)
        for e in range(NE):
            xb = xT[e % 2]

            def consume(tt, db, e=e):
                b = yi[0] % 2
                yi[0] += 1
                if b == 0:
                    P.op("act", I("activation", out=yst[b].t[:, :], in_=C.ps2(db), func=AF.Copy), R=[C.PS[db], C.PS[db + 1]], W=[yst[b]])
                else:
                    P.op("dve", I("tensor_copy", yst[b].t[:, :], C.ps2(db)), R=[C.PS[db], C.PS[db + 1]], W=[yst[b]])
                r0 = e * CAP + tt * 128
                P.dma("sync", C.ybuf[r0:r0 + 128, :], yst[b].t[:, :], sem_y[b], R=[yst[b]], W=[C.Dyb])

            def hook(fb, e=e):
                if e + 1 < NE:
                    if fb < NST:
                        emit_gather(e + 1, fb)
                    if 1 <= fb <= NST:
                        emit_tr(e + 1, fb - 1)

            ffn_pass(C, F, xb, C.d["moe_wg"][mi][e], C.d["moe_wu"][mi][e], C.d["moe_wd"][mi][e], consume, blk_hook=hook)
        P.barrier()
    if stop < 3:
        return
    with ExitStack() as es:
        sb = lambda n, s, d: es.enter_context(nc.sbuf_tensor(uniq(n), s, d))
        gam = TT(sb("n_gam", [128, D], F32))
        bet = TT(sb("n_bet", [128, D], F32))
        hin = [TT(sb(f"n_hin{i}", [128, D], F32)) for i in range(2)]
        y0 = [TT(sb(f"n_y0{i}", [128, D], F32)) for i in range(2)]
        y1 = [TT(sb(f"n_y1{i}", [128, D], F32)) for i in range(2)]
        pre = [TT(sb(f"n_pre{i}", [128, D], F32)) for i in range(2)]
        sem_c = P.dma_sem("n_c")
        sem_h = [P.dma_sem("n_h0", sw=True), P.dma_sem("n_h1", sw=True)]
        sem_a = [P.dma_sem("n_a0", sw=True), P.dma_sem("n_a1", sw=True)]
        sem_b = [P.dma_sem("n_b0", sw=True), P.dma_sem("n_b1", sw=True)]
        P.dma("sync", gam.t[:, :], bcast_row(C.d["ln2_g"][l], D), sem_c, W=[gam])
        P.dma("sync", bet.t[:, :], bcast_row(C.d["ln2_b"][l], D), sem_c, W=[bet])
        P.batch(sem_c, [gam, bet])
        L = alloc_ln(C, es, want_T=not final)
        for tt in range(NT):
            b = tt % 2
            P.dma("pool", hin[b].t[:, :], C.hb[tt * 128:(tt + 1) * 128, :], sem_h[b], R=[C.Dhb], W=[hin[b]])
            P.gather(y0[b].t[:, :], C.ybuf, C.sloti.t[:, 0, tt:tt + 1], sem_a[b], R=[C.Dyb, C.sloti], W=[y0[b]])
            P.gather(y1[b].t[:, :], C.ybuf, C.sloti.t[:, 1, tt:tt + 1], sem_b[b], R=[C.Dyb, C.sloti], W=[y1[b]])
            P.op("act", I("activation", out=y0[b].t[:, :], in_=y0[b].t[:, :], func=AF.Identity, scale=C.gsel.t[:, 0, tt:tt + 1]),
                 R=[y0[b], C.gsel], W=[y0[b]])
            P.op("dve", I("scalar_tensor_tensor", pre[b].t[:, :], hin[b].t[:, :], ALPHA, y0[b].t[:, :], op0=ALU.mult, op1=ALU.add),
                 R=[y0[b], hin[b]], W=[pre[b]])
            P.op("dve", I("scalar_tensor_tensor", pre[b].t[:, :], y1[b].t[:, :], C.gsel.t[:, 1, tt:tt + 1], pre[b].t[:, :], op0=ALU.mult, op1=ALU.add),
                 R=[y1[b], C.gsel, pre[b]], W=[pre[b]])
            if final:
                ln_tile(C, L, tt, pre[b], gam, bet, (C.Dout, C.out[tt * 128:(tt + 1) * 128, :]), None)
            else:
                ln_tile(C, L, tt, pre[b], gam, bet, (C.Dha, C.ha[tt * 128:(tt + 1) * 128, :]), (C.DhTa, C.hTa), tr_banks=0 + 2 * b)
        ln_flush(C, L)
        P.barrier()


IN_SPECS = None


def host_constants():
    c = {}
    c["c_ident"] = np.eye(128, dtype=np.float32)
    j = np.arange(128)[:, None]
    s = np.arange(128)[None, :]
    c["c_negU"] = -(j >= s).astype(np.float32)
    c["c_negO"] = -np.ones((128, 128), np.float32)
    c["c_lowI"] = (j <= s).astype(np.float32)
    c["c_ones"] = np.ones((128, 128), np.float32)
    p = np.arange(128)[:, None, None]
    dj = np.arange(4)[None, :, None]
    col = np.arange(512)[None, None, :]
    c["c_masks"] = ((128 * (3 - dj) + p) < col).astype(np.float32)
    invc = np.zeros((128, 4, 16), np.float32)
    for g in range(4):
        w = 2 << g
        invc[:, g, :] = 1.0 / np.minimum(np.arange(16) + 1, w)
    c["c_invc"] = invc
    c["c_iota"] = np.broadcast_to(np.arange(CAP, dtype=np.float32)[None, :], (128, CAP)).copy()
    c["c_ecap"] = np.broadcast_to((np.arange(NE, dtype=np.float32) * CAP)[None, None, :], (128, NT, NE)).copy()
    return c


def host_layout(inp):
    o = {}
    f = lambda a: np.ascontiguousarray(a, dtype=np.float32)
    o["ln_in_g"] = f(inp["ln_in_g"])
    o["ln_in_b"] = f(inp["ln_in_b"])
    o["w_in"] = f(inp["w_in"].reshape(DEPTH, 8, 128, 2048).transpose(0, 2, 1, 3))
    o["pool_w"] = f(inp["pool_w"].transpose(0, 2, 1, 3))
    o["pool_scale"] = f(inp["pool_scale"].reshape(DEPTH, 4, 128).transpose(0, 2, 1))
    o["w_out"] = f(inp["w_out"].reshape(DEPTH, 8, 128, D).transpose(0, 2, 1, 3))
    for nm in ("ln1_g", "ln1_b", "ln2_g", "ln2_b"):
        o[nm] = f(inp[nm])
    o["ffn_wg"] = f(inp["ffn_wg"].reshape(-1, 8, 128, NFC, 128).transpose(0, 3, 2, 1, 4))
    o["ffn_wu"] = f(inp["ffn_wu"].reshape(-1, 8, 128, NFC, 128).transpose(0, 3, 2, 1, 4))
    o["ffn_wd"] = f(inp["ffn_wd"])
    o["moe_router"] = f(inp["moe_router"].reshape(-1, 8, 128, NE).transpose(0, 2, 1, 3))
    o["moe_wg"] = f(inp["moe_wg"].reshape(-1, NE, 8, 128, NFC, 128).transpose(0, 1, 4, 3, 2, 5))
    o["moe_wu"] = f(inp["moe_wu"].reshape(-1, NE, 8, 128, NFC, 128).transpose(0, 1, 4, 3, 2, 5))
    o["moe_wd"] = f(inp["moe_wd"])
    o.update(host_constants())
    return o


def build(shapes, phases="all", debug_out=()):
    nc = bass.Bass("TRN2", target_bir_lowering=False)
    C = Ctx()
    C.nc = nc
    C.d = {}
    for name, shp in shapes.items():
        C.d[name] = nc.dram_tensor(name, list(shp), F32, kind="ExternalInput").ap()
    kind = lambda n: "ExternalOutput" if n in debug_out else "Internal"
    C.out = nc.dram_tensor("out", [T, D], F32, kind="ExternalOutput").ap()
    C.ha = nc.dram_tensor("s_ha", [T, D], F32, kind=kind("s_ha")).ap()
    C.hb = nc.dram_tensor("s_hb", [T, D], F32, kind=kind("s_hb")).ap()
    C.hTa = nc.dram_tensor("s_hTa", [D, T], BF16, kind=kind("s_hTa")).ap()
    C.hTb = nc.dram_tensor("s_hTb", [D, T], BF16, kind=kind("s_hTb")).ap()
    C.qT = nc.dram_tensor("s_qT", [512, T], BF16, kind=kind("s_qT")).ap()
    C.kT = nc.dram_tensor("s_kT", [512, T], BF16, kind=kind("s_kT")).ap()
    C.v = nc.dram_tensor("s_v", [T, 512], BF16, kind=kind("s_v")).ap()
    C.ycT = nc.dram_tensor("s_ycT", [D, T], BF16, kind=kind("s_ycT")).ap()
    C.ybuf = nc.dram_tensor("s_ybuf", [NE * CAP, D], F32, kind=kind("s_ybuf")).ap()
    C.Dout, C.Dha, C.Dhb, C.DhTa, C.DhTb, C.Dv, C.Dyb = TT(None), TT(None), TT(None), TT(None), TT(None), TT(None), TT(None)
    C.DqT = [TT(None) for _ in range(4)]
    C.DkT = [TT(None) for _ in range(4)]
    C.DycT = [TT(None) for _ in range(8)]
    with ExitStack() as es:
        P = Prog(nc, es)
        C.P = P
        sb = lambda n, s, d: es.enter_context(nc.sbuf_tensor(uniq(n), s, d))
        ps = es.enter_context(nc.psum_tensor("ps", [128, 8, 512], F32))
        C.ps = ps
        C.PS = [TT(None, f"bank{i}", excl=True) for i in range(8)]
        C.bank = lambda b: ps[:, b, :]
        C.ps2 = lambda b: ps[:, b:b + 2, :].rearrange("p a b -> p (a b)")
        C.ps2v = lambda b, a, w: ps[:, b:b + 2, :].rearrange("p a b -> p (a b)").rearrange("p (a w) -> p a w", w=w)
        C.ident = TT(sb("k_ident", [128, 128], F32))
        C.negU = TT(sb("k_negU", [128, 128], BF16))
        C.negO = TT(sb("k_negO", [128, 128], BF16))
        C.lowI = TT(sb("k_lowI", [128, 128], BF16))
        C.ones = TT(sb("k_ones", [128, 128], BF16))
        C.masks = TT(sb("k_masks", [128, 4, 512], BF16))
        C.gate = TT(sb("k_gate", [128, NT, NE], F32))
        C.sel = TT(sb("k_sel", [128, NT, NE], F32))
        C.slotf = TT(sb("k_slotf", [128, 2, NT], F32))
        C.sloti = TT(sb("k_sloti", [128, 2, NT], U32))
        C.gsel = TT(sb("k_gsel", [128, 2, NT], F32))
        C.toki = TT(sb("k_toki", [128, NE, NST], U32))
        sem_k = P.dma_sem("k_c", sw=True)
        sem_k2 = P.dma_sem("k_c2")
        P.dma("sync", C.ident.t[:, :], C.d["c_ident"], sem_k2, W=[C.ident])
        P.dma("pool", C.negU.t[:, :], C.d["c_negU"], sem_k, W=[C.negU])
        P.dma("pool", C.negO.t[:, :], C.d["c_negO"], sem_k, W=[C.negO])
        P.dma("pool", C.lowI.t[:, :], C.d["c_lowI"], sem_k, W=[C.lowI])
        P.dma("pool", C.ones.t[:, :], C.d["c_ones"], sem_k, W=[C.ones])
        P.dma("pool", C.masks.t[:, :, :], C.d["c_masks"], sem_k, W=[C.masks])
        P.batch(sem_k, [C.negU, C.negO, C.lowI, C.ones, C.masks])

        def want(p):
            if phases == "all":
                return not (len(p) == 3 and p[2] in "ab")
            return p in phases

        if want("A"):
            phase_ln_in(C)
        for l in range(DEPTH):
            final = (l == DEPTH - 1)
            if want(f"B{l}"):
                phase_proj(C, l)
            if want(f"C{l}"):
                phase_attn(C, l)
            if want(f"D{l}"):
                phase_mix_ln1(C, l)
            if want(f"E{l}") or want(f"E{l}a") or want(f"E{l}b"):
                if l % 2 == 0:
                    phase_ffn_dense(C, l, final)
                else:
                    phase_moe(C, l, final, stop=(1 if want(f"E{l}a") else 2 if want(f"E{l}b") else 3))
        if "k_dbg" in debug_out:
            sem_d = P.dma_sem("dbg")
            for nm, tt_, shp, dt in (("k_gate", C.gate, [128, NT, NE], F32), ("k_sel", C.sel, [128, NT, NE], F32),
                                     ("k_slotf", C.slotf, [128, 2, NT], F32), ("k_sloti", C.sloti, [128, 2, NT], U32),
                                     ("k_gsel", C.gsel, [128, 2, NT], F32), ("k_toki", C.toki, [128, NE, NST], U32)):
                dd_ = nc.dram_tensor("dbg_" + nm, shp, dt, kind="ExternalOutput").ap()
                P.dma("sync", dd_, tt_.t[:, :, :], sem_d, R=[tt_])
        P.barrier()
        print("bass ops:", P.nops, {e: len(P.q[e]) for e in ENG})
        P.flush()
    return nc


_CACHE = {}


def kernel(**inputs):
    lay = host_layout(inputs)
    x = np.ascontiguousarray(inputs["x"], dtype=np.float32)
    shapes = {"x": (T, D)}
    for k, v in lay.items():
        shapes[k] = v.shape
    if "nc" not in _CACHE:
        import os
        dbg = ("s_ha", "s_hb", "s_hTa", "s_hTb", "s_qT", "s_kT", "s_v", "s_ycT", "s_ybuf") if os.environ.get("SCRATCH_OUT") == "1" else ()
        _CACHE["nc"] = build(shapes, debug_out=dbg)
    nc = _CACHE["nc"]
    in_maps = []
    for b in range(NCORES):
        m = {"x": x[b]}
        m.update(lay)
        in_maps.append(m)
    res = run_bass_kernel_spmd(nc, in_maps, core_ids=list(range(NCORES)))
    out = np.stack([np.asarray(res.results[b]["out"], dtype=np.float32) for b in range(NCORES)], axis=0)
    return out
```

```python
import numpy as np
from contextlib import ExitStack
import concourse.bass as bass
import concourse.mybir as mybir
from concourse.bass_utils import run_bass_kernel_spmd

F32 = mybir.dt.float32
BF16 = mybir.dt.bfloat16
I32 = mybir.dt.int32
U32 = mybir.dt.uint32
AF = mybir.ActivationFunctionType
ALU = mybir.AluOpType
AX = mybir.AxisListType

NCORES = 8
T = 4096
D = 1024
NT = T // 128
DEPTH = 2
DFF = 2816
NFC = DFF // 128
NE = 8
CAP = 1280
NST = CAP // 128
ALPHA = float((2 * DEPTH) ** 0.25)
EPS = 1e-5
ENG = ("sync", "act", "pe", "dve", "pool")


_UNIQ = [0]


def uniq(n):
    _UNIQ[0] += 1
    return f"{n}_{_UNIQ[0]}"


def I(meth, *a, **k):
    return lambda e: getattr(e, meth)(*a, **k)


class TT:
    __slots__ = ("t", "w", "r", "name", "excl")

    def __init__(self, t, name="", excl=False):
        self.t = t
        self.w = None
        self.r = {}
        self.name = name
        self.excl = excl


class Prog:
    def __init__(self, nc, es):
        self.nc = nc
        self.es = es
        self.q = {e: [] for e in ENG}
        self.waited = {e: {} for e in ENG}
        self.psem = {e: self._mksem("p_" + e) for e in ENG}
        self.dsems = []
        self.dpool_sw = []
        self.dpool_hw = []
        self.nops = 0

    def _mksem(self, name):
        h = self.es.enter_context(self.nc.semaphore(name))
        return {"h": h, "n": 0, "name": name}

    def dma_sem(self, name, sw=False):
        pool = self.dpool_sw if sw else self.dpool_hw
        if pool:
            s = pool.pop()
        else:
            s = self._mksem(uniq(name))
            s["sw"] = sw
        self.dsems.append(s)
        return s

    def _wait(self, eng, dep):
        s, v = dep
        if eng == "pe" and s is self.psem["pe"]:
            return
        if self.waited[eng].get(s["name"], 0) >= v:
            return
        self.waited[eng][s["name"]] = v
        self.q[eng].append(("wait", s["h"], v))

    def _deps(self, R, W):
        deps = []
        for b in R:
            if b.w is not None:
                deps.append(b.w)
        for b in W:
            if b.w is not None:
                deps.append(b.w)
            deps.extend(b.r.values())
        return deps

    def _mark(self, dep, R, W):
        for b in R:
            b.r[dep[0]["name"]] = dep
        for b in W:
            b.w = dep
            b.r = {}

    def op(self, eng, fn, R=(), W=()):
        if any(b.excl for b in R):
            W = list(W) + [b for b in R if b.excl and b not in W]
            R = [b for b in R if not b.excl]
        for d in self._deps(R, W):
            self._wait(eng, d)
        s = self.psem[eng]
        s["n"] += 1
        dep = (s, s["n"])
        self.q[eng].append(("op", fn, s["h"], 1))
        self._mark(dep, R, W)
        self.nops += 1
        return dep

    def dma(self, eng, out, in_, sem, R=(), W=()):
        assert bool(sem.get("sw")) == (eng == "pool"), (eng, sem["name"])
        for d in self._deps(R, W):
            if d[0] is sem:
                continue
            self._wait(eng, d)
        sem["n"] += 16
        dep = (sem, sem["n"])
        self.q[eng].append(("op", lambda e: e.dma_start(out=out, in_=in_), sem["h"], 16))
        self._mark(dep, R, W)
        self.nops += 1
        return dep

    def batch(self, sem, tts):
        for b in tts:
            b.w = (sem, sem["n"])

    def gather(self, out, in_, idx_ap, sem, R=(), W=()):
        eng = "pool"
        for d in self._deps(R, W):
            self._wait(eng, d)
        sem["n"] += 16
        dep = (sem, sem["n"])
        self.q[eng].append(("op", lambda e: e.indirect_dma_start(
            out=out, out_offset=None, in_=in_,
            in_offset=bass.IndirectOffsetOnAxis(ap=idx_ap, axis=0)), sem["h"], 16))
        self._mark(dep, R, W)
        self.nops += 1
        return dep

    def barrier(self):
        sems = list(self.psem.values()) + self.dsems
        for e in ENG:
            for s in sems:
                if s["n"] > 0:
                    self._wait(e, (s, s["n"]))
        for s_ in self.dsems:
            (self.dpool_sw if s_.get("sw") else self.dpool_hw).append(s_)
        self.dsems = []
        for e in ENG:
            if self.psem[e]["n"] > 8000:
                self.psem[e] = self._mksem(uniq("p_" + e))

    def flush(self):
        nc = self.nc
        q = self.q

        def run(e, lst):
            for it in lst:
                if it[0] == "wait":
                    e.wait_ge(it[1], it[2])
                else:
                    ins = it[1](e)
                    ins.then_inc(it[2], it[3])

        with nc.Block() as block:
            @block.sync
            def _(e):
                run(e, q["sync"])

            @block.scalar
            def _(e):
                run(e, q["act"])

            @block.tensor
            def _(e):
                run(e, q["pe"])

            @block.vector
            def _(e):
                run(e, q["dve"])

            @block.gpsimd
            def _(e):
                run(e, q["pool"])


class Ctx:
    pass


def bcast_row(ap1d, n):
    return bass.AP(ap1d.tensor, ap1d.offset, [[0, 128], [1, n]])


def alloc_ln(C, es, want_T=True):
    nc, P = C.nc, C.P
    L = Ctx()
    sb = lambda n, s, d: es.enter_context(nc.sbuf_tensor(uniq(n), s, d))
    L.stats = [TT(sb(f"ln_stats{i}", [128, 12], F32)) for i in range(2)]
    L.mv = [TT(sb(f"ln_mv{i}", [128, 8], F32)) for i in range(2)]
    L.xn = [TT(sb(f"ln_xn{i}", [128, D], F32)) for i in range(2)]
    L.ho = [TT(sb(f"ln_ho{i}", [128, D], F32)) for i in range(2)]
    L.want_T = want_T
    if want_T:
        L.hTs = [TT(sb(f"ln_hTs{i}", [128, 8, 512], BF16)) for i in range(2)]
    L.sem_st = [P.dma_sem("ln_st0"), P.dma_sem("ln_st1")]
    L.sem_sT = [P.dma_sem("ln_sT0"), P.dma_sem("ln_sT1")]
    L.cnt = 0
    L.pend = []
    return L


def ln_tile(C, L, tt, pre, gam, bet, h_dst, hT_dst, tr_banks=None, router=None):
    i = L.cnt
    L.cnt += 1
    L.pend.append(dict(i=i, tt=tt, pre=pre, gam=gam, bet=bet, h_dst=h_dst, hT_dst=hT_dst, tr=tr_banks, router=router))
    _ln_step(C, L)


def ln_flush(C, L):
    for _ in range(2):
        L.pend.append(None)
        _ln_step(C, L)
    L.pend = []


def _ln_step(C, L):
    P = C.P
    n = len(L.pend)
    t0 = L.pend[n - 1]
    t1 = L.pend[n - 2] if n >= 2 else None
    t2 = L.pend[n - 3] if n >= 3 else None
    if t0 is not None:
        b = t0["i"] % 2
        st, mv, pre = L.stats[b], L.mv[b], t0["pre"]
        P.op("dve", I("bn_stats", st.t[:, 0:6], pre.t[:, 0:512]), R=[pre], W=[st])
        P.op("dve", I("bn_stats", st.t[:, 6:12], pre.t[:, 512:1024]), R=[pre], W=[st])
        P.op("dve", I("bn_aggr", mv.t[:, 0:2], st.t[:, :]), R=[st], W=[mv])
        P.op("dve", I("tensor_scalar_add", mv.t[:, 2:3], mv.t[:, 1:2], EPS), R=[mv], W=[mv])
    if t0 is not None:
        b = t0["i"] % 2
        mv = L.mv[b]
        P.op("act", I("activation", out=mv.t[:, 3:4], in_=mv.t[:, 2:3], func=AF.Ln), R=[mv], W=[mv])
        P.op("act", I("activation", out=mv.t[:, 4:5], in_=mv.t[:, 3:4], func=AF.Exp, scale=-0.5), R=[mv], W=[mv])
    if t1 is not None:
        b = t1["i"] % 2
        mv, xn, ho, pre = L.mv[b], L.xn[b], L.ho[b], t1["pre"]
        P.op("dve", I("scalar_tensor_tensor", xn.t[:, :], pre.t[:, :], mv.t[:, 0:1], t1["gam"].t[:, :], op0=ALU.subtract, op1=ALU.mult),
             R=[pre, mv, t1["gam"]], W=[xn])
        P.op("dve", I("scalar_tensor_tensor", ho.t[:, :], xn.t[:, :], mv.t[:, 4:5], t1["bet"].t[:, :], op0=ALU.mult, op1=ALU.add),
             R=[xn, mv, t1["bet"]], W=[ho])
        P.dma("sync", t1["h_dst"][1], ho.t[:, :], L.sem_st[b], R=[ho], W=[t1["h_dst"][0]])
        if t1["router"] is not None:
            t1["router"](t1["tt"], ho)
    if t2 is not None and t2["hT_dst"] is not None:
        b = t2["i"] % 2
        ho = L.ho[b]
        tt = t2["tt"]
        slot = tt % 4
        hTs = L.hTs[(tt // 4) % 2]
        bk = t2["tr"]
        flat = C.ps2(bk)
        for k in range(8):
            P.op("pe", I("transpose", flat[:, k * 128:(k + 1) * 128], ho.t[:, k * 128:(k + 1) * 128], C.ident.t[:, :]),
                 R=[ho, C.ident], W=[C.PS[bk], C.PS[bk + 1]])
        P.op("act", I("activation", out=hTs.t[:, :, slot * 128:(slot + 1) * 128],
                      in_=C.ps2v(bk, 8, 128), func=AF.Copy),
             R=[C.PS[bk], C.PS[bk + 1]], W=[hTs])
        if slot == 3:
            c0 = (tt - 3) * 128
            dst = t2["hT_dst"][1].rearrange("(c p) t -> p c t", p=128)[:, :, c0:c0 + 512]
            P.dma("sync", dst, hTs.t[:, :, :], L.sem_sT[(tt // 4) % 2], R=[hTs], W=[t2["hT_dst"][0]])


def phase_ln_in(C):
    nc, P = C.nc, C.P
    with ExitStack() as es:
        sb = lambda n, s, d: es.enter_context(nc.sbuf_tensor(uniq(n), s, d))
        gam = TT(sb("a_gam", [128, D], F32))
        bet = TT(sb("a_bet", [128, D], F32))
        NXB = 4
        xin = [TT(sb(f"a_xin{i}", [128, D], F32)) for i in range(NXB)]
        sem_c = P.dma_sem("a_c")
        sem_x = [P.dma_sem(f"a_x{i}", sw=True) for i in range(NXB)]
        P.dma("sync", gam.t[:, :], bcast_row(C.d["ln_in_g"], D), sem_c, W=[gam])
        P.dma("sync", bet.t[:, :], bcast_row(C.d["ln_in_b"], D), sem_c, W=[bet])
        P.batch(sem_c, [gam, bet])
        L = alloc_ln(C, es)
        for tt in range(NT):
            b = tt % NXB
            P.dma("pool", xin[b].t[:, :], C.d["x"][tt * 128:(tt + 1) * 128, :], sem_x[b], W=[xin[b]])
            ln_tile(C, L, tt, xin[b], gam, bet, (C.Dha, C.ha[tt * 128:(tt + 1) * 128, :]), (C.DhTa, C.hTa), tr_banks=0 + 2 * (tt % 2))
        ln_flush(C, L)
        P.barrier()


def phase_proj(C, l):
    nc, P = C.nc, C.P
    with ExitStack() as es:
        sb = lambda n, s, d: es.enter_context(nc.sbuf_tensor(uniq(n), s, d))
        wi = TT(sb("b_wi", [128, 8, 2048], BF16))
        hT = [TT(sb(f"b_hT{k}", [128, T], BF16)) for k in range(8)]
        pw = TT(sb("b_pw", [128, 4, 128], BF16))
        psc = TT(sb("b_psc", [128, 4], F32))
        invc = TT(sb("b_invc", [128, 4, 16], F32))
        U = TT(sb("b_U", [128, 16 + T], F32))
        SA = TT(sb("b_SA", [128, 16 + T], F32))
        SB = TT(sb("b_SB", [128, 16 + T], F32))
        dd = TT(sb("b_d", [128, T], BF16))
        stg = [TT(sb(f"b_stg{i}", [128, T], BF16)) for i in range(2)]
        vst = [TT(sb(f"b_vst{i}", [128, 4, 512], BF16)) for i in range(2)]
        sem_w = P.dma_sem("b_w", sw=True)
        sem_h = P.dma_sem("b_h")
        sem_c = P.dma_sem("b_c")
        sem_cw = P.dma_sem("b_cw", sw=True)
        sem_s = [P.dma_sem("b_s0"), P.dma_sem("b_s1")]
        sem_v = [P.dma_sem("b_v0"), P.dma_sem("b_v1")]
        for q4 in range(4):
            P.dma("pool", wi.t[:, :, q4 * 512:(q4 + 1) * 512], C.d["w_in"][l][:, :, q4 * 512:(q4 + 1) * 512], sem_w, W=[wi])
        for k in range(8):
            P.dma("sync", hT[k].t[:, :], C.hTa[k * 128:(k + 1) * 128, :], sem_h, R=[C.DhTa], W=[hT[k]])
        P.dma("pool", pw.t[:, :, :], C.d["pool_w"][l], sem_cw, W=[pw])
        P.dma("sync", psc.t[:, :], C.d["pool_scale"][l], sem_c, W=[psc])
        P.dma("sync", invc.t[:, :, :], C.d["c_invc"], sem_c, W=[invc])
        P.batch(sem_c, [psc, invc])
        P.batch(sem_h, hT)
        for buf in (U, SA, SB):
            P.op("dve", I("memset", buf.t[:, 0:16], 0.0), W=[buf])

        nbank = [0]

        def next_bank():
            b = nbank[0] % 4
            nbank[0] += 1
            return b

        evac_eng = [0]

        def evac(out_ap, in_ap, R, W, scale=None, only_act=False):
            e = 0 if only_act else evac_eng[0] % 2
            evac_eng[0] += 1
            if e == 0:
                if scale is None:
                    P.op("act", I("activation", out=out_ap, in_=in_ap, func=AF.Copy), R=R, W=W)
                else:
                    P.op("act", I("activation", out=out_ap, in_=in_ap, func=AF.Identity, scale=scale), R=R, W=W)
            else:
                if scale is None:
                    P.op("dve", I("tensor_copy", out_ap, in_ap), R=R, W=W)
                else:
                    P.op("dve", I("tensor_scalar_mul", out_ap, in_ap, scale), R=R, W=W)

        def fm_chunk(c, out_fn):
            for tg in range(8):
                bk = next_bank()
                for k in range(8):
                    P.op("pe", I("matmul", C.bank(bk), wi.t[:, k, c * 128:(c + 1) * 128], hT[k].t[:, tg * 512:(tg + 1) * 512],
                                 start=(k == 0), stop=(k == 7)), R=[wi, hT[k]], W=[C.PS[bk]])
                out_fn(tg, bk)

        def qk_chunk(c, only_act=False):
            so = stg[sidx[0] % 2]
            ss = sem_s[sidx[0] % 2]
            sidx[0] += 1
            sc = 0.125 if c < 8 else None
            fm_chunk(c, lambda tg, bk, so=so, sc=sc: evac(so.t[:, tg * 512:(tg + 1) * 512], C.bank(bk), [C.PS[bk]], [so], scale=sc, only_act=only_act))
            if c < 8:
                P.dma("sync", C.qT[(c - 4) * 128:(c - 3) * 128, :], so.t[:, :], ss, R=[so], W=[C.DqT[c - 4]])
            else:
                P.dma("sync", C.kT[(c - 8) * 128:(c - 7) * 128, :], so.t[:, :], ss, R=[so], W=[C.DkT[c - 8]])

        sidx = [0]
        for g in range(4):
            w = 2 << g
            fm_chunk(g, lambda tg, bk: evac(U.t[:, 16 + tg * 512:16 + (tg + 1) * 512], C.bank(bk), [C.PS[bk]], [U]))
            src = U
            bufs = [SA, SB]
            nlev = g + 1
            for lev in range(nlev):
                dst = bufs[lev % 2]
                sh = 1 << lev
                eng = "dve"
                P.op(eng, I("tensor_tensor", dst.t[:, 16:16 + T], src.t[:, 16:16 + T], src.t[:, 16 - sh:16 + T - sh], op=ALU.add),
                     R=[src], W=[dst])
                src = dst
            P.op("dve", I("scalar_tensor_tensor", dd.t[:, :], src.t[:, 16:16 + T], 1.0 / w, U.t[:, 16:16 + T],
                          op0=ALU.mult, op1=ALU.subtract), R=[src, U], W=[dd])
            P.op("dve", I("tensor_tensor", SA.t[:, 0:16] if src is SB else SB.t[:, 0:16], src.t[:, 16:32], invc.t[:, g, :], op=ALU.mult),
                 R=[src, invc], W=[SA if src is SB else SB])
            tmp = SA if src is SB else SB
            P.op("dve", I("tensor_tensor", dd.t[:, 0:16], tmp.t[:, 0:16], U.t[:, 16:32], op=ALU.subtract),
                 R=[tmp, U], W=[dd])
            P.op("dve", I("memset", tmp.t[:, 0:16], 0.0), W=[tmp])
            qk_chunk(4 + 2 * g, only_act=True)
            qk_chunk(5 + 2 * g, only_act=True)
            so = stg[sidx[0] % 2]
            ss = sem_s[sidx[0] % 2]
            sidx[0] += 1
            for tg in range(8):
                bk = next_bank()
                P.op("pe", I("matmul", C.bank(bk), pw.t[:, g, :], dd.t[:, tg * 512:(tg + 1) * 512], start=True, stop=True),
                     R=[pw, dd], W=[C.PS[bk]])
                evac(so.t[:, tg * 512:(tg + 1) * 512], C.bank(bk), [C.PS[bk], psc], [so], scale=psc.t[:, g:g + 1])
            P.dma("sync", C.ycT[g * 128:(g + 1) * 128, :], so.t[:, :], ss, R=[so], W=[C.DycT[g]])
        vv = C.v.rearrange("(n p) c -> p n c", p=128)
        for tt in range(NT):
            bk = next_bank()
            for k in range(8):
                P.op("pe", I("matmul", C.bank(bk), hT[k].t[:, tt * 128:(tt + 1) * 128], wi.t[:, k, 1536:2048],
                             start=(k == 0), stop=(k == 7)), R=[wi, hT[k]], W=[C.PS[bk]])
            vs = vst[(tt // 4) % 2]
            evac(vs.t[:, tt % 4, :], C.bank(bk), [C.PS[bk]], [vs])
            if tt % 4 == 3:
                P.dma("sync", vv[:, tt - 3:tt + 1, :], vs.t[:, :, :], sem_v[(tt // 4) % 2], R=[vs], W=[C.Dv])
        P.barrier()


def phase_attn(C, l):
    nc, P = C.nc, C.P
    with ExitStack() as es:
        sb = lambda n, s, d: es.enter_context(nc.sbuf_tensor(uniq(n), s, d))
        qT = [TT(sb(f"c_qT{i}", [128, T], BF16)) for i in range(4)]
        kT = [TT(sb(f"c_kT{i}", [128, T], BF16)) for i in range(4)]
        V = TT(sb("c_V", [128, NT, 512], BF16))
        E = [TT(sb(f"c_E{i}", [128, 2048], F32)) for i in range(2)]
        SP = [TT(sb(f"c_SP{i}", [128, 2048], BF16)) for i in range(4)]
        CA = [TT(sb(f"c_CA{i}", [128, 2048], BF16)) for i in range(4)]
        AT = [TT(sb(f"c_AT{i}", [128, 2048], BF16)) for i in range(3)]
        ys = [TT(sb(f"c_ys{i}", [128, T], BF16)) for i in range(2)]
        sem_l = P.dma_sem("c_l")
        sem_s = [P.dma_sem("c_s0"), P.dma_sem("c_s1")]
        sem_l0 = P.dma_sem("c_l0")
        sem_lv = P.dma_sem("c_lv")
        vv = C.v.rearrange("(n p) c -> p n c", p=128)
        P.dma("sync", qT[0].t[:, :], C.qT[0:128, :], sem_l0, R=[C.DqT[0]], W=[qT[0]])
        P.dma("sync", kT[0].t[:, :], C.kT[0:128, :], sem_l0, R=[C.DkT[0]], W=[kT[0]])
        P.batch(sem_l0, [qT[0], kT[0]])
        for i in range(4):
            P.dma("sync", V.t[:, i * 8:(i + 1) * 8, :], vv[:, i * 8:(i + 1) * 8, :], sem_lv, R=[C.Dv], W=[V])
        for i in range(1, 4):
            P.dma("sync", qT[i].t[:, :], C.qT[i * 128:(i + 1) * 128, :], sem_l, R=[C.DqT[i]], W=[qT[i]])
            P.dma("sync", kT[i].t[:, :], C.kT[i * 128:(i + 1) * 128, :], sem_l, R=[C.DkT[i]], W=[kT[i]])
        P.batch(sem_l, qT[1:] + kT[1:])

        ZP = [0, 2]
        BP = 4
        OB = [6, 7]
        quads = []
        for hp in range(4):
            for e in range(2):
                for g in range(8):
                    nq = g + 1
                    for qi in range(nq):
                        js = [4 * g + 3 - 4 * qi - k for k in range(4)]
                        quads.append(dict(hp=hp, e=e, g=g, js=js, first=(qi == 0), last=(qi == nq - 1)))
        n = len(quads)
        masks2 = C.masks.t[:, :, :].rearrange("p a b -> p (a b)")

        def zq(s):
            q = quads[s]
            hp, e, g = q["hp"], q["e"], q["g"]
            pr = slice(64 * e, 64 * e + 64)
            for k in range(4):
                bk = ZP[k // 2] + k % 2
                j = q["js"][k]
                P.op("pe", I("matmul", C.bank(bk), kT[hp].t[pr, j * 128:(j + 1) * 128], qT[hp].t[pr, g * 512:(g + 1) * 512],
                             start=True, stop=True), R=[kT[hp], qT[hp]], W=[C.PS[bk]])

        def act1(s):
            Eb = E[s % 2]
            z4 = C.ps[:, 0:4, :].rearrange("p a b -> p (a b)")
            P.op("act", I("activation", out=Eb.t[:, :], in_=z4, func=AF.Exp),
                 R=[C.PS[0], C.PS[1], C.PS[2], C.PS[3]], W=[Eb])

        def act2(s):
            P.op("act", I("activation", out=SP[s % 4].t[:, :], in_=E[s % 2].t[:, :], func=AF.Ln, bias=1.0), R=[E[s % 2]], W=[SP[s % 4]])

        def dve3(s):
            q = quads[s]
            S, Cq, Cn = SP[s % 4], CA[s % 4], CA[(s + 1) % 4]
            sl = lambda k: slice(k * 512, (k + 1) * 512)
            if q["first"]:
                P.op("dve", I("tensor_tensor", S.t[:, :], S.t[:, :], masks2, op=ALU.mult), R=[S, C.masks], W=[S])
                P.op("dve", I("tensor_tensor", Cq.t[:, sl(2)], S.t[:, sl(0)], S.t[:, sl(1)], op=ALU.add), R=[S], W=[Cq])
            else:
                P.op("dve", I("tensor_tensor", Cq.t[:, sl(1)], Cq.t[:, sl(0)], S.t[:, sl(0)], op=ALU.add), R=[S, Cq], W=[Cq])
                P.op("dve", I("tensor_tensor", Cq.t[:, sl(2)], Cq.t[:, sl(1)], S.t[:, sl(1)], op=ALU.add), R=[S, Cq], W=[Cq])
            P.op("dve", I("tensor_tensor", Cq.t[:, sl(3)], Cq.t[:, sl(2)], S.t[:, sl(2)], op=ALU.add), R=[S, Cq], W=[Cq])
            if not q["last"]:
                P.op("dve", I("tensor_tensor", Cn.t[:, sl(0)], Cq.t[:, sl(3)], S.t[:, sl(3)], op=ALU.add), R=[S, Cq], W=[Cn])

        def bq(s, p):
            q = quads[s]
            hp, e, g = q["hp"], q["e"], q["g"]
            pr = slice(64 * e, 64 * e + 64)
            S, Cq = SP[s % 4], CA[s % 4]
            for k in (2 * p, 2 * p + 1):
                bk = BP + k % 2
                j = q["js"][k]
                if q["first"] and k == 0:
                    car = None
                elif q["first"] and k == 1:
                    car = (S, S.t[:, 0:512])
                else:
                    car = (Cq, Cq.t[:, k * 512:(k + 1) * 512])
                P.op("pe", I("matmul", C.bank(bk), kT[hp].t[pr, j * 128:(j + 1) * 128], qT[hp].t[pr, g * 512:(g + 1) * 512],
                             start=True, stop=False), R=[kT[hp], qT[hp]], W=[C.PS[bk]])
                P.op("pe", I("matmul", C.bank(bk), C.negU.t[:, :], S.t[:, k * 512:(k + 1) * 512], start=False, stop=(car is None)),
                     R=[C.negU, S], W=[C.PS[bk]])
                if car is not None:
                    P.op("pe", I("matmul", C.bank(bk), C.negO.t[:, :], car[1], start=False, stop=True),
                         R=[C.negO, car[0]], W=[C.PS[bk]])

        def act3(s, p):
            A = AT[s % 3]
            P.op("act", I("activation", out=A.t[:, p * 1024:(p + 1) * 1024], in_=C.ps2(BP), func=AF.Exp),
                 R=[C.PS[BP], C.PS[BP + 1]], W=[A])

        def mask_att(s):
            q = quads[s]
            if q["first"]:
                A = AT[s % 3]
                P.op("dve", I("tensor_tensor", A.t[:, :], A.t[:, :], masks2, op=ALU.mult), R=[A, C.masks], W=[A])

        def av(s):
            q = quads[s]
            hp, e, g = q["hp"], q["e"], q["g"]
            gi = (hp * 2 + e) * 8 + g
            ob = OB[gi % 2]
            A = AT[s % 3]
            pr = slice(64 * e, 64 * e + 64)
            for k in range(4):
                j = q["js"][k]
                P.op("pe", I("matmul", C.bank(ob)[pr, :], V.t[:, j, hp * 128 + 64 * e: hp * 128 + 64 * e + 64], A.t[:, k * 512:(k + 1) * 512],
                             start=(q["first"] and k == 0), stop=(q["last"] and k == 3)), R=[V, A], W=[C.PS[ob]])
            if q["last"]:
                yb = ys[hp % 2]
                P.op("dve", I("tensor_copy", yb.t[pr, g * 512:(g + 1) * 512], C.bank(ob)[pr, :]), R=[C.PS[ob]], W=[yb])
                if e == 1 and g == 7:
                    P.dma("sync", C.ycT[512 + hp * 128:512 + (hp + 1) * 128, :], yb.t[:, :], sem_s[hp % 2], R=[yb], W=[C.DycT[4 + hp]])

        for s in range(n + 3):
            if s < n:
                zq(s)
            if 0 <= s - 2 < n:
                bq(s - 2, 0)
                act3(s - 2, 0)
            if s < n:
                act1(s)
            if 0 <= s - 3 < n:
                av(s - 3)
            if 0 <= s - 2 < n:
                bq(s - 2, 1)
                act3(s - 2, 1)
            if s < n:
                act2(s)
                dve3(s)
            if 0 <= s - 2 < n:
                mask_att(s - 2)
        P.barrier()


def phase_mix_ln1(C, l):
    nc, P = C.nc, C.P
    moe = (l % 2 == 1)
    with ExitStack() as es:
        sb = lambda n, s, d: es.enter_context(nc.sbuf_tensor(uniq(n), s, d))
        yc = [TT(sb(f"d_yc{k}", [128, T], BF16)) for k in range(8)]
        wo = TT(sb("d_wo", [128, 8, D], BF16))
        gam = TT(sb("d_gam", [128, D], F32))
        bet = TT(sb("d_bet", [128, D], F32))
        hin = [TT(sb(f"d_hin{i}", [128, D], F32)) for i in range(4)]
        pre = [TT(sb(f"d_pre{i}", [128, D], F32)) for i in range(2)]
        sem_c = P.dma_sem("d_c")
        sem_cw = P.dma_sem("d_cw", sw=True)
        sem_y = P.dma_sem("d_y")
        sem_h = [P.dma_sem(f"d_h{i}", sw=True) for i in range(4)]
        P.dma("pool", wo.t[:, :, :], C.d["w_out"][l], sem_cw, W=[wo])
        P.dma("sync", gam.t[:, :], bcast_row(C.d["ln1_g"][l], D), sem_c, W=[gam])
        P.dma("sync", bet.t[:, :], bcast_row(C.d["ln1_b"][l], D), sem_c, W=[bet])
        for k in range(8):
            P.dma("sync", yc[k].t[:, :], C.ycT[k * 128:(k + 1) * 128, :], sem_y, R=[C.DycT[k]], W=[yc[k]])
        P.batch(sem_y, yc)
        L = alloc_ln(C, es)
        router = None
        P.batch(sem_c, [gam, bet])
        if moe:
            rt = TT(sb("d_rt", [128, 8, NE], F32))
            P.dma("sync", rt.t[:, :, :], C.d["moe_router"][l // 2], sem_c, W=[rt])
            P.batch(sem_c, [gam, bet, rt])
            hT32 = [TT(sb(f"d_hT32{i}", [128, 8, 128], F32)) for i in range(2)]
            lgt = TT(sb("d_lgt", [128, NT, NE], F32))
            wk = TT(sb("d_wk", [128, NT, NE], F32))
            m1 = TT(sb("d_m1", [128, NT], F32))
            m2 = TT(sb("d_m2", [128, NT], F32))

            def router(tt, ho):
                b = tt % 2
                bk = 4 + 2 * b
                flat = C.ps2(bk)
                for k in range(8):
                    P.op("pe", I("transpose", flat[:, k * 128:(k + 1) * 128], ho.t[:, k * 128:(k + 1) * 128], C.ident.t[:, :]),
                         R=[ho, C.ident], W=[C.PS[bk], C.PS[bk + 1]])
                P.op("dve", I("tensor_copy", hT32[b].t[:, :, :], C.ps2v(bk, 8, 128)), R=[C.PS[bk], C.PS[bk + 1]], W=[hT32[b]])
                lb = C.bank(bk)[:, 0:NE]
                for k in range(8):
                    P.op("pe", I("matmul", lb, hT32[b].t[:, k, :], rt.t[:, k, :], start=(k == 0), stop=(k == 7)),
                         R=[hT32[b], rt], W=[C.PS[bk], C.PS[bk + 1]])
                P.op("dve", I("tensor_copy", lgt.t[:, tt, :], lb), R=[C.PS[bk], C.PS[bk + 1]], W=[lgt])

            def router_finish():
                fl = lambda t_: t_.t[:, :, :].rearrange("p a b -> p (a b)")
                P.op("dve", I("tensor_reduce", m1.t[:, :], lgt.t[:, :, :], AX.X, ALU.max), R=[lgt], W=[m1])
                for e_ in range(NE):
                    P.op("dve", I("tensor_tensor", wk.t[:, :, e_], lgt.t[:, :, e_], m1.t[:, :], op=ALU.is_equal), R=[lgt, m1], W=[wk])
                P.op("dve", I("scalar_tensor_tensor", fl(wk), fl(wk), -1e30, fl(lgt), op0=ALU.mult, op1=ALU.add), R=[wk, lgt], W=[wk])
                P.op("dve", I("tensor_reduce", m2.t[:, :], wk.t[:, :, :], AX.X, ALU.max), R=[wk], W=[m2])
                for e_ in range(NE):
                    P.op("dve", I("tensor_tensor", wk.t[:, :, e_], lgt.t[:, :, e_], m1.t[:, :], op=ALU.subtract), R=[lgt, m1], W=[wk])
                P.op("act", I("activation", out=fl(wk), in_=fl(wk), func=AF.Exp), R=[wk], W=[wk])
                for e_ in range(NE):
                    P.op("dve", I("tensor_tensor", C.sel.t[:, :, e_], lgt.t[:, :, e_], m2.t[:, :], op=ALU.is_ge), R=[lgt, m2], W=[C.sel])
                P.op("dve", I("tensor_tensor", fl(wk), fl(wk), fl(C.sel), op=ALU.mult), R=[wk, C.sel], W=[wk])
                P.op("dve", I("tensor_reduce", m1.t[:, :], wk.t[:, :, :], AX.X, ALU.add), R=[wk], W=[m1])
                P.op("dve", I("reciprocal", m1.t[:, :], m1.t[:, :]), R=[m1], W=[m1])
                for e_ in range(NE):
                    P.op("dve", I("tensor_tensor", C.gate.t[:, :, e_], wk.t[:, :, e_], m1.t[:, :], op=ALU.mult), R=[wk, m1], W=[C.gate])

        for tt in range(NT):
            b = tt % 2
            hb_ = tt % 4
            P.dma("pool", hin[hb_].t[:, :], C.ha[tt * 128:(tt + 1) * 128, :], sem_h[hb_], R=[C.Dha], W=[hin[hb_]])
            bk = 0 + 2 * b
            for nh in range(2):
                for k in range(8):
                    P.op("pe", I("matmul", C.bank(bk + nh), yc[k].t[:, tt * 128:(tt + 1) * 128], wo.t[:, k, nh * 512:(nh + 1) * 512],
                                 start=(k == 0), stop=(k == 7)), R=[yc[k], wo], W=[C.PS[bk], C.PS[bk + 1]])
            P.op("dve", I("scalar_tensor_tensor", pre[b].t[:, :], hin[hb_].t[:, :], ALPHA, C.ps2(bk), op0=ALU.mult, op1=ALU.add),
                 R=[hin[hb_], C.PS[bk], C.PS[bk + 1]], W=[pre[b]])
            ln_tile(C, L, tt, pre[b], gam, bet, (C.Dhb, C.hb[tt * 128:(tt + 1) * 128, :]),
                    None if moe else (C.DhTb, C.hTb), tr_banks=4 + 2 * b, router=router)
        ln_flush(C, L)
        if moe:
            router_finish()
        P.barrier()


def alloc_ffn(C, es, ntok):
    nc, P = C.nc, C.P
    F = Ctx()
    sb = lambda n, s, d: es.enter_context(nc.sbuf_tensor(uniq(n), s, d))
    F.ntok = ntok
    F.wd = TT(sb("f_wd", [128, NFC, D], BF16))
    F.wg = [TT(sb(f"f_wg{i}", [128, 2, 8, 128], BF16)) for i in range(3)]
    F.wu = [TT(sb(f"f_wu{i}", [128, 2, 8, 128], BF16)) for i in range(3)]
    F.act = TT(sb("f_act", [128, NFC, ntok], BF16))
    F.sg = [TT(sb(f"f_sg{i}", [128, 512], BF16)) for i in range(2)]
    F.sem_wd = P.dma_sem("f_wd", sw=True)
    F.sem_wg = [P.dma_sem(f"f_wg{i}", sw=True) for i in range(3)]
    F.sem_wu = [P.dma_sem(f"f_wu{i}", sw=True) for i in range(3)]
    F.cnt = 0
    F.blk = 0
    return F


def ffn_pass(C, F, xT, wg_d, wu_d, wd_d, consume, GB=(0, 1), UB=(2, 3), DB=4, blk_hook=None):
    P = C.P
    ntok = F.ntok
    subs = []
    t0 = 0
    while t0 < ntok:
        w = min(512, ntok - t0)
        subs.append((t0, w))
        t0 += w
    wdv = wd_d.rearrange("(c p) n -> p c n", p=128)
    for fb in range(NFC // 2):
        if fb == 2:
            P.dma("pool", F.wd.t[:, 0:11, :], wdv[:, 0:11, :], F.sem_wd, W=[F.wd])
            P.dma("pool", F.wd.t[:, 11:22, :], wdv[:, 11:22, :], F.sem_wd, W=[F.wd])
        bi = F.blk % 3
        F.blk += 1
        wg, wu = F.wg[bi], F.wu[bi]
        P.dma("pool", wg.t[:, :, :, :], wg_d[2 * fb:2 * fb + 2].rearrange("c p k f -> p c k f"), F.sem_wg[bi], W=[wg])
        P.dma("pool", wu.t[:, :, :, :], wu_d[2 * fb:2 * fb + 2].rearrange("c p k f -> p c k f"), F.sem_wu[bi], W=[wu])
        for ci in range(2):
            fc = 2 * fb + ci
            for (t0, w) in subs:
                i = F.cnt
                F.cnt += 1
                gb, ub = GB[i % 2], UB[i % 2]
                for k in range(8):
                    P.op("pe", I("matmul", C.bank(gb)[:, 0:w], wg.t[:, ci, k, :], xT[k].t[:, t0:t0 + w], start=(k == 0), stop=(k == 7)),
                         R=[wg, xT[k]], W=[C.PS[gb]])
                for k in range(8):
                    P.op("pe", I("matmul", C.bank(ub)[:, 0:w], wu.t[:, ci, k, :], xT[k].t[:, t0:t0 + w], start=(k == 0), stop=(k == 7)),
                         R=[wu, xT[k]], W=[C.PS[ub]])
                sg = F.sg[i % 2]
                P.op("act", I("activation", out=sg.t[:, 0:w], in_=C.bank(gb)[:, 0:w], func=AF.Silu), R=[C.PS[gb]], W=[sg])
                P.op("dve", I("tensor_tensor", F.act.t[:, fc, t0:t0 + w], sg.t[:, 0:w], C.bank(ub)[:, 0:w], op=ALU.mult),
                     R=[sg, C.PS[ub]], W=[F.act])
        if blk_hook is not None:
            blk_hook(fb)
    dpairs = (DB, GB[0], UB[0])
    for tt in range(ntok // 128):
        db = dpairs[tt % 3]
        for nh in range(2):
            for fc in range(NFC):
                P.op("pe", I("matmul", C.bank(db + nh), F.act.t[:, fc, tt * 128:(tt + 1) * 128], F.wd.t[:, fc, nh * 512:(nh + 1) * 512],
                             start=(fc == 0), stop=(fc == NFC - 1)), R=[F.act, F.wd], W=[C.PS[db], C.PS[db + 1]])
        consume(tt, db)


def phase_ffn_dense(C, l, final):
    nc, P = C.nc, C.P
    TG = 1024
    with ExitStack() as es:
        sb = lambda n, s, d: es.enter_context(nc.sbuf_tensor(uniq(n), s, d))
        gam = TT(sb("e_gam", [128, D], F32))
        bet = TT(sb("e_bet", [128, D], F32))
        hin = [TT(sb(f"e_hin{i}", [128, D], F32)) for i in range(2)]
        pre = [TT(sb(f"e_pre{i}", [128, D], F32)) for i in range(2)]
        xT = [[TT(sb(f"e_xT{j}_{k}", [128, TG], BF16)) for k in range(8)] for j in range(1)]
        sem_c = P.dma_sem("e_c")
        sem_x = [P.dma_sem("e_x0"), P.dma_sem("e_x1")]
        sem_h = [P.dma_sem("e_h0", sw=True), P.dma_sem("e_h1", sw=True)]
        P.dma("sync", gam.t[:, :], bcast_row(C.d["ln2_g"][l], D), sem_c, W=[gam])
        P.dma("sync", bet.t[:, :], bcast_row(C.d["ln2_b"][l], D), sem_c, W=[bet])
        P.batch(sem_c, [gam, bet])
        L = alloc_ln(C, es, want_T=not final)
        F = alloc_ffn(C, es, TG)
        i = l // 2
        cnt = [0]
        for grp in range(T // TG):
            xb = xT[0]
            for k in range(8):
                P.dma("sync", xb[k].t[:, :], C.hTb[k * 128:(k + 1) * 128, grp * TG:(grp + 1) * TG], sem_x[grp % 2], R=[C.DhTb], W=[xb[k]])
            P.batch(sem_x[grp % 2], xb)

            def consume(tt, db, grp=grp):
                gt = grp * (TG // 128) + tt
                b = cnt[0] % 2
                cnt[0] += 1
                P.dma("pool", hin[b].t[:, :], C.hb[gt * 128:(gt + 1) * 128, :], sem_h[b], R=[C.Dhb], W=[hin[b]])
                P.op("dve", I("scalar_tensor_tensor", pre[b].t[:, :], hin[b].t[:, :], ALPHA, C.ps2(db), op0=ALU.mult, op1=ALU.add),
                     R=[hin[b], C.PS[db], C.PS[db + 1]], W=[pre[b]])
                if final:
                    ln_tile(C, L, gt, pre[b], gam, bet, (C.Dout, C.out[gt * 128:(gt + 1) * 128, :]), None)
                else:
                    ln_tile(C, L, gt, pre[b], gam, bet, (C.Dha, C.ha[gt * 128:(gt + 1) * 128, :]), (C.DhTa, C.hTa), tr_banks=6)

            ffn_pass(C, F, xb, C.d["ffn_wg"][i], C.d["ffn_wu"][i], C.d["ffn_wd"][i], consume)
        ln_flush(C, L)
        P.barrier()


def phase_moe(C, l, final, stop=3):
    nc, P = C.nc, C.P
    mi = l // 2
    with ExitStack() as es:
        sb = lambda n, s, d: es.enter_context(nc.sbuf_tensor(uniq(n), s, d))
        selb = TT(sb("m_selb", [128, NT, NE], BF16))
        csel = TT(sb("m_csel", [128, NT, NE], BF16))
        cum = TT(sb("m_cum", [128, NT, NE], F32))
        pos = TT(sb("m_pos", [128, NT, NE], F32))
        pre_ = TT(sb("m_pre", [128, NT, NE], F32))
        tmp = TT(sb("m_tmp", [128, NT, NE], F32))
        tmp2 = TT(sb("m_tmp2", [128, NT, NE], F32))
        iot = TT(sb("m_iot", [128, CAP], F32))
        ecap = TT(sb("m_ecap", [128, NT, NE], F32))
        A = [TT(sb(f"m_A{i}", [128, NT, CAP], BF16)) for i in range(2)]
        tokf = TT(sb("m_tokf", [128, NE, NST], F32))
        sem_c = P.dma_sem("m_c")
        P.dma("sync", iot.t[:, :], C.d["c_iota"], sem_c, W=[iot])
        P.dma("sync", ecap.t[:, :, :], C.d["c_ecap"], sem_c, W=[ecap])
        P.batch(sem_c, [iot, ecap])
        P.op("dve", I("tensor_copy", selb.t[:, :, :], C.sel.t[:, :, :]), R=[C.sel], W=[selb])
        P.op("dve", I("memset", csel.t[:, 0, :], 0.0), W=[csel])
        for tt in range(1, NT):
            P.op("dve", I("tensor_tensor", csel.t[:, tt, :], csel.t[:, tt - 1, :], selb.t[:, tt - 1, :], op=ALU.add), R=[csel, selb], W=[csel])
        bk = 0
        for tt in range(NT):
            o = C.bank(bk)[:, tt * NE:(tt + 1) * NE]
            P.op("pe", I("matmul", o, C.lowI.t[:, :], selb.t[:, tt, :], start=True, stop=False), R=[C.lowI, selb], W=[C.PS[bk]])
            P.op("pe", I("matmul", o, C.ones.t[:, :], csel.t[:, tt, :], start=False, stop=True), R=[C.ones, csel], W=[C.PS[bk]])
        P.op("dve", I("tensor_copy", cum.t[:, :, :], C.bank(bk)[:, 0:NT * NE].rearrange("p (a b) -> p a b", b=NE)), R=[C.PS[bk]], W=[cum])
        P.op("dve", I("tensor_tensor", pos.t[:, :, :], cum.t[:, :, :], C.sel.t[:, :, :], op=ALU.subtract), R=[cum, C.sel], W=[pos])
        P.op("dve", I("memset", pre_.t[:, :, :], 0.0), W=[pre_])
        P.op("dve", I("tensor_copy", pre_.t[:, :, 1:NE], C.sel.t[:, :, 0:NE - 1]), R=[C.sel], W=[pre_])
        for sh in (1, 2, 4):
            P.op("dve", I("tensor_copy", tmp.t[:, :, :], pre_.t[:, :, :]), R=[pre_], W=[tmp])
            P.op("dve", I("tensor_tensor", pre_.t[:, :, sh:NE], tmp.t[:, :, sh:NE], tmp.t[:, :, 0:NE - sh], op=ALU.add), R=[tmp], W=[pre_])
        P.op("dve", I("tensor_tensor", pos.t[:, :, :], pos.t[:, :, :], ecap.t[:, :, :], op=ALU.add), R=[pos, ecap], W=[pos])
        for kk in range(2):
            P.op("dve", I("tensor_scalar", tmp.t[:, :, :], pre_.t[:, :, :], float(kk), None, op0=ALU.is_equal), R=[pre_], W=[tmp])
            P.op("dve", I("tensor_tensor", tmp.t[:, :, :], tmp.t[:, :, :], C.sel.t[:, :, :], op=ALU.mult), R=[tmp, C.sel], W=[tmp])
            P.op("dve", I("tensor_tensor", tmp2.t[:, :, :], tmp.t[:, :, :], pos.t[:, :, :], op=ALU.mult), R=[tmp, pos], W=[tmp2])
            P.op("dve", I("tensor_reduce", C.slotf.t[:, kk, :], tmp2.t[:, :, :], AX.X, ALU.add), R=[tmp2], W=[C.slotf])
            P.op("dve", I("tensor_tensor", tmp2.t[:, :, :], tmp.t[:, :, :], C.gate.t[:, :, :], op=ALU.mult), R=[tmp, C.gate], W=[tmp2])
            P.op("dve", I("tensor_reduce", C.gsel.t[:, kk, :], tmp2.t[:, :, :], AX.X, ALU.add), R=[tmp2], W=[C.gsel])
        P.op("dve", I("tensor_copy", C.sloti.t[:, :, :], C.slotf.t[:, :, :]), R=[C.slotf], W=[C.sloti])
        ai = 0
        for e in range(NE):
            bk = 1 + (e % 2)
            Ab = A[e % 2]
            for tt in range(NT):
                eng = "dve"
                ai += 1
                P.op(eng, I("tensor_scalar", Ab.t[:, tt, :], iot.t[:, :], cum.t[:, tt, e:e + 1], None, op0=ALU.is_ge), R=[iot, cum], W=[Ab])
            for st in range(NST):
                for tt in range(NT):
                    P.op("pe", I("matmul", C.bank(bk)[:, st:st + 1], Ab.t[:, tt, st * 128:(st + 1) * 128], C.ones.t[:, 0:1],
                                 start=(tt == 0), stop=(tt == NT - 1)), R=[Ab, C.ones], W=[C.PS[bk]])
            P.op("dve", I("tensor_scalar", tokf.t[:, e, :], C.bank(bk)[:, 0:NST], float(T - 1), None, op0=ALU.min), R=[C.PS[bk]], W=[tokf])
        P.op("dve", I("tensor_copy", C.toki.t[:, :, :], tokf.t[:, :, :]), R=[tokf], W=[C.toki])
        P.barrier()
    if stop < 2:
        return
    with ExitStack() as es:
        sb = lambda n, s, d: es.enter_context(nc.sbuf_tensor(uniq(n), s, d))
        xg = [TT(sb(f"m_xg{i}", [128, D], F32)) for i in range(2)]
        xT = [[TT(sb(f"m_xT{j}_{k}", [128, CAP], BF16)) for k in range(8)] for j in range(2)]
        yst = [TT(sb(f"m_yst{i}", [128, D], F32)) for i in range(2)]
        sem_g = [P.dma_sem("m_g0", sw=True), P.dma_sem("m_g1", sw=True)]
        sem_y = [P.dma_sem("m_y0"), P.dma_sem("m_y1")]
        F = alloc_ffn(C, es, CAP)
        yi = [0]

        def emit_gather(e, st):
            b = st % 2
            P.gather(xg[b].t[:, :], C.hb, C.toki.t[:, e, st:st + 1], sem_g[b], R=[C.Dhb, C.toki], W=[xg[b]])

        def emit_tr(e, st):
            b = st % 2
            xb = xT[e % 2]
            bk = 6
            flat = C.ps2(bk)
            for k in range(8):
                P.op("pe", I("transpose", flat[:, k * 128:(k + 1) * 128], xg[b].t[:, k * 128:(k + 1) * 128], C.ident.t[:, :]),
                     R=[xg[b], C.ident], W=[C.PS[bk], C.PS[bk + 1]])
            for k in range(8):
                if k % 2 == 0:
                    P.op("act", I("activation", out=xb[k].t[:, st * 128:(st + 1) * 128], in_=flat[:, k * 128:(k + 1) * 128], func=AF.Copy),
                         R=[C.PS[bk], C.PS[bk + 1]], W=[xb[k]])
                else:
                    P.op("dve", I("tensor_copy", xb[k].t[:, st * 128:(st + 1) * 128], flat[:, k * 128:(k + 1) * 128]),
                         R=[C.PS[bk], C.PS[bk + 1]], W=[xb[k]])

        for st in range(NST):
            emit_gather(0, st)
            emit_tr(0, st)
        for e in range(NE):
            xb = xT[e % 2]

            def consume(tt, db, e=e):
                b = yi[0] % 2
                yi[0] += 1
                if b == 0:
                    P.op("act", I("activation", out=yst[b].t[:, :], in_=C.ps2(db), func=AF.Copy), R=[C.PS[db], C.PS[db + 1]], W=[yst[b]])
                else:
                    P.op("dve", I("tensor_copy", yst[b].t[:, :], C.ps2(db)), R=[C.PS[db], C.PS[db + 1]], W=[yst[b]])
                r0 = e * CAP + tt * 128
                P.dma("sync", C.ybuf[r0:r0 + 128, :], yst[b].t[:, :], sem_y[b], R=[yst[b]], W=[C.Dyb])

            def hook(fb, e=e):
                if e + 1 < NE:
                    if fb < NST:
                        emit_gather(e + 1, fb)
                    if 1 <= fb <= NST:
                        emit_tr(e + 1, fb - 1)

            ffn_pass(C, F, xb, C.d["moe_wg"][mi][e], C.d["moe_wu"][mi][e], C.d["moe_wd"][mi][e], consume, blk_hook=hook)
        P.barrier()
    if stop < 3:
        return
    with ExitStack() as es:
        sb = lambda n, s, d: es.enter_context(nc.sbuf_tensor(uniq(n), s, d))
        gam = TT(sb("n_gam", [128, D], F32))
        bet = TT(sb("n_bet", [128, D], F32))
        hin = [TT(sb(f"n_hin{i}", [128, D], F32)) for i in range(4)]
        y0 = [TT(sb(f"n_y0{i}", [128, D], F32)) for i in range(4)]
        y1 = [TT(sb(f"n_y1{i}", [128, D], F32)) for i in range(4)]
        pre = [TT(sb(f"n_pre{i}", [128, D], F32)) for i in range(2)]
        sem_c = P.dma_sem("n_c")
        sem_h = [P.dma_sem(f"n_h{i}", sw=True) for i in range(4)]
        sem_a = [P.dma_sem(f"n_a{i}", sw=True) for i in range(4)]
        sem_b = [P.dma_sem(f"n_b{i}", sw=True) for i in range(4)]
        P.dma("sync", gam.t[:, :], bcast_row(C.d["ln2_g"][l], D), sem_c, W=[gam])
        P.dma("sync", bet.t[:, :], bcast_row(C.d["ln2_b"][l], D), sem_c, W=[bet])
        P.batch(sem_c, [gam, bet])
        L = alloc_ln(C, es, want_T=not final)
        for tt in range(NT):
            b = tt % 2
            r_ = tt % 4
            P.dma("pool", hin[r_].t[:, :], C.hb[tt * 128:(tt + 1) * 128, :], sem_h[r_], R=[C.Dhb], W=[hin[r_]])
            P.gather(y0[r_].t[:, :], C.ybuf, C.sloti.t[:, 0, tt:tt + 1], sem_a[r_], R=[C.Dyb, C.sloti], W=[y0[r_]])
            P.gather(y1[r_].t[:, :], C.ybuf, C.sloti.t[:, 1, tt:tt + 1], sem_b[r_], R=[C.Dyb, C.sloti], W=[y1[r_]])
            P.op("act", I("activation", out=y0[r_].t[:, :], in_=y0[r_].t[:, :], func=AF.Identity, scale=C.gsel.t[:, 0, tt:tt + 1]),
                 R=[y0[r_], C.gsel], W=[y0[r_]])
            P.op("dve", I("scalar_tensor_tensor", pre[b].t[:, :], hin[r_].t[:, :], ALPHA, y0[r_].t[:, :], op0=ALU.mult, op1=ALU.add),
                 R=[y0[r_], hin[r_]], W=[pre[b]])
            P.op("dve", I("scalar_tensor_tensor", pre[b].t[:, :], y1[r_].t[:, :], C.gsel.t[:, 1, tt:tt + 1], pre[b].t[:, :], op0=ALU.mult, op1=ALU.add),
                 R=[y1[r_], C.gsel, pre[b]], W=[pre[b]])
            if final:
                ln_tile(C, L, tt, pre[b], gam, bet, (C.Dout, C.out[tt * 128:(tt + 1) * 128, :]), None)
            else:
                ln_tile(C, L, tt, pre[b], gam, bet, (C.Dha, C.ha[tt * 128:(tt + 1) * 128, :]), (C.DhTa, C.hTa), tr_banks=0 + 2 * b)
        ln_flush(C, L)
        P.barrier()


IN_SPECS = None


def host_constants():
    c = {}
    c["c_ident"] = np.eye(128, dtype=np.float32)
    j = np.arange(128)[:, None]
    s = np.arange(128)[None, :]
    c["c_negU"] = -(j >= s).astype(np.float32)
    c["c_negO"] = -np.ones((128, 128), np.float32)
    c["c_lowI"] = (j <= s).astype(np.float32)
    c["c_ones"] = np.ones((128, 128), np.float32)
    p = np.arange(128)[:, None, None]
    dj = np.arange(4)[None, :, None]
    col = np.arange(512)[None, None, :]
    c["c_masks"] = ((128 * (3 - dj) + p) < col).astype(np.float32)
    invc = np.zeros((128, 4, 16), np.float32)
    for g in range(4):
        w = 2 << g
        invc[:, g, :] = 1.0 / np.minimum(np.arange(16) + 1, w)
    c["c_invc"] = invc
    c["c_iota"] = np.broadcast_to(np.arange(CAP, dtype=np.float32)[None, :], (128, CAP)).copy()
    c["c_ecap"] = np.broadcast_to((np.arange(NE, dtype=np.float32) * CAP)[None, None, :], (128, NT, NE)).copy()
    return c


def host_layout(inp):
    o = {}
    f = lambda a: np.ascontiguousarray(a, dtype=np.float32)
    o["ln_in_g"] = f(inp["ln_in_g"])
    o["ln_in_b"] = f(inp["ln_in_b"])
    o["w_in"] = f(inp["w_in"].reshape(DEPTH, 8, 128, 2048).transpose(0, 2, 1, 3))
    o["pool_w"] = f(inp["pool_w"].transpose(0, 2, 1, 3))
    o["pool_scale"] = f(inp["pool_scale"].reshape(DEPTH, 4, 128).transpose(0, 2, 1))
    o["w_out"] = f(inp["w_out"].reshape(DEPTH, 8, 128, D).transpose(0, 2, 1, 3))
    for nm in ("ln1_g", "ln1_b", "ln2_g", "ln2_b"):
        o[nm] = f(inp[nm])
    o["ffn_wg"] = f(inp["ffn_wg"].reshape(-1, 8, 128, NFC, 128).transpose(0, 3, 2, 1, 4))
    o["ffn_wu"] = f(inp["ffn_wu"].reshape(-1, 8, 128, NFC, 128).transpose(0, 3, 2, 1, 4))
    o["ffn_wd"] = f(inp["ffn_wd"])
    o["moe_router"] = f(inp["moe_router"].reshape(-1, 8, 128, NE).transpose(0, 2, 1, 3))
    o["moe_wg"] = f(inp["moe_wg"].reshape(-1, NE, 8, 128, NFC, 128).transpose(0, 1, 4, 3, 2, 5))
    o["moe_wu"] = f(inp["moe_wu"].reshape(-1, NE, 8, 128, NFC, 128).transpose(0, 1, 4, 3, 2, 5))
    o["moe_wd"] = f(inp["moe_wd"])
    o.update(host_constants())
    return o


def build(shapes, phases="all", debug_out=()):
    nc = bass.Bass("TRN2", target_bir_lowering=False)
    C = Ctx()
    C.nc = nc
    C.d = {}
    for name, shp in shapes.items():
        C.d[name] = nc.dram_tensor(name, list(shp), F32, kind="ExternalInput").ap()
    kind = lambda n: "ExternalOutput" if n in debug_out else "Internal"
    C.out = nc.dram_tensor("out", [T, D], F32, kind="ExternalOutput").ap()
    C.ha = nc.dram_tensor("s_ha", [T, D], F32, kind=kind("s_ha")).ap()
    C.hb = nc.dram_tensor("s_hb", [T, D], F32, kind=kind("s_hb")).ap()
    C.hTa = nc.dram_tensor("s_hTa", [D, T], BF16, kind=kind("s_hTa")).ap()
    C.hTb = nc.dram_tensor("s_hTb", [D, T], BF16, kind=kind("s_hTb")).ap()
    C.qT = nc.dram_tensor("s_qT", [512, T], BF16, kind=kind("s_qT")).ap()
    C.kT = nc.dram_tensor("s_kT", [512, T], BF16, kind=kind("s_kT")).ap()
    C.v = nc.dram_tensor("s_v", [T, 512], BF16, kind=kind("s_v")).ap()
    C.ycT = nc.dram_tensor("s_ycT", [D, T], BF16, kind=kind("s_ycT")).ap()
    C.ybuf = nc.dram_tensor("s_ybuf", [NE * CAP, D], F32, kind=kind("s_ybuf")).ap()
    C.Dout, C.Dha, C.Dhb, C.DhTa, C.DhTb, C.Dv, C.Dyb = TT(None), TT(None), TT(None), TT(None), TT(None), TT(None), TT(None)
    C.DqT = [TT(None) for _ in range(4)]
    C.DkT = [TT(None) for _ in range(4)]
    C.DycT = [TT(None) for _ in range(8)]
    with ExitStack() as es:
        P = Prog(nc, es)
        C.P = P
        sb = lambda n, s, d: es.enter_context(nc.sbuf_tensor(uniq(n), s, d))
        ps = es.enter_context(nc.psum_tensor("ps", [128, 8, 512], F32))
        C.ps = ps
        C.PS = [TT(None, f"bank{i}", excl=True) for i in range(8)]
        C.bank = lambda b: ps[:, b, :]
        C.ps2 = lambda b: ps[:, b:b + 2, :].rearrange("p a b -> p (a b)")
        C.ps2v = lambda b, a, w: ps[:, b:b + 2, :].rearrange("p a b -> p (a b)").rearrange("p (a w) -> p a w", w=w)
        C.ident = TT(sb("k_ident", [128, 128], F32))
        C.negU = TT(sb("k_negU", [128, 128], BF16))
        C.negO = TT(sb("k_negO", [128, 128], BF16))
        C.lowI = TT(sb("k_lowI", [128, 128], BF16))
        C.ones = TT(sb("k_ones", [128, 128], BF16))
        C.masks = TT(sb("k_masks", [128, 4, 512], BF16))
        C.gate = TT(sb("k_gate", [128, NT, NE], F32))
        C.sel = TT(sb("k_sel", [128, NT, NE], F32))
        C.slotf = TT(sb("k_slotf", [128, 2, NT], F32))
        C.sloti = TT(sb("k_sloti", [128, 2, NT], U32))
        C.gsel = TT(sb("k_gsel", [128, 2, NT], F32))
        C.toki = TT(sb("k_toki", [128, NE, NST], U32))
        sem_k = P.dma_sem("k_c", sw=True)
        sem_k2 = P.dma_sem("k_c2")
        P.dma("sync", C.ident.t[:, :], C.d["c_ident"], sem_k2, W=[C.ident])
        P.dma("pool", C.negU.t[:, :], C.d["c_negU"], sem_k, W=[C.negU])
        P.dma("pool", C.negO.t[:, :], C.d["c_negO"], sem_k, W=[C.negO])
        P.dma("pool", C.lowI.t[:, :], C.d["c_lowI"], sem_k, W=[C.lowI])
        P.dma("pool", C.ones.t[:, :], C.d["c_ones"], sem_k, W=[C.ones])
        P.dma("pool", C.masks.t[:, :, :], C.d["c_masks"], sem_k, W=[C.masks])
        P.batch(sem_k, [C.negU, C.negO, C.lowI, C.ones, C.masks])

        def want(p):
            if phases == "all":
                return not (len(p) == 3 and p[2] in "ab")
            return p in phases

        if want("A"):
            phase_ln_in(C)
        for l in range(DEPTH):
            final = (l == DEPTH - 1)
            if want(f"B{l}"):
                phase_proj(C, l)
            if want(f"C{l}"):
                phase_attn(C, l)
            if want(f"D{l}"):
                phase_mix_ln1(C, l)
            if want(f"E{l}") or want(f"E{l}a") or want(f"E{l}b"):
                if l % 2 == 0:
                    phase_ffn_dense(C, l, final)
                else:
                    phase_moe(C, l, final, stop=(1 if want(f"E{l}a") else 2 if want(f"E{l}b") else 3))
        if "k_dbg" in debug_out:
            sem_d = P.dma_sem("dbg")
            for nm, tt_, shp, dt in (("k_gate", C.gate, [128, NT, NE], F32), ("k_sel", C.sel, [128, NT, NE], F32),
                                     ("k_slotf", C.slotf, [128, 2, NT], F32), ("k_sloti", C.sloti, [128, 2, NT], U32),
                                     ("k_gsel", C.gsel, [128, 2, NT], F32), ("k_toki", C.toki, [128, NE, NST], U32)):
                dd_ = nc.dram_tensor("dbg_" + nm, shp, dt, kind="ExternalOutput").ap()
                P.dma("sync", dd_, tt_.t[:, :, :], sem_d, R=[tt_])
        P.barrier()
        print("bass ops:", P.nops, {e: len(P.q[e]) for e in ENG})
        P.flush()
    return nc


_CACHE = {}


def kernel(**inputs):
    lay = host_layout(inputs)
    x = np.ascontiguousarray(inputs["x"], dtype=np.float32)
    shapes = {"x": (T, D)}
    for k, v in lay.items():
        shapes[k] = v.shape
    if "nc" not in _CACHE:
        import os
        dbg = ("s_ha", "s_hb", "s_hTa", "s_hTb", "s_qT", "s_kT", "s_v", "s_ycT", "s_ybuf") if os.environ.get("SCRATCH_OUT") == "1" else ()
        _CACHE["nc"] = build(shapes, debug_out=dbg)
    nc = _CACHE["nc"]
    in_maps = []
    for b in range(NCORES):
        m = {"x": x[b]}
        m.update(lay)
        in_maps.append(m)
    res = run_bass_kernel_spmd(nc, in_maps, core_ids=list(range(NCORES)))
    out = np.stack([np.asarray(res.results[b]["out"], dtype=np.float32) for b in range(NCORES)], axis=0)
    return out
```

```python
import numpy as np
from contextlib import ExitStack
import concourse.bass as bass
import concourse.mybir as mybir
from concourse.bass_utils import run_bass_kernel_spmd

F32 = mybir.dt.float32
BF16 = mybir.dt.bfloat16
I32 = mybir.dt.int32
U32 = mybir.dt.uint32
AF = mybir.ActivationFunctionType
ALU = mybir.AluOpType
AX = mybir.AxisListType

NCORES = 8
T = 4096
D = 1024
NT = T // 128
DEPTH = 2
DFF = 2816
NFC = DFF // 128
NE = 8
CAP = 1280
NST = CAP // 128
ALPHA = float((2 * DEPTH) ** 0.25)
EPS = 1e-5
ENG = ("sync", "act", "pe", "dve", "pool")


_UNIQ = [0]


def uniq(n):
    _UNIQ[0] += 1
    return f"{n}_{_UNIQ[0]}"


def I(meth, *a, **k):
    return lambda e: getattr(e, meth)(*a, **k)


class TT:
    __slots__ = ("t", "w", "r", "name", "excl")

    def __init__(self, t, name="", excl=False):
        self.t = t
        self.w = None
        self.r = {}
        self.name = name
        self.excl = excl


class Prog:
    def __init__(self, nc, es):
        self.nc = nc
        self.es = es
        self.q = {e: [] for e in ENG}
        self.waited = {e: {} for e in ENG}
        self.psem = {e: self._mksem("p_" + e) for e in ENG}
        self.dsems = []
        self.dpool_sw = []
        self.dpool_hw = []
        self.nops = 0

    def _mksem(self, name):
        h = self.es.enter_context(self.nc.semaphore(name))
        return {"h": h, "n": 0, "name": name}

    def dma_sem(self, name, sw=False):
        pool = self.dpool_sw if sw else self.dpool_hw
        if pool:
            s = pool.pop()
        else:
            s = self._mksem(uniq(name))
            s["sw"] = sw
        self.dsems.append(s)
        return s

    def _wait(self, eng, dep):
        s, v = dep
        if eng == "pe" and s is self.psem["pe"]:
            return
        if self.waited[eng].get(s["name"], 0) >= v:
            return
        self.waited[eng][s["name"]] = v
        self.q[eng].append(("wait", s["h"], v))

    def _deps(self, R, W):
        deps = []
        for b in R:
            if b.w is not None:
                deps.append(b.w)
        for b in W:
            if b.w is not None:
                deps.append(b.w)
            deps.extend(b.r.values())
        return deps

    def _mark(self, dep, R, W):
        for b in R:
            b.r[dep[0]["name"]] = dep
        for b in W:
            b.w = dep
            b.r = {}

    def op(self, eng, fn, R=(), W=()):
        if any(b.excl for b in R):
            W = list(W) + [b for b in R if b.excl and b not in W]
            R = [b for b in R if not b.excl]
        for d in self._deps(R, W):
            self._wait(eng, d)
        s = self.psem[eng]
        s["n"] += 1
        dep = (s, s["n"])
        self.q[eng].append(("op", fn, s["h"], 1))
        self._mark(dep, R, W)
        self.nops += 1
        return dep

    def dma(self, eng, out, in_, sem, R=(), W=()):
        assert bool(sem.get("sw")) == (eng == "pool"), (eng, sem["name"])
        for d in self._deps(R, W):
            if d[0] is sem:
                continue
            self._wait(eng, d)
        sem["n"] += 16
        dep = (sem, sem["n"])
        self.q[eng].append(("op", lambda e: e.dma_start(out=out, in_=in_), sem["h"], 16))
        self._mark(dep, R, W)
        self.nops += 1
        return dep

    def batch(self, sem, tts):
        for b in tts:
            b.w = (sem, sem["n"])

    def gather(self, out, in_, idx_ap, sem, R=(), W=()):
        eng = "pool"
        for d in self._deps(R, W):
            self._wait(eng, d)
        sem["n"] += 16
        dep = (sem, sem["n"])
        self.q[eng].append(("op", lambda e: e.indirect_dma_start(
            out=out, out_offset=None, in_=in_,
            in_offset=bass.IndirectOffsetOnAxis(ap=idx_ap, axis=0)), sem["h"], 16))
        self._mark(dep, R, W)
        self.nops += 1
        return dep

    def barrier(self):
        sems = list(self.psem.values()) + self.dsems
        for e in ENG:
            for s in sems:
                if s["n"] > 0:
                    self._wait(e, (s, s["n"]))
        for s_ in self.dsems:
            (self.dpool_sw if s_.get("sw") else self.dpool_hw).append(s_)
        self.dsems = []
        for e in ENG:
            if self.psem[e]["n"] > 8000:
                self.psem[e] = self._mksem(uniq("p_" + e))

    def flush(self):
        nc = self.nc
        q = self.q

        def run(e, lst):
            for it in lst:
                if it[0] == "wait":
                    e.wait_ge(it[1], it[2])
                else:
                    ins = it[1](e)
                    ins.then_inc(it[2], it[3])

        with nc.Block() as block:
            @block.sync
            def _(e):
                run(e, q["sync"])

            @block.scalar
            def _(e):
                run(e, q["act"])

            @block.tensor
            def _(e):
                run(e, q["pe"])

            @block.vector
            def _(e):
                run(e, q["dve"])

            @block.gpsimd
            def _(e):
                run(e, q["pool"])


class Ctx:
    pass


def bcast_row(ap1d, n):
    return bass.AP(ap1d.tensor, ap1d.offset, [[0, 128], [1, n]])


def alloc_ln(C, es, want_T=True):
    nc, P = C.nc, C.P
    L = Ctx()
    sb = lambda n, s, d: es.enter_context(nc.sbuf_tensor(uniq(n), s, d))
    L.stats = [TT(sb(f"ln_stats{i}", [128, 12], F32)) for i in range(2)]
    L.mv = [TT(sb(f"ln_mv{i}", [128, 8], F32)) for i in range(2)]
    L.xn = [TT(sb(f"ln_xn{i}", [128, D], F32)) for i in range(2)]
    L.ho = [TT(sb(f"ln_ho{i}", [128, D], F32)) for i in range(2)]
    L.want_T = want_T
    if want_T:
        L.hTs = [TT(sb(f"ln_hTs{i}", [128, 8, 512], BF16)) for i in range(2)]
    L.sem_st = [P.dma_sem("ln_st0"), P.dma_sem("ln_st1")]
    L.sem_sT = [P.dma_sem("ln_sT0"), P.dma_sem("ln_sT1")]
    L.cnt = 0
    L.pend = []
    return L


def ln_tile(C, L, tt, pre, gam, bet, h_dst, hT_dst, tr_banks=None, router=None):
    i = L.cnt
    L.cnt += 1
    L.pend.append(dict(i=i, tt=tt, pre=pre, gam=gam, bet=bet, h_dst=h_dst, hT_dst=hT_dst, tr=tr_banks, router=router))
    _ln_step(C, L)


def ln_flush(C, L):
    for _ in range(2):
        L.pend.append(None)
        _ln_step(C, L)
    L.pend = []


def _ln_step(C, L):
    P = C.P
    n = len(L.pend)
    t0 = L.pend[n - 1]
    t1 = L.pend[n - 2] if n >= 2 else None
    t2 = L.pend[n - 3] if n >= 3 else None
    if t0 is not None:
        b = t0["i"] % 2
        st, mv, pre = L.stats[b], L.mv[b], t0["pre"]
        P.op("dve", I("bn_stats", st.t[:, 0:6], pre.t[:, 0:512]), R=[pre], W=[st])
        P.op("dve", I("bn_stats", st.t[:, 6:12], pre.t[:, 512:1024]), R=[pre], W=[st])
        P.op("dve", I("bn_aggr", mv.t[:, 0:2], st.t[:, :]), R=[st], W=[mv])
        P.op("dve", I("tensor_scalar_add", mv.t[:, 2:3], mv.t[:, 1:2], EPS), R=[mv], W=[mv])
    if t0 is not None:
        b = t0["i"] % 2
        mv = L.mv[b]
        P.op("act", I("activation", out=mv.t[:, 3:4], in_=mv.t[:, 2:3], func=AF.Ln), R=[mv], W=[mv])
        P.op("act", I("activation", out=mv.t[:, 4:5], in_=mv.t[:, 3:4], func=AF.Exp, scale=-0.5), R=[mv], W=[mv])
    if t1 is not None:
        b = t1["i"] % 2
        mv, xn, ho, pre = L.mv[b], L.xn[b], L.ho[b], t1["pre"]
        P.op("dve", I("scalar_tensor_tensor", xn.t[:, :], pre.t[:, :], mv.t[:, 0:1], t1["gam"].t[:, :], op0=ALU.subtract, op1=ALU.mult),
             R=[pre, mv, t1["gam"]], W=[xn])
        P.op("dve", I("scalar_tensor_tensor", ho.t[:, :], xn.t[:, :], mv.t[:, 4:5], t1["bet"].t[:, :], op0=ALU.mult, op1=ALU.add),
             R=[xn, mv, t1["bet"]], W=[ho])
        P.dma("sync", t1["h_dst"][1], ho.t[:, :], L.sem_st[b], R=[ho], W=[t1["h_dst"][0]])
        if t1["router"] is not None:
            t1["router"](t1["tt"], ho)
    if t2 is not None and t2["hT_dst"] is not None:
        b = t2["i"] % 2
        ho = L.ho[b]
        tt = t2["tt"]
        slot = tt % 4
        hTs = L.hTs[(tt // 4) % 2]
        bk = t2["tr"]
        flat = C.ps2(bk)
        for k in range(8):
            P.op("pe", I("transpose", flat[:, k * 128:(k + 1) * 128], ho.t[:, k * 128:(k + 1) * 128], C.ident.t[:, :]),
                 R=[ho, C.ident], W=[C.PS[bk], C.PS[bk + 1]])
        P.op("act", I("activation", out=hTs.t[:, :, slot * 128:(slot + 1) * 128],
                      in_=C.ps2v(bk, 8, 128), func=AF.Copy),
             R=[C.PS[bk], C.PS[bk + 1]], W=[hTs])
        if slot == 3:
            c0 = (tt - 3) * 128
            dst = t2["hT_dst"][1].rearrange("(c p) t -> p c t", p=128)[:, :, c0:c0 + 512]
            P.dma("sync", dst, hTs.t[:, :, :], L.sem_sT[(tt // 4) % 2], R=[hTs], W=[t2["hT_dst"][0]])


def phase_ln_in(C):
    nc, P = C.nc, C.P
    with ExitStack() as es:
        sb = lambda n, s, d: es.enter_context(nc.sbuf_tensor(uniq(n), s, d))
        gam = TT(sb("a_gam", [128, D], F32))
        bet = TT(sb("a_bet", [128, D], F32))
        NXB = 4
        xin = [TT(sb(f"a_xin{i}", [128, D], F32)) for i in range(NXB)]
        sem_c = P.dma_sem("a_c")
        sem_x = [P.dma_sem(f"a_x{i}", sw=True) for i in range(NXB)]
        P.dma("sync", gam.t[:, :], bcast_row(C.d["ln_in_g"], D), sem_c, W=[gam])
        P.dma("sync", bet.t[:, :], bcast_row(C.d["ln_in_b"], D), sem_c, W=[bet])
        P.batch(sem_c, [gam, bet])
        L = alloc_ln(C, es)
        for tt in range(NT):
            b = tt % NXB
            P.dma("pool", xin[b].t[:, :], C.d["x"][tt * 128:(tt + 1) * 128, :], sem_x[b], W=[xin[b]])
            ln_tile(C, L, tt, xin[b], gam, bet, (C.Dha, C.ha[tt * 128:(tt + 1) * 128, :]), (C.DhTa, C.hTa), tr_banks=0 + 2 * (tt % 2))
        ln_flush(C, L)
        P.barrier()


def phase_proj(C, l):
    nc, P = C.nc, C.P
    with ExitStack() as es:
        sb = lambda n, s, d: es.enter_context(nc.sbuf_tensor(uniq(n), s, d))
        wi = TT(sb("b_wi", [128, 8, 2048], BF16))
        hT = [TT(sb(f"b_hT{k}", [128, T], BF16)) for k in range(8)]
        pw = TT(sb("b_pw", [128, 4, 128], BF16))
        psc = TT(sb("b_psc", [128, 4], F32))
        invc = TT(sb("b_invc", [128, 4, 16], F32))
        U = TT(sb("b_U", [128, 16 + T], F32))
        SA = TT(sb("b_SA", [128, 16 + T], F32))
        SB = TT(sb("b_SB", [128, 16 + T], F32))
        dd = TT(sb("b_d", [128, T], BF16))
        stg = [TT(sb(f"b_stg{i}", [128, T], BF16)) for i in range(2)]
        vst = [TT(sb(f"b_vst{i}", [128, 4, 512], BF16)) for i in range(2)]
        sem_w = P.dma_sem("b_w", sw=True)
        sem_h = P.dma_sem("b_h")
        sem_c = P.dma_sem("b_c")
        sem_cw = P.dma_sem("b_cw", sw=True)
        sem_s = [P.dma_sem("b_s0"), P.dma_sem("b_s1")]
        sem_v = [P.dma_sem("b_v0"), P.dma_sem("b_v1")]
        for q4 in range(4):
            P.dma("pool", wi.t[:, :, q4 * 512:(q4 + 1) * 512], C.d["w_in"][l][:, :, q4 * 512:(q4 + 1) * 512], sem_w, W=[wi])
        for k in range(8):
            P.dma("sync", hT[k].t[:, :], C.hTa[k * 128:(k + 1) * 128, :], sem_h, R=[C.DhTa], W=[hT[k]])
        P.dma("pool", pw.t[:, :, :], C.d["pool_w"][l], sem_cw, W=[pw])
        P.dma("sync", psc.t[:, :], C.d["pool_scale"][l], sem_c, W=[psc])
        P.dma("sync", invc.t[:, :, :], C.d["c_invc"], sem_c, W=[invc])
        P.batch(sem_c, [psc, invc])
        P.batch(sem_h, hT)
        for buf in (U, SA, SB):
            P.op("dve", I("memset", buf.t[:, 0:16], 0.0), W=[buf])

        nbank = [0]

        def next_bank():
            b = nbank[0] % 4
            nbank[0] += 1
            return b

        evac_eng = [0]

        def evac(out_ap, in_ap, R, W, scale=None, only_act=False):
            e = 0 if only_act else evac_eng[0] % 2
            evac_eng[0] += 1
            if e == 0:
                if scale is None:
                    P.op("act", I("activation", out=out_ap, in_=in_ap, func=AF.Copy), R=R, W=W)
                else:
                    P.op("act", I("activation", out=out_ap, in_=in_ap, func=AF.Identity, scale=scale), R=R, W=W)
            else:
                if scale is None:
                    P.op("dve", I("tensor_copy", out_ap, in_ap), R=R, W=W)
                else:
                    P.op("dve", I("tensor_scalar_mul", out_ap, in_ap, scale), R=R, W=W)

        def fm_chunk(c, out_fn):
            for tg in range(8):
                bk = next_bank()
                for k in range(8):
                    P.op("pe", I("matmul", C.bank(bk), wi.t[:, k, c * 128:(c + 1) * 128], hT[k].t[:, tg * 512:(tg + 1) * 512],
                                 start=(k == 0), stop=(k == 7)), R=[wi, hT[k]], W=[C.PS[bk]])
                out_fn(tg, bk)

        def qk_chunk(c, only_act=False):
            so = stg[sidx[0] % 2]
            ss = sem_s[sidx[0] % 2]
            sidx[0] += 1
            sc = 0.125 if c < 8 else None
            fm_chunk(c, lambda tg, bk, so=so, sc=sc: evac(so.t[:, tg * 512:(tg + 1) * 512], C.bank(bk), [C.PS[bk]], [so], scale=sc, only_act=only_act))
            if c < 8:
                P.dma("sync", C.qT[(c - 4) * 128:(c - 3) * 128, :], so.t[:, :], ss, R=[so], W=[C.DqT[c - 4]])
            else:
                P.dma("sync", C.kT[(c - 8) * 128:(c - 7) * 128, :], so.t[:, :], ss, R=[so], W=[C.DkT[c - 8]])

        sidx = [0]
        for g in range(4):
            w = 2 << g
            fm_chunk(g, lambda tg, bk: evac(U.t[:, 16 + tg * 512:16 + (tg + 1) * 512], C.bank(bk), [C.PS[bk]], [U]))
            src = U
            bufs = [SA, SB]
            nlev = g + 1
            for lev in range(nlev):
                dst = bufs[lev % 2]
                sh = 1 << lev
                eng = "dve"
                P.op(eng, I("tensor_tensor", dst.t[:, 16:16 + T], src.t[:, 16:16 + T], src.t[:, 16 - sh:16 + T - sh], op=ALU.add),
                     R=[src], W=[dst])
                src = dst
            P.op("dve", I("scalar_tensor_tensor", dd.t[:, :], src.t[:, 16:16 + T], 1.0 / w, U.t[:, 16:16 + T],
                          op0=ALU.mult, op1=ALU.subtract), R=[src, U], W=[dd])
            P.op("dve", I("tensor_tensor", SA.t[:, 0:16] if src is SB else SB.t[:, 0:16], src.t[:, 16:32], invc.t[:, g, :], op=ALU.mult),
                 R=[src, invc], W=[SA if src is SB else SB])
            tmp = SA if src is SB else SB
            P.op("dve", I("tensor_tensor", dd.t[:, 0:16], tmp.t[:, 0:16], U.t[:, 16:32], op=ALU.subtract),
                 R=[tmp, U], W=[dd])
            P.op("dve", I("memset", tmp.t[:, 0:16], 0.0), W=[tmp])
            qk_chunk(4 + 2 * g, only_act=True)
            qk_chunk(5 + 2 * g, only_act=True)
            so = stg[sidx[0] % 2]
            ss = sem_s[sidx[0] % 2]
            sidx[0] += 1
            for tg in range(8):
                bk = next_bank()
                P.op("pe", I("matmul", C.bank(bk), pw.t[:, g, :], dd.t[:, tg * 512:(tg + 1) * 512], start=True, stop=True),
                     R=[pw, dd], W=[C.PS[bk]])
                evac(so.t[:, tg * 512:(tg + 1) * 512], C.bank(bk), [C.PS[bk], psc], [so], scale=psc.t[:, g:g + 1])
            P.dma("sync", C.ycT[g * 128:(g + 1) * 128, :], so.t[:, :], ss, R=[so], W=[C.DycT[g]])
        vv = C.v.rearrange("(n p) c -> p n c", p=128)
        for tt in range(NT):
            bk = next_bank()
            for k in range(8):
                P.op("pe", I("matmul", C.bank(bk), hT[k].t[:, tt * 128:(tt + 1) * 128], wi.t[:, k, 1536:2048],
                             start=(k == 0), stop=(k == 7)), R=[wi, hT[k]], W=[C.PS[bk]])
            vs = vst[(tt // 4) % 2]
            evac(vs.t[:, tt % 4, :], C.bank(bk), [C.PS[bk]], [vs])
            if tt % 4 == 3:
                P.dma("sync", vv[:, tt - 3:tt + 1, :], vs.t[:, :, :], sem_v[(tt // 4) % 2], R=[vs], W=[C.Dv])
        P.barrier()


def phase_attn(C, l):
    nc, P = C.nc, C.P
    with ExitStack() as es:
        sb = lambda n, s, d: es.enter_context(nc.sbuf_tensor(uniq(n), s, d))
        qT = [TT(sb(f"c_qT{i}", [128, T], BF16)) for i in range(4)]
        kT = [TT(sb(f"c_kT{i}", [128, T], BF16)) for i in range(4)]
        V = TT(sb("c_V", [128, NT, 512], BF16))
        E = [TT(sb(f"c_E{i}", [128, 2048], F32)) for i in range(2)]
        SP = [TT(sb(f"c_SP{i}", [128, 2048], BF16)) for i in range(4)]
        CA = [TT(sb(f"c_CA{i}", [128, 2048], BF16)) for i in range(4)]
        AT = [TT(sb(f"c_AT{i}", [128, 2048], BF16)) for i in range(3)]
        ys = [TT(sb(f"c_ys{i}", [128, T], BF16)) for i in range(2)]
        sem_l = P.dma_sem("c_l")
        sem_s = [P.dma_sem("c_s0"), P.dma_sem("c_s1")]
        sem_l0 = P.dma_sem("c_l0")
        sem_lv = P.dma_sem("c_lv")
        vv = C.v.rearrange("(n p) c -> p n c", p=128)
        P.dma("sync", qT[0].t[:, :], C.qT[0:128, :], sem_l0, R=[C.DqT[0]], W=[qT[0]])
        P.dma("sync", kT[0].t[:, :], C.kT[0:128, :], sem_l0, R=[C.DkT[0]], W=[kT[0]])
        P.batch(sem_l0, [qT[0], kT[0]])
        for i in range(4):
            P.dma("sync", V.t[:, i * 8:(i + 1) * 8, :], vv[:, i * 8:(i + 1) * 8, :], sem_lv, R=[C.Dv], W=[V])
        for i in range(1, 4):
            P.dma("sync", qT[i].t[:, :], C.qT[i * 128:(i + 1) * 128, :], sem_l, R=[C.DqT[i]], W=[qT[i]])
            P.dma("sync", kT[i].t[:, :], C.kT[i * 128:(i + 1) * 128, :], sem_l, R=[C.DkT[i]], W=[kT[i]])
        P.batch(sem_l, qT[1:] + kT[1:])

        ZP = [0, 2]
        BP = 4
        OB = [6, 7]
        quads = []
        for hp in range(4):
            for e in range(2):
                for g in range(8):
                    nq = g + 1
                    for qi in range(nq):
                        js = [4 * g + 3 - 4 * qi - k for k in range(4)]
                        quads.append(dict(hp=hp, e=e, g=g, js=js, first=(qi == 0), last=(qi == nq - 1)))
        n = len(quads)
        masks2 = C.masks.t[:, :, :].rearrange("p a b -> p (a b)")

        def zq(s):
            q = quads[s]
            hp, e, g = q["hp"], q["e"], q["g"]
            pr = slice(64 * e, 64 * e + 64)
            for k in range(4):
                bk = ZP[k // 2] + k % 2
                j = q["js"][k]
                P.op("pe", I("matmul", C.bank(bk), kT[hp].t[pr, j * 128:(j + 1) * 128], qT[hp].t[pr, g * 512:(g + 1) * 512],
                             start=True, stop=True), R=[kT[hp], qT[hp]], W=[C.PS[bk]])

        def act1(s):
            Eb = E[s % 2]
            z4 = C.ps[:, 0:4, :].rearrange("p a b -> p (a b)")
            P.op("act", I("activation", out=Eb.t[:, :], in_=z4, func=AF.Exp),
                 R=[C.PS[0], C.PS[1], C.PS[2], C.PS[3]], W=[Eb])

        def act2(s):
            P.op("act", I("activation", out=SP[s % 4].t[:, :], in_=E[s % 2].t[:, :], func=AF.Ln, bias=1.0), R=[E[s % 2]], W=[SP[s % 4]])

        def dve3(s):
            q = quads[s]
            S, Cq, Cn = SP[s % 4], CA[s % 4], CA[(s + 1) % 4]
            sl = lambda k: slice(k * 512, (k + 1) * 512)
            if q["first"]:
                P.op("dve", I("tensor_tensor", S.t[:, :], S.t[:, :], masks2, op=ALU.mult), R=[S, C.masks], W=[S])
                P.op("dve", I("tensor_tensor", Cq.t[:, sl(2)], S.t[:, sl(0)], S.t[:, sl(1)], op=ALU.add), R=[S], W=[Cq])
            else:
                P.op("dve", I("tensor_tensor", Cq.t[:, sl(1)], Cq.t[:, sl(0)], S.t[:, sl(0)], op=ALU.add), R=[S, Cq], W=[Cq])
                P.op("dve", I("tensor_tensor", Cq.t[:, sl(2)], Cq.t[:, sl(1)], S.t[:, sl(1)], op=ALU.add), R=[S, Cq], W=[Cq])
            P.op("dve", I("tensor_tensor", Cq.t[:, sl(3)], Cq.t[:, sl(2)], S.t[:, sl(2)], op=ALU.add), R=[S, Cq], W=[Cq])
            if not q["last"]:
                P.op("dve", I("tensor_tensor", Cn.t[:, sl(0)], Cq.t[:, sl(3)], S.t[:, sl(3)], op=ALU.add), R=[S, Cq], W=[Cn])

        def bq(s, p):
            q = quads[s]
            hp, e, g = q["hp"], q["e"], q["g"]
            pr = slice(64 * e, 64 * e + 64)
            S, Cq = SP[s % 4], CA[s % 4]
            for k in (2 * p, 2 * p + 1):
                bk = BP + k % 2
                j = q["js"][k]
                if q["first"] and k == 0:
                    car = None
                elif q["first"] and k == 1:
                    car = (S, S.t[:, 0:512])
                else:
                    car = (Cq, Cq.t[:, k * 512:(k + 1) * 512])
                P.op("pe", I("matmul", C.bank(bk), kT[hp].t[pr, j * 128:(j + 1) * 128], qT[hp].t[pr, g * 512:(g + 1) * 512],
                             start=True, stop=False), R=[kT[hp], qT[hp]], W=[C.PS[bk]])
                P.op("pe", I("matmul", C.bank(bk), C.negU.t[:, :], S.t[:, k * 512:(k + 1) * 512], start=False, stop=(car is None)),
                     R=[C.negU, S], W=[C.PS[bk]])
                if car is not None:
                    P.op("pe", I("matmul", C.bank(bk), C.negO.t[:, :], car[1], start=False, stop=True),
                         R=[C.negO, car[0]], W=[C.PS[bk]])

        def act3(s, p):
            A = AT[s % 3]
            P.op("act", I("activation", out=A.t[:, p * 1024:(p + 1) * 1024], in_=C.ps2(BP), func=AF.Exp),
                 R=[C.PS[BP], C.PS[BP + 1]], W=[A])

        def mask_att(s):
            q = quads[s]
            if q["first"]:
                A = AT[s % 3]
                P.op("dve", I("tensor_tensor", A.t[:, :], A.t[:, :], masks2, op=ALU.mult), R=[A, C.masks], W=[A])

        def av(s):
            q = quads[s]
            hp, e, g = q["hp"], q["e"], q["g"]
            gi = (hp * 2 + e) * 8 + g
            ob = OB[gi % 2]
            A = AT[s % 3]
            pr = slice(64 * e, 64 * e + 64)
            for k in range(4):
                j = q["js"][k]
                P.op("pe", I("matmul", C.bank(ob)[pr, :], V.t[:, j, hp * 128 + 64 * e: hp * 128 + 64 * e + 64], A.t[:, k * 512:(k + 1) * 512],
                             start=(q["first"] and k == 0), stop=(q["last"] and k == 3)), R=[V, A], W=[C.PS[ob]])
            if q["last"]:
                yb = ys[hp % 2]
                P.op("dve", I("tensor_copy", yb.t[pr, g * 512:(g + 1) * 512], C.bank(ob)[pr, :]), R=[C.PS[ob]], W=[yb])
                if e == 1 and g == 7:
                    P.dma("sync", C.ycT[512 + hp * 128:512 + (hp + 1) * 128, :], yb.t[:, :], sem_s[hp % 2], R=[yb], W=[C.DycT[4 + hp]])

        for s in range(n + 3):
            if s < n:
                zq(s)
            if 0 <= s - 2 < n:
                bq(s - 2, 0)
                act3(s - 2, 0)
            if s < n:
                act1(s)
            if 0 <= s - 3 < n:
                av(s - 3)
            if 0 <= s - 2 < n:
                bq(s - 2, 1)
                act3(s - 2, 1)
            if s < n:
                act2(s)
                dve3(s)
            if 0 <= s - 2 < n:
                mask_att(s - 2)
        P.barrier()


def phase_mix_ln1(C, l):
    nc, P = C.nc, C.P
    moe = (l % 2 == 1)
    with ExitStack() as es:
        sb = lambda n, s, d: es.enter_context(nc.sbuf_tensor(uniq(n), s, d))
        yc = [TT(sb(f"d_yc{k}", [128, T], BF16)) for k in range(8)]
        wo = TT(sb("d_wo", [128, 8, D], BF16))
        gam = TT(sb("d_gam", [128, D], F32))
        bet = TT(sb("d_bet", [128, D], F32))
        hin = [TT(sb(f"d_hin{i}", [128, D], F32)) for i in range(4)]
        pre = [TT(sb(f"d_pre{i}", [128, D], F32)) for i in range(2)]
        sem_c = P.dma_sem("d_c")
        sem_cw = P.dma_sem("d_cw", sw=True)
        sem_y = P.dma_sem("d_y")
        sem_h = [P.dma_sem(f"d_h{i}", sw=True) for i in range(4)]
        P.dma("pool", wo.t[:, :, :], C.d["w_out"][l], sem_cw, W=[wo])
        P.dma("sync", gam.t[:, :], bcast_row(C.d["ln1_g"][l], D), sem_c, W=[gam])
        P.dma("sync", bet.t[:, :], bcast_row(C.d["ln1_b"][l], D), sem_c, W=[bet])
        for k in range(8):
            P.dma("sync", yc[k].t[:, :], C.ycT[k * 128:(k + 1) * 128, :], sem_y, R=[C.DycT[k]], W=[yc[k]])
        P.batch(sem_y, yc)
        L = alloc_ln(C, es)
        router = None
        P.batch(sem_c, [gam, bet])
        if moe:
            rt = TT(sb("d_rt", [128, 8, NE], F32))
            P.dma("sync", rt.t[:, :, :], C.d["moe_router"][l // 2], sem_c, W=[rt])
            P.batch(sem_c, [gam, bet, rt])
            hT32 = [TT(sb(f"d_hT32{i}", [128, 8, 128], F32)) for i in range(2)]
            lgt = TT(sb("d_lgt", [128, NT, NE], F32))
            wk = TT(sb("d_wk", [128, NT, NE], F32))
            m1 = TT(sb("d_m1", [128, NT], F32))
            m2 = TT(sb("d_m2", [128, NT], F32))

            def router(tt, ho):
                b = tt % 2
                bk = 4 + 2 * b
                flat = C.ps2(bk)
                for k in range(8):
                    P.op("pe", I("transpose", flat[:, k * 128:(k + 1) * 128], ho.t[:, k * 128:(k + 1) * 128], C.ident.t[:, :]),
                         R=[ho, C.ident], W=[C.PS[bk], C.PS[bk + 1]])
                P.op("dve", I("tensor_copy", hT32[b].t[:, :, :], C.ps2v(bk, 8, 128)), R=[C.PS[bk], C.PS[bk + 1]], W=[hT32[b]])
                lb = C.bank(bk)[:, 0:NE]
                for k in range(8):
                    P.op("pe", I("matmul", lb, hT32[b].t[:, k, :], rt.t[:, k, :], start=(k == 0), stop=(k == 7)),
                         R=[hT32[b], rt], W=[C.PS[bk], C.PS[bk + 1]])
                P.op("dve", I("tensor_copy", lgt.t[:, tt, :], lb), R=[C.PS[bk], C.PS[bk + 1]], W=[lgt])

            def router_finish():
                fl = lambda t_: t_.t[:, :, :].rearrange("p a b -> p (a b)")
                P.op("dve", I("tensor_reduce", m1.t[:, :], lgt.t[:, :, :], AX.X, ALU.max), R=[lgt], W=[m1])
                for e_ in range(NE):
                    P.op("dve", I("tensor_tensor", wk.t[:, :, e_], lgt.t[:, :, e_], m1.t[:, :], op=ALU.is_equal), R=[lgt, m1], W=[wk])
                P.op("dve", I("scalar_tensor_tensor", fl(wk), fl(wk), -1e30, fl(lgt), op0=ALU.mult, op1=ALU.add), R=[wk, lgt], W=[wk])
                P.op("dve", I("tensor_reduce", m2.t[:, :], wk.t[:, :, :], AX.X, ALU.max), R=[wk], W=[m2])
                for e_ in range(NE):
                    P.op("dve", I("tensor_tensor", wk.t[:, :, e_], lgt.t[:, :, e_], m1.t[:, :], op=ALU.subtract), R=[lgt, m1], W=[wk])
                P.op("act", I("activation", out=fl(wk), in_=fl(wk), func=AF.Exp), R=[wk], W=[wk])
                for e_ in range(NE):
                    P.op("dve", I("tensor_tensor", C.sel.t[:, :, e_], lgt.t[:, :, e_], m2.t[:, :], op=ALU.is_ge), R=[lgt, m2], W=[C.sel])
                P.op("dve", I("tensor_tensor", fl(wk), fl(wk), fl(C.sel), op=ALU.mult), R=[wk, C.sel], W=[wk])
                P.op("dve", I("tensor_reduce", m1.t[:, :], wk.t[:, :, :], AX.X, ALU.add), R=[wk], W=[m1])
                P.op("dve", I("reciprocal", m1.t[:, :], m1.t[:, :]), R=[m1], W=[m1])
                for e_ in range(NE):
                    P.op("dve", I("tensor_tensor", C.gate.t[:, :, e_], wk.t[:, :, e_], m1.t[:, :], op=ALU.mult), R=[wk, m1], W=[C.gate])

        for tt in range(NT):
            b = tt % 2
            hb_ = tt % 4
            P.dma("pool", hin[hb_].t[:, :], C.ha[tt * 128:(tt + 1) * 128, :], sem_h[hb_], R=[C.Dha], W=[hin[hb_]])
            bk = 0 + 2 * b
            for nh in range(2):
                for k in range(8):
                    P.op("pe", I("matmul", C.bank(bk + nh), yc[k].t[:, tt * 128:(tt + 1) * 128], wo.t[:, k, nh * 512:(nh + 1) * 512],
                                 start=(k == 0), stop=(k == 7)), R=[yc[k], wo], W=[C.PS[bk], C.PS[bk + 1]])
            P.op("dve", I("scalar_tensor_tensor", pre[b].t[:, :], hin[hb_].t[:, :], ALPHA, C.ps2(bk), op0=ALU.mult, op1=ALU.add),
                 R=[hin[hb_], C.PS[bk], C.PS[bk + 1]], W=[pre[b]])
            ln_tile(C, L, tt, pre[b], gam, bet, (C.Dhb, C.hb[tt * 128:(tt + 1) * 128, :]),
                    None if moe else (C.DhTb, C.hTb), tr_banks=4 + 2 * b, router=router)
        ln_flush(C, L)
        if moe:
            router_finish()
        P.barrier()


def alloc_ffn(C, es, ntok):
    nc, P = C.nc, C.P
    F = Ctx()
    sb = lambda n, s, d: es.enter_context(nc.sbuf_tensor(uniq(n), s, d))
    F.ntok = ntok
    F.wd = TT(sb("f_wd", [128, NFC, D], BF16))
    F.wg = [TT(sb(f"f_wg{i}", [128, 2, 8, 128], BF16)) for i in range(3)]
    F.wu = [TT(sb(f"f_wu{i}", [128, 2, 8, 128], BF16)) for i in range(3)]
    F.act = TT(sb("f_act", [128, NFC, ntok], BF16))
    F.sg = [TT(sb(f"f_sg{i}", [128, 512], BF16)) for i in range(2)]
    F.sem_wd = P.dma_sem("f_wd", sw=True)
    F.sem_wg = [P.dma_sem(f"f_wg{i}", sw=True) for i in range(3)]
    F.sem_wu = [P.dma_sem(f"f_wu{i}", sw=True) for i in range(3)]
    F.cnt = 0
    F.blk = 0
    F.pref = None
    return F


def ffn_pass(C, F, xT, wg_d, wu_d, wd_d, consume, GB=(0, 1), UB=(2, 3), DB=4, blk_hook=None, nxt=None, mid_hook=None):
    P = C.P
    ntok = F.ntok
    subs = []
    t0 = 0
    while t0 < ntok:
        w = min(512, ntok - t0)
        subs.append((t0, w))
        t0 += w
    wdv = wd_d.rearrange("(c p) n -> p c n", p=128)
    for fb in range(NFC // 2):
        if fb == 2:
            P.dma("pool", F.wd.t[:, 0:11, :], wdv[:, 0:11, :], F.sem_wd, W=[F.wd])
            P.dma("pool", F.wd.t[:, 11:22, :], wdv[:, 11:22, :], F.sem_wd, W=[F.wd])
        if fb == 0 and F.pref is not None:
            bi = F.pref
            F.pref = None
            wg, wu = F.wg[bi], F.wu[bi]
        else:
            bi = F.blk % 3
            F.blk += 1
            wg, wu = F.wg[bi], F.wu[bi]
            P.dma("pool", wg.t[:, :, :, :], wg_d[2 * fb:2 * fb + 2].rearrange("c p k f -> p c k f"), F.sem_wg[bi], W=[wg])
            P.dma("pool", wu.t[:, :, :, :], wu_d[2 * fb:2 * fb + 2].rearrange("c p k f -> p c k f"), F.sem_wu[bi], W=[wu])
        for ci in range(2):
            fc = 2 * fb + ci
            for (t0, w) in subs:
                i = F.cnt
                F.cnt += 1
                gb, ub = GB[i % 2], UB[i % 2]
                for k in range(8):
                    P.op("pe", I("matmul", C.bank(gb)[:, 0:w], wg.t[:, ci, k, :], xT[k].t[:, t0:t0 + w], start=(k == 0), stop=(k == 7)),
                         R=[wg, xT[k]], W=[C.PS[gb]])
                for k in range(8):
                    P.op("pe", I("matmul", C.bank(ub)[:, 0:w], wu.t[:, ci, k, :], xT[k].t[:, t0:t0 + w], start=(k == 0), stop=(k == 7)),
                         R=[wu, xT[k]], W=[C.PS[ub]])
                sg = F.sg[i % 2]
                P.op("act", I("activation", out=sg.t[:, 0:w], in_=C.bank(gb)[:, 0:w], func=AF.Silu), R=[C.PS[gb]], W=[sg])
                P.op("dve", I("tensor_tensor", F.act.t[:, fc, t0:t0 + w], sg.t[:, 0:w], C.bank(ub)[:, 0:w], op=ALU.mult),
                     R=[sg, C.PS[ub]], W=[F.act])
        if blk_hook is not None:
            blk_hook(fb)
    if mid_hook is not None:
        mid_hook()
    if nxt is not None:
        bi = F.blk % 3
        F.blk += 1
        P.dma("pool", F.wg[bi].t[:, :, :, :], nxt[0][0:2].rearrange("c p k f -> p c k f"), F.sem_wg[bi], W=[F.wg[bi]])
        P.dma("pool", F.wu[bi].t[:, :, :, :], nxt[1][0:2].rearrange("c p k f -> p c k f"), F.sem_wu[bi], W=[F.wu[bi]])
        F.pref = bi
    dpairs = (DB, GB[0], UB[0])
    for tt in range(ntok // 128):
        db = dpairs[tt % 3]
        for nh in range(2):
            for fc in range(NFC):
                P.op("pe", I("matmul", C.bank(db + nh), F.act.t[:, fc, tt * 128:(tt + 1) * 128], F.wd.t[:, fc, nh * 512:(nh + 1) * 512],
                             start=(fc == 0), stop=(fc == NFC - 1)), R=[F.act, F.wd], W=[C.PS[db], C.PS[db + 1]])
        consume(tt, db)


def phase_ffn_dense(C, l, final):
    nc, P = C.nc, C.P
    TG = 1024
    with ExitStack() as es:
        sb = lambda n, s, d: es.enter_context(nc.sbuf_tensor(uniq(n), s, d))
        gam = TT(sb("e_gam", [128, D], F32))
        bet = TT(sb("e_bet", [128, D], F32))
        hin = [TT(sb(f"e_hin{i}", [128, D], F32)) for i in range(2)]
        pre = [TT(sb(f"e_pre{i}", [128, D], F32)) for i in range(2)]
        xT = [[TT(sb(f"e_xT{j}_{k}", [128, TG], BF16)) for k in range(8)] for j in range(1)]
        sem_c = P.dma_sem("e_c")
        sem_x = [P.dma_sem("e_x0"), P.dma_sem("e_x1")]
        sem_h = [P.dma_sem("e_h0", sw=True), P.dma_sem("e_h1", sw=True)]
        P.dma("sync", gam.t[:, :], bcast_row(C.d["ln2_g"][l], D), sem_c, W=[gam])
        P.dma("sync", bet.t[:, :], bcast_row(C.d["ln2_b"][l], D), sem_c, W=[bet])
        P.batch(sem_c, [gam, bet])
        L = alloc_ln(C, es, want_T=not final)
        F = alloc_ffn(C, es, TG)
        i = l // 2
        cnt = [0]
        NG = T // TG
        xb = xT[0]

        def load_x(grp):
            for k in range(8):
                P.dma("sync", xb[k].t[:, :], C.hTb[k * 128:(k + 1) * 128, grp * TG:(grp + 1) * TG], sem_x[grp % 2], R=[C.DhTb], W=[xb[k]])
            P.batch(sem_x[grp % 2], xb)

        load_x(0)
        for grp in range(NG):
            def consume(tt, db, grp=grp):
                gt = grp * (TG // 128) + tt
                b = cnt[0] % 2
                cnt[0] += 1
                P.dma("pool", hin[b].t[:, :], C.hb[gt * 128:(gt + 1) * 128, :], sem_h[b], R=[C.Dhb], W=[hin[b]])
                P.op("dve", I("scalar_tensor_tensor", pre[b].t[:, :], hin[b].t[:, :], ALPHA, C.ps2(db), op0=ALU.mult, op1=ALU.add),
                     R=[hin[b], C.PS[db], C.PS[db + 1]], W=[pre[b]])
                if final:
                    ln_tile(C, L, gt, pre[b], gam, bet, (C.Dout, C.out[gt * 128:(gt + 1) * 128, :]), None)
                else:
                    ln_tile(C, L, gt, pre[b], gam, bet, (C.Dha, C.ha[gt * 128:(gt + 1) * 128, :]), (C.DhTa, C.hTa), tr_banks=6)

            more = grp + 1 < NG
            ffn_pass(C, F, xb, C.d["ffn_wg"][i], C.d["ffn_wu"][i], C.d["ffn_wd"][i], consume,
                     nxt=(C.d["ffn_wg"][i], C.d["ffn_wu"][i]) if more else None,
                     mid_hook=(lambda g=grp: load_x(g + 1)) if more else None)
        ln_flush(C, L)
        P.barrier()


def phase_moe(C, l, final, stop=3):
    nc, P = C.nc, C.P
    mi = l // 2
    with ExitStack() as es:
        sb = lambda n, s, d: es.enter_context(nc.sbuf_tensor(uniq(n), s, d))
        selb = TT(sb("m_selb", [128, NT, NE], BF16))
        csel = TT(sb("m_csel", [128, NT, NE], BF16))
        cum = TT(sb("m_cum", [128, NT, NE], F32))
        pos = TT(sb("m_pos", [128, NT, NE], F32))
        pre_ = TT(sb("m_pre", [128, NT, NE], F32))
        tmp = TT(sb("m_tmp", [128, NT, NE], F32))
        tmp2 = TT(sb("m_tmp2", [128, NT, NE], F32))
        iot = TT(sb("m_iot", [128, CAP], F32))
        ecap = TT(sb("m_ecap", [128, NT, NE], F32))
        A = [TT(sb(f"m_A{i}", [128, NT, CAP], BF16)) for i in range(2)]
        tokf = TT(sb("m_tokf", [128, NE, NST], F32))
        sem_c = P.dma_sem("m_c")
        P.dma("sync", iot.t[:, :], C.d["c_iota"], sem_c, W=[iot])
        P.dma("sync", ecap.t[:, :, :], C.d["c_ecap"], sem_c, W=[ecap])
        P.batch(sem_c, [iot, ecap])
        P.op("dve", I("tensor_copy", selb.t[:, :, :], C.sel.t[:, :, :]), R=[C.sel], W=[selb])
        P.op("dve", I("memset", csel.t[:, 0, :], 0.0), W=[csel])
        for tt in range(1, NT):
            P.op("dve", I("tensor_tensor", csel.t[:, tt, :], csel.t[:, tt - 1, :], selb.t[:, tt - 1, :], op=ALU.add), R=[csel, selb], W=[csel])
        bk = 0
        for tt in range(NT):
            o = C.bank(bk)[:, tt * NE:(tt + 1) * NE]
            P.op("pe", I("matmul", o, C.lowI.t[:, :], selb.t[:, tt, :], start=True, stop=False), R=[C.lowI, selb], W=[C.PS[bk]])
            P.op("pe", I("matmul", o, C.ones.t[:, :], csel.t[:, tt, :], start=False, stop=True), R=[C.ones, csel], W=[C.PS[bk]])
        P.op("dve", I("tensor_copy", cum.t[:, :, :], C.bank(bk)[:, 0:NT * NE].rearrange("p (a b) -> p a b", b=NE)), R=[C.PS[bk]], W=[cum])
        P.op("dve", I("tensor_tensor", pos.t[:, :, :], cum.t[:, :, :], C.sel.t[:, :, :], op=ALU.subtract), R=[cum, C.sel], W=[pos])
        P.op("dve", I("memset", pre_.t[:, :, :], 0.0), W=[pre_])
        P.op("dve", I("tensor_copy", pre_.t[:, :, 1:NE], C.sel.t[:, :, 0:NE - 1]), R=[C.sel], W=[pre_])
        for sh in (1, 2, 4):
            P.op("dve", I("tensor_copy", tmp.t[:, :, :], pre_.t[:, :, :]), R=[pre_], W=[tmp])
            P.op("dve", I("tensor_tensor", pre_.t[:, :, sh:NE], tmp.t[:, :, sh:NE], tmp.t[:, :, 0:NE - sh], op=ALU.add), R=[tmp], W=[pre_])
        P.op("dve", I("tensor_tensor", pos.t[:, :, :], pos.t[:, :, :], ecap.t[:, :, :], op=ALU.add), R=[pos, ecap], W=[pos])
        for kk in range(2):
            P.op("dve", I("tensor_scalar", tmp.t[:, :, :], pre_.t[:, :, :], float(kk), None, op0=ALU.is_equal), R=[pre_], W=[tmp])
            P.op("dve", I("tensor_tensor", tmp.t[:, :, :], tmp.t[:, :, :], C.sel.t[:, :, :], op=ALU.mult), R=[tmp, C.sel], W=[tmp])
            P.op("dve", I("tensor_tensor", tmp2.t[:, :, :], tmp.t[:, :, :], pos.t[:, :, :], op=ALU.mult), R=[tmp, pos], W=[tmp2])
            P.op("dve", I("tensor_reduce", C.slotf.t[:, kk, :], tmp2.t[:, :, :], AX.X, ALU.add), R=[tmp2], W=[C.slotf])
            P.op("dve", I("tensor_tensor", tmp2.t[:, :, :], tmp.t[:, :, :], C.gate.t[:, :, :], op=ALU.mult), R=[tmp, C.gate], W=[tmp2])
            P.op("dve", I("tensor_reduce", C.gsel.t[:, kk, :], tmp2.t[:, :, :], AX.X, ALU.add), R=[tmp2], W=[C.gsel])
        P.op("dve", I("tensor_copy", C.sloti.t[:, :, :], C.slotf.t[:, :, :]), R=[C.slotf], W=[C.sloti])
        ai = 0
        for e in range(NE):
            bk = 1 + (e % 2)
            Ab = A[e % 2]
            for tt in range(NT):
                eng = "dve"
                ai += 1
                P.op(eng, I("tensor_scalar", Ab.t[:, tt, :], iot.t[:, :], cum.t[:, tt, e:e + 1], None, op0=ALU.is_ge), R=[iot, cum], W=[Ab])
            for st in range(NST):
                for tt in range(NT):
                    P.op("pe", I("matmul", C.bank(bk)[:, st:st + 1], Ab.t[:, tt, st * 128:(st + 1) * 128], C.ones.t[:, 0:1],
                                 start=(tt == 0), stop=(tt == NT - 1)), R=[Ab, C.ones], W=[C.PS[bk]])
            P.op("dve", I("tensor_scalar", tokf.t[:, e, :], C.bank(bk)[:, 0:NST], float(T - 1), None, op0=ALU.min), R=[C.PS[bk]], W=[tokf])
        P.op("dve", I("tensor_copy", C.toki.t[:, :, :], tokf.t[:, :, :]), R=[tokf], W=[C.toki])
        P.barrier()
    if stop < 2:
        return
    with ExitStack() as es:
        sb = lambda n, s, d: es.enter_context(nc.sbuf_tensor(uniq(n), s, d))
        xg = [TT(sb(f"m_xg{i}", [128, D], F32)) for i in range(2)]
        xT = [[TT(sb(f"m_xT{j}_{k}", [128, CAP], BF16)) for k in range(8)] for j in range(2)]
        yst = [TT(sb(f"m_yst{i}", [128, D], F32)) for i in range(2)]
        sem_g = [P.dma_sem("m_g0", sw=True), P.dma_sem("m_g1", sw=True)]
        sem_y = [P.dma_sem("m_y0"), P.dma_sem("m_y1")]
        F = alloc_ffn(C, es, CAP)
        yi = [0]

        def emit_gather(e, st):
            b = st % 2
            P.gather(xg[b].t[:, :], C.hb, C.toki.t[:, e, st:st + 1], sem_g[b], R=[C.Dhb, C.toki], W=[xg[b]])

        def emit_tr(e, st):
            b = st % 2
            xb = xT[e % 2]
            bk = 6
            flat = C.ps2(bk)
            for k in range(8):
                P.op("pe", I("transpose", flat[:, k * 128:(k + 1) * 128], xg[b].t[:, k * 128:(k + 1) * 128], C.ident.t[:, :]),
                     R=[xg[b], C.ident], W=[C.PS[bk], C.PS[bk + 1]])
            for k in range(8):
                if k % 2 == 0:
                    P.op("act", I("activation", out=xb[k].t[:, st * 128:(st + 1) * 128], in_=flat[:, k * 128:(k + 1) * 128], func=AF.Copy),
                         R=[C.PS[bk], C.PS[bk + 1]], W=[xb[k]])
                else:
                    P.op("dve", I("tensor_copy", xb[k].t[:, st * 128:(st + 1) * 128], flat[:, k * 128:(k + 1) * 128]),
                         R=[C.PS[bk], C.PS[bk + 1]], W=[xb[k]])

        for st in range(NST):
            emit_gather(0, st)
            emit_tr(0, st)
        for e in range(NE):
            xb = xT[e % 2]

            def consume(tt, db, e=e):
                b = yi[0] % 2
                yi[0] += 1
                if b == 0:
                    P.op("act", I("activation", out=yst[b].t[:, :], in_=C.ps2(db), func=AF.Copy), R=[C.PS[db], C.PS[db + 1]], W=[yst[b]])
                else:
                    P.op("dve", I("tensor_copy", yst[b].t[:, :], C.ps2(db)), R=[C.PS[db], C.PS[db + 1]], W=[yst[b]])
                r0 = e * CAP + tt * 128
                P.dma("sync", C.ybuf[r0:r0 + 128, :], yst[b].t[:, :], sem_y[b], R=[yst[b]], W=[C.Dyb])

            def hook(fb, e=e):
                if e + 1 < NE:
                    if fb < NST:
                        emit_gather(e + 1, fb)
                    if 1 <= fb <= NST:
                        emit_tr(e + 1, fb - 1)

            ffn_pass(C, F, xb, C.d["moe_wg"][mi][e], C.d["moe_wu"][mi][e], C.d["moe_wd"][mi][e], consume, blk_hook=hook)
        P.barrier()
    if stop < 3:
        return
    with ExitStack() as es:
        sb = lambda n, s, d: es.enter_context(nc.sbuf_tensor(uniq(n), s, d))
        gam = TT(sb("n_gam", [128, D], F32))
        bet = TT(sb("n_bet", [128, D], F32))
        hin = [TT(sb(f"n_hin{i}", [128, D], F32)) for i in range(4)]
        y0 = [TT(sb(f"n_y0{i}", [128, D], F32)) for i in range(4)]
        y1 = [TT(sb(f"n_y1{i}", [128, D], F32)) for i in range(4)]
        pre = [TT(sb(f"n_pre{i}", [128, D], F32)) for i in range(2)]
        sem_c = P.dma_sem("n_c")
        sem_h = [P.dma_sem(f"n_h{i}", sw=True) for i in range(4)]
        sem_a = [P.dma_sem(f"n_a{i}", sw=True) for i in range(4)]
        sem_b = [P.dma_sem(f"n_b{i}", sw=True) for i in range(4)]
        P.dma("sync", gam.t[:, :], bcast_row(C.d["ln2_g"][l], D), sem_c, W=[gam])
        P.dma("sync", bet.t[:, :], bcast_row(C.d["ln2_b"][l], D), sem_c, W=[bet])
        P.batch(sem_c, [gam, bet])
        L = alloc_ln(C, es, want_T=not final)
        for tt in range(NT):
            b = tt % 2
            r_ = tt % 4
            P.dma("pool", hin[r_].t[:, :], C.hb[tt * 128:(tt + 1) * 128, :], sem_h[r_], R=[C.Dhb], W=[hin[r_]])
            P.gather(y0[r_].t[:, :], C.ybuf, C.sloti.t[:, 0, tt:tt + 1], sem_a[r_], R=[C.Dyb, C.sloti], W=[y0[r_]])
            P.gather(y1[r_].t[:, :], C.ybuf, C.sloti.t[:, 1, tt:tt + 1], sem_b[r_], R=[C.Dyb, C.sloti], W=[y1[r_]])
            P.op("act", I("activation", out=y0[r_].t[:, :], in_=y0[r_].t[:, :], func=AF.Identity, scale=C.gsel.t[:, 0, tt:tt + 1]),
                 R=[y0[r_], C.gsel], W=[y0[r_]])
            P.op("dve", I("scalar_tensor_tensor", pre[b].t[:, :], hin[r_].t[:, :], ALPHA, y0[r_].t[:, :], op0=ALU.mult, op1=ALU.add),
                 R=[y0[r_], hin[r_]], W=[pre[b]])
            P.op("dve", I("scalar_tensor_tensor", pre[b].t[:, :], y1[r_].t[:, :], C.gsel.t[:, 1, tt:tt + 1], pre[b].t[:, :], op0=ALU.mult, op1=ALU.add),
                 R=[y1[r_], C.gsel, pre[b]], W=[pre[b]])
            if final:
                ln_tile(C, L, tt, pre[b], gam, bet, (C.Dout, C.out[tt * 128:(tt + 1) * 128, :]), None)
            else:
                ln_tile(C, L, tt, pre[b], gam, bet, (C.Dha, C.ha[tt * 128:(tt + 1) * 128, :]), (C.DhTa, C.hTa), tr_banks=0 + 2 * b)
        ln_flush(C, L)
        P.barrier()


IN_SPECS = None


def host_constants():
    c = {}
    c["c_ident"] = np.eye(128, dtype=np.float32)
    j = np.arange(128)[:, None]
    s = np.arange(128)[None, :]
    c["c_negU"] = -(j >= s).astype(np.float32)
    c["c_negO"] = -np.ones((128, 128), np.float32)
    c["c_lowI"] = (j <= s).astype(np.float32)
    c["c_ones"] = np.ones((128, 128), np.float32)
    p = np.arange(128)[:, None, None]
    dj = np.arange(4)[None, :, None]
    col = np.arange(512)[None, None, :]
    c["c_masks"] = ((128 * (3 - dj) + p) < col).astype(np.float32)
    invc = np.zeros((128, 4, 16), np.float32)
    for g in range(4):
        w = 2 << g
        invc[:, g, :] = 1.0 / np.minimum(np.arange(16) + 1, w)
    c["c_invc"] = invc
    c["c_iota"] = np.broadcast_to(np.arange(CAP, dtype=np.float32)[None, :], (128, CAP)).copy()
    c["c_ecap"] = np.broadcast_to((np.arange(NE, dtype=np.float32) * CAP)[None, None, :], (128, NT, NE)).copy()
    return c


def host_layout(inp):
    o = {}
    f = lambda a: np.ascontiguousarray(a, dtype=np.float32)
    o["ln_in_g"] = f(inp["ln_in_g"])
    o["ln_in_b"] = f(inp["ln_in_b"])
    o["w_in"] = f(inp["w_in"].reshape(DEPTH, 8, 128, 2048).transpose(0, 2, 1, 3))
    o["pool_w"] = f(inp["pool_w"].transpose(0, 2, 1, 3))
    o["pool_scale"] = f(inp["pool_scale"].reshape(DEPTH, 4, 128).transpose(0, 2, 1))
    o["w_out"] = f(inp["w_out"].reshape(DEPTH, 8, 128, D).transpose(0, 2, 1, 3))
    for nm in ("ln1_g", "ln1_b", "ln2_g", "ln2_b"):
        o[nm] = f(inp[nm])
    o["ffn_wg"] = f(inp["ffn_wg"].reshape(-1, 8, 128, NFC, 128).transpose(0, 3, 2, 1, 4))
    o["ffn_wu"] = f(inp["ffn_wu"].reshape(-1, 8, 128, NFC, 128).transpose(0, 3, 2, 1, 4))
    o["ffn_wd"] = f(inp["ffn_wd"])
    o["moe_router"] = f(inp["moe_router"].reshape(-1, 8, 128, NE).transpose(0, 2, 1, 3))
    o["moe_wg"] = f(inp["moe_wg"].reshape(-1, NE, 8, 128, NFC, 128).transpose(0, 1, 4, 3, 2, 5))
    o["moe_wu"] = f(inp["moe_wu"].reshape(-1, NE, 8, 128, NFC, 128).transpose(0, 1, 4, 3, 2, 5))
    o["moe_wd"] = f(inp["moe_wd"])
    o.update(host_constants())
    return o


def build(shapes, phases="all", debug_out=()):
    nc = bass.Bass("TRN2", target_bir_lowering=False)
    C = Ctx()
    C.nc = nc
    C.d = {}
    for name, shp in shapes.items():
        C.d[name] = nc.dram_tensor(name, list(shp), F32, kind="ExternalInput").ap()
    kind = lambda n: "ExternalOutput" if n in debug_out else "Internal"
    C.out = nc.dram_tensor("out", [T, D], F32, kind="ExternalOutput").ap()
    C.ha = nc.dram_tensor("s_ha", [T, D], F32, kind=kind("s_ha")).ap()
    C.hb = nc.dram_tensor("s_hb", [T, D], F32, kind=kind("s_hb")).ap()
    C.hTa = nc.dram_tensor("s_hTa", [D, T], BF16, kind=kind("s_hTa")).ap()
    C.hTb = nc.dram_tensor("s_hTb", [D, T], BF16, kind=kind("s_hTb")).ap()
    C.qT = nc.dram_tensor("s_qT", [512, T], BF16, kind=kind("s_qT")).ap()
    C.kT = nc.dram_tensor("s_kT", [512, T], BF16, kind=kind("s_kT")).ap()
    C.v = nc.dram_tensor("s_v", [T, 512], BF16, kind=kind("s_v")).ap()
    C.ycT = nc.dram_tensor("s_ycT", [D, T], BF16, kind=kind("s_ycT")).ap()
    C.ybuf = nc.dram_tensor("s_ybuf", [NE * CAP, D], F32, kind=kind("s_ybuf")).ap()
    C.Dout, C.Dha, C.Dhb, C.DhTa, C.DhTb, C.Dv, C.Dyb = TT(None), TT(None), TT(None), TT(None), TT(None), TT(None), TT(None)
    C.DqT = [TT(None) for _ in range(4)]
    C.DkT = [TT(None) for _ in range(4)]
    C.DycT = [TT(None) for _ in range(8)]
    with ExitStack() as es:
        P = Prog(nc, es)
        C.P = P
        sb = lambda n, s, d: es.enter_context(nc.sbuf_tensor(uniq(n), s, d))
        ps = es.enter_context(nc.psum_tensor("ps", [128, 8, 512], F32))
        C.ps = ps
        C.PS = [TT(None, f"bank{i}", excl=True) for i in range(8)]
        C.bank = lambda b: ps[:, b, :]
        C.ps2 = lambda b: ps[:, b:b + 2, :].rearrange("p a b -> p (a b)")
        C.ps2v = lambda b, a, w: ps[:, b:b + 2, :].rearrange("p a b -> p (a b)").rearrange("p (a w) -> p a w", w=w)
        C.ident = TT(sb("k_ident", [128, 128], F32))
        C.negU = TT(sb("k_negU", [128, 128], BF16))
        C.negO = TT(sb("k_negO", [128, 128], BF16))
        C.lowI = TT(sb("k_lowI", [128, 128], BF16))
        C.ones = TT(sb("k_ones", [128, 128], BF16))
        C.masks = TT(sb("k_masks", [128, 4, 512], BF16))
        C.gate = TT(sb("k_gate", [128, NT, NE], F32))
        C.sel = TT(sb("k_sel", [128, NT, NE], F32))
        C.slotf = TT(sb("k_slotf", [128, 2, NT], F32))
        C.sloti = TT(sb("k_sloti", [128, 2, NT], U32))
        C.gsel = TT(sb("k_gsel", [128, 2, NT], F32))
        C.toki = TT(sb("k_toki", [128, NE, NST], U32))
        sem_k = P.dma_sem("k_c", sw=True)
        sem_k2 = P.dma_sem("k_c2")
        P.dma("sync", C.ident.t[:, :], C.d["c_ident"], sem_k2, W=[C.ident])
        P.dma("pool", C.negU.t[:, :], C.d["c_negU"], sem_k, W=[C.negU])
        P.dma("pool", C.negO.t[:, :], C.d["c_negO"], sem_k, W=[C.negO])
        P.dma("pool", C.lowI.t[:, :], C.d["c_lowI"], sem_k, W=[C.lowI])
        P.dma("pool", C.ones.t[:, :], C.d["c_ones"], sem_k, W=[C.ones])
        P.dma("pool", C.masks.t[:, :, :], C.d["c_masks"], sem_k, W=[C.masks])
        P.batch(sem_k, [C.negU, C.negO, C.lowI, C.ones, C.masks])

        def want(p):
            if phases == "all":
                return not (len(p) == 3 and p[2] in "ab")
            return p in phases

        if want("A"):
            phase_ln_in(C)
        for l in range(DEPTH):
            final = (l == DEPTH - 1)
            if want(f"B{l}"):
                phase_proj(C, l)
            if want(f"C{l}"):
                phase_attn(C, l)
            if want(f"D{l}"):
                phase_mix_ln1(C, l)
            if want(f"E{l}") or want(f"E{l}a") or want(f"E{l}b"):
                if l % 2 == 0:
                    phase_ffn_dense(C, l, final)
                else:
                    phase_moe(C, l, final, stop=(1 if want(f"E{l}a") else 2 if want(f"E{l}b") else 3))
        if "k_dbg" in debug_out:
            sem_d = P.dma_sem("dbg")
            for nm, tt_, shp, dt in (("k_gate", C.gate, [128, NT, NE], F32), ("k_sel", C.sel, [128, NT, NE], F32),
                                     ("k_slotf", C.slotf, [128, 2, NT], F32), ("k_sloti", C.sloti, [128, 2, NT], U32),
                                     ("k_gsel", C.gsel, [128, 2, NT], F32), ("k_toki", C.toki, [128, NE, NST], U32)):
                dd_ = nc.dram_tensor("dbg_" + nm, shp, dt, kind="ExternalOutput").ap()
                P.dma("sync", dd_, tt_.t[:, :, :], sem_d, R=[tt_])
        P.barrier()
        print("bass ops:", P.nops, {e: len(P.q[e]) for e in ENG})
        P.flush()
    return nc


_CACHE = {}


def kernel(**inputs):
    lay = host_layout(inputs)
    x = np.ascontiguousarray(inputs["x"], dtype=np.float32)
    shapes = {"x": (T, D)}
    for k, v in lay.items():
        shapes[k] = v.shape
    if "nc" not in _CACHE:
        import os
        dbg = ("s_ha", "s_hb", "s_hTa", "s_hTb", "s_qT", "s_kT", "s_v", "s_ycT", "s_ybuf") if os.environ.get("SCRATCH_OUT") == "1" else ()
        _CACHE["nc"] = build(shapes, debug_out=dbg)
    nc = _CACHE["nc"]
    in_maps = []
    for b in range(NCORES):
        m = {"x": x[b]}
        m.update(lay)
        in_maps.append(m)
    res = run_bass_kernel_spmd(nc, in_maps, core_ids=list(range(NCORES)))
    out = np.stack([np.asarray(res.results[b]["out"], dtype=np.float32) for b in range(NCORES)], axis=0)
    return out
```

```python
import numpy as np
from contextlib import ExitStack
import concourse.bass as bass
import concourse.mybir as mybir
from concourse.bass_utils import run_bass_kernel_spmd

F32 = mybir.dt.float32
BF16 = mybir.dt.bfloat16
I32 = mybir.dt.int32
U32 = mybir.dt.uint32
AF = mybir.ActivationFunctionType
ALU = mybir.AluOpType
AX = mybir.AxisListType

NCORES = 8
T = 4096
D = 1024
NT = T // 128
DEPTH = 2
DFF = 2816
NFC = DFF // 128
NE = 8
CAP = 1280
NST = CAP // 128
ALPHA = float((2 * DEPTH) ** 0.25)
EPS = 1e-5
ENG = ("sync", "act", "pe", "dve", "pool")


_UNIQ = [0]


def uniq(n):
    _UNIQ[0] += 1
    return f"{n}_{_UNIQ[0]}"


def I(meth, *a, **k):
    return lambda e: getattr(e, meth)(*a, **k)


class TT:
    __slots__ = ("t", "w", "r", "name", "excl")

    def __init__(self, t, name="", excl=False):
        self.t = t
        self.w = None
        self.r = {}
        self.name = name
        self.excl = excl


class Prog:
    def __init__(self, nc, es):
        self.nc = nc
        self.es = es
        self.q = {e: [] for e in ENG}
        self.waited = {e: {} for e in ENG}
        self.psem = {e: self._mksem("p_" + e) for e in ENG}
        self.dsems = []
        self.dpool_sw = []
        self.dpool_hw = []
        self.nops = 0

    def _mksem(self, name):
        h = self.es.enter_context(self.nc.semaphore(name))
        return {"h": h, "n": 0, "name": name}

    def dma_sem(self, name, sw=False):
        pool = self.dpool_sw if sw else self.dpool_hw
        if pool:
            s = pool.pop()
        else:
            s = self._mksem(uniq(name))
            s["sw"] = sw
        self.dsems.append(s)
        return s

    def _wait(self, eng, dep):
        s, v = dep
        if eng == "pe" and s is self.psem["pe"]:
            return
        if self.waited[eng].get(s["name"], 0) >= v:
            return
        self.waited[eng][s["name"]] = v
        self.q[eng].append(("wait", s["h"], v))

    def _deps(self, R, W):
        deps = []
        for b in R:
            if b.w is not None:
                deps.append(b.w)
        for b in W:
            if b.w is not None:
                deps.append(b.w)
            deps.extend(b.r.values())
        return deps

    def _mark(self, dep, R, W):
        for b in R:
            b.r[dep[0]["name"]] = dep
        for b in W:
            b.w = dep
            b.r = {}

    def op(self, eng, fn, R=(), W=()):
        if any(b.excl for b in R):
            W = list(W) + [b for b in R if b.excl and b not in W]
            R = [b for b in R if not b.excl]
        for d in self._deps(R, W):
            self._wait(eng, d)
        s = self.psem[eng]
        s["n"] += 1
        dep = (s, s["n"])
        self.q[eng].append(("op", fn, s["h"], 1))
        self._mark(dep, R, W)
        self.nops += 1
        return dep

    def dma(self, eng, out, in_, sem, R=(), W=()):
        assert bool(sem.get("sw")) == (eng == "pool"), (eng, sem["name"])
        for d in self._deps(R, W):
            if d[0] is sem:
                continue
            self._wait(eng, d)
        sem["n"] += 16
        dep = (sem, sem["n"])
        self.q[eng].append(("op", lambda e: e.dma_start(out=out, in_=in_), sem["h"], 16))
        self._mark(dep, R, W)
        self.nops += 1
        return dep

    def batch(self, sem, tts):
        for b in tts:
            b.w = (sem, sem["n"])

    def gather(self, out, in_, idx_ap, sem, R=(), W=()):
        eng = "pool"
        for d in self._deps(R, W):
            self._wait(eng, d)
        sem["n"] += 16
        dep = (sem, sem["n"])
        self.q[eng].append(("op", lambda e: e.indirect_dma_start(
            out=out, out_offset=None, in_=in_,
            in_offset=bass.IndirectOffsetOnAxis(ap=idx_ap, axis=0)), sem["h"], 16))
        self._mark(dep, R, W)
        self.nops += 1
        return dep

    def barrier(self):
        sems = list(self.psem.values()) + self.dsems
        for e in ENG:
            for s in sems:
                if s["n"] > 0:
                    self._wait(e, (s, s["n"]))
        for s_ in self.dsems:
            (self.dpool_sw if s_.get("sw") else self.dpool_hw).append(s_)
        self.dsems = []
        for e in ENG:
            if self.psem[e]["n"] > 8000:
                self.psem[e] = self._mksem(uniq("p_" + e))

    def flush(self):
        nc = self.nc
        q = self.q

        def run(e, lst):
            for it in lst:
                if it[0] == "wait":
                    e.wait_ge(it[1], it[2])
                else:
                    ins = it[1](e)
                    ins.then_inc(it[2], it[3])

        with nc.Block() as block:
            @block.sync
            def _(e):
                run(e, q["sync"])

            @block.scalar
            def _(e):
                run(e, q["act"])

            @block.tensor
            def _(e):
                run(e, q["pe"])

            @block.vector
            def _(e):
                run(e, q["dve"])

            @block.gpsimd
            def _(e):
                run(e, q["pool"])


class Ctx:
    pass


def bcast_row(ap1d, n):
    return bass.AP(ap1d.tensor, ap1d.offset, [[0, 128], [1, n]])


def alloc_ln(C, es, want_T=True):
    nc, P = C.nc, C.P
    L = Ctx()
    sb = lambda n, s, d: es.enter_context(nc.sbuf_tensor(uniq(n), s, d))
    L.stats = [TT(sb(f"ln_stats{i}", [128, 12], F32)) for i in range(2)]
    L.mv = [TT(sb(f"ln_mv{i}", [128, 8], F32)) for i in range(2)]
    L.xn = [TT(sb(f"ln_xn{i}", [128, D], F32)) for i in range(2)]
    L.ho = [TT(sb(f"ln_ho{i}", [128, D], F32)) for i in range(2)]
    L.want_T = want_T
    if want_T:
        L.hTs = [TT(sb(f"ln_hTs{i}", [128, 8, 512], BF16)) for i in range(2)]
    L.sem_st = [P.dma_sem("ln_st0"), P.dma_sem("ln_st1")]
    L.sem_sT = [P.dma_sem("ln_sT0"), P.dma_sem("ln_sT1")]
    L.cnt = 0
    L.pend = []
    return L


def ln_tile(C, L, tt, pre, gam, bet, h_dst, hT_dst, tr_banks=None, router=None):
    i = L.cnt
    L.cnt += 1
    L.pend.append(dict(i=i, tt=tt, pre=pre, gam=gam, bet=bet, h_dst=h_dst, hT_dst=hT_dst, tr=tr_banks, router=router))
    _ln_step(C, L)


def ln_flush(C, L):
    for _ in range(2):
        L.pend.append(None)
        _ln_step(C, L)
    L.pend = []


def _ln_step(C, L):
    P = C.P
    n = len(L.pend)
    t0 = L.pend[n - 1]
    t1 = L.pend[n - 2] if n >= 2 else None
    t2 = L.pend[n - 3] if n >= 3 else None
    if t0 is not None:
        b = t0["i"] % 2
        st, mv, pre = L.stats[b], L.mv[b], t0["pre"]
        P.op("dve", I("bn_stats", st.t[:, 0:6], pre.t[:, 0:512]), R=[pre], W=[st])
        P.op("dve", I("bn_stats", st.t[:, 6:12], pre.t[:, 512:1024]), R=[pre], W=[st])
        P.op("dve", I("bn_aggr", mv.t[:, 0:2], st.t[:, :]), R=[st], W=[mv])
        P.op("dve", I("tensor_scalar_add", mv.t[:, 2:3], mv.t[:, 1:2], EPS), R=[mv], W=[mv])
    if t0 is not None:
        b = t0["i"] % 2
        mv = L.mv[b]
        P.op("act", I("activation", out=mv.t[:, 3:4], in_=mv.t[:, 2:3], func=AF.Ln), R=[mv], W=[mv])
        P.op("act", I("activation", out=mv.t[:, 4:5], in_=mv.t[:, 3:4], func=AF.Exp, scale=-0.5), R=[mv], W=[mv])
    if t1 is not None:
        b = t1["i"] % 2
        mv, xn, ho, pre = L.mv[b], L.xn[b], L.ho[b], t1["pre"]
        P.op("dve", I("scalar_tensor_tensor", xn.t[:, :], pre.t[:, :], mv.t[:, 0:1], t1["gam"].t[:, :], op0=ALU.subtract, op1=ALU.mult),
             R=[pre, mv, t1["gam"]], W=[xn])
        P.op("dve", I("scalar_tensor_tensor", ho.t[:, :], xn.t[:, :], mv.t[:, 4:5], t1["bet"].t[:, :], op0=ALU.mult, op1=ALU.add),
             R=[xn, mv, t1["bet"]], W=[ho])
        P.dma("sync", t1["h_dst"][1], ho.t[:, :], L.sem_st[b], R=[ho], W=[t1["h_dst"][0]])
        if t1["router"] is not None:
            t1["router"](t1["tt"], ho)
    if t2 is not None and t2["hT_dst"] is not None:
        b = t2["i"] % 2
        ho = L.ho[b]
        tt = t2["tt"]
        slot = tt % 4
        hTs = L.hTs[(tt // 4) % 2]
        bk = t2["tr"]
        flat = C.ps2(bk)
        for k in range(8):
            P.op("pe", I("transpose", flat[:, k * 128:(k + 1) * 128], ho.t[:, k * 128:(k + 1) * 128], C.ident.t[:, :]),
                 R=[ho, C.ident], W=[C.PS[bk], C.PS[bk + 1]])
        P.op("act", I("activation", out=hTs.t[:, :, slot * 128:(slot + 1) * 128],
                      in_=C.ps2v(bk, 8, 128), func=AF.Copy),
             R=[C.PS[bk], C.PS[bk + 1]], W=[hTs])
        if slot == 3:
            c0 = (tt - 3) * 128
            dst = t2["hT_dst"][1].rearrange("(c p) t -> p c t", p=128)[:, :, c0:c0 + 512]
            P.dma("sync", dst, hTs.t[:, :, :], L.sem_sT[(tt // 4) % 2], R=[hTs], W=[t2["hT_dst"][0]])


def phase_ln_in(C):
    nc, P = C.nc, C.P
    with ExitStack() as es:
        sb = lambda n, s, d: es.enter_context(nc.sbuf_tensor(uniq(n), s, d))
        gam = TT(sb("a_gam", [128, D], F32))
        bet = TT(sb("a_bet", [128, D], F32))
        NXB = 4
        xin = [TT(sb(f"a_xin{i}", [128, D], F32)) for i in range(NXB)]
        sem_c = P.dma_sem("a_c")
        sem_x = [P.dma_sem(f"a_x{i}", sw=True) for i in range(NXB)]
        P.dma("sync", gam.t[:, :], bcast_row(C.d["ln_in_g"], D), sem_c, W=[gam])
        P.dma("sync", bet.t[:, :], bcast_row(C.d["ln_in_b"], D), sem_c, W=[bet])
        P.batch(sem_c, [gam, bet])
        L = alloc_ln(C, es)
        for tt in range(NT):
            b = tt % NXB
            P.dma("pool", xin[b].t[:, :], C.d["x"][tt * 128:(tt + 1) * 128, :], sem_x[b], W=[xin[b]])
            ln_tile(C, L, tt, xin[b], gam, bet, (C.Dha, C.ha[tt * 128:(tt + 1) * 128, :]), (C.DhTa, C.hTa), tr_banks=0 + 2 * (tt % 2))
        ln_flush(C, L)
        P.barrier()


def phase_proj(C, l):
    nc, P = C.nc, C.P
    with ExitStack() as es:
        sb = lambda n, s, d: es.enter_context(nc.sbuf_tensor(uniq(n), s, d))
        wi = TT(sb("b_wi", [128, 8, 2048], BF16))
        hT = [TT(sb(f"b_hT{k}", [128, T], BF16)) for k in range(8)]
        pw = TT(sb("b_pw", [128, 4, 128], BF16))
        psc = TT(sb("b_psc", [128, 4], F32))
        invc = TT(sb("b_invc", [128, 4, 16], F32))
        U = TT(sb("b_U", [128, 16 + T], F32))
        SA = TT(sb("b_SA", [128, 16 + T], F32))
        SB = TT(sb("b_SB", [128, 16 + T], F32))
        dd = TT(sb("b_d", [128, T], BF16))
        stg = [TT(sb(f"b_stg{i}", [128, T], BF16)) for i in range(2)]
        vst = [TT(sb(f"b_vst{i}", [128, 4, 512], BF16)) for i in range(2)]
        sem_w = P.dma_sem("b_w", sw=True)
        sem_h = P.dma_sem("b_h")
        sem_c = P.dma_sem("b_c")
        sem_cw = P.dma_sem("b_cw", sw=True)
        sem_s = [P.dma_sem("b_s0"), P.dma_sem("b_s1")]
        sem_v = [P.dma_sem("b_v0"), P.dma_sem("b_v1")]
        for q4 in range(4):
            P.dma("pool", wi.t[:, :, q4 * 512:(q4 + 1) * 512], C.d["w_in"][l][:, :, q4 * 512:(q4 + 1) * 512], sem_w, W=[wi])
        for k in range(8):
            P.dma("sync", hT[k].t[:, :], C.hTa[k * 128:(k + 1) * 128, :], sem_h, R=[C.DhTa], W=[hT[k]])
        P.dma("pool", pw.t[:, :, :], C.d["pool_w"][l], sem_cw, W=[pw])
        P.dma("sync", psc.t[:, :], C.d["pool_scale"][l], sem_c, W=[psc])
        P.dma("sync", invc.t[:, :, :], C.d["c_invc"], sem_c, W=[invc])
        P.batch(sem_c, [psc, invc])
        P.batch(sem_h, hT)
        for buf in (U, SA, SB):
            P.op("dve", I("memset", buf.t[:, 0:16], 0.0), W=[buf])

        nbank = [0]

        def next_bank():
            b = nbank[0] % 4
            nbank[0] += 1
            return b

        evac_eng = [0]

        def evac(out_ap, in_ap, R, W, scale=None, only_act=False):
            e = 0 if only_act else evac_eng[0] % 2
            evac_eng[0] += 1
            if e == 0:
                if scale is None:
                    P.op("act", I("activation", out=out_ap, in_=in_ap, func=AF.Copy), R=R, W=W)
                else:
                    P.op("act", I("activation", out=out_ap, in_=in_ap, func=AF.Identity, scale=scale), R=R, W=W)
            else:
                if scale is None:
                    P.op("dve", I("tensor_copy", out_ap, in_ap), R=R, W=W)
                else:
                    P.op("dve", I("tensor_scalar_mul", out_ap, in_ap, scale), R=R, W=W)

        def fm_chunk(c, out_fn):
            for tg in range(8):
                bk = next_bank()
                for k in range(8):
                    P.op("pe", I("matmul", C.bank(bk), wi.t[:, k, c * 128:(c + 1) * 128], hT[k].t[:, tg * 512:(tg + 1) * 512],
                                 start=(k == 0), stop=(k == 7)), R=[wi, hT[k]], W=[C.PS[bk]])
                out_fn(tg, bk)

        def qk_chunk(c, only_act=False):
            so = stg[sidx[0] % 2]
            ss = sem_s[sidx[0] % 2]
            sidx[0] += 1
            sc = 0.125 if c < 8 else None
            fm_chunk(c, lambda tg, bk, so=so, sc=sc: evac(so.t[:, tg * 512:(tg + 1) * 512], C.bank(bk), [C.PS[bk]], [so], scale=sc, only_act=only_act))
            if c < 8:
                P.dma("sync", C.qT[(c - 4) * 128:(c - 3) * 128, :], so.t[:, :], ss, R=[so], W=[C.DqT[c - 4]])
            else:
                P.dma("sync", C.kT[(c - 8) * 128:(c - 7) * 128, :], so.t[:, :], ss, R=[so], W=[C.DkT[c - 8]])

        sidx = [0]
        for g in range(4):
            w = 2 << g
            fm_chunk(g, lambda tg, bk: evac(U.t[:, 16 + tg * 512:16 + (tg + 1) * 512], C.bank(bk), [C.PS[bk]], [U]))
            src = U
            bufs = [SA, SB]
            nlev = g + 1
            for lev in range(nlev):
                dst = bufs[lev % 2]
                sh = 1 << lev
                eng = "dve"
                P.op(eng, I("tensor_tensor", dst.t[:, 16:16 + T], src.t[:, 16:16 + T], src.t[:, 16 - sh:16 + T - sh], op=ALU.add),
                     R=[src], W=[dst])
                src = dst
            P.op("dve", I("scalar_tensor_tensor", dd.t[:, :], src.t[:, 16:16 + T], 1.0 / w, U.t[:, 16:16 + T],
                          op0=ALU.mult, op1=ALU.subtract), R=[src, U], W=[dd])
            P.op("dve", I("tensor_tensor", SA.t[:, 0:16] if src is SB else SB.t[:, 0:16], src.t[:, 16:32], invc.t[:, g, :], op=ALU.mult),
                 R=[src, invc], W=[SA if src is SB else SB])
            tmp = SA if src is SB else SB
            P.op("dve", I("tensor_tensor", dd.t[:, 0:16], tmp.t[:, 0:16], U.t[:, 16:32], op=ALU.subtract),
                 R=[tmp, U], W=[dd])
            P.op("dve", I("memset", tmp.t[:, 0:16], 0.0), W=[tmp])
            qk_chunk(4 + 2 * g, only_act=True)
            qk_chunk(5 + 2 * g, only_act=True)
            so = stg[sidx[0] % 2]
            ss = sem_s[sidx[0] % 2]
            sidx[0] += 1
            for tg in range(8):
                bk = next_bank()
                P.op("pe", I("matmul", C.bank(bk), pw.t[:, g, :], dd.t[:, tg * 512:(tg + 1) * 512], start=True, stop=True),
                     R=[pw, dd], W=[C.PS[bk]])
                evac(so.t[:, tg * 512:(tg + 1) * 512], C.bank(bk), [C.PS[bk], psc], [so], scale=psc.t[:, g:g + 1])
            P.dma("sync", C.ycT[g * 128:(g + 1) * 128, :], so.t[:, :], ss, R=[so], W=[C.DycT[g]])
        vv = C.v.rearrange("(n p) c -> p n c", p=128)
        for tt in range(NT):
            bk = next_bank()
            for k in range(8):
                P.op("pe", I("matmul", C.bank(bk), hT[k].t[:, tt * 128:(tt + 1) * 128], wi.t[:, k, 1536:2048],
                             start=(k == 0), stop=(k == 7)), R=[wi, hT[k]], W=[C.PS[bk]])
            vs = vst[(tt // 4) % 2]
            evac(vs.t[:, tt % 4, :], C.bank(bk), [C.PS[bk]], [vs])
            if tt % 4 == 3:
                P.dma("sync", vv[:, tt - 3:tt + 1, :], vs.t[:, :, :], sem_v[(tt // 4) % 2], R=[vs], W=[C.Dv])
        P.barrier()


def phase_attn(C, l):
    nc, P = C.nc, C.P
    with ExitStack() as es:
        sb = lambda n, s, d: es.enter_context(nc.sbuf_tensor(uniq(n), s, d))
        qT = [TT(sb(f"c_qT{i}", [128, T], BF16)) for i in range(4)]
        kT = [TT(sb(f"c_kT{i}", [128, T], BF16)) for i in range(4)]
        V = TT(sb("c_V", [128, NT, 512], BF16))
        E = [TT(sb(f"c_E{i}", [128, 2048], F32)) for i in range(2)]
        SP = [TT(sb(f"c_SP{i}", [128, 2048], BF16)) for i in range(4)]
        CA = [TT(sb(f"c_CA{i}", [128, 2048], BF16)) for i in range(4)]
        AT = [TT(sb(f"c_AT{i}", [128, 2048], BF16)) for i in range(3)]
        ys = [TT(sb(f"c_ys{i}", [128, T], BF16)) for i in range(2)]
        sem_l = P.dma_sem("c_l")
        sem_s = [P.dma_sem("c_s0"), P.dma_sem("c_s1")]
        sem_l0 = P.dma_sem("c_l0")
        sem_lv = P.dma_sem("c_lv")
        vv = C.v.rearrange("(n p) c -> p n c", p=128)
        P.dma("sync", qT[0].t[:, :], C.qT[0:128, :], sem_l0, R=[C.DqT[0]], W=[qT[0]])
        P.dma("sync", kT[0].t[:, :], C.kT[0:128, :], sem_l0, R=[C.DkT[0]], W=[kT[0]])
        P.batch(sem_l0, [qT[0], kT[0]])
        for i in range(4):
            P.dma("sync", V.t[:, i * 8:(i + 1) * 8, :], vv[:, i * 8:(i + 1) * 8, :], sem_lv, R=[C.Dv], W=[V])
        for i in range(1, 4):
            P.dma("sync", qT[i].t[:, :], C.qT[i * 128:(i + 1) * 128, :], sem_l, R=[C.DqT[i]], W=[qT[i]])
            P.dma("sync", kT[i].t[:, :], C.kT[i * 128:(i + 1) * 128, :], sem_l, R=[C.DkT[i]], W=[kT[i]])
        P.batch(sem_l, qT[1:] + kT[1:])

        ZP = [0, 2]
        BP = 4
        OB = [6, 7]
        quads = []
        for hp in range(4):
            for e in range(2):
                for g in range(8):
                    nq = g + 1
                    for qi in range(nq):
                        js = [4 * g + 3 - 4 * qi - k for k in range(4)]
                        quads.append(dict(hp=hp, e=e, g=g, js=js, first=(qi == 0), last=(qi == nq - 1)))
        n = len(quads)
        masks2 = C.masks.t[:, :, :].rearrange("p a b -> p (a b)")

        def zq(s):
            q = quads[s]
            hp, e, g = q["hp"], q["e"], q["g"]
            pr = slice(64 * e, 64 * e + 64)
            for k in range(4):
                bk = ZP[k // 2] + k % 2
                j = q["js"][k]
                P.op("pe", I("matmul", C.bank(bk), kT[hp].t[pr, j * 128:(j + 1) * 128], qT[hp].t[pr, g * 512:(g + 1) * 512],
                             start=True, stop=True), R=[kT[hp], qT[hp]], W=[C.PS[bk]])

        def act1(s):
            Eb = E[s % 2]
            z4 = C.ps[:, 0:4, :].rearrange("p a b -> p (a b)")
            P.op("act", I("activation", out=Eb.t[:, :], in_=z4, func=AF.Exp),
                 R=[C.PS[0], C.PS[1], C.PS[2], C.PS[3]], W=[Eb])

        def act2(s):
            P.op("act", I("activation", out=SP[s % 4].t[:, :], in_=E[s % 2].t[:, :], func=AF.Ln, bias=1.0), R=[E[s % 2]], W=[SP[s % 4]])

        def dve3(s):
            q = quads[s]
            S, Cq, Cn = SP[s % 4], CA[s % 4], CA[(s + 1) % 4]
            sl = lambda k: slice(k * 512, (k + 1) * 512)
            if q["first"]:
                P.op("dve", I("tensor_tensor", S.t[:, :], S.t[:, :], masks2, op=ALU.mult), R=[S, C.masks], W=[S])
                P.op("dve", I("tensor_tensor", Cq.t[:, sl(2)], S.t[:, sl(0)], S.t[:, sl(1)], op=ALU.add), R=[S], W=[Cq])
            else:
                P.op("dve", I("tensor_tensor", Cq.t[:, sl(1)], Cq.t[:, sl(0)], S.t[:, sl(0)], op=ALU.add), R=[S, Cq], W=[Cq])
                P.op("dve", I("tensor_tensor", Cq.t[:, sl(2)], Cq.t[:, sl(1)], S.t[:, sl(1)], op=ALU.add), R=[S, Cq], W=[Cq])
            P.op("dve", I("tensor_tensor", Cq.t[:, sl(3)], Cq.t[:, sl(2)], S.t[:, sl(2)], op=ALU.add), R=[S, Cq], W=[Cq])
            if not q["last"]:
                P.op("dve", I("tensor_tensor", Cn.t[:, sl(0)], Cq.t[:, sl(3)], S.t[:, sl(3)], op=ALU.add), R=[S, Cq], W=[Cn])

        def bq(s, p):
            q = quads[s]
            hp, e, g = q["hp"], q["e"], q["g"]
            pr = slice(64 * e, 64 * e + 64)
            S, Cq = SP[s % 4], CA[s % 4]
            for k in (2 * p, 2 * p + 1):
                bk = BP + k % 2
                j = q["js"][k]
                if q["first"] and k == 0:
                    car = None
                elif q["first"] and k == 1:
                    car = (S, S.t[:, 0:512])
                else:
                    car = (Cq, Cq.t[:, k * 512:(k + 1) * 512])
                P.op("pe", I("matmul", C.bank(bk), kT[hp].t[pr, j * 128:(j + 1) * 128], qT[hp].t[pr, g * 512:(g + 1) * 512],
                             start=True, stop=False), R=[kT[hp], qT[hp]], W=[C.PS[bk]])
                P.op("pe", I("matmul", C.bank(bk), C.negU.t[:, :], S.t[:, k * 512:(k + 1) * 512], start=False, stop=(car is None)),
                     R=[C.negU, S], W=[C.PS[bk]])
                if car is not None:
                    P.op("pe", I("matmul", C.bank(bk), C.negO.t[:, :], car[1], start=False, stop=True),
                         R=[C.negO, car[0]], W=[C.PS[bk]])

        def act3(s, p):
            A = AT[s % 3]
            P.op("act", I("activation", out=A.t[:, p * 1024:(p + 1) * 1024], in_=C.ps2(BP), func=AF.Exp),
                 R=[C.PS[BP], C.PS[BP + 1]], W=[A])

        def mask_att(s):
            q = quads[s]
            if q["first"]:
                A = AT[s % 3]
                P.op("dve", I("tensor_tensor", A.t[:, :], A.t[:, :], masks2, op=ALU.mult), R=[A, C.masks], W=[A])

        def av(s):
            q = quads[s]
            hp, e, g = q["hp"], q["e"], q["g"]
            gi = (hp * 2 + e) * 8 + g
            ob = OB[gi % 2]
            A = AT[s % 3]
            pr = slice(64 * e, 64 * e + 64)
            for k in range(4):
                j = q["js"][k]
                P.op("pe", I("matmul", C.bank(ob)[pr, :], V.t[:, j, hp * 128 + 64 * e: hp * 128 + 64 * e + 64], A.t[:, k * 512:(k + 1) * 512],
                             start=(q["first"] and k == 0), stop=(q["last"] and k == 3)), R=[V, A], W=[C.PS[ob]])
            if q["last"]:
                yb = ys[hp % 2]
                P.op("dve", I("tensor_copy", yb.t[pr, g * 512:(g + 1) * 512], C.bank(ob)[pr, :]), R=[C.PS[ob]], W=[yb])
                if e == 1 and g == 7:
                    P.dma("sync", C.ycT[512 + hp * 128:512 + (hp + 1) * 128, :], yb.t[:, :], sem_s[hp % 2], R=[yb], W=[C.DycT[4 + hp]])

        for s in range(n + 3):
            if s < n:
                zq(s)
                act1(s)
            if 0 <= s - 2 < n:
                bq(s - 2, 0)
                act3(s - 2, 0)
            if 0 <= s - 3 < n:
                av(s - 3)
            if s < n:
                act2(s)
                dve3(s)
            if 0 <= s - 2 < n:
                bq(s - 2, 1)
                act3(s - 2, 1)
                mask_att(s - 2)
        P.barrier()


def phase_mix_ln1(C, l):
    nc, P = C.nc, C.P
    moe = (l % 2 == 1)
    with ExitStack() as es:
        sb = lambda n, s, d: es.enter_context(nc.sbuf_tensor(uniq(n), s, d))
        yc = [TT(sb(f"d_yc{k}", [128, T], BF16)) for k in range(8)]
        wo = TT(sb("d_wo", [128, 8, D], BF16))
        gam = TT(sb("d_gam", [128, D], F32))
        bet = TT(sb("d_bet", [128, D], F32))
        hin = [TT(sb(f"d_hin{i}", [128, D], F32)) for i in range(4)]
        pre = [TT(sb(f"d_pre{i}", [128, D], F32)) for i in range(2)]
        sem_c = P.dma_sem("d_c")
        sem_cw = P.dma_sem("d_cw", sw=True)
        sem_y = P.dma_sem("d_y")
        sem_h = [P.dma_sem(f"d_h{i}", sw=True) for i in range(4)]
        P.dma("pool", wo.t[:, :, :], C.d["w_out"][l], sem_cw, W=[wo])
        P.dma("sync", gam.t[:, :], bcast_row(C.d["ln1_g"][l], D), sem_c, W=[gam])
        P.dma("sync", bet.t[:, :], bcast_row(C.d["ln1_b"][l], D), sem_c, W=[bet])
        for k in range(8):
            P.dma("sync", yc[k].t[:, :], C.ycT[k * 128:(k + 1) * 128, :], sem_y, R=[C.DycT[k]], W=[yc[k]])
        P.batch(sem_y, yc)
        L = alloc_ln(C, es)
        router = None
        P.batch(sem_c, [gam, bet])
        if moe:
            rt = TT(sb("d_rt", [128, 8, NE], F32))
            P.dma("sync", rt.t[:, :, :], C.d["moe_router"][l // 2], sem_c, W=[rt])
            P.batch(sem_c, [gam, bet, rt])
            hT32 = [TT(sb(f"d_hT32{i}", [128, 8, 128], F32)) for i in range(2)]
            lgt = TT(sb("d_lgt", [128, NT, NE], F32))
            wk = TT(sb("d_wk", [128, NT, NE], F32))
            m1 = TT(sb("d_m1", [128, NT], F32))
            m2 = TT(sb("d_m2", [128, NT], F32))

            def router(tt, ho):
                b = tt % 2
                bk = 4 + 2 * b
                flat = C.ps2(bk)
                for k in range(8):
                    P.op("pe", I("transpose", flat[:, k * 128:(k + 1) * 128], ho.t[:, k * 128:(k + 1) * 128], C.ident.t[:, :]),
                         R=[ho, C.ident], W=[C.PS[bk], C.PS[bk + 1]])
                P.op("dve", I("tensor_copy", hT32[b].t[:, :, :], C.ps2v(bk, 8, 128)), R=[C.PS[bk], C.PS[bk + 1]], W=[hT32[b]])
                lb = C.bank(bk)[:, 0:NE]
                for k in range(8):
                    P.op("pe", I("matmul", lb, hT32[b].t[:, k, :], rt.t[:, k, :], start=(k == 0), stop=(k == 7)),
                         R=[hT32[b], rt], W=[C.PS[bk], C.PS[bk + 1]])
                P.op("dve", I("tensor_copy", lgt.t[:, tt, :], lb), R=[C.PS[bk], C.PS[bk + 1]], W=[lgt])

            def router_finish():
                fl = lambda t_: t_.t[:, :, :].rearrange("p a b -> p (a b)")
                P.op("dve", I("tensor_reduce", m1.t[:, :], lgt.t[:, :, :], AX.X, ALU.max), R=[lgt], W=[m1])
                for e_ in range(NE):
                    P.op("dve", I("tensor_tensor", wk.t[:, :, e_], lgt.t[:, :, e_], m1.t[:, :], op=ALU.is_equal), R=[lgt, m1], W=[wk])
                P.op("dve", I("scalar_tensor_tensor", fl(wk), fl(wk), -1e30, fl(lgt), op0=ALU.mult, op1=ALU.add), R=[wk, lgt], W=[wk])
                P.op("dve", I("tensor_reduce", m2.t[:, :], wk.t[:, :, :], AX.X, ALU.max), R=[wk], W=[m2])
                for e_ in range(NE):
                    P.op("dve", I("tensor_tensor", wk.t[:, :, e_], lgt.t[:, :, e_], m1.t[:, :], op=ALU.subtract), R=[lgt, m1], W=[wk])
                P.op("act", I("activation", out=fl(wk), in_=fl(wk), func=AF.Exp), R=[wk], W=[wk])
                for e_ in range(NE):
                    P.op("dve", I("tensor_tensor", C.sel.t[:, :, e_], lgt.t[:, :, e_], m2.t[:, :], op=ALU.is_ge), R=[lgt, m2], W=[C.sel])
                P.op("dve", I("tensor_tensor", fl(wk), fl(wk), fl(C.sel), op=ALU.mult), R=[wk, C.sel], W=[wk])
                P.op("dve", I("tensor_reduce", m1.t[:, :], wk.t[:, :, :], AX.X, ALU.add), R=[wk], W=[m1])
                P.op("dve", I("reciprocal", m1.t[:, :], m1.t[:, :]), R=[m1], W=[m1])
                for e_ in range(NE):
                    P.op("dve", I("tensor_tensor", C.gate.t[:, :, e_], wk.t[:, :, e_], m1.t[:, :], op=ALU.mult), R=[wk, m1], W=[C.gate])

        for tt in range(NT):
            b = tt % 2
            hb_ = tt % 4
            P.dma("pool", hin[hb_].t[:, :], C.ha[tt * 128:(tt + 1) * 128, :], sem_h[hb_], R=[C.Dha], W=[hin[hb_]])
            bk = 0 + 2 * b
            for nh in range(2):
                for k in range(8):
                    P.op("pe", I("matmul", C.bank(bk + nh), yc[k].t[:, tt * 128:(tt + 1) * 128], wo.t[:, k, nh * 512:(nh + 1) * 512],
                                 start=(k == 0), stop=(k == 7)), R=[yc[k], wo], W=[C.PS[bk], C.PS[bk + 1]])
            P.op("dve", I("scalar_tensor_tensor", pre[b].t[:, :], hin[hb_].t[:, :], ALPHA, C.ps2(bk), op0=ALU.mult, op1=ALU.add),
                 R=[hin[hb_], C.PS[bk], C.PS[bk + 1]], W=[pre[b]])
            ln_tile(C, L, tt, pre[b], gam, bet, (C.Dhb, C.hb[tt * 128:(tt + 1) * 128, :]),
                    None if moe else (C.DhTb, C.hTb), tr_banks=4 + 2 * b, router=router)
        ln_flush(C, L)
        if moe:
            router_finish()
        P.barrier()


def alloc_ffn(C, es, ntok):
    nc, P = C.nc, C.P
    F = Ctx()
    sb = lambda n, s, d: es.enter_context(nc.sbuf_tensor(uniq(n), s, d))
    F.ntok = ntok
    F.wd = TT(sb("f_wd", [128, NFC, D], BF16))
    F.wg = [TT(sb(f"f_wg{i}", [128, 2, 8, 128], BF16)) for i in range(3)]
    F.wu = [TT(sb(f"f_wu{i}", [128, 2, 8, 128], BF16)) for i in range(3)]
    F.act = TT(sb("f_act", [128, NFC, ntok], BF16))
    F.sg = [TT(sb(f"f_sg{i}", [128, 512], BF16)) for i in range(2)]
    F.sem_wd = P.dma_sem("f_wd", sw=True)
    F.sem_wg = [P.dma_sem(f"f_wg{i}", sw=True) for i in range(3)]
    F.sem_wu = [P.dma_sem(f"f_wu{i}", sw=True) for i in range(3)]
    F.cnt = 0
    F.blk = 0
    F.pref = None
    return F


def ffn_pass(C, F, xT, wg_d, wu_d, wd_d, consume, GB=(0, 1), UB=(2, 3), DB=4, blk_hook=None, nxt=None, mid_hook=None):
    P = C.P
    ntok = F.ntok
    subs = []
    t0 = 0
    while t0 < ntok:
        w = min(512, ntok - t0)
        subs.append((t0, w))
        t0 += w
    wdv = wd_d.rearrange("(c p) n -> p c n", p=128)
    for fb in range(NFC // 2):
        if fb == 2:
            P.dma("pool", F.wd.t[:, 0:11, :], wdv[:, 0:11, :], F.sem_wd, W=[F.wd])
            P.dma("pool", F.wd.t[:, 11:22, :], wdv[:, 11:22, :], F.sem_wd, W=[F.wd])
        if fb == 0 and F.pref is not None:
            bi = F.pref
            F.pref = None
            wg, wu = F.wg[bi], F.wu[bi]
        else:
            bi = F.blk % 3
            F.blk += 1
            wg, wu = F.wg[bi], F.wu[bi]
            P.dma("pool", wg.t[:, :, :, :], wg_d[2 * fb:2 * fb + 2].rearrange("c p k f -> p c k f"), F.sem_wg[bi], W=[wg])
            P.dma("pool", wu.t[:, :, :, :], wu_d[2 * fb:2 * fb + 2].rearrange("c p k f -> p c k f"), F.sem_wu[bi], W=[wu])
        for ci in range(2):
            fc = 2 * fb + ci
            for (t0, w) in subs:
                i = F.cnt
                F.cnt += 1
                gb, ub = GB[i % 2], UB[i % 2]
                for k in range(8):
                    P.op("pe", I("matmul", C.bank(gb)[:, 0:w], wg.t[:, ci, k, :], xT[k].t[:, t0:t0 + w], start=(k == 0), stop=(k == 7)),
                         R=[wg, xT[k]], W=[C.PS[gb]])
                for k in range(8):
                    P.op("pe", I("matmul", C.bank(ub)[:, 0:w], wu.t[:, ci, k, :], xT[k].t[:, t0:t0 + w], start=(k == 0), stop=(k == 7)),
                         R=[wu, xT[k]], W=[C.PS[ub]])
                sg = F.sg[i % 2]
                P.op("act", I("activation", out=sg.t[:, 0:w], in_=C.bank(gb)[:, 0:w], func=AF.Silu), R=[C.PS[gb]], W=[sg])
                P.op("dve", I("tensor_tensor", F.act.t[:, fc, t0:t0 + w], sg.t[:, 0:w], C.bank(ub)[:, 0:w], op=ALU.mult),
                     R=[sg, C.PS[ub]], W=[F.act])
        if blk_hook is not None:
            blk_hook(fb)
    if mid_hook is not None:
        mid_hook()
    if nxt is not None:
        bi = F.blk % 3
        F.blk += 1
        P.dma("pool", F.wg[bi].t[:, :, :, :], nxt[0][0:2].rearrange("c p k f -> p c k f"), F.sem_wg[bi], W=[F.wg[bi]])
        P.dma("pool", F.wu[bi].t[:, :, :, :], nxt[1][0:2].rearrange("c p k f -> p c k f"), F.sem_wu[bi], W=[F.wu[bi]])
        F.pref = bi
    dpairs = (DB, GB[0], UB[0])
    for tt in range(ntok // 128):
        db = dpairs[tt % 3]
        for nh in range(2):
            for fc in range(NFC):
                P.op("pe", I("matmul", C.bank(db + nh), F.act.t[:, fc, tt * 128:(tt + 1) * 128], F.wd.t[:, fc, nh * 512:(nh + 1) * 512],
                             start=(fc == 0), stop=(fc == NFC - 1)), R=[F.act, F.wd], W=[C.PS[db], C.PS[db + 1]])
        consume(tt, db)


def phase_ffn_dense(C, l, final):
    nc, P = C.nc, C.P
    TG = 1024
    with ExitStack() as es:
        sb = lambda n, s, d: es.enter_context(nc.sbuf_tensor(uniq(n), s, d))
        gam = TT(sb("e_gam", [128, D], F32))
        bet = TT(sb("e_bet", [128, D], F32))
        hin = [TT(sb(f"e_hin{i}", [128, D], F32)) for i in range(2)]
        pre = [TT(sb(f"e_pre{i}", [128, D], F32)) for i in range(2)]
        xT = [[TT(sb(f"e_xT{j}_{k}", [128, TG], BF16)) for k in range(8)] for j in range(1)]
        sem_c = P.dma_sem("e_c")
        sem_x = [P.dma_sem("e_x0"), P.dma_sem("e_x1")]
        sem_h = [P.dma_sem("e_h0", sw=True), P.dma_sem("e_h1", sw=True)]
        P.dma("sync", gam.t[:, :], bcast_row(C.d["ln2_g"][l], D), sem_c, W=[gam])
        P.dma("sync", bet.t[:, :], bcast_row(C.d["ln2_b"][l], D), sem_c, W=[bet])
        P.batch(sem_c, [gam, bet])
        L = alloc_ln(C, es, want_T=not final)
        F = alloc_ffn(C, es, TG)
        i = l // 2
        cnt = [0]
        NG = T // TG
        xb = xT[0]

        def load_x(grp):
            for k in range(8):
                P.dma("sync", xb[k].t[:, :], C.hTb[k * 128:(k + 1) * 128, grp * TG:(grp + 1) * TG], sem_x[grp % 2], R=[C.DhTb], W=[xb[k]])
            P.batch(sem_x[grp % 2], xb)

        load_x(0)
        for grp in range(NG):
            def consume(tt, db, grp=grp):
                gt = grp * (TG // 128) + tt
                b = cnt[0] % 2
                cnt[0] += 1
                P.dma("pool", hin[b].t[:, :], C.hb[gt * 128:(gt + 1) * 128, :], sem_h[b], R=[C.Dhb], W=[hin[b]])
                P.op("dve", I("scalar_tensor_tensor", pre[b].t[:, :], hin[b].t[:, :], ALPHA, C.ps2(db), op0=ALU.mult, op1=ALU.add),
                     R=[hin[b], C.PS[db], C.PS[db + 1]], W=[pre[b]])
                if final:
                    ln_tile(C, L, gt, pre[b], gam, bet, (C.Dout, C.out[gt * 128:(gt + 1) * 128, :]), None)
                else:
                    ln_tile(C, L, gt, pre[b], gam, bet, (C.Dha, C.ha[gt * 128:(gt + 1) * 128, :]), (C.DhTa, C.hTa), tr_banks=6)

            more = grp + 1 < NG
            ffn_pass(C, F, xb, C.d["ffn_wg"][i], C.d["ffn_wu"][i], C.d["ffn_wd"][i], consume,
                     nxt=(C.d["ffn_wg"][i], C.d["ffn_wu"][i]) if more else None,
                     mid_hook=(lambda g=grp: load_x(g + 1)) if more else None)
        ln_flush(C, L)
        P.barrier()


def phase_moe(C, l, final, stop=3):
    nc, P = C.nc, C.P
    mi = l // 2
    with ExitStack() as es:
        sb = lambda n, s, d: es.enter_context(nc.sbuf_tensor(uniq(n), s, d))
        selb = TT(sb("m_selb", [128, NT, NE], BF16))
        csel = TT(sb("m_csel", [128, NT, NE], BF16))
        cum = TT(sb("m_cum", [128, NT, NE], F32))
        pos = TT(sb("m_pos", [128, NT, NE], F32))
        pre_ = TT(sb("m_pre", [128, NT, NE], F32))
        tmp = TT(sb("m_tmp", [128, NT, NE], F32))
        tmp2 = TT(sb("m_tmp2", [128, NT, NE], F32))
        iot = TT(sb("m_iot", [128, CAP], F32))
        ecap = TT(sb("m_ecap", [128, NT, NE], F32))
        A = [TT(sb(f"m_A{i}", [128, NT, CAP], BF16)) for i in range(2)]
        tokf = TT(sb("m_tokf", [128, NE, NST], F32))
        sem_c = P.dma_sem("m_c")
        P.dma("sync", iot.t[:, :], C.d["c_iota"], sem_c, W=[iot])
        P.dma("sync", ecap.t[:, :, :], C.d["c_ecap"], sem_c, W=[ecap])
        P.batch(sem_c, [iot, ecap])
        P.op("dve", I("tensor_copy", selb.t[:, :, :], C.sel.t[:, :, :]), R=[C.sel], W=[selb])
        P.op("dve", I("memset", csel.t[:, 0, :], 0.0), W=[csel])
        for tt in range(1, NT):
            P.op("dve", I("tensor_tensor", csel.t[:, tt, :], csel.t[:, tt - 1, :], selb.t[:, tt - 1, :], op=ALU.add), R=[csel, selb], W=[csel])
        bk = 0
        for tt in range(NT):
            o = C.bank(bk)[:, tt * NE:(tt + 1) * NE]
            P.op("pe", I("matmul", o, C.lowI.t[:, :], selb.t[:, tt, :], start=True, stop=False), R=[C.lowI, selb], W=[C.PS[bk]])
            P.op("pe", I("matmul", o, C.ones.t[:, :], csel.t[:, tt, :], start=False, stop=True), R=[C.ones, csel], W=[C.PS[bk]])
        P.op("dve", I("tensor_copy", cum.t[:, :, :], C.bank(bk)[:, 0:NT * NE].rearrange("p (a b) -> p a b", b=NE)), R=[C.PS[bk]], W=[cum])
        P.op("dve", I("tensor_tensor", pos.t[:, :, :], cum.t[:, :, :], C.sel.t[:, :, :], op=ALU.subtract), R=[cum, C.sel], W=[pos])
        P.op("dve", I("memset", pre_.t[:, :, :], 0.0), W=[pre_])
        P.op("dve", I("tensor_copy", pre_.t[:, :, 1:NE], C.sel.t[:, :, 0:NE - 1]), R=[C.sel], W=[pre_])
        for sh in (1, 2, 4):
            P.op("dve", I("tensor_copy", tmp.t[:, :, :], pre_.t[:, :, :]), R=[pre_], W=[tmp])
            P.op("dve", I("tensor_tensor", pre_.t[:, :, sh:NE], tmp.t[:, :, sh:NE], tmp.t[:, :, 0:NE - sh], op=ALU.add), R=[tmp], W=[pre_])
        P.op("dve", I("tensor_tensor", pos.t[:, :, :], pos.t[:, :, :], ecap.t[:, :, :], op=ALU.add), R=[pos, ecap], W=[pos])
        for kk in range(2):
            P.op("dve", I("tensor_scalar", tmp.t[:, :, :], pre_.t[:, :, :], float(kk), None, op0=ALU.is_equal), R=[pre_], W=[tmp])
            P.op("dve", I("tensor_tensor", tmp.t[:, :, :], tmp.t[:, :, :], C.sel.t[:, :, :], op=ALU.mult), R=[tmp, C.sel], W=[tmp])
            P.op("dve", I("tensor_tensor", tmp2.t[:, :, :], tmp.t[:, :, :], pos.t[:, :, :], op=ALU.mult), R=[tmp, pos], W=[tmp2])
            P.op("dve", I("tensor_reduce", C.slotf.t[:, kk, :], tmp2.t[:, :, :], AX.X, ALU.add), R=[tmp2], W=[C.slotf])
            P.op("dve", I("tensor_tensor", tmp2.t[:, :, :], tmp.t[:, :, :], C.gate.t[:, :, :], op=ALU.mult), R=[tmp, C.gate], W=[tmp2])
            P.op("dve", I("tensor_reduce", C.gsel.t[:, kk, :], tmp2.t[:, :, :], AX.X, ALU.add), R=[tmp2], W=[C.gsel])
        P.op("dve", I("tensor_copy", C.sloti.t[:, :, :], C.slotf.t[:, :, :]), R=[C.slotf], W=[C.sloti])
        ai = 0
        for e in range(NE):
            bk = 1 + (e % 2)
            Ab = A[e % 2]
            for tt in range(NT):
                eng = "dve"
                ai += 1
                P.op(eng, I("tensor_scalar", Ab.t[:, tt, :], iot.t[:, :], cum.t[:, tt, e:e + 1], None, op0=ALU.is_ge), R=[iot, cum], W=[Ab])
            for st in range(NST):
                for tt in range(NT):
                    P.op("pe", I("matmul", C.bank(bk)[:, st:st + 1], Ab.t[:, tt, st * 128:(st + 1) * 128], C.ones.t[:, 0:1],
                                 start=(tt == 0), stop=(tt == NT - 1)), R=[Ab, C.ones], W=[C.PS[bk]])
            P.op("dve", I("tensor_scalar", tokf.t[:, e, :], C.bank(bk)[:, 0:NST], float(T - 1), None, op0=ALU.min), R=[C.PS[bk]], W=[tokf])
        P.op("dve", I("tensor_copy", C.toki.t[:, :, :], tokf.t[:, :, :]), R=[tokf], W=[C.toki])
        P.barrier()
    if stop < 2:
        return
    with ExitStack() as es:
        sb = lambda n, s, d: es.enter_context(nc.sbuf_tensor(uniq(n), s, d))
        xg = [TT(sb(f"m_xg{i}", [128, D], F32)) for i in range(2)]
        xT = [[TT(sb(f"m_xT{j}_{k}", [128, CAP], BF16)) for k in range(8)] for j in range(2)]
        yst = [TT(sb(f"m_yst{i}", [128, D], F32)) for i in range(2)]
        sem_g = [P.dma_sem("m_g0", sw=True), P.dma_sem("m_g1", sw=True)]
        sem_y = [P.dma_sem("m_y0"), P.dma_sem("m_y1")]
        F = alloc_ffn(C, es, CAP)
        yi = [0]

        def emit_gather(e, st):
            b = st % 2
            P.gather(xg[b].t[:, :], C.hb, C.toki.t[:, e, st:st + 1], sem_g[b], R=[C.Dhb, C.toki], W=[xg[b]])

        def emit_tr(e, st):
            b = st % 2
            xb = xT[e % 2]
            bk = 6
            flat = C.ps2(bk)
            for k in range(8):
                P.op("pe", I("transpose", flat[:, k * 128:(k + 1) * 128], xg[b].t[:, k * 128:(k + 1) * 128], C.ident.t[:, :]),
                     R=[xg[b], C.ident], W=[C.PS[bk], C.PS[bk + 1]])
            for k in range(8):
                if k % 2 == 0:
                    P.op("act", I("activation", out=xb[k].t[:, st * 128:(st + 1) * 128], in_=flat[:, k * 128:(k + 1) * 128], func=AF.Copy),
                         R=[C.PS[bk], C.PS[bk + 1]], W=[xb[k]])
                else:
                    P.op("dve", I("tensor_copy", xb[k].t[:, st * 128:(st + 1) * 128], flat[:, k * 128:(k + 1) * 128]),
                         R=[C.PS[bk], C.PS[bk + 1]], W=[xb[k]])

        for st in range(NST):
            emit_gather(0, st)
            emit_tr(0, st)
        for e in range(NE):
            xb = xT[e % 2]

            def consume(tt, db, e=e):
                b = yi[0] % 2
                yi[0] += 1
                if b == 0:
                    P.op("act", I("activation", out=yst[b].t[:, :], in_=C.ps2(db), func=AF.Copy), R=[C.PS[db], C.PS[db + 1]], W=[yst[b]])
                else:
                    P.op("dve", I("tensor_copy", yst[b].t[:, :], C.ps2(db)), R=[C.PS[db], C.PS[db + 1]], W=[yst[b]])
                r0 = e * CAP + tt * 128
                P.dma("sync", C.ybuf[r0:r0 + 128, :], yst[b].t[:, :], sem_y[b], R=[yst[b]], W=[C.Dyb])

            def hook(fb, e=e):
                if e + 1 < NE:
                    if fb < NST:
                        emit_gather(e + 1, fb)
                    if 1 <= fb <= NST:
                        emit_tr(e + 1, fb - 1)

            ffn_pass(C, F, xb, C.d["moe_wg"][mi][e], C.d["moe_wu"][mi][e], C.d["moe_wd"][mi][e], consume, blk_hook=hook)
        P.barrier()
    if stop < 3:
        return
    with ExitStack() as es:
        sb = lambda n, s, d: es.enter_context(nc.sbuf_tensor(uniq(n), s, d))
        gam = TT(sb("n_gam", [128, D], F32))
        bet = TT(sb("n_bet", [128, D], F32))
        hin = [TT(sb(f"n_hin{i}", [128, D], F32)) for i in range(4)]
        y0 = [TT(sb(f"n_y0{i}", [128, D], F32)) for i in range(4)]
        y1 = [TT(sb(f"n_y1{i}", [128, D], F32)) for i in range(4)]
        pre = [TT(sb(f"n_pre{i}", [128, D], F32)) for i in range(2)]
        sem_c = P.dma_sem("n_c")
        sem_h = [P.dma_sem(f"n_h{i}", sw=True) for i in range(4)]
        sem_a = [P.dma_sem(f"n_a{i}", sw=True) for i in range(4)]
        sem_b = [P.dma_sem(f"n_b{i}", sw=True) for i in range(4)]
        P.dma("sync", gam.t[:, :], bcast_row(C.d["ln2_g"][l], D), sem_c, W=[gam])
        P.dma("sync", bet.t[:, :], bcast_row(C.d["ln2_b"][l], D), sem_c, W=[bet])
        P.batch(sem_c, [gam, bet])
        L = alloc_ln(C, es, want_T=not final)
        for tt in range(NT):
            b = tt % 2
            r_ = tt % 4
            P.dma("pool", hin[r_].t[:, :], C.hb[tt * 128:(tt + 1) * 128, :], sem_h[r_], R=[C.Dhb], W=[hin[r_]])
            P.gather(y0[r_].t[:, :], C.ybuf, C.sloti.t[:, 0, tt:tt + 1], sem_a[r_], R=[C.Dyb, C.sloti], W=[y0[r_]])
            P.gather(y1[r_].t[:, :], C.ybuf, C.sloti.t[:, 1, tt:tt + 1], sem_b[r_], R=[C.Dyb, C.sloti], W=[y1[r_]])
            P.op("act", I("activation", out=y0[r_].t[:, :], in_=y0[r_].t[:, :], func=AF.Identity, scale=C.gsel.t[:, 0, tt:tt + 1]),
                 R=[y0[r_], C.gsel], W=[y0[r_]])
            P.op("dve", I("scalar_tensor_tensor", pre[b].t[:, :], hin[r_].t[:, :], ALPHA, y0[r_].t[:, :], op0=ALU.mult, op1=ALU.add),
                 R=[y0[r_], hin[r_]], W=[pre[b]])
            P.op("dve", I("scalar_tensor_tensor", pre[b].t[:, :], y1[r_].t[:, :], C.gsel.t[:, 1, tt:tt + 1], pre[b].t[:, :], op0=ALU.mult, op1=ALU.add),
                 R=[y1[r_], C.gsel, pre[b]], W=[pre[b]])
            if final:
                ln_tile(C, L, tt, pre[b], gam, bet, (C.Dout, C.out[tt * 128:(tt + 1) * 128, :]), None)
            else:
                ln_tile(C, L, tt, pre[b], gam, bet, (C.Dha, C.ha[tt * 128:(tt + 1) * 128, :]), (C.DhTa, C.hTa), tr_banks=0 + 2 * b)
        ln_flush(C, L)
        P.barrier()


IN_SPECS = None


def host_constants():
    c = {}
    c["c_ident"] = np.eye(128, dtype=np.float32)
    j = np.arange(128)[:, None]
    s = np.arange(128)[None, :]
    c["c_negU"] = -(j >= s).astype(np.float32)
    c["c_negO"] = -np.ones((128, 128), np.float32)
    c["c_lowI"] = (j <= s).astype(np.float32)
    c["c_ones"] = np.ones((128, 128), np.float32)
    p = np.arange(128)[:, None, None]
    dj = np.arange(4)[None, :, None]
    col = np.arange(512)[None, None, :]
    c["c_masks"] = ((128 * (3 - dj) + p) < col).astype(np.float32)
    invc = np.zeros((128, 4, 16), np.float32)
    for g in range(4):
        w = 2 << g
        invc[:, g, :] = 1.0 / np.minimum(np.arange(16) + 1, w)
    c["c_invc"] = invc
    c["c_iota"] = np.broadcast_to(np.arange(CAP, dtype=np.float32)[None, :], (128, CAP)).copy()
    c["c_ecap"] = np.broadcast_to((np.arange(NE, dtype=np.float32) * CAP)[None, None, :], (128, NT, NE)).copy()
    return c


def host_layout(inp):
    o = {}
    f = lambda a: np.ascontiguousarray(a, dtype=np.float32)
    o["ln_in_g"] = f(inp["ln_in_g"])
    o["ln_in_b"] = f(inp["ln_in_b"])
    o["w_in"] = f(inp["w_in"].reshape(DEPTH, 8, 128, 2048).transpose(0, 2, 1, 3))
    o["pool_w"] = f(inp["pool_w"].transpose(0, 2, 1, 3))
    o["pool_scale"] = f(inp["pool_scale"].reshape(DEPTH, 4, 128).transpose(0, 2, 1))
    o["w_out"] = f(inp["w_out"].reshape(DEPTH, 8, 128, D).transpose(0, 2, 1, 3))
    for nm in ("ln1_g", "ln1_b", "ln2_g", "ln2_b"):
        o[nm] = f(inp[nm])
    o["ffn_wg"] = f(inp["ffn_wg"].reshape(-1, 8, 128, NFC, 128).transpose(0, 3, 2, 1, 4))
    o["ffn_wu"] = f(inp["ffn_wu"].reshape(-1, 8, 128, NFC, 128).transpose(0, 3, 2, 1, 4))
    o["ffn_wd"] = f(inp["ffn_wd"])
    o["moe_router"] = f(inp["moe_router"].reshape(-1, 8, 128, NE).transpose(0, 2, 1, 3))
    o["moe_wg"] = f(inp["moe_wg"].reshape(-1, NE, 8, 128, NFC, 128).transpose(0, 1, 4, 3, 2, 5))
    o["moe_wu"] = f(inp["moe_wu"].reshape(-1, NE, 8, 128, NFC, 128).transpose(0, 1, 4, 3, 2, 5))
    o["moe_wd"] = f(inp["moe_wd"])
    o.update(host_constants())
    return o


def build(shapes, phases="all", debug_out=()):
    nc = bass.Bass("TRN2", target_bir_lowering=False)
    C = Ctx()
    C.nc = nc
    C.d = {}
    for name, shp in shapes.items():
        C.d[name] = nc.dram_tensor(name, list(shp), F32, kind="ExternalInput").ap()
    kind = lambda n: "ExternalOutput" if n in debug_out else "Internal"
    C.out = nc.dram_tensor("out", [T, D], F32, kind="ExternalOutput").ap()
    C.ha = nc.dram_tensor("s_ha", [T, D], F32, kind=kind("s_ha")).ap()
    C.hb = nc.dram_tensor("s_hb", [T, D], F32, kind=kind("s_hb")).ap()
    C.hTa = nc.dram_tensor("s_hTa", [D, T], BF16, kind=kind("s_hTa")).ap()
    C.hTb = nc.dram_tensor("s_hTb", [D, T], BF16, kind=kind("s_hTb")).ap()
    C.qT = nc.dram_tensor("s_qT", [512, T], BF16, kind=kind("s_qT")).ap()
    C.kT = nc.dram_tensor("s_kT", [512, T], BF16, kind=kind("s_kT")).ap()
    C.v = nc.dram_tensor("s_v", [T, 512], BF16, kind=kind("s_v")).ap()
    C.ycT = nc.dram_tensor("s_ycT", [D, T], BF16, kind=kind("s_ycT")).ap()
    C.ybuf = nc.dram_tensor("s_ybuf", [NE * CAP, D], F32, kind=kind("s_ybuf")).ap()
    C.Dout, C.Dha, C.Dhb, C.DhTa, C.DhTb, C.Dv, C.Dyb = TT(None), TT(None), TT(None), TT(None), TT(None), TT(None), TT(None)
    C.DqT = [TT(None) for _ in range(4)]
    C.DkT = [TT(None) for _ in range(4)]
    C.DycT = [TT(None) for _ in range(8)]
    with ExitStack() as es:
        P = Prog(nc, es)
        C.P = P
        sb = lambda n, s, d: es.enter_context(nc.sbuf_tensor(uniq(n), s, d))
        ps = es.enter_context(nc.psum_tensor("ps", [128, 8, 512], F32))
        C.ps = ps
        C.PS = [TT(None, f"bank{i}", excl=True) for i in range(8)]
        C.bank = lambda b: ps[:, b, :]
        C.ps2 = lambda b: ps[:, b:b + 2, :].rearrange("p a b -> p (a b)")
        C.ps2v = lambda b, a, w: ps[:, b:b + 2, :].rearrange("p a b -> p (a b)").rearrange("p (a w) -> p a w", w=w)
        C.ident = TT(sb("k_ident", [128, 128], F32))
        C.negU = TT(sb("k_negU", [128, 128], BF16))
        C.negO = TT(sb("k_negO", [128, 128], BF16))
        C.lowI = TT(sb("k_lowI", [128, 128], BF16))
        C.ones = TT(sb("k_ones", [128, 128], BF16))
        C.masks = TT(sb("k_masks", [128, 4, 512], BF16))
        C.gate = TT(sb("k_gate", [128, NT, NE], F32))
        C.sel = TT(sb("k_sel", [128, NT, NE], F32))
        C.slotf = TT(sb("k_slotf", [128, 2, NT], F32))
        C.sloti = TT(sb("k_sloti", [128, 2, NT], U32))
        C.gsel = TT(sb("k_gsel", [128, 2, NT], F32))
        C.toki = TT(sb("k_toki", [128, NE, NST], U32))
        sem_k = P.dma_sem("k_c", sw=True)
        sem_k2 = P.dma_sem("k_c2")
        P.dma("sync", C.ident.t[:, :], C.d["c_ident"], sem_k2, W=[C.ident])
        P.dma("pool", C.negU.t[:, :], C.d["c_negU"], sem_k, W=[C.negU])
        P.dma("pool", C.negO.t[:, :], C.d["c_negO"], sem_k, W=[C.negO])
        P.dma("pool", C.lowI.t[:, :], C.d["c_lowI"], sem_k, W=[C.lowI])
        P.dma("pool", C.ones.t[:, :], C.d["c_ones"], sem_k, W=[C.ones])
        P.dma("pool", C.masks.t[:, :, :], C.d["c_masks"], sem_k, W=[C.masks])
        P.batch(sem_k, [C.negU, C.negO, C.lowI, C.ones, C.masks])

        def want(p):
            if phases == "all":
                return not (len(p) == 3 and p[2] in "ab")
            return p in phases

        if want("A"):
            phase_ln_in(C)
        for l in range(DEPTH):
            final = (l == DEPTH - 1)
            if want(f"B{l}"):
                phase_proj(C, l)
            if want(f"C{l}"):
                phase_attn(C, l)
            if want(f"D{l}"):
                phase_mix_ln1(C, l)
            if want(f"E{l}") or want(f"E{l}a") or want(f"E{l}b"):
                if l % 2 == 0:
                    phase_ffn_dense(C, l, final)
                else:
                    phase_moe(C, l, final, stop=(1 if want(f"E{l}a") else 2 if want(f"E{l}b") else 3))
        if "k_dbg" in debug_out:
            sem_d = P.dma_sem("dbg")
            for nm, tt_, shp, dt in (("k_gate", C.gate, [128, NT, NE], F32), ("k_sel", C.sel, [128, NT, NE], F32),
                                     ("k_slotf", C.slotf, [128, 2, NT], F32), ("k_sloti", C.sloti, [128, 2, NT], U32),
                                     ("k_gsel", C.gsel, [128, 2, NT], F32), ("k_toki", C.toki, [128, NE, NST], U32)):
                dd_ = nc.dram_tensor("dbg_" + nm, shp, dt, kind="ExternalOutput").ap()
                P.dma("sync", dd_, tt_.t[:, :, :], sem_d, R=[tt_])
        P.barrier()
        print("bass ops:", P.nops, {e: len(P.q[e]) for e in ENG})
        P.flush()
    return nc


_CACHE = {}


def kernel(**inputs):
    lay = host_layout(inputs)
    x = np.ascontiguousarray(inputs["x"], dtype=np.float32)
    shapes = {"x": (T, D)}
    for k, v in lay.items():
        shapes[k] = v.shape
    if "nc" not in _CACHE:
        import os
        dbg = ("s_ha", "s_hb", "s_hTa", "s_hTb", "s_qT", "s_kT", "s_v", "s_ycT", "s_ybuf") if os.environ.get("SCRATCH_OUT") == "1" else ()
        _CACHE["nc"] = build(shapes, debug_out=dbg)
    nc = _CACHE["nc"]
    in_maps = []
    for b in range(NCORES):
        m = {"x": x[b]}
        m.update(lay)
        in_maps.append(m)
    res = run_bass_kernel_spmd(nc, in_maps, core_ids=list(range(NCORES)))
    out = np.stack([np.asarray(res.results[b]["out"], dtype=np.float32) for b in range(NCORES)], axis=0)
    return out
```
